# Optimizing a Trainium2 kernel written in Bass

```python
import math
import jax, jax.numpy as jnp
from jax import lax
import numpy as np

D_MODEL = 2048
BATCH = 4
SEQ = 4096
DEPTH = 1

DA_HEADS = 8
DA_HEAD_DIM = 64
DA_V_DIM = 2 * DA_HEAD_DIM
DA_QK_WIDTH = DA_HEADS * 2 * DA_HEAD_DIM
DA_V_WIDTH = DA_HEADS * DA_V_DIM
ROPE_THETA = 500000.0
ROPE_DIM = DA_HEAD_DIM // 4
Q_BLOCK = 128
RW_HEADS = 16
RW_HEAD_DIM = 64
RW_WIDTH = RW_HEADS * RW_HEAD_DIM
DECAY_LORA = 64
AAA_LORA = 64
GATE_LORA = 160
GN_EPS = 64e-5
N_BRANCH = 2
N_EXPERTS = 16
EXPERT_FF = 1024
CAPACITY_FACTOR = 2
RMS_EPS = 1e-6

DA_COLS = 2 * DA_QK_WIDTH + DA_V_WIDTH
RW_COLS = 3 * RW_WIDTH + 2 * DECAY_LORA + 2 * AAA_LORA + GATE_LORA
GATE_COLS = N_BRANCH * D_MODEL
IN_COLS = DA_COLS + RW_COLS + GATE_COLS
RW_SPLITS = tuple(int(v) for v in np.cumsum(
    [RW_WIDTH, RW_WIDTH, RW_WIDTH, DECAY_LORA, DECAY_LORA, AAA_LORA, AAA_LORA]))

kernel_name = "hybrid_diffattn_rwkv7_ecmoe_encoder"


def rms_norm(x, g, eps=RMS_EPS):
    xf = x.astype(jnp.float32)
    y = xf * lax.rsqrt(jnp.mean(xf * xf, axis=-1, keepdims=True) + eps)
    return (y * g.astype(jnp.float32)).astype(x.dtype)


def rope_tables(T):
    inv = ROPE_THETA ** (-(jnp.arange(0, ROPE_DIM, 2, dtype=jnp.float32) / ROPE_DIM))
    ang = jnp.arange(T, dtype=jnp.float32)[:, None] * inv[None, :]
    return jnp.cos(ang), jnp.sin(ang)


def apply_partial_rope(x, cos, sin):
    c = cos[None, :, None, None, :].astype(x.dtype)
    s = sin[None, :, None, None, :].astype(x.dtype)
    half = ROPE_DIM // 2
    x1 = x[..., :half]
    x2 = x[..., half:ROPE_DIM]
    return jnp.concatenate([x1 * c - x2 * s, x2 * c + x1 * s, x[..., ROPE_DIM:]], axis=-1)


def diff_attention_branch(p, cos, sin, q_norm_g, k_norm_g, lq1, lk1, lq2, lk2, subln_g, lam_init):
    B, T, _ = p.shape
    q = p[..., :DA_QK_WIDTH].reshape(B, T, DA_HEADS, 2, DA_HEAD_DIM)
    k = p[..., DA_QK_WIDTH:2 * DA_QK_WIDTH].reshape(B, T, DA_HEADS, 2, DA_HEAD_DIM)
    v = p[..., 2 * DA_QK_WIDTH:].reshape(B, T, DA_HEADS, DA_V_DIM)
    q = apply_partial_rope(rms_norm(q, q_norm_g), cos, sin)
    k = apply_partial_rope(rms_norm(k, k_norm_g), cos, sin)
    lam = (jnp.exp(jnp.sum(lq1.astype(jnp.float32) * lk1.astype(jnp.float32)))
           - jnp.exp(jnp.sum(lq2.astype(jnp.float32) * lk2.astype(jnp.float32))) + lam_init)
    scale = DA_HEAD_DIM ** -0.5
    qh = q.transpose(0, 2, 3, 1, 4)
    kh = k.transpose(0, 2, 3, 1, 4)
    vh = v.transpose(0, 2, 1, 3)
    nb = T // Q_BLOCK
    qb = qh.reshape(B, DA_HEADS, 2, nb, Q_BLOCK, DA_HEAD_DIM).transpose(3, 0, 1, 2, 4, 5)

    def block(qi):
        s = jnp.einsum('bhsqd,bhskd->bhsqk', qi, kh).astype(jnp.float32) * scale
        pr = jax.nn.softmax(s, axis=-1)
        a = pr[:, :, 0] - lam * pr[:, :, 1]
        return jnp.einsum('bhqk,bhkv->bhqv', a.astype(vh.dtype), vh)

    o = lax.map(block, qb)
    o = o.transpose(1, 0, 3, 2, 4).reshape(B, T, DA_HEADS, DA_V_DIM)
    o = rms_norm(o, subln_g) * (1.0 - lam_init)
    return o.reshape(B, T, DA_V_WIDTH)


def centred_shift(p):
    prev = jnp.pad(p[:, :-1], ((0, 0), (1, 0), (0, 0)))
    nxt = jnp.pad(p[:, 1:], ((0, 0), (0, 1), (0, 0)))
    return 0.5 * (prev + nxt)


def wkv7_scan(r, w, k, v, a, b, reverse):
    B, T, H, N = r.shape
    xs = tuple(t.astype(jnp.float32).transpose(1, 0, 2, 3) for t in (r, w, k, v, a, b))

    def step(S, inp):
        r_t, w_t, k_t, v_t, a_t, b_t = inp
        sa = jnp.einsum('bhvk,bhk->bhv', S, a_t)
        S = S * w_t[:, :, None, :] + sa[..., None] * b_t[:, :, None, :] + v_t[..., None] * k_t[:, :, None, :]
        return S, jnp.einsum('bhvk,bhk->bhv', S, r_t)

    S0 = jnp.zeros((B, H, N, N), jnp.float32)
    _, y = lax.scan(step, S0, xs, reverse=reverse)
    return y.transpose(1, 0, 2, 3)


def rwkv7_branch(p, shift_mu, w0_f, w2_f, w0_b, w2_b, a0_f, a2_f, a0_b, a2_b, g2,
                 k_k, k_a, r_k, ln_x_w, ln_x_b):
    B, T, _ = p.shape
    dt = p.dtype
    p = p + shift_mu * (centred_shift(p) - p)
    r, k, v, wd_f, wd_b, ad_f, ad_b, gd = jnp.split(p, RW_SPLITS, axis=-1)

    def decay(w0, wd, w2):
        wl = (w0 + jnp.tanh(wd) @ w2).astype(jnp.float32)
        return jnp.exp(-jnp.exp(-jax.nn.softplus(-wl) - 0.5))

    dec_f = decay(w0_f, wd_f, w2_f)
    dec_b = decay(w0_b, wd_b, w2_b)
    a_f = jax.nn.sigmoid(a0_f + ad_f @ a2_f)
    a_b = jax.nn.sigmoid(a0_b + ad_b @ a2_b)
    g = jax.nn.sigmoid(gd) @ g2

    hs = lambda t: t.reshape(B, T, RW_HEADS, RW_HEAD_DIM)
    r, k, v, a_f, a_b, dec_f, dec_b = map(hs, (r, k, v, a_f, a_b, dec_f, dec_b))
    kk = (k * k_k.reshape(RW_HEADS, RW_HEAD_DIM)).astype(jnp.float32)
    kk = kk / jnp.maximum(jnp.sqrt(jnp.sum(kk * kk, axis=-1, keepdims=True)), 1e-12)
    k_ah = k_a.reshape(RW_HEADS, RW_HEAD_DIM)
    k_f = k * (1 + (a_f - 1) * k_ah)
    k_b = k * (1 + (a_b - 1) * k_ah)
    y = (wkv7_scan(r, dec_f, k_f, v, -kk, kk * a_f, reverse=False)
         + wkv7_scan(r, dec_b, k_b, v, -kk, kk * a_b, reverse=True))
    mean = jnp.mean(y, axis=-1, keepdims=True)
    var = jnp.mean(jnp.square(y - mean), axis=-1, keepdims=True)
    y = ((y - mean) * lax.rsqrt(var + GN_EPS) * ln_x_w.reshape(RW_HEADS, RW_HEAD_DIM).astype(jnp.float32)
         + ln_x_b.reshape(RW_HEADS, RW_HEAD_DIM).astype(jnp.float32)).astype(dt)
    bonus = jnp.sum(r * (k_f + k_b) * r_k, axis=-1, keepdims=True) * v
    return (y + bonus).reshape(B, T, RW_WIDTH) * g


def expert_choice_ffn(hn, w_router, w_gate_e, w_up_e, w_down_e):
    B, T, D = hn.shape
    cap = CAPACITY_FACTOR * T // N_EXPERTS
    aff = jax.nn.softmax((hn @ w_router).astype(jnp.float32), axis=-1)
    gate, idx = lax.top_k(aff.transpose(0, 2, 1), cap)
    xe = jax.vmap(lambda h, i: h[i])(hn, idx)
    hid = jax.nn.silu(jnp.einsum('becd,edf->becf', xe, w_gate_e)) * jnp.einsum('becd,edf->becf', xe, w_up_e)
    ye = jnp.einsum('becf,efd->becd', hid, w_down_e) * gate[..., None].astype(hn.dtype)
    flat = (jnp.arange(B, dtype=jnp.int32)[:, None, None] * T + idx).reshape(-1)
    out = jnp.zeros((B * T, D), hn.dtype).at[flat].add(ye.reshape(-1, D))
    return out.reshape(B, T, D)


def setup_inputs(seed: int = 0) -> dict:
    key = jax.random.key(seed)
    ks = iter(jax.random.split(key, 40))
    f32 = jnp.float32
    nrm = lambda shape, s: jax.random.normal(next(ks), shape, f32) * s
    gain = lambda shape: 1.0 + 0.1 * jax.random.normal(next(ks), shape, f32)
    L = DEPTH
    return {
        "x": jax.random.normal(next(ks), (BATCH, SEQ, D_MODEL), f32),
        "attn_norm_g": gain((L, D_MODEL)),
        "w_in": nrm((L, D_MODEL, IN_COLS), D_MODEL ** -0.5),
        "q_norm_g": gain((L, DA_HEAD_DIM)),
        "k_norm_g": gain((L, DA_HEAD_DIM)),
        "lambda_q1": nrm((L, DA_HEAD_DIM), 0.1),
        "lambda_k1": nrm((L, DA_HEAD_DIM), 0.1),
        "lambda_q2": nrm((L, DA_HEAD_DIM), 0.1),
        "lambda_k2": nrm((L, DA_HEAD_DIM), 0.1),
        "subln_g": gain((L, DA_V_DIM)),
        "shift_mu": jax.random.uniform(next(ks), (L, RW_COLS), f32),
        "w0_f": jax.random.uniform(next(ks), (L, RW_WIDTH), f32, -5.0, -0.5),
        "w2_f": nrm((L, DECAY_LORA, RW_WIDTH), 0.5 * DECAY_LORA ** -0.5),
        "w0_b": jax.random.uniform(next(ks), (L, RW_WIDTH), f32, -5.0, -0.5),
        "w2_b": nrm((L, DECAY_LORA, RW_WIDTH), 0.5 * DECAY_LORA ** -0.5),
        "a0_f": nrm((L, RW_WIDTH), 0.5),
        "a2_f": nrm((L, AAA_LORA, RW_WIDTH), 0.5 * AAA_LORA ** -0.5),
        "a0_b": nrm((L, RW_WIDTH), 0.5),
        "a2_b": nrm((L, AAA_LORA, RW_WIDTH), 0.5 * AAA_LORA ** -0.5),
        "g2": nrm((L, GATE_LORA, RW_WIDTH), GATE_LORA ** -0.5),
        "k_k": 0.85 + 0.1 * jax.random.normal(next(ks), (L, RW_WIDTH), f32),
        "k_a": gain((L, RW_WIDTH)),
        "r_k": nrm((L, RW_HEADS, RW_HEAD_DIM), 0.1),
        "ln_x_w": gain((L, RW_WIDTH)),
        "ln_x_b": nrm((L, RW_WIDTH), 0.02),
        "w_branch_a": nrm((L, DA_V_WIDTH, D_MODEL), DA_V_WIDTH ** -0.5),
        "w_branch_b": nrm((L, RW_WIDTH, D_MODEL), RW_WIDTH ** -0.5),
        "w_out": nrm((L, D_MODEL, D_MODEL), D_MODEL ** -0.5),
        "ffn_norm_g": gain((L, D_MODEL)),
        "w_router": nrm((L, D_MODEL, N_EXPERTS), D_MODEL ** -0.5),
        "w_gate_e": nrm((L, N_EXPERTS, D_MODEL, EXPERT_FF), D_MODEL ** -0.5),
        "w_up_e": nrm((L, N_EXPERTS, D_MODEL, EXPERT_FF), D_MODEL ** -0.5),
        "w_down_e": nrm((L, N_EXPERTS, EXPERT_FF, D_MODEL), EXPERT_FF ** -0.5),
    }


def reference(x, attn_norm_g, w_in, q_norm_g, k_norm_g, lambda_q1, lambda_k1, lambda_q2, lambda_k2,
              subln_g, shift_mu, w0_f, w2_f, w0_b, w2_b, a0_f, a2_f, a0_b, a2_b, g2, k_k, k_a, r_k,
              ln_x_w, ln_x_b, w_branch_a, w_branch_b, w_out, ffn_norm_g, w_router,
              w_gate_e, w_up_e, w_down_e):
    B, T, _ = x.shape
    cos, sin = rope_tables(T)
    h = x
    for l in range(DEPTH):
        lam_init = 0.8 - 0.6 * math.exp(-0.3 * l)
        hn = rms_norm(h, attn_norm_g[l])
        proj = hn @ w_in[l]
        p_da = proj[..., :DA_COLS]
        p_rw = proj[..., DA_COLS:DA_COLS + RW_COLS]
        p_gate = proj[..., DA_COLS + RW_COLS:]
        y_a = diff_attention_branch(p_da, cos, sin, q_norm_g[l], k_norm_g[l], lambda_q1[l], lambda_k1[l],
                                    lambda_q2[l], lambda_k2[l], subln_g[l], lam_init)
        y_b = rwkv7_branch(p_rw, shift_mu[l], w0_f[l], w2_f[l], w0_b[l], w2_b[l], a0_f[l], a2_f[l],
                           a0_b[l], a2_b[l], g2[l], k_k[l], k_a[l], r_k[l], ln_x_w[l], ln_x_b[l])
        gates = jax.nn.sigmoid(p_gate)
        gate_a = gates[..., :D_MODEL]
        gate_b = gates[..., D_MODEL:]
        merged = gate_a * (y_a @ w_branch_a[l]) + gate_b * (y_b @ w_branch_b[l])
        h = h + merged @ w_out[l]
        h = h + expert_choice_ffn(rms_norm(h, ffn_norm_g[l]), w_router[l], w_gate_e[l], w_up_e[l], w_down_e[l])
    return h
```

```python
import os
import math
from contextlib import ExitStack
import numpy as np
import concourse.bass as bass
import concourse.mybir as mybir
from concourse.bass_utils import run_bass_kernel_spmd

F32 = mybir.dt.float32
BF16 = mybir.dt.bfloat16
I32 = mybir.dt.int32
U32 = mybir.dt.uint32
AF = mybir.ActivationFunctionType
ALU = mybir.AluOpType
AX = mybir.AxisListType

D = 2048
T = int(os.environ.get("KT", "4096"))
NT = T // 128
DA_COLS = 3072
RW_COLS = 3488
GATE_COLS = 4096
IN_COLS = DA_COLS + RW_COLS + GATE_COLS
TM_COLS = DA_COLS + RW_COLS
NE = 16
FF = 1024
CAP = T // 8
LAM_INIT = 0.8 - 0.6 * math.exp(-0.3 * 0)

DEBUG = bool(int(os.environ.get("KDEBUG", "0")))
SKIP_RWKV = bool(int(os.environ.get("KSKIP_RWKV", "0")))
CUT = int(os.environ.get("KCUT", "99"))
PHASES = os.environ.get("KPHASES", "1,2a,2b,3a,3b,3c,4a,4b,5g,6g").split(",")


class Buf:
    __slots__ = ("w", "r", "pr", "name")

    def __init__(self, name=""):
        self.w = {}
        self.r = {}
        self.pr = {}
        self.name = name


class Eng:
    def __init__(self, K, name, eng, is_pe=False):
        self.K = K
        self.name = name
        self.eng = eng
        self.is_pe = is_pe
        self.sem = K.new_sem("s_" + name)
        self.count = 0
        self.seen = {}
        self.dma_pool = []
        self.dma_i = 0

    def _wait(self, toks):
        for sem, val in toks.items():
            if self.is_pe and sem is self.sem:
                continue
            if self.seen.get(sem, 0) >= val:
                continue
            self.eng.wait_ge(sem, val)
            self.seen[sem] = val

    @staticmethod
    def _deps(reads, writes, pwrites):
        toks = {}

        def add(d):
            for s, v in d.items():
                if toks.get(s, 0) < v:
                    toks[s] = v
        for b in reads:
            add(b.w)
        for b in writes:
            add(b.w)
            add(b.r)
        for b in pwrites:
            add(b.r)
            add(b.pr)
        return toks

    @staticmethod
    def _commit(tok, reads, writes, pwrites):
        s, v = tok
        for b in reads:
            b.r[s] = v
        for b in writes:
            b.w = {s: v}
            b.pr = b.r
            b.r = {}
        for b in pwrites:
            b.w[s] = v
            for s2, v2 in b.r.items():
                if b.pr.get(s2, 0) < v2:
                    b.pr[s2] = v2
            b.r = {}

    def op(self, fn, reads=(), writes=(), pwrites=()):
        self._wait(self._deps(reads, writes, pwrites))
        ins = fn(self.eng)
        self.count += 1
        ins.then_inc(self.sem, 1)
        self._commit((self.sem, self.count), reads, writes, pwrites)
        return ins

    def dma(self, out, in_, reads=(), writes=(), pwrites=(), fn=None, **kw):
        self._wait(self._deps(reads, writes, pwrites))
        if len(self.dma_pool) < self.K.n_dma_sems:
            self.dma_pool.append([self.K.new_sem("d_%s%d" % (self.name, len(self.dma_pool))), 0])
        ent = self.dma_pool[self.dma_i % self.K.n_dma_sems]
        self.dma_i += 1
        sem, cur = ent
        if cur > 0 and self.seen.get(sem, 0) < cur:
            self.eng.wait_ge(sem, cur)
            self.seen[sem] = cur
        ins = fn(self.eng) if fn is not None else self.eng.dma_start(out=out, in_=in_, **kw)
        ent[1] = cur + 16
        ins.then_inc(sem, 16)
        self._commit((sem, cur + 16), reads, writes, pwrites)
        return ins


class Kern:
    def __init__(self, nc, n_dma_sems=10):
        self.nc = nc
        self.n_dma_sems = n_dma_sems
        self._sems = []
        self.E = {}

    def new_sem(self, name):
        cm = self.nc.semaphore(name)
        s = cm.__enter__()
        self._sems.append(cm)
        return s

    def setup(self, engines):
        for name, eng in engines.items():
            self.E[name] = Eng(self, name, eng, is_pe=(name == "pe"))

    def barrier(self):
        toks = {}
        for e in self.E.values():
            if e.count > 0:
                toks[e.sem] = e.count
            for sem, cur in e.dma_pool:
                if cur > 0:
                    toks[sem] = cur
        for e in self.E.values():
            for sem, val in toks.items():
                if sem is e.sem:
                    continue
                if e.seen.get(sem, 0) >= val:
                    continue
                e.eng.wait_ge(sem, val)
                e.seen[sem] = val


DECL = set()


def phase(name):
    if name in PHASES or name == "zero":
        with ExitStack() as st:
            yield st


def build():
    nc = bass.Bass("TRN2", target_bir_lowering=False)

    def din(name, shape, dt=F32):
        DECL.add(name)
        return nc.dram_tensor(name, list(shape), dt, kind="ExternalInput").ap()

    def dscr(name, shape, dt, dbg=False):
        kind = "ExternalOutput" if (dbg and DEBUG) else "Internal"
        return nc.dram_tensor(name, list(shape), dt, kind=kind).ap()

    x = din("x", [T, D])
    w_in = din("w_in", [D, IN_COLS])
    g1T = din("g1T", [128, 16])
    g2T = din("g2T", [128, 16])
    ident_in = din("ident", [128, 128])
    gqk = din("gqk", [128, 2, 64])
    rope_c = din("rope_c", [128, NT, 8])
    rope_s = din("rope_s", [128, NT, 8])
    lam_in = din("lam_in", [128, 4, 64])
    subg = din("subg", [128, 128])
    w_ba = din("w_ba", [1024, D])
    w_bb = din("w_bb", [1024, D])
    w_o = din("w_o", [D, D])
    w_r = din("w_r", [D, NE])
    if "6" in PHASES or "6g" in PHASES:
        w_g = din("w_g", [NE, D, FF])
        w_u = din("w_u", [NE, D, FF])
        w_d = din("w_d", [NE, FF, D])

    out = nc.dram_tensor("out", [T, D], F32, kind="ExternalOutput").ap()

    proj_tm = dscr("proj_tm", [T, TM_COLS], BF16, dbg=True)
    gateT = dscr("gateT", [GATE_COLS, T], BF16)
    qkT_d = dscr("qkT_d", [2048, T], BF16)
    y_a = dscr("y_a", [T, 1024], BF16, dbg=True)
    y_b = dscr("y_b", [T, 1024], BF16, dbg=True)
    mT_d = dscr("mT_d", [D, T], BF16)
    hn2T_d = dscr("hn2T_d", [D, T], BF16)
    aff_d = dscr("aff_d", [128, NT, NE], F32, dbg=True)
    xn2_d = dscr("xn2_d", [T, D], BF16)
    idx_d = dscr("idx_d", [NE, CAP], I32)
    val_d = dscr("val_d", [NE, CAP], F32)

    K = Kern(nc)
    K.setup({"sp": nc.sync, "act": nc.scalar, "dve": nc.vector, "pe": nc.tensor, "pool": nc.gpsimd})
    sp, act, dve, pe, pool = (K.E[n] for n in ("sp", "act", "dve", "pe", "pool"))

    gst = ExitStack()

    def sb(st, name, shape, dt):
        return st.enter_context(nc.sbuf_tensor(name, list(shape), dt))

    def ps(st, name, shape, dt):
        return st.enter_context(nc.psum_tensor(name, list(shape), dt))

    idf = sb(gst, "idf", [128, 128], F32)
    idb = sb(gst, "idb", [128, 128], BF16)
    b_idf, b_idb = Buf(), Buf()
    sp.dma(idf[:], ident_in, writes=[b_idf])
    dve.op(lambda e: e.tensor_copy(out=idb[:], in_=idf[:]), reads=[b_idf], writes=[b_idb])

    evq = [0]

    def evac_copy(dst, src, reads, writes=(), pwrites=()):
        evq[0] += 1
        if evq[0] % 2 == 0:
            dve.op(lambda e: e.tensor_copy(out=dst, in_=src), reads=reads, writes=writes, pwrites=pwrites)
        else:
            act.op(lambda e: e.activation(out=dst, in_=src, func=AF.Copy), reads=reads, writes=writes, pwrites=pwrites)

    def mm_group(ps_ap, ps_buf, pairs, reads):
        n = len(pairs)
        for i, (l, r) in enumerate(pairs):
            if i == 0:
                pe.op(lambda e: e.matmul(ps_ap, lhsT=l, rhs=r, start=True, stop=(n == 1)), reads=reads, writes=[ps_buf])
            else:
                pe.op(lambda e: e.matmul(ps_ap, lhsT=l, rhs=r, start=False, stop=(i == n - 1)), reads=reads, pwrites=[ps_buf])

    def rmsnorm_T(st, tag, src_tile, b_src, gT_t, b_gT, dstT_ap, b_dst, pts, b_pts, scratch, store_ap=None):
        junk, b_junk, ssq, b_ssq, xn, b_xn = scratch
        dve.op(lambda e: e.memset(ssq[:], 0.0), writes=[b_ssq])
        act.op(lambda e: e.activation(out=junk[:], in_=src_tile, func=AF.Square, accum_out=ssq[:]),
               reads=[b_src], writes=[b_junk, b_ssq])
        act.op(lambda e: e.activation(out=ssq[:], in_=ssq[:], func=AF.Sqrt, scale=1.0 / D, bias=1e-6),
               reads=[b_ssq], writes=[b_ssq])
        dve.op(lambda e: e.reciprocal(out=ssq[:], in_=ssq[:]), reads=[b_ssq], writes=[b_ssq])
        dve.op(lambda e: e.tensor_scalar(out=xn[:], in0=src_tile, scalar1=ssq[:, 0:1], scalar2=None, op0=ALU.mult),
               reads=[b_src, b_ssq], writes=[b_xn])
        if store_ap is not None:
            sp.dma(store_ap, xn[:], reads=[b_xn])
        for half in range(2):
            for c in range(8):
                cc = half * 8 + c
                pe.op(lambda e: e.transpose(out=pts[half][:, c, :], in_=xn[:, cc * 128:(cc + 1) * 128], identity=idb[:]),
                      reads=[b_xn, b_idb], writes=[b_pts[half]] if c == 0 else (), pwrites=() if c == 0 else [b_pts[half]])
            dve.op(lambda e: e.tensor_tensor(out=dstT_ap[:, half * 8:(half + 1) * 8, :], in0=pts[half][:],
                                             in1=gT_t[:, half * 8:(half + 1) * 8].unsqueeze(2).broadcast_to([128, 8, 128]),
                                             op=ALU.mult),
                   reads=[b_pts[half], b_gT], pwrites=[b_dst])

    for st in phase("1"):
        HT = min(2048, T)
        hnT = sb(st, "hnT", [128, 16, HT], BF16)
        xt = sb(st, "xt", [128, 2, D], F32)
        junk = sb(st, "junk1", [128, D], BF16)
        xn = sb(st, "xn1", [128, D], BF16)
        ssq = sb(st, "ssq1", [128, 1], F32)
        g1 = sb(st, "g1", [128, 16], F32)
        wbf = sb(st, "wbf", [128, 2, 16, 512], BF16)
        stage = sb(st, "stage1", [128, 4, 512], BF16)
        pt = [ps(st, "pt1a", [128, 8, 128], BF16), ps(st, "pt1b", [128, 8, 128], BF16)]
        pm = [ps(st, "pm1_%d" % i, [128, 512], F32) for i in range(4)]
        b_hnT = Buf(); b_xt = [Buf(), Buf()]; b_junk = Buf(); b_xn = Buf(); b_ssq = Buf(); b_g1 = Buf()
        b_wbf = [Buf(), Buf()]; b_stage = [Buf() for _ in range(4)]; b_pt = [Buf(), Buf()]; b_pm = [Buf() for _ in range(4)]
        sp.dma(g1[:], g1T, writes=[b_g1])
        w_v = w_in.rearrange("(c p) n -> p c n", p=128)
        chunks = []
        c0 = 0
        while c0 < TM_COLS:
            cw = min(512, TM_COLS - c0)
            chunks.append(("tm", c0, cw))
            c0 += cw
        for gc in range(GATE_COLS // 512):
            chunks.append(("gate", TM_COLS + gc * 512, 512))
        wi = 0
        si = 0
        pi = 0
        NH16 = HT // 128
        for half in range(T // HT):
            for t16 in range(NH16):
                tt = half * NH16 + t16
                bi = tt % 2
                sp.dma(xt[:, bi, :], x[tt * 128:(tt + 1) * 128, :], writes=[b_xt[bi]])
                rmsnorm_T(st, "p1", xt[:, bi, :], b_xt[bi], g1, b_g1, hnT[:, :, t16 * 128:(t16 + 1) * 128], b_hnT,
                          pt, b_pt, (junk, b_junk, ssq, b_ssq, xn, b_xn))
            for kind, c0, cw in chunks:
                wb = wi % 2
                wi += 1
                pool.dma(wbf[:, wb, :, 0:cw], w_v[:, :, c0:c0 + cw], writes=[b_wbf[wb]])
                if kind == "tm":
                    for t16 in range(NH16):
                        tt = half * NH16 + t16
                        p = pi % 4
                        pi += 1
                        mm_group(pm[p][:, 0:cw], b_pm[p],
                                 [(hnT[:, kc, t16 * 128:(t16 + 1) * 128], wbf[:, wb, kc, 0:cw]) for kc in range(16)],
                                 reads=[b_hnT, b_wbf[wb]])
                        s = si % 4
                        si += 1
                        evac_copy(stage[:, s, 0:cw], pm[p][:, 0:cw], reads=[b_pm[p]], writes=[b_stage[s]])
                        sp.dma(proj_tm[tt * 128:(tt + 1) * 128, c0:c0 + cw], stage[:, s, 0:cw], reads=[b_stage[s]])
                else:
                    gcol = c0 - TM_COLS
                    for blk in range(4):
                        for tc4 in range(HT // 512):
                            p = pi % 4
                            pi += 1
                            mm_group(pm[p][:], b_pm[p],
                                     [(wbf[:, wb, kc, blk * 128:(blk + 1) * 128], hnT[:, kc, tc4 * 512:(tc4 + 1) * 512]) for kc in range(16)],
                                     reads=[b_hnT, b_wbf[wb]])
                            s = si % 4
                            si += 1
                            act.op(lambda e: e.activation(out=stage[:, s, :], in_=pm[p][:], func=AF.Sigmoid),
                                   reads=[b_pm[p]], writes=[b_stage[s]])
                            sp.dma(gateT[gcol + blk * 128:gcol + (blk + 1) * 128, half * HT + tc4 * 512:half * HT + (tc4 + 1) * 512],
                                   stage[:, s, :], reads=[b_stage[s]])
        K.barrier()

    for st in phase("2a"):
        qk = sb(st, "qk", [128, 2, 2048], BF16)
        sq = sb(st, "sq", [128, 32, 64], F32)
        ss = sb(st, "ss", [128, 32], F32)
        qn = sb(st, "qn", [128, 32, 64], F32)
        tmp = sb(st, "ropetmp", [128, 4, 32, 8], F32)
        qb = sb(st, "qb", [128, 32, 64], BF16)
        gq = sb(st, "gq", [128, 2, 64], F32)
        rc = sb(st, "rc", [128, NT, 8], F32)
        rs_ = sb(st, "rs", [128, NT, 8], F32)
        stg = sb(st, "stg2", [128, 2, 16, 512], BF16)
        pt = [ps(st, "pt2a", [128, 8, 128], BF16), ps(st, "pt2b", [128, 8, 128], BF16)]
        b_qk = [Buf(), Buf()]; b_sq = Buf(); b_ss = Buf(); b_qn = Buf(); b_tmp = Buf(); b_qb = Buf()
        b_c = Buf(); b_stg = [Buf(), Buf()]; b_pt = [Buf(), Buf()]
        sp.dma(gq[:], gqk, writes=[b_c])
        sp.dma(rc[:], rope_c, pwrites=[b_c])
        sp.dma(rs_[:], rope_s, pwrites=[b_c])
        for tt in range(NT):
            bi = tt % 2
            sp.dma(qk[:, bi, :], proj_tm[tt * 128:(tt + 1) * 128, 0:2048], writes=[b_qk[bi]])
            qv = qk[:, bi, :].rearrange("p (g d) -> p g d", d=64)
            dve.op(lambda e: e.tensor_tensor(out=sq[:], in0=qv, in1=qv, op=ALU.mult), reads=[b_qk[bi]], writes=[b_sq])
            dve.op(lambda e: e.tensor_reduce(out=ss[:], in_=sq[:], axis=AX.X, op=ALU.add), reads=[b_sq], writes=[b_ss])
            act.op(lambda e: e.activation(out=ss[:], in_=ss[:], func=AF.Sqrt, scale=1.0 / 64, bias=1e-6), reads=[b_ss], writes=[b_ss])
            dve.op(lambda e: e.reciprocal(out=ss[:], in_=ss[:]), reads=[b_ss], writes=[b_ss])
            dve.op(lambda e: e.tensor_tensor(out=qn[:], in0=qv, in1=ss[:].unsqueeze(2).broadcast_to([128, 32, 64]), op=ALU.mult),
                   reads=[b_qk[bi], b_ss], writes=[b_qn])
            for i in range(2):
                dve.op(lambda e: e.tensor_tensor(out=qn[:, i * 16:(i + 1) * 16, :], in0=qn[:, i * 16:(i + 1) * 16, :],
                                                 in1=gq[:, i:i + 1, :].broadcast_to([128, 16, 64]), op=ALU.mult),
                       reads=[b_qn, b_c], writes=[b_qn])
            cb = rc[:, tt:tt + 1, :].broadcast_to([128, 32, 8])
            sbb = rs_[:, tt:tt + 1, :].broadcast_to([128, 32, 8])
            x1 = qn[:, :, 0:8]
            x2 = qn[:, :, 8:16]
            dve.op(lambda e: e.tensor_tensor(out=tmp[:, 0], in0=x1, in1=cb, op=ALU.mult), reads=[b_qn, b_c], writes=[b_tmp])
            dve.op(lambda e: e.tensor_tensor(out=tmp[:, 1], in0=x2, in1=sbb, op=ALU.mult), reads=[b_qn, b_c], pwrites=[b_tmp])
            dve.op(lambda e: e.tensor_tensor(out=tmp[:, 2], in0=x2, in1=cb, op=ALU.mult), reads=[b_qn, b_c], pwrites=[b_tmp])
            dve.op(lambda e: e.tensor_tensor(out=tmp[:, 3], in0=x1, in1=sbb, op=ALU.mult), reads=[b_qn, b_c], pwrites=[b_tmp])
            dve.op(lambda e: e.tensor_copy(out=qb[:], in_=qn[:]), reads=[b_qn], writes=[b_qb])
            dve.op(lambda e: e.tensor_tensor(out=qb[:, :, 0:8], in0=tmp[:, 0], in1=tmp[:, 1], op=ALU.subtract), reads=[b_tmp], writes=[b_qb])
            dve.op(lambda e: e.tensor_tensor(out=qb[:, :, 8:16], in0=tmp[:, 2], in1=tmp[:, 3], op=ALU.add), reads=[b_tmp], writes=[b_qb])
            qbf = qb[:].rearrange("p g d -> p (g d)")
            sg = (tt // 4) % 2
            for half in range(2):
                for c in range(8):
                    cc = half * 8 + c
                    pe.op(lambda e: e.transpose(out=pt[half][:, c, :], in_=qbf[:, cc * 128:(cc + 1) * 128], identity=idb[:]),
                          reads=[b_qb, b_idb], writes=[b_pt[half]] if c == 0 else (), pwrites=() if c == 0 else [b_pt[half]])
                evac_copy(stg[:, sg, half * 8:(half + 1) * 8, (tt % 4) * 128:(tt % 4 + 1) * 128], pt[half][:],
                          reads=[b_pt[half]], writes=[b_stg[sg]] if (tt % 4 == 0 and half == 0) else (),
                          pwrites=() if (tt % 4 == 0 and half == 0) else [b_stg[sg]])
            if tt % 4 == 3:
                t0 = (tt // 4) * 512
                sp.dma(qkT_d.rearrange("(c p) t -> p c t", p=128)[:, :, t0:t0 + 512], stg[:, sg, :, :], reads=[b_stg[sg]])
        K.barrier()

    for st in phase("2b"):
        V1 = sb(st, "V1", [128, NT, 8, 144], BF16)
        qT = sb(st, "qT", [128, 2, T], BF16)
        kT1 = sb(st, "kT1", [128, 2, T], BF16)
        kT2 = sb(st, "kT2", [128, 2, T], BF16)
        PT = sb(st, "PT", [128, 3, 512], BF16)
        lamt = sb(st, "lamt", [128, 4, 64], F32)
        lamp = sb(st, "lamp", [128, 2, 64], F32)
        lam2 = sb(st, "lam2", [128, 2], F32)
        lam = sb(st, "lam", [128, 1], F32)
        sgt = sb(st, "sgt", [128, 128], F32)
        rr = sb(st, "rr", [128, 2], F32)
        o1 = sb(st, "o1", [128, 128], F32)
        o2 = sb(st, "o2", [128, 128], F32)
        osq = sb(st, "osq", [128, 128], F32)
        oms = sb(st, "oms", [128, 1], F32)
        yst = sb(st, "yst", [128, 2, 128], BF16)
        pS = [ps(st, "pS%d" % i, [128, 512], F32) for i in range(2)]
        pO = [ps(st, "pO%d" % i, [128, 2, 256], F32) for i in range(4)]
        b_V1 = Buf(); b_V1z = Buf(); b_q = [Buf(), Buf()]; b_k1 = [Buf(), Buf()]; b_k2 = [Buf(), Buf()]
        b_PT = [Buf() for _ in range(3)]; b_pS = [Buf(), Buf()]; b_pO = [[b_, b_] for b_ in (Buf(), Buf(), Buf(), Buf())]
        b_lam = Buf(); b_sg = Buf(); b_rr = Buf(); b_o1 = Buf(); b_o2 = Buf(); b_osq = Buf(); b_oms = Buf(); b_yst = [Buf(), Buf()]
        sp.dma(lamt[:], lam_in, writes=[b_lam])
        sp.dma(sgt[:], subg, writes=[b_sg])
        dve.op(lambda e: e.tensor_tensor(out=lamp[:, 0, :], in0=lamt[:, 0, :], in1=lamt[:, 1, :], op=ALU.mult), reads=[b_lam], writes=[b_lam])
        dve.op(lambda e: e.tensor_tensor(out=lamp[:, 1, :], in0=lamt[:, 2, :], in1=lamt[:, 3, :], op=ALU.mult), reads=[b_lam], writes=[b_lam])
        dve.op(lambda e: e.tensor_reduce(out=lam2[:], in_=lamp[:], axis=AX.X, op=ALU.add), reads=[b_lam], writes=[b_lam])
        act.op(lambda e: e.activation(out=lam2[:], in_=lam2[:], func=AF.Exp), reads=[b_lam], writes=[b_lam])
        dve.op(lambda e: e.tensor_tensor(out=lam[:], in0=lam2[:, 0:1], in1=lam2[:, 1:2], op=ALU.subtract), reads=[b_lam], writes=[b_lam])
        dve.op(lambda e: e.tensor_scalar(out=lam[:], in0=lam[:], scalar1=LAM_INIT, scalar2=None, op0=ALU.add), reads=[b_lam], writes=[b_lam])
        pool.op(lambda e: e.memset(kT1[:], 0.0), writes=[b_k1[0], b_k1[1]])
        pool.op(lambda e: e.memset(kT2[:], 0.0), writes=[b_k2[0], b_k2[1]])
        pool.op(lambda e: e.memset(V1[:], 1.0), writes=[b_V1, b_V1z])
        for tt in range(NT):
            sp.dma(V1[:, tt, :, 0:128], proj_tm[tt * 128:(tt + 1) * 128, 2048:3072].rearrange("p (h v) -> p h v", v=128), reads=[b_V1z], pwrites=[b_V1])
        sci = 0
        pti = 0
        for h in range(8):
            hb = h % 2
            sp.dma(qT[:, hb, :], qkT_d[h * 128:(h + 1) * 128, :], writes=[b_q[hb]])
            sp.dma(kT1[0:64, hb, :], qkT_d[1024 + h * 128:1024 + h * 128 + 64, :], writes=[b_k1[hb]])
            sp.dma(kT2[64:128, hb, :], qkT_d[1024 + h * 128 + 64:1024 + (h + 1) * 128, :], writes=[b_k2[hb]])
            kTs = [kT1, kT2]
            b_ks = [b_k1, b_k2]
            for qc in range(T // 512):
                for kt in range(NT):
                    for s in range(2):
                        pb = sci % 2
                        sci += 1
                        pe.op(lambda e: e.matmul(pS[pb][:], lhsT=kTs[s][:, hb, kt * 128:(kt + 1) * 128], rhs=qT[:, hb, qc * 512:(qc + 1) * 512],
                                                 start=True, stop=True),
                              reads=[b_ks[s][hb], b_q[hb]], writes=[b_pS[pb]])
                        pi_ = pti % 3
                        pti += 1
                        act.op(lambda e: e.activation(out=PT[:, pi_, :], in_=pS[pb][:], func=AF.Exp, scale=0.125),
                               reads=[b_pS[pb]], writes=[b_PT[pi_]])
                        for qs in range(4):
                            if kt == 0:
                                pe.op(lambda e: e.matmul(pO[qs][:, s, 0:129], lhsT=PT[:, pi_, qs * 128:(qs + 1) * 128], rhs=V1[:, kt, h, 0:129],
                                                         start=(s == 0), stop=False, skip_group_check=True),
                                      reads=[b_PT[pi_], b_V1], writes=[b_pO[qs][s]])
                            else:
                                pe.op(lambda e: e.matmul(pO[qs][:, s, 0:129], lhsT=PT[:, pi_, qs * 128:(qs + 1) * 128], rhs=V1[:, kt, h, 0:129],
                                                         start=False, stop=(kt == NT - 1), skip_group_check=True),
                                      reads=[b_PT[pi_], b_V1], pwrites=[b_pO[qs][s]])
                for qs in range(4):
                    qt = qc * 4 + qs
                    dve.op(lambda e: e.tensor_copy(out=rr[:, 0:1], in_=pO[qs][:, 0, 128:129]), reads=[b_pO[qs][0]], writes=[b_rr])
                    dve.op(lambda e: e.tensor_copy(out=rr[:, 1:2], in_=pO[qs][:, 1, 128:129]), reads=[b_pO[qs][1]], writes=[b_rr])
                    dve.op(lambda e: e.reciprocal(out=rr[:], in_=rr[:]), reads=[b_rr], writes=[b_rr])
                    dve.op(lambda e: e.tensor_tensor(out=rr[:, 1:2], in0=rr[:, 1:2], in1=lam[:], op=ALU.mult), reads=[b_rr, b_lam], writes=[b_rr])
                    dve.op(lambda e: e.tensor_scalar(out=o1[:], in0=pO[qs][:, 0, 0:128], scalar1=rr[:, 0:1], scalar2=None, op0=ALU.mult),
                           reads=[b_pO[qs][0], b_rr], writes=[b_o1])
                    dve.op(lambda e: e.tensor_scalar(out=o2[:], in0=pO[qs][:, 1, 0:128], scalar1=rr[:, 1:2], scalar2=None, op0=ALU.mult),
                           reads=[b_pO[qs][1], b_rr], writes=[b_o2])
                    dve.op(lambda e: e.tensor_tensor(out=o1[:], in0=o1[:], in1=o2[:], op=ALU.subtract), reads=[b_o1, b_o2], writes=[b_o1])
                    dve.op(lambda e: e.memset(oms[:], 0.0), writes=[b_oms])
                    act.op(lambda e: e.activation(out=osq[:], in_=o1[:], func=AF.Square, accum_out=oms[:]), reads=[b_o1], writes=[b_osq, b_oms])
                    act.op(lambda e: e.activation(out=oms[:], in_=oms[:], func=AF.Sqrt, scale=1.0 / 128, bias=1e-6), reads=[b_oms], writes=[b_oms])
                    dve.op(lambda e: e.reciprocal(out=oms[:], in_=oms[:]), reads=[b_oms], writes=[b_oms])
                    dve.op(lambda e: e.tensor_scalar(out=o1[:], in0=o1[:], scalar1=oms[:, 0:1], scalar2=(1.0 - LAM_INIT), op0=ALU.mult, op1=ALU.mult),
                           reads=[b_o1, b_oms], writes=[b_o1])
                    yb_ = qt % 2
                    dve.op(lambda e: e.tensor_tensor(out=yst[:, yb_, :], in0=o1[:], in1=sgt[:], op=ALU.mult), reads=[b_o1, b_sg], writes=[b_yst[yb_]])
                    sp.dma(y_a[qt * 128:(qt + 1) * 128, h * 128:(h + 1) * 128], yst[:, yb_, :], reads=[b_yst[yb_]])
        K.barrier()


    mu_in = din("mu_rep", [128, RW_COLS])
    w0_in = din("w0_rep", [128, 2, 1024])
    a0_in = din("a0_rep", [128, 2, 1024])
    kk_in = din("kk_rep", [128, 1024])
    ka_in = din("ka_rep", [128, 1024])
    rk_in = din("rk_rep", [128, 1024])
    lnw_in = din("lnw_rep", [128, 1024])
    lnb_in = din("lnb_rep", [128, 1024])
    w2_in = din("w2_in", [4, 64, 1024])
    g2_in = din("g2_in", [160, 1024])
    masks_in = din("masks_in", [128, 4, 128])
    RWS = {n: dscr("rw_" + n, [T, 1024], F32, dbg=True) for n in
           ("R", "V", "KK", "LW0", "LW1", "BB0", "BB1", "KE0", "KE1", "G", "BON", "Y0", "Y1")}

    def rwkv_phase():
        for st in phase("3a"):
            P = sb(st, "rP", [128, 3, RW_COLS], BF16)
            xs = sb(st, "rxs", [128, RW_COLS], F32)
            tt_ = sb(st, "rtt", [128, RW_COLS], F32)
            mu = sb(st, "rmu", [128, RW_COLS], F32)
            w0 = sb(st, "rw0", [128, 2, 1024], F32)
            a0 = sb(st, "ra0", [128, 2, 1024], F32)
            kkc = sb(st, "rkkc", [128, 1024], F32)
            kac = sb(st, "rkac", [128, 1024], F32)
            rkc = sb(st, "rrkc", [128, 1024], F32)
            w2 = sb(st, "rw2", [64, 4, 1024], BF16)
            g2a = sb(st, "rg2a", [128, 1024], BF16)
            g2b = sb(st, "rg2b", [32, 1024], BF16)
            L = sb(st, "rL", [128, 416], BF16)
            LT = sb(st, "rLT", [128, 6, 128], BF16)
            asg = sb(st, "rasg", [128, 2, 1024], F32)
            o = [sb(st, "ro%d" % i, [128, 1024], F32) for i in range(4)]
            kk = sb(st, "rkk", [128, 1024], F32)
            ke = sb(st, "rke", [128, 2, 1024], F32)
            s16 = sb(st, "rs16", [128, 16], F32)
            pl = ps(st, "rpl", [128, 6, 128], BF16)
            pm = [ps(st, "rpm%d" % i, [128, 2, 512], F32) for i in range(2)]
            b_P = Buf(); b_Pz = Buf(); b_xs = Buf(); b_tt = Buf(); b_c = Buf(); b_L = Buf(); b_LT = Buf(); b_asg = Buf()
            b_o = [Buf() for _ in range(4)]; b_kk = Buf(); b_ke = Buf(); b_s16 = Buf(); b_pl = Buf(); b_pm = [Buf(), Buf()]
            sp.dma(mu[:], mu_in, writes=[b_c])
            sp.dma(w0[:], w0_in, pwrites=[b_c])
            sp.dma(a0[:], a0_in, pwrites=[b_c])
            sp.dma(kkc[:], kk_in, pwrites=[b_c])
            sp.dma(kac[:], ka_in, pwrites=[b_c])
            sp.dma(rkc[:], rk_in, pwrites=[b_c])
            pool.dma(w2[:], w2_in.rearrange("f k n -> k f n"), pwrites=[b_c])
            pool.dma(g2a[:], g2_in[0:128, :], pwrites=[b_c])
            pool.dma(g2b[:], g2_in[128:160, :], pwrites=[b_c])
            oi = [0]

            def outbuf():
                oi[0] += 1
                return oi[0] % 4

            def store(name, tt, i):
                sp.dma(RWS[name][tt * 128:(tt + 1) * 128, :], o[i][:], reads=[b_o[i]])

            for tt in range(NT):
                r0 = tt * 128
                dve.op(lambda e: e.memset(P[:, 1:3, :], 0.0), writes=[b_P, b_Pz])
                sp.dma(P[:, 0, :], proj_tm[r0:r0 + 128, DA_COLS:TM_COLS], pwrites=[b_P])
                if tt == 0:
                    sp.dma(P[1:128, 1, :], proj_tm[0:127, DA_COLS:TM_COLS], reads=[b_Pz], pwrites=[b_P])
                else:
                    sp.dma(P[:, 1, :], proj_tm[r0 - 1:r0 + 127, DA_COLS:TM_COLS], reads=[b_Pz], pwrites=[b_P])
                if tt == NT - 1:
                    sp.dma(P[0:127, 2, :], proj_tm[r0 + 1:r0 + 128, DA_COLS:TM_COLS], reads=[b_Pz], pwrites=[b_P])
                else:
                    sp.dma(P[:, 2, :], proj_tm[r0 + 1:r0 + 129, DA_COLS:TM_COLS], reads=[b_Pz], pwrites=[b_P])
                dve.op(lambda e: e.tensor_tensor(out=tt_[:], in0=P[:, 1, :], in1=P[:, 2, :], op=ALU.add), reads=[b_P], writes=[b_tt])
                dve.op(lambda e: e.scalar_tensor_tensor(out=tt_[:], in0=tt_[:], scalar=0.5, in1=P[:, 0, :], op0=ALU.mult, op1=ALU.subtract),
                       reads=[b_tt, b_P], writes=[b_tt])
                dve.op(lambda e: e.tensor_tensor(out=tt_[:], in0=tt_[:], in1=mu[:], op=ALU.mult), reads=[b_tt, b_c], writes=[b_tt])
                dve.op(lambda e: e.tensor_tensor(out=xs[:], in0=tt_[:], in1=P[:, 0, :], op=ALU.add), reads=[b_tt, b_P], writes=[b_xs])
                rr_ = xs[:, 0:1024]
                kk_ = xs[:, 1024:2048]
                vv_ = xs[:, 2048:3072]
                if CUT <= 1:
                    continue
                act.op(lambda e: e.activation(out=L[:, 0:128], in_=xs[:, 3072:3200], func=AF.Tanh), reads=[b_xs], writes=[b_L])
                act.op(lambda e: e.activation(out=L[:, 128:256], in_=xs[:, 3200:3328], func=AF.Copy), reads=[b_xs], pwrites=[b_L])
                act.op(lambda e: e.activation(out=L[:, 256:416], in_=xs[:, 3328:3488], func=AF.Sigmoid), reads=[b_xs], pwrites=[b_L])
                for i in range(4):
                    pe.op(lambda e: e.transpose(out=pl[0:64, i, :], in_=L[:, i * 64:(i + 1) * 64], identity=idb[:]),
                          reads=[b_L, b_idb], writes=[b_pl] if i == 0 else (), pwrites=() if i == 0 else [b_pl])
                pe.op(lambda e: e.transpose(out=pl[:, 4, :], in_=L[:, 256:384], identity=idb[:]), reads=[b_L, b_idb], pwrites=[b_pl])
                pe.op(lambda e: e.transpose(out=pl[0:32, 5, :], in_=L[:, 384:416], identity=idb[:]), reads=[b_L, b_idb], pwrites=[b_pl])
                dve.op(lambda e: e.tensor_copy(out=LT[0:64, 0:4, :], in_=pl[0:64, 0:4, :]), reads=[b_pl], writes=[b_LT])
                dve.op(lambda e: e.tensor_copy(out=LT[:, 4, :], in_=pl[:, 4, :]), reads=[b_pl], pwrites=[b_LT])
                dve.op(lambda e: e.tensor_copy(out=LT[0:32, 5, :], in_=pl[0:32, 5, :]), reads=[b_pl], pwrites=[b_LT])
                if CUT <= 2:
                    continue
                i = outbuf()
                dve.op(lambda e: e.tensor_copy(out=o[i][:], in_=rr_), reads=[b_xs], writes=[b_o[i]])
                store("R", tt, i)
                i = outbuf()
                dve.op(lambda e: e.tensor_copy(out=o[i][:], in_=vv_), reads=[b_xs], writes=[b_o[i]])
                store("V", tt, i)
                if CUT <= 3:
                    continue
                for d in range(2):
                    p = pm[d % 2]
                    for hf in range(2):
                        pe.op(lambda e: e.matmul(p[:, hf, :], lhsT=LT[0:64, d, :], rhs=w2[:, d, hf * 512:(hf + 1) * 512], start=True, stop=True),
                              reads=[b_LT, b_c], writes=[b_pm[d % 2]] if hf == 0 else (), pwrites=() if hf == 0 else [b_pm[d % 2]])
                    i = outbuf()
                    dve.op(lambda e: e.tensor_tensor(out=o[i][:], in0=p[:].rearrange("p a b -> p (a b)"), in1=w0[:, d, :], op=ALU.add),
                           reads=[b_pm[d % 2], b_c], writes=[b_o[i]])
                    act.op(lambda e: e.activation(out=o[i][:], in_=o[i][:], func=AF.Sigmoid), reads=[b_o[i]], writes=[b_o[i]])
                    dve.op(lambda e: e.tensor_scalar(out=o[i][:], in0=o[i][:], scalar1=-math.exp(-0.5), scalar2=None, op0=ALU.mult),
                           reads=[b_o[i]], writes=[b_o[i]])
                    store("LW%d" % d, tt, i)
                if CUT <= 4:
                    continue
                for d in range(2):
                    p = pm[d % 2]
                    for hf in range(2):
                        pe.op(lambda e: e.matmul(p[:, hf, :], lhsT=LT[0:64, 2 + d, :], rhs=w2[:, 2 + d, hf * 512:(hf + 1) * 512], start=True, stop=True),
                              reads=[b_LT, b_c], writes=[b_pm[d % 2]] if hf == 0 else (), pwrites=() if hf == 0 else [b_pm[d % 2]])
                    dve.op(lambda e: e.tensor_tensor(out=asg[:, d, :], in0=p[:].rearrange("p a b -> p (a b)"), in1=a0[:, d, :], op=ALU.add),
                           reads=[b_pm[d % 2], b_c], writes=[b_asg] if d == 0 else (), pwrites=() if d == 0 else [b_asg])
                act.op(lambda e: e.activation(out=asg[:], in_=asg[:], func=AF.Sigmoid), reads=[b_asg], writes=[b_asg])
                if CUT <= 5:
                    continue
                p = pm[0]
                for hf in range(2):
                    pe.op(lambda e: e.matmul(p[:, hf, :], lhsT=LT[:, 4, :], rhs=g2a[:, hf * 512:(hf + 1) * 512], start=True, stop=False),
                          reads=[b_LT, b_c], writes=[b_pm[0]] if hf == 0 else (), pwrites=() if hf == 0 else [b_pm[0]])
                    pe.op(lambda e: e.matmul(p[:, hf, :], lhsT=LT[0:32, 5, :], rhs=g2b[:, hf * 512:(hf + 1) * 512], start=False, stop=True),
                          reads=[b_LT, b_c], pwrites=[b_pm[0]])
                i = outbuf()
                act.op(lambda e: e.activation(out=o[i][:], in_=p[:].rearrange("p a b -> p (a b)"), func=AF.Copy), reads=[b_pm[0]], writes=[b_o[i]])
                store("G", tt, i)
                if CUT <= 6:
                    continue
                dve.op(lambda e: e.tensor_tensor(out=kk[:], in0=kk_, in1=kkc[:], op=ALU.mult), reads=[b_xs, b_c], writes=[b_kk])
                dve.op(lambda e: e.tensor_tensor(out=tt_[:, 0:1024], in0=kk[:], in1=kk[:], op=ALU.mult), reads=[b_kk], writes=[b_tt])
                dve.op(lambda e: e.tensor_reduce(out=s16[:], in_=tt_[:, 0:1024].rearrange("p (h d) -> p h d", d=64), axis=AX.X, op=ALU.add),
                       reads=[b_tt], writes=[b_s16])
                act.op(lambda e: e.activation(out=s16[:], in_=s16[:], func=AF.Sqrt), reads=[b_s16], writes=[b_s16])
                dve.op(lambda e: e.tensor_scalar(out=s16[:], in0=s16[:], scalar1=1e-12, scalar2=None, op0=ALU.max), reads=[b_s16], writes=[b_s16])
                dve.op(lambda e: e.reciprocal(out=s16[:], in_=s16[:]), reads=[b_s16], writes=[b_s16])
                i = outbuf()
                dve.op(lambda e: e.tensor_tensor(out=o[i][:].rearrange("p (h d) -> p h d", d=64), in0=kk[:].rearrange("p (h d) -> p h d", d=64),
                                                 in1=s16[:].unsqueeze(2).broadcast_to([128, 16, 64]), op=ALU.mult),
                       reads=[b_kk, b_s16], writes=[b_o[i]])
                ikk = i
                for d in range(2):
                    i = outbuf()
                    dve.op(lambda e: e.tensor_tensor(out=o[i][:], in0=o[ikk][:], in1=asg[:, d, :], op=ALU.mult), reads=[b_o[ikk], b_asg], writes=[b_o[i]])
                    store("BB%d" % d, tt, i)
                dve.op(lambda e: e.tensor_scalar(out=o[ikk][:], in0=o[ikk][:], scalar1=-1.0, scalar2=None, op0=ALU.mult), reads=[b_o[ikk]], writes=[b_o[ikk]])
                store("KK", tt, ikk)
                for d in range(2):
                    dve.op(lambda e: e.tensor_tensor(out=ke[:, d, :], in0=asg[:, d, :], in1=kac[:], op=ALU.mult),
                           reads=[b_asg, b_c], writes=[b_ke] if d == 0 else (), pwrites=() if d == 0 else [b_ke])
                    dve.op(lambda e: e.tensor_tensor(out=ke[:, d, :], in0=ke[:, d, :], in1=kac[:], op=ALU.subtract),
                           reads=[b_ke, b_c], writes=[b_ke])
                    dve.op(lambda e: e.tensor_tensor(out=ke[:, d, :], in0=ke[:, d, :], in1=kk_, op=ALU.mult),
                           reads=[b_ke, b_xs], writes=[b_ke])
                    dve.op(lambda e: e.tensor_tensor(out=ke[:, d, :], in0=ke[:, d, :], in1=kk_, op=ALU.add),
                           reads=[b_ke, b_xs], writes=[b_ke])
                    i = outbuf()
                    dve.op(lambda e: e.tensor_copy(out=o[i][:], in_=ke[:, d, :]), reads=[b_ke], writes=[b_o[i]])
                    store("KE%d" % d, tt, i)
                if CUT <= 7:
                    continue
                dve.op(lambda e: e.tensor_tensor(out=tt_[:, 0:1024], in0=ke[:, 0, :], in1=ke[:, 1, :], op=ALU.add), reads=[b_ke], writes=[b_tt])
                dve.op(lambda e: e.tensor_tensor(out=tt_[:, 0:1024], in0=tt_[:, 0:1024], in1=rr_, op=ALU.mult), reads=[b_tt, b_xs], writes=[b_tt])
                dve.op(lambda e: e.tensor_tensor(out=tt_[:, 0:1024], in0=tt_[:, 0:1024], in1=rkc[:], op=ALU.mult), reads=[b_tt, b_c], writes=[b_tt])
                dve.op(lambda e: e.tensor_reduce(out=s16[:], in_=tt_[:, 0:1024].rearrange("p (h d) -> p h d", d=64), axis=AX.X, op=ALU.add),
                       reads=[b_tt], writes=[b_s16])
                i = outbuf()
                dve.op(lambda e: e.tensor_tensor(out=o[i][:].rearrange("p (h d) -> p h d", d=64), in0=vv_.rearrange("p (h d) -> p h d", d=64),
                                                 in1=s16[:].unsqueeze(2).broadcast_to([128, 16, 64]), op=ALU.mult),
                       reads=[b_xs, b_s16], writes=[b_o[i]])
                store("BON", tt, i)
            K.barrier()

        for st in phase("3b"):
            mk = sb(st, "smk", [128, 4, 128], F32)
            ld = sb(st, "sld", [128, 2, 6, 1024], F32)
            e4 = sb(st, "se4", [128, 4, 1024], F32)
            bfs = sb(st, "sbfs", [128, 7, 1024], BF16)
            dG = sb(st, "sdG", [64, 1024], F32)
            XT = sb(st, "sXT", [64, 4, 4, 128], BF16)
            pr5 = sb(st, "spr5", [128, 5, 4, 128], BF16)
            Xp = sb(st, "sXp", [128, 2, 4, 192], BF16)
            Pp = sb(st, "sPp", [128, 2, 2, 4, 128], BF16)
            Rh = sb(st, "sRh", [64, 16, 128], BF16)
            Qm = sb(st, "sQm", [128, 16, 128], BF16)
            Gm = sb(st, "sGm", [64, 16, 64], BF16)
            Hm = sb(st, "sHm", [128, 16, 64], BF16)
            ST = sb(st, "sST", [64, 16, 64], BF16)
            yo = sb(st, "syo", [128, 2, 1024], F32)
            pT = [ps(st, "spT%d" % i, [64, 8, 128], BF16) for i in range(2)]
            pA = ps(st, "spA", [128, 2, 512], F32)
            pB = ps(st, "spB", [128, 2, 512], F32)
            pX = ps(st, "spX", [128, 2, 512], F32)
            b_mk = Buf(); b_ld = [Buf(), Buf()]; b_e4 = Buf(); b_bfs = Buf(); b_dG = Buf(); b_XT = Buf(); b_pr5 = Buf()
            b_Xp = [Buf(), Buf()]; b_Pp = [Buf(), Buf()]; b_Rh = Buf(); b_Qm = Buf(); b_Gm = Buf(); b_Hm = Buf(); b_ST = Buf()
            b_yo = [Buf(), Buf()]; b_pT = [Buf(), Buf()]; b_pA = [Buf(), Buf()]; b_pB = [Buf(), Buf()]; b_pX = [Buf(), Buf()]
            slot = [(pA, 0, b_pA[0]), (pA, 1, b_pA[1]), (pB, 0, b_pB[0]), (pB, 1, b_pB[1]), (pX, 0, b_pX[0]), (pX, 1, b_pX[1])]

            def sl(i, shape3):
                t_, j, b = slot[i]
                a, bb_ = shape3
                return t_[:, j, 0:a * bb_].rearrange("p (a b) -> p a b", b=bb_), b

            sp.dma(mk[:], masks_in, writes=[b_mk])
            SU, SL_, UI, LI = 0, 1, 2, 3
            li = 0
            yi = 0
            for d in range(2):
                cum_i, cum_s, mb_s, mb_i, ma_s = (UI, SL_, SU, UI, SL_) if d == 0 else (LI, SU, SL_, LI, SU)
                dve.op(lambda e: e.memset(ST[:], 0.0), writes=[b_ST])
                corder = range(NT) if d == 0 else range(NT - 1, -1, -1)
                names = ("R", "V", "KK", "LW%d" % d, "BB%d" % d, "KE%d" % d)
                for c in corder:
                    lb = li % 2
                    li += 1
                    for qi, nm in enumerate(names):
                        sp.dma(ld[:, lb, qi, :], RWS[nm][c * 128:(c + 1) * 128, :],
                               writes=[b_ld[lb]] if qi == 0 else (), pwrites=() if qi == 0 else [b_ld[lb]])
                    r_, v_, kk_, lw_, bb_, ke_ = (ld[:, lb, qi, :] for qi in range(6))
                    for hf in range(2):
                        pe.op(lambda e: e.matmul(pA[:, hf, :], lhsT=mk[:, cum_i, :], rhs=lw_[:, hf * 512:(hf + 1) * 512], start=True, stop=True),
                              reads=[b_mk, b_ld[lb]], writes=[b_pA[hf]])
                        pe.op(lambda e: e.matmul(pB[:, hf, :], lhsT=mk[:, cum_s, :], rhs=lw_[:, hf * 512:(hf + 1) * 512], start=True, stop=True),
                              reads=[b_mk, b_ld[lb]], writes=[b_pB[hf]])
                    gam = pA[:].rearrange("p a b -> p (a b)")
                    gsf = pB[:].rearrange("p a b -> p (a b)")
                    act.op(lambda e: e.activation(out=e4[:, 0, :], in_=gam, func=AF.Exp), reads=b_pA, writes=[b_e4])
                    act.op(lambda e: e.activation(out=e4[:, 2, :], in_=gam, func=AF.Exp, scale=-1.0), reads=b_pA, pwrites=[b_e4])
                    act.op(lambda e: e.activation(out=e4[:, 3, :], in_=gsf, func=AF.Exp), reads=b_pB, pwrites=[b_e4])
                    act.op(lambda e: e.activation(out=e4[:, 1, :], in_=lw_, func=AF.Exp, scale=-1.0), reads=[b_ld[lb]], pwrites=[b_e4])
                    dve.op(lambda e: e.tensor_tensor(out=e4[:, 1, :], in0=e4[:, 1, :], in1=e4[:, 0, :], op=ALU.mult), reads=[b_e4], pwrites=[b_e4])
                    dve.op(lambda e: e.tensor_tensor(out=dG[:], in0=e4[0:64, 0, :], in1=e4[0:64, 3, :], op=ALU.mult),
                           reads=[b_e4], writes=[b_dG])
                    dve.op(lambda e: e.tensor_tensor(out=dG[:].rearrange("p (h k) -> p h k", k=64), in0=dG[:].rearrange("p (h k) -> p h k", k=64),
                                                     in1=idf[0:64, 0:64].unsqueeze(1).broadcast_to([64, 16, 64]), op=ALU.mult),
                           reads=[b_dG, b_idf], writes=[b_dG])
                    dve.op(lambda e: e.tensor_tensor(out=bfs[:, 0, :], in0=kk_, in1=e4[:, 1, :], op=ALU.mult),
                           reads=[b_ld[lb], b_e4], writes=[b_bfs])
                    dve.op(lambda e: e.tensor_tensor(out=bfs[:, 1, :], in0=bb_, in1=e4[:, 2, :], op=ALU.mult), reads=[b_ld[lb], b_e4], pwrites=[b_bfs])
                    dve.op(lambda e: e.tensor_tensor(out=bfs[:, 2, :], in0=ke_, in1=e4[:, 2, :], op=ALU.mult), reads=[b_ld[lb], b_e4], pwrites=[b_bfs])
                    dve.op(lambda e: e.tensor_tensor(out=bfs[:, 3, :], in0=r_, in1=e4[:, 0, :], op=ALU.mult), reads=[b_ld[lb], b_e4], pwrites=[b_bfs])
                    dve.op(lambda e: e.tensor_tensor(out=bfs[:, 4, :], in0=bb_, in1=e4[:, 3, :], op=ALU.mult), reads=[b_ld[lb], b_e4], pwrites=[b_bfs])
                    dve.op(lambda e: e.tensor_tensor(out=bfs[:, 5, :], in0=ke_, in1=e4[:, 3, :], op=ALU.mult), reads=[b_ld[lb], b_e4], pwrites=[b_bfs])
                    act.op(lambda e: e.activation(out=bfs[:, 6, :], in_=v_, func=AF.Copy), reads=[b_ld[lb], b_bfs], pwrites=[b_bfs])
                    for g4 in range(4):
                        for hh in range(4):
                            h = g4 * 4 + hh
                            tb_ = hh // 2
                            for kd in range(4):
                                first = (hh % 2 == 0 and kd == 0)
                                pe.op(lambda e: e.transpose(out=pT[tb_][:, (hh % 2) * 4 + kd, :], in_=bfs[:, kd, h * 64:(h + 1) * 64], identity=idb[:]),
                                      reads=[b_bfs, b_idb], writes=[b_pT[tb_]] if first else (), pwrites=() if first else [b_pT[tb_]])
                        for tb_ in range(2):
                            evac_copy(XT[:, tb_ * 2:(tb_ + 1) * 2, :, :], pT[tb_][:].rearrange("p (a k) t -> p a k t", k=4), reads=[b_pT[tb_]],
                                      writes=[b_XT] if tb_ == 0 else (), pwrites=() if tb_ == 0 else [b_XT])
                        A_, B_, K_, R_ = 0, 1, 2, 3
                        prods = [(B_, A_, mb_s), (A_, B_, ma_s), (A_, K_, ma_s), (B_, R_, mb_i), (K_, R_, mb_i)]
                        for pi_, (l_, r2_, m_) in enumerate(prods):
                            ap3, bslot = sl(pi_, (4, 128))
                            for hh in range(4):
                                pe.op(lambda e: e.matmul(ap3[:, hh, :], lhsT=XT[:, hh, l_, :], rhs=XT[:, hh, r2_, :], start=True, stop=True),
                                      reads=[b_XT], writes=[bslot] if hh == 0 else (), pwrites=() if hh == 0 else [bslot])
                            dve.op(lambda e: e.tensor_tensor(out=pr5[:, pi_, :, :], in0=ap3, in1=mk[:, m_, :].unsqueeze(1).broadcast_to([128, 4, 128]), op=ALU.mult),
                                   reads=[bslot, b_mk], writes=[b_pr5] if pi_ == 0 else (), pwrites=() if pi_ == 0 else [b_pr5])
                        act.op(lambda e: e.activation(out=Xp[:, 0, :, 0:64], in_=bfs[:, 0, g4 * 256:(g4 + 1) * 256].rearrange("p (h k) -> p h k", k=64), func=AF.Copy),
                               reads=[b_bfs], writes=[b_Xp[0]])
                        act.op(lambda e: e.activation(out=Xp[:, 0, :, 64:192], in_=pr5[:, 2, :, :], func=AF.Copy), reads=[b_pr5], pwrites=[b_Xp[0]])
                        xi = 0
                        for it in range(7):
                            if it == 0:
                                Pc, PTc, bP = pr5[:, 0], pr5[:, 1], b_pr5
                            else:
                                Pc, PTc, bP = Pp[:, it % 2, 0], Pp[:, it % 2, 1], b_Pp[it % 2]
                            xa = pX[:].rearrange("p a (h x) -> p (a h) x", x=256)[:, :, 0:192]
                            for hh in range(4):
                                pe.op(lambda e: e.matmul(xa[:, hh, :], lhsT=Pc[:, hh, :], rhs=Xp[:, xi, hh, :], start=True, stop=True),
                                      reads=[bP, b_Xp[xi]], writes=[b_pX[0], b_pX[1]] if hh == 0 else (), pwrites=() if hh == 0 else [b_pX[0], b_pX[1]])
                            dve.op(lambda e: e.tensor_tensor(out=Xp[:, 1 - xi, :, :], in0=xa, in1=Xp[:, xi, :, :], op=ALU.add),
                                   reads=[b_pX[0], b_pX[1], b_Xp[xi]], writes=[b_Xp[1 - xi]])
                            xi = 1 - xi
                            if it < 6:
                                nP = (it + 1) % 2
                                a0_, bs0 = sl(0, (4, 128))
                                a1_, bs1 = sl(1, (4, 128))
                                for hh in range(4):
                                    pe.op(lambda e: e.matmul(a0_[:, hh, :], lhsT=PTc[:, hh, :], rhs=Pc[:, hh, :], start=True, stop=True),
                                          reads=[bP], writes=[bs0] if hh == 0 else (), pwrites=() if hh == 0 else [bs0])
                                for hh in range(4):
                                    pe.op(lambda e: e.matmul(a1_[:, hh, :], lhsT=Pc[:, hh, :], rhs=PTc[:, hh, :], start=True, stop=True),
                                          reads=[bP], writes=[bs1] if hh == 0 else (), pwrites=() if hh == 0 else [bs1])
                                act.op(lambda e: e.activation(out=Pp[:, nP, 0], in_=a0_, func=AF.Copy), reads=[bs0], writes=[b_Pp[nP]])
                                act.op(lambda e: e.activation(out=Pp[:, nP, 1], in_=a1_, func=AF.Copy), reads=[bs1], pwrites=[b_Pp[nP]])
                        Xf = Xp[:, xi]
                        bXf = b_Xp[xi]
                        hs = slice(g4 * 4, g4 * 4 + 4)
                        a2_, bs2 = sl(2, (4, 128))
                        for hh in range(4):
                            pe.op(lambda e: e.matmul(a2_[0:64, hh, :], lhsT=Xf[:, hh, 0:64], rhs=pr5[:, 3, hh, :], start=True, stop=True),
                                  reads=[bXf, b_pr5], writes=[bs2] if hh == 0 else (), pwrites=() if hh == 0 else [bs2])
                        dve.op(lambda e: e.tensor_tensor(out=Rh[:, hs, :], in0=a2_[0:64], in1=XT[:, :, 3, :], op=ALU.add),
                               reads=[bs2, b_XT], pwrites=[b_Rh])
                        a3_, bs3 = sl(3, (4, 128))
                        for hh in range(4):
                            pe.op(lambda e: e.matmul(a3_[:, hh, :], lhsT=Xf[:, hh, 64:192], rhs=pr5[:, 3, hh, :], start=True, stop=True),
                                  reads=[bXf, b_pr5], writes=[bs3] if hh == 0 else (), pwrites=() if hh == 0 else [bs3])
                        dve.op(lambda e: e.tensor_tensor(out=Qm[:, hs, :], in0=a3_, in1=pr5[:, 4, :, :], op=ALU.add),
                               reads=[bs3, b_pr5], pwrites=[b_Qm])
                        a0_, bs0 = sl(0, (4, 64))
                        a1_, bs1 = sl(1, (4, 64))
                        for hh in range(4):
                            h = g4 * 4 + hh
                            pe.op(lambda e: e.matmul(a0_[0:64, hh, :], lhsT=Xf[:, hh, 0:64], rhs=bfs[:, 4, h * 64:(h + 1) * 64], start=True, stop=True),
                                  reads=[bXf, b_bfs], writes=[bs0] if hh == 0 else (), pwrites=() if hh == 0 else [bs0])
                        for hh in range(4):
                            h = g4 * 4 + hh
                            pe.op(lambda e: e.matmul(a1_[:, hh, :], lhsT=Xf[:, hh, 64:192], rhs=bfs[:, 4, h * 64:(h + 1) * 64], start=True, stop=True),
                                  reads=[bXf, b_bfs], writes=[bs1] if hh == 0 else (), pwrites=() if hh == 0 else [bs1])
                        dve.op(lambda e: e.tensor_tensor(out=Gm[:, hs, :], in0=a0_[0:64], in1=dG[:, g4 * 256:(g4 + 1) * 256].rearrange("p (h k) -> p h k", k=64), op=ALU.add),
                               reads=[bs0, b_dG], pwrites=[b_Gm])
                        dve.op(lambda e: e.tensor_tensor(out=Hm[:, hs, :], in0=a1_, in1=bfs[:, 5, g4 * 256:(g4 + 1) * 256].rearrange("p (h k) -> p h k", k=64), op=ALU.add),
                               reads=[bs1, b_bfs], pwrites=[b_Hm])
                    yv = pA[:].rearrange("p a (h v) -> p (a h) v", v=64)
                    sv = pB[0:64].rearrange("p a (h v) -> p (a h) v", v=64)
                    for h in range(16):
                        vh = bfs[:, 6, h * 64:(h + 1) * 64]
                        pe.op(lambda e: e.matmul(yv[:, h, :], lhsT=Rh[:, h, :], rhs=ST[:, h, :], start=True, stop=False),
                              reads=[b_Rh, b_ST], writes=b_pA if h == 0 else (), pwrites=() if h == 0 else b_pA)
                        pe.op(lambda e: e.matmul(yv[:, h, :], lhsT=Qm[:, h, :], rhs=vh, start=False, stop=True),
                              reads=[b_Qm, b_bfs], pwrites=b_pA)
                        pe.op(lambda e: e.matmul(sv[:, h, :], lhsT=Gm[:, h, :], rhs=ST[:, h, :], start=True, stop=False),
                              reads=[b_Gm, b_ST], writes=b_pB if h == 0 else (), pwrites=() if h == 0 else b_pB)
                        pe.op(lambda e: e.matmul(sv[:, h, :], lhsT=Hm[:, h, :], rhs=vh, start=False, stop=True),
                              reads=[b_Hm, b_bfs], pwrites=b_pB)
                    yb2 = yi % 2
                    yi += 1
                    act.op(lambda e: e.activation(out=yo[:, yb2, :], in_=pA[:].rearrange("p a b -> p (a b)"), func=AF.Copy), reads=b_pA, writes=[b_yo[yb2]])
                    dve.op(lambda e: e.tensor_copy(out=ST[:], in_=sv), reads=b_pB, writes=[b_ST])
                    sp.dma(RWS["Y%d" % d][c * 128:(c + 1) * 128, :], yo[:, yb2, :], reads=[b_yo[yb2]])
            K.barrier()

        for st in phase("3c"):
            ld = sb(st, "cld", [128, 2, 4, 1024], F32)
            lw_ = sb(st, "clw", [128, 1024], F32)
            lb_ = sb(st, "clb", [128, 1024], F32)
            y = sb(st, "cy", [128, 1024], F32)
            sq = sb(st, "csq", [128, 1024], F32)
            m16 = sb(st, "cm16", [128, 16], F32)
            v16 = sb(st, "cv16", [128, 16], F32)
            yb16 = sb(st, "cyb", [128, 2, 1024], BF16)
            b_ld = [Buf(), Buf()]; b_c = Buf(); b_y = Buf(); b_sq = Buf(); b_m = Buf(); b_v = Buf(); b_yb = [Buf(), Buf()]
            sp.dma(lw_[:], lnw_in, writes=[b_c])
            sp.dma(lb_[:], lnb_in, pwrites=[b_c])
            h3 = lambda ap: ap.rearrange("p (h d) -> p h d", d=64)
            for tt in range(NT):
                lb = tt % 2
                for qi, nm in enumerate(("Y0", "Y1", "BON", "G")):
                    sp.dma(ld[:, lb, qi, :], RWS[nm][tt * 128:(tt + 1) * 128, :], writes=[b_ld[lb]] if qi == 0 else (), pwrites=() if qi == 0 else [b_ld[lb]])
                dve.op(lambda e: e.tensor_tensor(out=y[:], in0=ld[:, lb, 0, :], in1=ld[:, lb, 1, :], op=ALU.add), reads=[b_ld[lb]], writes=[b_y])
                dve.op(lambda e: e.tensor_reduce(out=m16[:], in_=h3(y[:]), axis=AX.X, op=ALU.add), reads=[b_y], writes=[b_m])
                dve.op(lambda e: e.tensor_scalar(out=m16[:], in0=m16[:], scalar1=1.0 / 64, scalar2=None, op0=ALU.mult), reads=[b_m], writes=[b_m])
                dve.op(lambda e: e.tensor_tensor(out=h3(y[:]), in0=h3(y[:]), in1=m16[:].unsqueeze(2).broadcast_to([128, 16, 64]), op=ALU.subtract),
                       reads=[b_y, b_m], writes=[b_y])
                dve.op(lambda e: e.tensor_tensor(out=sq[:], in0=y[:], in1=y[:], op=ALU.mult), reads=[b_y], writes=[b_sq])
                dve.op(lambda e: e.tensor_reduce(out=v16[:], in_=h3(sq[:]), axis=AX.X, op=ALU.add), reads=[b_sq], writes=[b_v])
                act.op(lambda e: e.activation(out=v16[:], in_=v16[:], func=AF.Sqrt, scale=1.0 / 64, bias=64e-5), reads=[b_v], writes=[b_v])
                dve.op(lambda e: e.reciprocal(out=v16[:], in_=v16[:]), reads=[b_v], writes=[b_v])
                dve.op(lambda e: e.tensor_tensor(out=h3(y[:]), in0=h3(y[:]), in1=v16[:].unsqueeze(2).broadcast_to([128, 16, 64]), op=ALU.mult),
                       reads=[b_y, b_v], writes=[b_y])
                dve.op(lambda e: e.tensor_tensor(out=y[:], in0=y[:], in1=lw_[:], op=ALU.mult), reads=[b_y, b_c], writes=[b_y])
                dve.op(lambda e: e.tensor_tensor(out=y[:], in0=y[:], in1=lb_[:], op=ALU.add), reads=[b_y, b_c], writes=[b_y])
                dve.op(lambda e: e.tensor_tensor(out=y[:], in0=y[:], in1=ld[:, lb, 2, :], op=ALU.add), reads=[b_y, b_ld[lb]], writes=[b_y])
                dve.op(lambda e: e.tensor_tensor(out=yb16[:, lb, :], in0=y[:], in1=ld[:, lb, 3, :], op=ALU.mult), reads=[b_y, b_ld[lb]], writes=[b_yb[lb]])
                sp.dma(y_b[tt * 128:(tt + 1) * 128, :], yb16[:, lb, :], reads=[b_yb[lb]])
            K.barrier()

    if SKIP_RWKV:
        for st in phase("zero"):
            z = sb(st, "zz", [128, 1024], BF16)
            b_z = Buf()
            dve.op(lambda e: e.memset(z[:], 0.0), writes=[b_z])
            for tt in range(NT):
                sp.dma(y_b[tt * 128:(tt + 1) * 128, :], z[:], reads=[b_z])
            K.barrier()
    else:
        rwkv_phase()

    for st in phase("4a"):
        Wa = sb(st, "Wa", [128, 8, D], BF16)
        Wb = sb(st, "Wb", [128, 8, D], BF16)
        yt = sb(st, "yt", [128, 2, 1024], BF16)
        yT = sb(st, "yT", [128, 2, 8, 512], BF16)
        gt = sb(st, "gt", [128, 2, 2, 512], BF16)
        ma = sb(st, "ma", [128, 512], F32)
        mbt = sb(st, "mbt", [128, 512], F32)
        mst = sb(st, "mst", [128, 2, 512], BF16)
        pt = [ps(st, "pt4a", [128, 8, 128], BF16), ps(st, "pt4b", [128, 8, 128], BF16)]
        pm = [ps(st, "pm4_%d" % i, [128, 512], F32) for i in range(4)]
        b_W = Buf(); b_yt = [Buf(), Buf()]; b_yT = [Buf(), Buf()]; b_gt = [Buf(), Buf()]; b_ma = Buf(); b_mb = Buf()
        b_mst = [Buf(), Buf()]; b_pt = [Buf(), Buf()]; b_pm = [Buf() for _ in range(4)]
        pool.dma(Wa[:], w_ba.rearrange("(c p) n -> p c n", p=128), writes=[b_W])
        pool.dma(Wb[:], w_bb.rearrange("(c p) n -> p c n", p=128), pwrites=[b_W])
        li = 0
        gi = 0
        for tb in range(T // 512):
            for br, ysrc in enumerate((y_a, y_b)):
                for t4 in range(4):
                    tt = tb * 4 + t4
                    lb = li % 2
                    li += 1
                    sp.dma(yt[:, lb, :], ysrc[tt * 128:(tt + 1) * 128, :], writes=[b_yt[lb]])
                    for c in range(8):
                        pe.op(lambda e: e.transpose(out=pt[lb][:, c, :], in_=yt[:, lb, c * 128:(c + 1) * 128], identity=idb[:]),
                              reads=[b_yt[lb], b_idb], writes=[b_pt[lb]] if c == 0 else (), pwrites=() if c == 0 else [b_pt[lb]])
                    evac_copy(yT[:, br, :, t4 * 128:(t4 + 1) * 128], pt[lb][:], reads=[b_pt[lb]],
                              writes=[b_yT[br]] if t4 == 0 else (), pwrites=() if t4 == 0 else [b_yT[br]])
            for fb in range(16):
                gb = gi % 2
                gi += 1
                sp.dma(gt[:, gb, 0, :], gateT[fb * 128:(fb + 1) * 128, tb * 512:(tb + 1) * 512], writes=[b_gt[gb]])
                sp.dma(gt[:, gb, 1, :], gateT[D + fb * 128:D + (fb + 1) * 128, tb * 512:(tb + 1) * 512], pwrites=[b_gt[gb]])
                pa = (2 * fb) % 4
                pb = (2 * fb + 1) % 4
                mm_group(pm[pa][:], b_pm[pa], [(Wa[:, c, fb * 128:(fb + 1) * 128], yT[:, 0, c, :]) for c in range(8)], reads=[b_W, b_yT[0]])
                mm_group(pm[pb][:], b_pm[pb], [(Wb[:, c, fb * 128:(fb + 1) * 128], yT[:, 1, c, :]) for c in range(8)], reads=[b_W, b_yT[1]])
                dve.op(lambda e: e.tensor_tensor(out=ma[:], in0=pm[pa][:], in1=gt[:, gb, 0, :], op=ALU.mult), reads=[b_pm[pa], b_gt[gb]], writes=[b_ma])
                dve.op(lambda e: e.tensor_tensor(out=mbt[:], in0=pm[pb][:], in1=gt[:, gb, 1, :], op=ALU.mult), reads=[b_pm[pb], b_gt[gb]], writes=[b_mb])
                dve.op(lambda e: e.tensor_tensor(out=mst[:, gb, :], in0=ma[:], in1=mbt[:], op=ALU.add), reads=[b_ma, b_mb], writes=[b_mst[gb]])
                sp.dma(mT_d[fb * 128:(fb + 1) * 128, tb * 512:(tb + 1) * 512], mst[:, gb, :], reads=[b_mst[gb]])
        K.barrier()

    aff = sb(gst, "aff", [128, NT, NE], F32)
    b_aff = Buf()
    for st in phase("4b"):
        Wo = sb(st, "Wo", [128, 16, D], BF16)
        Wr = sb(st, "Wr", [128, 16, NE], BF16)
        g2 = sb(st, "g2", [128, 16], F32)
        mT = sb(st, "mT", [128, 2, 16, 512], BF16)
        xt = sb(st, "xt4", [128, 2, D], F32)
        ht = sb(st, "ht", [128, 2, D], F32)
        junk = sb(st, "junk4", [128, D], BF16)
        xn = sb(st, "xn4", [128, D], BF16)
        ssq = sb(st, "ssq4", [128, 1], F32)
        hst = sb(st, "hst", [128, 2, 16, 512], BF16)
        lg = sb(st, "lg", [128, NE], F32)
        mx = sb(st, "mx", [128, 1], F32)
        sm = sb(st, "sm", [128, 1], F32)
        pt = [ps(st, "pt5a", [128, 8, 128], BF16), ps(st, "pt5b", [128, 8, 128], BF16)]
        pm = [ps(st, "pm5_%d" % i, [128, 512], F32) for i in range(4)]
        pr = ps(st, "pr5", [128, NE], F32)
        b_Wo = Buf(); b_Wr = Buf(); b_g2 = Buf(); b_mT = [Buf(), Buf()]; b_xt = [Buf(), Buf()]; b_ht = [Buf(), Buf()]
        b_junk = Buf(); b_xn = Buf(); b_ssq = Buf(); b_hst = [Buf(), Buf()]; b_lg = Buf(); b_mx = Buf(); b_sm = Buf()
        b_pt = [Buf(), Buf()]; b_pm = [Buf() for _ in range(4)]; b_pr = Buf()
        pool.dma(Wo[:], w_o.rearrange("(c p) n -> p c n", p=128), writes=[b_Wo])
        pool.dma(Wr[:], w_r.rearrange("(c p) n -> p c n", p=128), writes=[b_Wr])
        sp.dma(g2[:], g2T, writes=[b_g2])
        pi = 0
        for tb in range(T // 512):
            mb = tb % 2
            sp.dma(mT[:, mb, :, :], mT_d.rearrange("(c p) t -> p c t", p=128)[:, :, tb * 512:(tb + 1) * 512], writes=[b_mT[mb]])
            for t4 in range(4):
                tt = tb * 4 + t4
                bi = tt % 2
                sp.dma(xt[:, bi, :], x[tt * 128:(tt + 1) * 128, :], writes=[b_xt[bi]])
                for cc in range(4):
                    p = pi % 4
                    pi += 1
                    mm_group(pm[p][:], b_pm[p],
                             [(mT[:, mb, kc, t4 * 128:(t4 + 1) * 128], Wo[:, kc, cc * 512:(cc + 1) * 512]) for kc in range(16)],
                             reads=[b_mT[mb], b_Wo])
                    dve.op(lambda e: e.tensor_tensor(out=ht[:, bi, cc * 512:(cc + 1) * 512], in0=pm[p][:], in1=xt[:, bi, cc * 512:(cc + 1) * 512], op=ALU.add),
                           reads=[b_pm[p], b_xt[bi]], writes=[b_ht[bi]] if cc == 0 else (), pwrites=() if cc == 0 else [b_ht[bi]])
                sp.dma(out[tt * 128:(tt + 1) * 128, :], ht[:, bi, :], reads=[b_ht[bi]])
                rmsnorm_T(st, "p4", ht[:, bi, :], b_ht[bi], g2, b_g2, hst[:, mb, :, t4 * 128:(t4 + 1) * 128], b_hst[mb],
                          pt, b_pt, (junk, b_junk, ssq, b_ssq, xn, b_xn), store_ap=xn2_d[tt * 128:(tt + 1) * 128, :])
                mm_group(pr[:], b_pr, [(hst[:, mb, kc, t4 * 128:(t4 + 1) * 128], Wr[:, kc, :]) for kc in range(16)], reads=[b_hst[mb], b_Wr])
                dve.op(lambda e: e.tensor_reduce(out=mx[:], in_=pr[:], axis=AX.X, op=ALU.max), reads=[b_pr], writes=[b_mx])
                dve.op(lambda e: e.tensor_scalar(out=mx[:], in0=mx[:], scalar1=-1.0, scalar2=None, op0=ALU.mult), reads=[b_mx], writes=[b_mx])
                dve.op(lambda e: e.memset(sm[:], 0.0), writes=[b_sm])
                act.op(lambda e: e.activation(out=lg[:], in_=pr[:], func=AF.Exp, bias=mx[:, 0:1], accum_out=sm[:]),
                       reads=[b_pr, b_mx], writes=[b_lg, b_sm])
                dve.op(lambda e: e.reciprocal(out=sm[:], in_=sm[:]), reads=[b_sm], writes=[b_sm])
                dve.op(lambda e: e.tensor_scalar(out=aff[:, tt, :], in0=lg[:], scalar1=sm[:, 0:1], scalar2=None, op0=ALU.mult),
                       reads=[b_lg, b_sm], pwrites=[b_aff])
            sp.dma(hn2T_d.rearrange("(c p) t -> p c t", p=128)[:, :, tb * 512:(tb + 1) * 512], hst[:, mb, :, :], reads=[b_hst[mb]])
        if DEBUG:
            sp.dma(aff_d, aff[:], reads=[b_aff])
        K.barrier()

    coef = sb(gst, "coef", [128, NT, NE], F32)
    b_coef = Buf()
    for st in phase("5"):
        affT = sb(st, "affT", [NE, T], F32)
        work = sb(st, "work", [NE, T], F32)
        cT = sb(st, "cT", [NE, T], F32)
        m8 = sb(st, "m8", [NE, 8], F32)
        pa = [ps(st, "pa%d" % i, [NE, 4, 128], F32) for i in range(2)]
        pc = [ps(st, "pc%d" % i, [128, 4, NE], F32) for i in range(2)]
        b_affT = Buf(); b_work = Buf(); b_cT = Buf(); b_m8 = Buf(); b_pa = [Buf(), Buf()]; b_pc = [Buf(), Buf()]
        for g in range(NT // 4):
            pb = g % 2
            for j in range(4):
                tt = g * 4 + j
                pe.op(lambda e: e.transpose(out=pa[pb][:, j, :], in_=aff[:, tt, :], identity=idf[:]),
                      reads=[b_aff, b_idf], writes=[b_pa[pb]] if j == 0 else (), pwrites=() if j == 0 else [b_pa[pb]])
            dve.op(lambda e: e.tensor_copy(out=affT[:, g * 512:(g + 1) * 512], in_=pa[pb][:].rearrange("p a b -> p (a b)")),
                   reads=[b_pa[pb]], pwrites=[b_affT])
        dve.op(lambda e: e.tensor_copy(out=work[:], in_=affT[:]), reads=[b_affT], writes=[b_work])
        for r in range(CAP // 8):
            dve.op(lambda e: e.max(out=m8[:], in_=work[:]), reads=[b_work], writes=[b_m8])
            if r < CAP // 8 - 1:
                dve.op(lambda e: e.match_replace(out=work[:], in_to_replace=m8[:], in_values=work[:], imm_value=-1.0),
                       reads=[b_m8, b_work], writes=[b_work])
        dve.op(lambda e: e.scalar_tensor_tensor(out=cT[:], in0=affT[:], scalar=m8[:, 7:8], in1=affT[:], op0=ALU.is_ge, op1=ALU.mult),
               reads=[b_affT, b_m8], writes=[b_cT])
        for g in range(NT // 4):
            pb = g % 2
            for j in range(4):
                tt = g * 4 + j
                pe.op(lambda e: e.transpose(out=pc[pb][:, j, :], in_=cT[:, tt * 128:(tt + 1) * 128], identity=idf[0:NE, 0:NE]),
                      reads=[b_cT, b_idf], writes=[b_pc[pb]] if j == 0 else (), pwrites=() if j == 0 else [b_pc[pb]])
            dve.op(lambda e: e.tensor_copy(out=coef[:, g * 4:(g + 1) * 4, :], in_=pc[pb][:]), reads=[b_pc[pb]], pwrites=[b_coef])
        K.barrier()

    for st in phase("6"):
        Wg = sb(st, "Wg", [128, 16, FF], BF16)
        Wu = sb(st, "Wu", [128, 16, FF], BF16)
        Wd = sb(st, "Wd", [128, 8, D], BF16)
        hT = sb(st, "hT6", [128, 2, 16, 512], BF16)
        sg = sb(st, "sg6", [128, 2, 512], F32)
        hid = sb(st, "hid6", [128, 2, 8, 512], BF16)
        yo = sb(st, "yo6", [128, 2, D], F32)
        pg = [ps(st, "pg6_%d" % i, [128, 512], F32) for i in range(2)]
        pu = [ps(st, "pu6_%d" % i, [128, 512], F32) for i in range(2)]
        po = [ps(st, "po6_%d" % i, [128, 512], F32) for i in range(4)]
        b_Wg = Buf(); b_Wu = Buf(); b_Wd = Buf(); b_hT = [Buf(), Buf()]; b_sg = [Buf(), Buf()]; b_hid = [Buf(), Buf()]
        b_yo = [Buf(), Buf()]; b_pg = [Buf(), Buf()]; b_pu = [Buf(), Buf()]; b_po = [Buf() for _ in range(4)]
        b_out = [Buf() for _ in range(NT)]
        hi = 0
        fi = 0
        oi = 0
        yi = 0
        for ex in range(NE):
            pool.dma(Wg[:], w_g[ex].rearrange("(c p) n -> p c n", p=128), writes=[b_Wg])
            pool.dma(Wu[:], w_u[ex].rearrange("(c p) n -> p c n", p=128), writes=[b_Wu])
            pool.dma(Wd[:], w_d[ex].rearrange("(c p) n -> p c n", p=128), writes=[b_Wd])
            for tb in range(T // 512):
                hb = hi % 2
                hi += 1
                sp.dma(hT[:, hb, :, :], hn2T_d.rearrange("(c p) t -> p c t", p=128)[:, :, tb * 512:(tb + 1) * 512], writes=[b_hT[hb]])
                for fb in range(8):
                    f2 = fi % 2
                    fi += 1
                    mm_group(pg[f2][:], b_pg[f2], [(Wg[:, kc, fb * 128:(fb + 1) * 128], hT[:, hb, kc, :]) for kc in range(16)], reads=[b_Wg, b_hT[hb]])
                    mm_group(pu[f2][:], b_pu[f2], [(Wu[:, kc, fb * 128:(fb + 1) * 128], hT[:, hb, kc, :]) for kc in range(16)], reads=[b_Wu, b_hT[hb]])
                    act.op(lambda e: e.activation(out=sg[:, f2, :], in_=pg[f2][:], func=AF.Silu), reads=[b_pg[f2]], writes=[b_sg[f2]])
                    dve.op(lambda e: e.tensor_tensor(out=hid[:, hb, fb, :], in0=sg[:, f2, :], in1=pu[f2][:], op=ALU.mult),
                           reads=[b_sg[f2], b_pu[f2]], writes=[b_hid[hb]] if fb == 0 else (), pwrites=() if fb == 0 else [b_hid[hb]])
                for t4 in range(4):
                    tt = tb * 4 + t4
                    yb_ = yi % 2
                    yi += 1
                    for cc in range(4):
                        p = oi % 4
                        oi += 1
                        mm_group(po[p][:], b_po[p], [(hid[:, hb, fb, t4 * 128:(t4 + 1) * 128], Wd[:, fb, cc * 512:(cc + 1) * 512]) for fb in range(8)],
                                 reads=[b_hid[hb], b_Wd])
                        evq[0] += 1
                        if evq[0] % 2 == 0:
                            dve.op(lambda e: e.tensor_scalar(out=yo[:, yb_, cc * 512:(cc + 1) * 512], in0=po[p][:], scalar1=coef[:, tt, ex:ex + 1], scalar2=None, op0=ALU.mult),
                                   reads=[b_po[p], b_coef], writes=[b_yo[yb_]] if cc == 0 else (), pwrites=() if cc == 0 else [b_yo[yb_]])
                        else:
                            act.op(lambda e: e.activation(out=yo[:, yb_, cc * 512:(cc + 1) * 512], in_=po[p][:], func=AF.Copy, scale=coef[:, tt, ex:ex + 1]),
                                   reads=[b_po[p], b_coef], writes=[b_yo[yb_]] if cc == 0 else (), pwrites=() if cc == 0 else [b_yo[yb_]])
                    pool.dma(out[tt * 128:(tt + 1) * 128, :], yo[:, yb_, :], reads=[b_yo[yb_]], writes=[b_out[tt]], accum_op=ALU.add)
        K.barrier()

    for st in phase("5g"):
        affT = sb(st, "affTg", [NE, T], F32)
        work = sb(st, "workg", [NE, T], F32)
        vals = sb(st, "valsg", [NE, CAP], F32)
        idxu = sb(st, "idxug", [NE, CAP], U32)
        pa = [ps(st, "pag%d" % i, [NE, 4, 128], F32) for i in range(2)]
        b_affT = Buf(); b_work = Buf(); b_vals = Buf(); b_idx = Buf(); b_pa = [Buf(), Buf()]
        for g in range(NT // 4):
            pb = g % 2
            for j in range(4):
                tt = g * 4 + j
                pe.op(lambda e: e.transpose(out=pa[pb][:, j, :], in_=aff[:, tt, :], identity=idf[:]),
                      reads=[b_aff, b_idf], writes=[b_pa[pb]] if j == 0 else (), pwrites=() if j == 0 else [b_pa[pb]])
            dve.op(lambda e: e.tensor_copy(out=affT[:, g * 512:(g + 1) * 512], in_=pa[pb][:].rearrange("p a b -> p (a b)")),
                   reads=[b_pa[pb]], pwrites=[b_affT])
        dve.op(lambda e: e.tensor_copy(out=work[:], in_=affT[:]), reads=[b_affT], writes=[b_work])
        for r in range(CAP // 8):
            v8 = vals[:, r * 8:(r + 1) * 8]
            dve.op(lambda e: e.max(out=v8, in_=work[:]), reads=[b_work], pwrites=[b_vals])
            dve.op(lambda e: e.max_index(out=idxu[:, r * 8:(r + 1) * 8], in_max=v8, in_values=work[:]), reads=[b_work, b_vals], pwrites=[b_idx])
            dve.op(lambda e: e.match_replace(out=work[:], in_to_replace=v8, in_values=work[:], imm_value=-1.0),
                   reads=[b_vals, b_work, b_idx], writes=[b_work])
        sp.dma(idx_d, idxu[:].bitcast(I32), reads=[b_idx])
        sp.dma(val_d, vals[:], reads=[b_vals])
        K.barrier()

    for st in phase("6g"):
        NS = CAP // 128
        Wg = sb(st, "Wgg", [128, 16, FF], BF16)
        Wu = sb(st, "Wug", [128, 16, FF], BF16)
        Wd = sb(st, "Wdg", [128, 8, D], BF16)
        g2 = sb(st, "g2g", [128, 16], F32)
        idx_t = sb(st, "idx_t", [128, 2, NS], I32)
        gat = sb(st, "gat", [128, 2, NS], F32)
        xe = sb(st, "xeg", [128, 2, D], BF16)
        xeT = sb(st, "xeTg", [128, 16, CAP], BF16)
        sg = sb(st, "sgg", [128, 2, CAP], F32)
        hid = sb(st, "hidg", [128, 8, CAP], BF16)
        yo = sb(st, "yog", [128, 2, D], F32)
        pt = [ps(st, "ptg%d" % i, [128, 8, 128], BF16) for i in range(2)]
        pg = ps(st, "pgg", [128, 512], F32)
        pu = ps(st, "pug", [128, 512], F32)
        po = [ps(st, "pog%d" % i, [128, 512], F32) for i in range(4)]
        b_Wg = Buf(); b_Wu = Buf(); b_Wd = Buf(); b_g2 = Buf(); b_it = [Buf(), Buf()]; b_xe = [Buf(), Buf()]; b_xeT = Buf()
        b_sg = [Buf(), Buf()]; b_hid = Buf(); b_yo = [Buf(), Buf()]; b_pt = [Buf(), Buf()]; b_pg = Buf(); b_pu = Buf()
        b_po = [Buf() for _ in range(4)]; b_outall = Buf()
        sp.dma(g2[:], g2T, writes=[b_g2])
        xi = 0
        fi = 0
        oi = 0
        yi = 0
        for ex in range(NE):
            eb = ex % 2
            pool.dma(Wg[:], w_g[ex].rearrange("(c p) n -> p c n", p=128), writes=[b_Wg])
            pool.dma(Wu[:], w_u[ex].rearrange("(c p) n -> p c n", p=128), writes=[b_Wu])
            pool.dma(Wd[:], w_d[ex].rearrange("(c p) n -> p c n", p=128), writes=[b_Wd])
            for j in range(NS):
                sp.dma(idx_t[:, eb, j:j + 1], idx_d[ex:ex + 1, j * 128:(j + 1) * 128].rearrange("o p -> p o"),
                       writes=[b_it[eb]] if j == 0 else (), pwrites=() if j == 0 else [b_it[eb]])
                sp.dma(gat[:, eb, j:j + 1], val_d[ex:ex + 1, j * 128:(j + 1) * 128].rearrange("o p -> p o"), pwrites=[b_it[eb]])
            for j in range(NS):
                xb = xi % 2
                xi += 1
                pool.dma(None, None, reads=[b_it[eb]], writes=[b_xe[xb]],
                         fn=lambda e: e.indirect_dma_start(out=xe[:, xb, :], out_offset=None, in_=xn2_d[:, :],
                                                           in_offset=bass.IndirectOffsetOnAxis(ap=idx_t[:, eb, j:j + 1], axis=0)))
                for half in range(2):
                    for c in range(8):
                        cc = half * 8 + c
                        pe.op(lambda e: e.transpose(out=pt[half][:, c, :], in_=xe[:, xb, cc * 128:(cc + 1) * 128], identity=idb[:]),
                              reads=[b_xe[xb], b_idb], writes=[b_pt[half]] if c == 0 else (), pwrites=() if c == 0 else [b_pt[half]])
                    first = (j == 0 and half == 0)
                    dve.op(lambda e: e.tensor_tensor(out=xeT[:, half * 8:(half + 1) * 8, j * 128:(j + 1) * 128], in0=pt[half][:],
                                                     in1=g2[:, half * 8:(half + 1) * 8].unsqueeze(2).broadcast_to([128, 8, 128]), op=ALU.mult),
                           reads=[b_pt[half], b_g2], writes=[b_xeT] if first else (), pwrites=() if first else [b_xeT])
            for fb in range(8):
                f2 = fi % 2
                fi += 1
                mm_group(pg[:, 0:CAP], b_pg, [(Wg[:, kc, fb * 128:(fb + 1) * 128], xeT[:, kc, :]) for kc in range(16)], reads=[b_Wg, b_xeT])
                mm_group(pu[:, 0:CAP], b_pu, [(Wu[:, kc, fb * 128:(fb + 1) * 128], xeT[:, kc, :]) for kc in range(16)], reads=[b_Wu, b_xeT])
                act.op(lambda e: e.activation(out=sg[:, f2, :], in_=pg[:, 0:CAP], func=AF.Silu), reads=[b_pg], writes=[b_sg[f2]])
                dve.op(lambda e: e.tensor_tensor(out=hid[:, fb, :], in0=sg[:, f2, :], in1=pu[:, 0:CAP], op=ALU.mult),
                       reads=[b_sg[f2], b_pu], writes=[b_hid] if fb == 0 else (), pwrites=() if fb == 0 else [b_hid])
            for j in range(NS):
                yb_ = yi % 2
                yi += 1
                for cc in range(4):
                    p = oi % 4
                    oi += 1
                    mm_group(po[p][:], b_po[p], [(hid[:, fb, j * 128:(j + 1) * 128], Wd[:, fb, cc * 512:(cc + 1) * 512]) for fb in range(8)],
                             reads=[b_hid, b_Wd])
                    evq[0] += 1
                    if evq[0] % 2 == 0:
                        dve.op(lambda e: e.tensor_scalar(out=yo[:, yb_, cc * 512:(cc + 1) * 512], in0=po[p][:], scalar1=gat[:, eb, j:j + 1], scalar2=None, op0=ALU.mult),
                               reads=[b_po[p], b_it[eb]], writes=[b_yo[yb_]] if cc == 0 else (), pwrites=() if cc == 0 else [b_yo[yb_]])
                    else:
                        act.op(lambda e: e.activation(out=yo[:, yb_, cc * 512:(cc + 1) * 512], in_=po[p][:], func=AF.Copy, scale=gat[:, eb, j:j + 1]),
                               reads=[b_po[p], b_it[eb]], writes=[b_yo[yb_]] if cc == 0 else (), pwrites=() if cc == 0 else [b_yo[yb_]])
                pool.dma(None, None, reads=[b_yo[yb_], b_it[eb]], writes=[b_outall],
                         fn=lambda e: e.indirect_dma_start(out=out[:, :], out_offset=bass.IndirectOffsetOnAxis(ap=idx_t[:, eb, j:j + 1], axis=0),
                                                           in_=yo[:, yb_, :], in_offset=None, compute_op=ALU.add))
        K.barrier()
    gst.close()
    return nc


_NC_CACHE = {}


def kernel(**inputs):
    f32 = np.float32
    g = lambda k: np.asarray(inputs[k], dtype=f32)
    x = g("x")
    rep = lambda v, n=128: np.ascontiguousarray(np.broadcast_to(v[None], (n,) + v.shape)).astype(f32)
    colT = lambda v: np.ascontiguousarray(v.reshape(16, 128).T).astype(f32)
    inv = 500000.0 ** (-(np.arange(0, 16, 2, dtype=f32) / 16.0))
    ang = np.arange(T, dtype=f32)[:, None] * inv[None, :]
    cos = np.cos(ang).astype(f32).reshape(NT, 128, 8).transpose(1, 0, 2)
    sin = np.sin(ang).astype(f32).reshape(NT, 128, 8).transpose(1, 0, 2)
    common = {
        "w_in": g("w_in")[0],
        "g1T": colT(g("attn_norm_g")[0]),
        "g2T": colT(g("ffn_norm_g")[0]),
        "ident": np.eye(128, dtype=f32),
        "gqk": rep(np.stack([g("q_norm_g")[0], g("k_norm_g")[0]])),
        "rope_c": np.ascontiguousarray(cos),
        "rope_s": np.ascontiguousarray(sin),
        "lam_in": rep(np.stack([g("lambda_q1")[0], g("lambda_k1")[0], g("lambda_q2")[0], g("lambda_k2")[0]])),
        "subg": rep(g("subln_g")[0]),
        "w_ba": g("w_branch_a")[0],
        "w_bb": g("w_branch_b")[0],
        "w_o": g("w_out")[0],
        "w_r": g("w_router")[0],
        "w_g": g("w_gate_e")[0],
        "w_u": g("w_up_e")[0],
        "w_d": g("w_down_e")[0],
        "mu_rep": rep(g("shift_mu")[0]),
        "w0_rep": rep(np.stack([g("w0_f")[0], g("w0_b")[0]])),
        "a0_rep": rep(np.stack([g("a0_f")[0], g("a0_b")[0]])),
        "kk_rep": rep(g("k_k")[0]),
        "ka_rep": rep(g("k_a")[0]),
        "rk_rep": rep(g("r_k")[0].reshape(1024)),
        "lnw_rep": rep(g("ln_x_w")[0]),
        "lnb_rep": rep(g("ln_x_b")[0]),
        "w2_in": np.ascontiguousarray(np.stack([g("w2_f")[0], g("w2_b")[0], g("a2_f")[0], g("a2_b")[0]])),
        "g2_in": g("g2")[0],
        "masks_in": np.ascontiguousarray(np.stack([np.triu(np.ones((128, 128), f32), 1), np.tril(np.ones((128, 128), f32), -1),
                                                   np.triu(np.ones((128, 128), f32), 0), np.tril(np.ones((128, 128), f32), 0)], axis=1)),
    }
    if "nc" not in _NC_CACHE:
        _NC_CACHE["nc"] = build()
    nc = _NC_CACHE["nc"]
    in_maps = []
    for c in range(8):
        m = {k: v for k, v in common.items() if k in DECL}
        m["x"] = np.ascontiguousarray(x[c // 2][:T])
        in_maps.append(m)
    res = run_bass_kernel_spmd(nc, in_maps, core_ids=list(range(8)))
    outp = np.empty((4, T, D), dtype=f32)
    for c in range(8):
        b, half = c // 2, c % 2
        o = np.asarray(res.results[c]["out"])
        outp[b, half * 2048:(half + 1) * 2048] = o[half * 2048:(half + 1) * 2048]
    if DEBUG:
        kernel.dbg = res.results
    return outp
```

```python
import os
import math
from contextlib import ExitStack
import numpy as np
import concourse.bass as bass
import concourse.mybir as mybir
from concourse.bass_utils import run_bass_kernel_spmd

F32 = mybir.dt.float32
BF16 = mybir.dt.bfloat16
I32 = mybir.dt.int32
U32 = mybir.dt.uint32
AF = mybir.ActivationFunctionType
ALU = mybir.AluOpType
AX = mybir.AxisListType

D = 2048
T = int(os.environ.get("KT", "4096"))
NT = T // 128
DA_COLS = 3072
RW_COLS = 3488
GATE_COLS = 4096
IN_COLS = DA_COLS + RW_COLS + GATE_COLS
TM_COLS = DA_COLS + RW_COLS
NE = 16
FF = 1024
CAP = T // 8
LAM_INIT = 0.8 - 0.6 * math.exp(-0.3 * 0)

DEBUG = bool(int(os.environ.get("KDEBUG", "0")))
SKIP_RWKV = bool(int(os.environ.get("KSKIP_RWKV", "0")))
CUT = int(os.environ.get("KCUT", "99"))
PHASES = os.environ.get("KPHASES", "1,2a,2b,3a,3b,3c,4a,4b,5g,6g").split(",")


class Buf:
    __slots__ = ("w", "r", "pr", "name")

    def __init__(self, name=""):
        self.w = {}
        self.r = {}
        self.pr = {}
        self.name = name


class Eng:
    def __init__(self, K, name, eng, is_pe=False):
        self.K = K
        self.name = name
        self.eng = eng
        self.is_pe = is_pe
        self.sem = K.new_sem("s_" + name)
        self.count = 0
        self.seen = {}
        self.dma_pool = []
        self.dma_i = 0

    def _wait(self, toks):
        for sem, val in toks.items():
            if self.is_pe and sem is self.sem:
                continue
            if self.seen.get(sem, 0) >= val:
                continue
            self.eng.wait_ge(sem, val)
            self.seen[sem] = val

    @staticmethod
    def _deps(reads, writes, pwrites):
        toks = {}

        def add(d):
            for s, v in d.items():
                if toks.get(s, 0) < v:
                    toks[s] = v
        for b in reads:
            add(b.w)
        for b in writes:
            add(b.w)
            add(b.r)
        for b in pwrites:
            add(b.r)
            add(b.pr)
        return toks

    @staticmethod
    def _commit(tok, reads, writes, pwrites):
        s, v = tok
        for b in reads:
            b.r[s] = v
        for b in writes:
            b.w = {s: v}
            b.pr = b.r
            b.r = {}
        for b in pwrites:
            b.w[s] = v
            for s2, v2 in b.r.items():
                if b.pr.get(s2, 0) < v2:
                    b.pr[s2] = v2
            b.r = {}

    def op(self, fn, reads=(), writes=(), pwrites=()):
        self._wait(self._deps(reads, writes, pwrites))
        ins = fn(self.eng)
        self.count += 1
        ins.then_inc(self.sem, 1)
        self._commit((self.sem, self.count), reads, writes, pwrites)
        return ins

    def dma(self, out, in_, reads=(), writes=(), pwrites=(), fn=None, **kw):
        self._wait(self._deps(reads, writes, pwrites))
        if len(self.dma_pool) < self.K.n_dma_sems:
            self.dma_pool.append([self.K.new_sem("d_%s%d" % (self.name, len(self.dma_pool))), 0])
        ent = self.dma_pool[self.dma_i % self.K.n_dma_sems]
        self.dma_i += 1
        sem, cur = ent
        if cur > 0 and self.seen.get(sem, 0) < cur:
            self.eng.wait_ge(sem, cur)
            self.seen[sem] = cur
        ins = fn(self.eng) if fn is not None else self.eng.dma_start(out=out, in_=in_, **kw)
        ent[1] = cur + 16
        ins.then_inc(sem, 16)
        self._commit((sem, cur + 16), reads, writes, pwrites)
        return ins


class Kern:
    def __init__(self, nc, n_dma_sems=10):
        self.nc = nc
        self.n_dma_sems = n_dma_sems
        self._sems = []
        self.E = {}

    def new_sem(self, name):
        cm = self.nc.semaphore(name)
        s = cm.__enter__()
        self._sems.append(cm)
        return s

    def setup(self, engines):
        for name, eng in engines.items():
            self.E[name] = Eng(self, name, eng, is_pe=(name == "pe"))

    def barrier(self):
        toks = {}
        for e in self.E.values():
            if e.count > 0:
                toks[e.sem] = e.count
            for sem, cur in e.dma_pool:
                if cur > 0:
                    toks[sem] = cur
        for e in self.E.values():
            for sem, val in toks.items():
                if sem is e.sem:
                    continue
                if e.seen.get(sem, 0) >= val:
                    continue
                e.eng.wait_ge(sem, val)
                e.seen[sem] = val


DECL = set()


def phase(name):
    if name in PHASES or name == "zero":
        with ExitStack() as st:
            yield st


def build():
    nc = bass.Bass("TRN2", target_bir_lowering=False)

    def din(name, shape, dt=F32):
        DECL.add(name)
        return nc.dram_tensor(name, list(shape), dt, kind="ExternalInput").ap()

    def dscr(name, shape, dt, dbg=False):
        kind = "ExternalOutput" if (dbg and DEBUG) else "Internal"
        return nc.dram_tensor(name, list(shape), dt, kind=kind).ap()

    x = din("x", [T, D])
    w_in = din("w_in", [D, IN_COLS])
    g1T = din("g1T", [128, 16])
    g2T = din("g2T", [128, 16])
    ident_in = din("ident", [128, 128])
    gqk = din("gqk", [128, 2, 64])
    rope_c = din("rope_c", [128, NT, 8])
    rope_s = din("rope_s", [128, NT, 8])
    lam_in = din("lam_in", [128, 4, 64])
    subg = din("subg", [128, 128])
    w_ba = din("w_ba", [1024, D])
    w_bb = din("w_bb", [1024, D])
    w_o = din("w_o", [D, D])
    w_r = din("w_r", [D, NE])
    if "6" in PHASES or "6g" in PHASES:
        w_g = din("w_g", [NE, D, FF])
        w_u = din("w_u", [NE, D, FF])
        w_d = din("w_d", [NE, FF, D])

    out = nc.dram_tensor("out", [T, D], F32, kind="ExternalOutput").ap()

    proj_tm = dscr("proj_tm", [T, TM_COLS], BF16, dbg=True)
    gateT = dscr("gateT", [GATE_COLS, T], BF16)
    qkT_d = dscr("qkT_d", [2048, T], BF16)
    y_a = dscr("y_a", [T, 1024], BF16, dbg=True)
    y_b = dscr("y_b", [T, 1024], BF16, dbg=True)
    mT_d = dscr("mT_d", [D, T], BF16)
    hn2T_d = dscr("hn2T_d", [D, T], BF16)
    aff_d = dscr("aff_d", [128, NT, NE], F32, dbg=True)
    xn2_d = dscr("xn2_d", [T, D], BF16)
    idx_d = dscr("idx_d", [NE, CAP], I32)
    val_d = dscr("val_d", [NE, CAP], F32)

    K = Kern(nc)
    K.setup({"sp": nc.sync, "act": nc.scalar, "dve": nc.vector, "pe": nc.tensor, "pool": nc.gpsimd})
    sp, act, dve, pe, pool = (K.E[n] for n in ("sp", "act", "dve", "pe", "pool"))

    gst = ExitStack()

    def sb(st, name, shape, dt):
        return st.enter_context(nc.sbuf_tensor(name, list(shape), dt))

    def ps(st, name, shape, dt):
        return st.enter_context(nc.psum_tensor(name, list(shape), dt))

    idf = sb(gst, "idf", [128, 128], F32)
    idb = sb(gst, "idb", [128, 128], BF16)
    b_idf, b_idb = Buf(), Buf()
    sp.dma(idf[:], ident_in, writes=[b_idf])
    dve.op(lambda e: e.tensor_copy(out=idb[:], in_=idf[:]), reads=[b_idf], writes=[b_idb])

    evq = [0]

    def evac_copy(dst, src, reads, writes=(), pwrites=()):
        evq[0] += 1
        if evq[0] % 2 == 0:
            dve.op(lambda e: e.tensor_copy(out=dst, in_=src), reads=reads, writes=writes, pwrites=pwrites)
        else:
            act.op(lambda e: e.activation(out=dst, in_=src, func=AF.Copy), reads=reads, writes=writes, pwrites=pwrites)

    def mm_group(ps_ap, ps_buf, pairs, reads):
        n = len(pairs)
        for i, (l, r) in enumerate(pairs):
            if i == 0:
                pe.op(lambda e: e.matmul(ps_ap, lhsT=l, rhs=r, start=True, stop=(n == 1)), reads=reads, writes=[ps_buf])
            else:
                pe.op(lambda e: e.matmul(ps_ap, lhsT=l, rhs=r, start=False, stop=(i == n - 1)), reads=reads, pwrites=[ps_buf])

    def rmsnorm_T(st, tag, src_tile, b_src, gT_t, b_gT, dstT_ap, b_dst, pts, b_pts, scratch, store_ap=None):
        junk, b_junk, ssq, b_ssq, xn, b_xn = scratch
        dve.op(lambda e: e.memset(ssq[:], 0.0), writes=[b_ssq])
        act.op(lambda e: e.activation(out=junk[:], in_=src_tile, func=AF.Square, accum_out=ssq[:]),
               reads=[b_src], writes=[b_junk, b_ssq])
        act.op(lambda e: e.activation(out=ssq[:], in_=ssq[:], func=AF.Sqrt, scale=1.0 / D, bias=1e-6),
               reads=[b_ssq], writes=[b_ssq])
        dve.op(lambda e: e.reciprocal(out=ssq[:], in_=ssq[:]), reads=[b_ssq], writes=[b_ssq])
        dve.op(lambda e: e.tensor_scalar(out=xn[:], in0=src_tile, scalar1=ssq[:, 0:1], scalar2=None, op0=ALU.mult),
               reads=[b_src, b_ssq], writes=[b_xn])
        if store_ap is not None:
            sp.dma(store_ap, xn[:], reads=[b_xn])
        for half in range(2):
            for c in range(8):
                cc = half * 8 + c
                pe.op(lambda e: e.transpose(out=pts[half][:, c, :], in_=xn[:, cc * 128:(cc + 1) * 128], identity=idb[:]),
                      reads=[b_xn, b_idb], writes=[b_pts[half]] if c == 0 else (), pwrites=() if c == 0 else [b_pts[half]])
            dve.op(lambda e: e.tensor_tensor(out=dstT_ap[:, half * 8:(half + 1) * 8, :], in0=pts[half][:],
                                             in1=gT_t[:, half * 8:(half + 1) * 8].unsqueeze(2).broadcast_to([128, 8, 128]),
                                             op=ALU.mult),
                   reads=[b_pts[half], b_gT], pwrites=[b_dst])

    for st in phase("1"):
        HT = min(2048, T)
        hnT = sb(st, "hnT", [128, 16, HT], BF16)
        xt = sb(st, "xt", [128, 2, D], F32)
        junk = sb(st, "junk1", [128, D], BF16)
        xn = sb(st, "xn1", [128, D], BF16)
        ssq = sb(st, "ssq1", [128, 1], F32)
        g1 = sb(st, "g1", [128, 16], F32)
        wbf = sb(st, "wbf", [128, 2, 16, 512], BF16)
        stage = sb(st, "stage1", [128, 4, 512], BF16)
        pt = [ps(st, "pt1a", [128, 8, 128], BF16), ps(st, "pt1b", [128, 8, 128], BF16)]
        pm = [ps(st, "pm1_%d" % i, [128, 512], F32) for i in range(4)]
        b_hnT = Buf(); b_xt = [Buf(), Buf()]; b_junk = Buf(); b_xn = Buf(); b_ssq = Buf(); b_g1 = Buf()
        b_wbf = [Buf(), Buf()]; b_stage = [Buf() for _ in range(4)]; b_pt = [Buf(), Buf()]; b_pm = [Buf() for _ in range(4)]
        sp.dma(g1[:], g1T, writes=[b_g1])
        w_v = w_in.rearrange("(c p) n -> p c n", p=128)
        chunks = []
        c0 = 0
        while c0 < TM_COLS:
            cw = min(512, TM_COLS - c0)
            chunks.append(("tm", c0, cw))
            c0 += cw
        for gc in range(GATE_COLS // 512):
            chunks.append(("gate", TM_COLS + gc * 512, 512))
        wi = 0
        si = 0
        pi = 0
        NH16 = HT // 128
        for half in range(T // HT):
            for t16 in range(NH16):
                tt = half * NH16 + t16
                bi = tt % 2
                sp.dma(xt[:, bi, :], x[tt * 128:(tt + 1) * 128, :], writes=[b_xt[bi]])
                rmsnorm_T(st, "p1", xt[:, bi, :], b_xt[bi], g1, b_g1, hnT[:, :, t16 * 128:(t16 + 1) * 128], b_hnT,
                          pt, b_pt, (junk, b_junk, ssq, b_ssq, xn, b_xn))
            for kind, c0, cw in chunks:
                wb = wi % 2
                wi += 1
                pool.dma(wbf[:, wb, :, 0:cw], w_v[:, :, c0:c0 + cw], writes=[b_wbf[wb]])
                if kind == "tm":
                    for t16 in range(NH16):
                        tt = half * NH16 + t16
                        p = pi % 4
                        pi += 1
                        mm_group(pm[p][:, 0:cw], b_pm[p],
                                 [(hnT[:, kc, t16 * 128:(t16 + 1) * 128], wbf[:, wb, kc, 0:cw]) for kc in range(16)],
                                 reads=[b_hnT, b_wbf[wb]])
                        s = si % 4
                        si += 1
                        evac_copy(stage[:, s, 0:cw], pm[p][:, 0:cw], reads=[b_pm[p]], writes=[b_stage[s]])
                        sp.dma(proj_tm[tt * 128:(tt + 1) * 128, c0:c0 + cw], stage[:, s, 0:cw], reads=[b_stage[s]])
                else:
                    gcol = c0 - TM_COLS
                    for blk in range(4):
                        for tc4 in range(HT // 512):
                            p = pi % 4
                            pi += 1
                            mm_group(pm[p][:], b_pm[p],
                                     [(wbf[:, wb, kc, blk * 128:(blk + 1) * 128], hnT[:, kc, tc4 * 512:(tc4 + 1) * 512]) for kc in range(16)],
                                     reads=[b_hnT, b_wbf[wb]])
                            s = si % 4
                            si += 1
                            act.op(lambda e: e.activation(out=stage[:, s, :], in_=pm[p][:], func=AF.Sigmoid),
                                   reads=[b_pm[p]], writes=[b_stage[s]])
                            sp.dma(gateT[gcol + blk * 128:gcol + (blk + 1) * 128, half * HT + tc4 * 512:half * HT + (tc4 + 1) * 512],
                                   stage[:, s, :], reads=[b_stage[s]])
        K.barrier()

    for st in phase("2a"):
        qk = sb(st, "qk", [128, 2, 2048], BF16)
        sq = sb(st, "sq", [128, 32, 64], F32)
        ss = sb(st, "ss", [128, 32], F32)
        qn = sb(st, "qn", [128, 32, 64], F32)
        tmp = sb(st, "ropetmp", [128, 4, 32, 8], F32)
        qb = sb(st, "qb", [128, 32, 64], BF16)
        gq = sb(st, "gq", [128, 2, 64], F32)
        rc = sb(st, "rc", [128, NT, 8], F32)
        rs_ = sb(st, "rs", [128, NT, 8], F32)
        stg = sb(st, "stg2", [128, 2, 16, 512], BF16)
        pt = [ps(st, "pt2a", [128, 8, 128], BF16), ps(st, "pt2b", [128, 8, 128], BF16)]
        b_qk = [Buf(), Buf()]; b_sq = Buf(); b_ss = Buf(); b_qn = Buf(); b_tmp = Buf(); b_qb = Buf()
        b_c = Buf(); b_stg = [Buf(), Buf()]; b_pt = [Buf(), Buf()]
        sp.dma(gq[:], gqk, writes=[b_c])
        sp.dma(rc[:], rope_c, pwrites=[b_c])
        sp.dma(rs_[:], rope_s, pwrites=[b_c])
        for tt in range(NT):
            bi = tt % 2
            sp.dma(qk[:, bi, :], proj_tm[tt * 128:(tt + 1) * 128, 0:2048], writes=[b_qk[bi]])
            qv = qk[:, bi, :].rearrange("p (g d) -> p g d", d=64)
            dve.op(lambda e: e.tensor_tensor(out=sq[:], in0=qv, in1=qv, op=ALU.mult), reads=[b_qk[bi]], writes=[b_sq])
            dve.op(lambda e: e.tensor_reduce(out=ss[:], in_=sq[:], axis=AX.X, op=ALU.add), reads=[b_sq], writes=[b_ss])
            act.op(lambda e: e.activation(out=ss[:], in_=ss[:], func=AF.Sqrt, scale=1.0 / 64, bias=1e-6), reads=[b_ss], writes=[b_ss])
            dve.op(lambda e: e.reciprocal(out=ss[:], in_=ss[:]), reads=[b_ss], writes=[b_ss])
            dve.op(lambda e: e.tensor_tensor(out=qn[:], in0=qv, in1=ss[:].unsqueeze(2).broadcast_to([128, 32, 64]), op=ALU.mult),
                   reads=[b_qk[bi], b_ss], writes=[b_qn])
            for i in range(2):
                dve.op(lambda e: e.tensor_tensor(out=qn[:, i * 16:(i + 1) * 16, :], in0=qn[:, i * 16:(i + 1) * 16, :],
                                                 in1=gq[:, i:i + 1, :].broadcast_to([128, 16, 64]), op=ALU.mult),
                       reads=[b_qn, b_c], writes=[b_qn])
            cb = rc[:, tt:tt + 1, :].broadcast_to([128, 32, 8])
            sbb = rs_[:, tt:tt + 1, :].broadcast_to([128, 32, 8])
            x1 = qn[:, :, 0:8]
            x2 = qn[:, :, 8:16]
            dve.op(lambda e: e.tensor_tensor(out=tmp[:, 0], in0=x1, in1=cb, op=ALU.mult), reads=[b_qn, b_c], writes=[b_tmp])
            dve.op(lambda e: e.tensor_tensor(out=tmp[:, 1], in0=x2, in1=sbb, op=ALU.mult), reads=[b_qn, b_c], pwrites=[b_tmp])
            dve.op(lambda e: e.tensor_tensor(out=tmp[:, 2], in0=x2, in1=cb, op=ALU.mult), reads=[b_qn, b_c], pwrites=[b_tmp])
            dve.op(lambda e: e.tensor_tensor(out=tmp[:, 3], in0=x1, in1=sbb, op=ALU.mult), reads=[b_qn, b_c], pwrites=[b_tmp])
            dve.op(lambda e: e.tensor_copy(out=qb[:], in_=qn[:]), reads=[b_qn], writes=[b_qb])
            dve.op(lambda e: e.tensor_tensor(out=qb[:, :, 0:8], in0=tmp[:, 0], in1=tmp[:, 1], op=ALU.subtract), reads=[b_tmp], writes=[b_qb])
            dve.op(lambda e: e.tensor_tensor(out=qb[:, :, 8:16], in0=tmp[:, 2], in1=tmp[:, 3], op=ALU.add), reads=[b_tmp], writes=[b_qb])
            qbf = qb[:].rearrange("p g d -> p (g d)")
            sg = (tt // 4) % 2
            for half in range(2):
                for c in range(8):
                    cc = half * 8 + c
                    pe.op(lambda e: e.transpose(out=pt[half][:, c, :], in_=qbf[:, cc * 128:(cc + 1) * 128], identity=idb[:]),
                          reads=[b_qb, b_idb], writes=[b_pt[half]] if c == 0 else (), pwrites=() if c == 0 else [b_pt[half]])
                evac_copy(stg[:, sg, half * 8:(half + 1) * 8, (tt % 4) * 128:(tt % 4 + 1) * 128], pt[half][:],
                          reads=[b_pt[half]], writes=[b_stg[sg]] if (tt % 4 == 0 and half == 0) else (),
                          pwrites=() if (tt % 4 == 0 and half == 0) else [b_stg[sg]])
            if tt % 4 == 3:
                t0 = (tt // 4) * 512
                sp.dma(qkT_d.rearrange("(c p) t -> p c t", p=128)[:, :, t0:t0 + 512], stg[:, sg, :, :], reads=[b_stg[sg]])
        K.barrier()

    for st in phase("2b"):
        V1 = sb(st, "V1", [128, NT, 8, 144], BF16)
        qT = sb(st, "qT", [128, 2, T], BF16)
        kT1 = sb(st, "kT1", [128, 2, T], BF16)
        kT2 = sb(st, "kT2", [128, 2, T], BF16)
        PT = sb(st, "PT", [128, 3, 2, 512], BF16)
        lamt = sb(st, "lamt", [128, 4, 64], F32)
        lamp = sb(st, "lamp", [128, 2, 64], F32)
        lam2 = sb(st, "lam2", [128, 2], F32)
        lam = sb(st, "lam", [128, 1], F32)
        sgt = sb(st, "sgt", [128, 128], F32)
        rr = sb(st, "rr", [128, 2], F32)
        o1 = sb(st, "o1", [128, 128], F32)
        o2 = sb(st, "o2", [128, 128], F32)
        osq = sb(st, "osq", [128, 128], F32)
        oms = sb(st, "oms", [128, 1], F32)
        yst = sb(st, "yst", [128, 2, 128], BF16)
        pS = [ps(st, "pS%d" % i, [128, 2, 512], F32) for i in range(2)]
        pO = [ps(st, "pO%d" % i, [128, 2, 256], F32) for i in range(4)]
        b_V1 = Buf(); b_V1z = Buf(); b_q = [Buf(), Buf()]; b_k1 = [Buf(), Buf()]; b_k2 = [Buf(), Buf()]
        b_PT = [Buf() for _ in range(3)]; b_pS = [Buf(), Buf()]; b_pO = [[b_, b_] for b_ in (Buf(), Buf(), Buf(), Buf())]
        b_lam = Buf(); b_sg = Buf(); b_rr = Buf(); b_o1 = Buf(); b_o2 = Buf(); b_osq = Buf(); b_oms = Buf(); b_yst = [Buf(), Buf()]
        sp.dma(lamt[:], lam_in, writes=[b_lam])
        sp.dma(sgt[:], subg, writes=[b_sg])
        dve.op(lambda e: e.tensor_tensor(out=lamp[:, 0, :], in0=lamt[:, 0, :], in1=lamt[:, 1, :], op=ALU.mult), reads=[b_lam], writes=[b_lam])
        dve.op(lambda e: e.tensor_tensor(out=lamp[:, 1, :], in0=lamt[:, 2, :], in1=lamt[:, 3, :], op=ALU.mult), reads=[b_lam], writes=[b_lam])
        dve.op(lambda e: e.tensor_reduce(out=lam2[:], in_=lamp[:], axis=AX.X, op=ALU.add), reads=[b_lam], writes=[b_lam])
        act.op(lambda e: e.activation(out=lam2[:], in_=lam2[:], func=AF.Exp), reads=[b_lam], writes=[b_lam])
        dve.op(lambda e: e.tensor_tensor(out=lam[:], in0=lam2[:, 0:1], in1=lam2[:, 1:2], op=ALU.subtract), reads=[b_lam], writes=[b_lam])
        dve.op(lambda e: e.tensor_scalar(out=lam[:], in0=lam[:], scalar1=LAM_INIT, scalar2=None, op0=ALU.add), reads=[b_lam], writes=[b_lam])
        pool.op(lambda e: e.memset(kT1[:], 0.0), writes=[b_k1[0], b_k1[1]])
        pool.op(lambda e: e.memset(kT2[:], 0.0), writes=[b_k2[0], b_k2[1]])
        pool.op(lambda e: e.memset(V1[:], 1.0), writes=[b_V1, b_V1z])
        for tt in range(NT):
            sp.dma(V1[:, tt, :, 0:128], proj_tm[tt * 128:(tt + 1) * 128, 2048:3072].rearrange("p (h v) -> p h v", v=128), reads=[b_V1z], pwrites=[b_V1])
        sci = 0
        pti = 0
        for h in range(8):
            hb = h % 2
            sp.dma(qT[:, hb, :], qkT_d[h * 128:(h + 1) * 128, :], writes=[b_q[hb]])
            sp.dma(kT1[0:64, hb, :], qkT_d[1024 + h * 128:1024 + h * 128 + 64, :], writes=[b_k1[hb]])
            sp.dma(kT2[64:128, hb, :], qkT_d[1024 + h * 128 + 64:1024 + (h + 1) * 128, :], writes=[b_k2[hb]])
            kTs = [kT1, kT2]
            b_ks = [b_k1, b_k2]
            for qc in range(T // 512):
                for kt in range(NT):
                    pb = sci % 2
                    sci += 1
                    for s in range(2):
                        pe.op(lambda e: e.matmul(pS[pb][:, s, :], lhsT=kTs[s][:, hb, kt * 128:(kt + 1) * 128], rhs=qT[:, hb, qc * 512:(qc + 1) * 512],
                                                 start=True, stop=True),
                              reads=[b_ks[s][hb], b_q[hb]], writes=[b_pS[pb]] if s == 0 else (), pwrites=() if s == 0 else [b_pS[pb]])
                    pi_ = pti % 3
                    pti += 1
                    act.op(lambda e: e.activation(out=PT[:, pi_, :, :], in_=pS[pb][:], func=AF.Exp, scale=0.125),
                           reads=[b_pS[pb]], writes=[b_PT[pi_]])
                    for s in range(2):
                        for qs in range(4):
                            if kt == 0:
                                pe.op(lambda e: e.matmul(pO[qs][:, s, 0:129], lhsT=PT[:, pi_, s, qs * 128:(qs + 1) * 128], rhs=V1[:, kt, h, 0:129],
                                                         start=(s == 0), stop=False, skip_group_check=True),
                                      reads=[b_PT[pi_], b_V1], writes=[b_pO[qs][s]])
                            else:
                                pe.op(lambda e: e.matmul(pO[qs][:, s, 0:129], lhsT=PT[:, pi_, s, qs * 128:(qs + 1) * 128], rhs=V1[:, kt, h, 0:129],
                                                         start=False, stop=(kt == NT - 1), skip_group_check=True),
                                      reads=[b_PT[pi_], b_V1], pwrites=[b_pO[qs][s]])
                for qs in range(4):
                    qt = qc * 4 + qs
                    dve.op(lambda e: e.tensor_copy(out=rr[:, 0:1], in_=pO[qs][:, 0, 128:129]), reads=[b_pO[qs][0]], writes=[b_rr])
                    dve.op(lambda e: e.tensor_copy(out=rr[:, 1:2], in_=pO[qs][:, 1, 128:129]), reads=[b_pO[qs][1]], writes=[b_rr])
                    dve.op(lambda e: e.reciprocal(out=rr[:], in_=rr[:]), reads=[b_rr], writes=[b_rr])
                    dve.op(lambda e: e.tensor_tensor(out=rr[:, 1:2], in0=rr[:, 1:2], in1=lam[:], op=ALU.mult), reads=[b_rr, b_lam], writes=[b_rr])
                    dve.op(lambda e: e.tensor_scalar(out=o1[:], in0=pO[qs][:, 0, 0:128], scalar1=rr[:, 0:1], scalar2=None, op0=ALU.mult),
                           reads=[b_pO[qs][0], b_rr], writes=[b_o1])
                    dve.op(lambda e: e.tensor_scalar(out=o2[:], in0=pO[qs][:, 1, 0:128], scalar1=rr[:, 1:2], scalar2=None, op0=ALU.mult),
                           reads=[b_pO[qs][1], b_rr], writes=[b_o2])
                    dve.op(lambda e: e.tensor_tensor(out=o1[:], in0=o1[:], in1=o2[:], op=ALU.subtract), reads=[b_o1, b_o2], writes=[b_o1])
                    dve.op(lambda e: e.memset(oms[:], 0.0), writes=[b_oms])
                    act.op(lambda e: e.activation(out=osq[:], in_=o1[:], func=AF.Square, accum_out=oms[:]), reads=[b_o1], writes=[b_osq, b_oms])
                    act.op(lambda e: e.activation(out=oms[:], in_=oms[:], func=AF.Sqrt, scale=1.0 / 128, bias=1e-6), reads=[b_oms], writes=[b_oms])
                    dve.op(lambda e: e.reciprocal(out=oms[:], in_=oms[:]), reads=[b_oms], writes=[b_oms])
                    dve.op(lambda e: e.tensor_scalar(out=o1[:], in0=o1[:], scalar1=oms[:, 0:1], scalar2=(1.0 - LAM_INIT), op0=ALU.mult, op1=ALU.mult),
                           reads=[b_o1, b_oms], writes=[b_o1])
                    yb_ = qt % 2
                    dve.op(lambda e: e.tensor_tensor(out=yst[:, yb_, :], in0=o1[:], in1=sgt[:], op=ALU.mult), reads=[b_o1, b_sg], writes=[b_yst[yb_]])
                    sp.dma(y_a[qt * 128:(qt + 1) * 128, h * 128:(h + 1) * 128], yst[:, yb_, :], reads=[b_yst[yb_]])
        K.barrier()


    mu_in = din("mu_rep", [128, RW_COLS])
    w0_in = din("w0_rep", [128, 2, 1024])
    a0_in = din("a0_rep", [128, 2, 1024])
    kk_in = din("kk_rep", [128, 1024])
    ka_in = din("ka_rep", [128, 1024])
    rk_in = din("rk_rep", [128, 1024])
    lnw_in = din("lnw_rep", [128, 1024])
    lnb_in = din("lnb_rep", [128, 1024])
    w2_in = din("w2_in", [4, 64, 1024])
    g2_in = din("g2_in", [160, 1024])
    masks_in = din("masks_in", [128, 4, 128])
    RWS = {n: dscr("rw_" + n, [T, 1024], F32, dbg=True) for n in
           ("R", "V", "KK", "LW0", "LW1", "BB0", "BB1", "KE0", "KE1", "G", "BON", "Y0", "Y1")}

    def rwkv_phase():
        for st in phase("3a"):
            P = sb(st, "rP", [128, 3, RW_COLS], BF16)
            xs = sb(st, "rxs", [128, RW_COLS], F32)
            tt_ = sb(st, "rtt", [128, RW_COLS], F32)
            mu = sb(st, "rmu", [128, RW_COLS], F32)
            w0 = sb(st, "rw0", [128, 2, 1024], F32)
            a0 = sb(st, "ra0", [128, 2, 1024], F32)
            kkc = sb(st, "rkkc", [128, 1024], F32)
            kac = sb(st, "rkac", [128, 1024], F32)
            rkc = sb(st, "rrkc", [128, 1024], F32)
            w2 = sb(st, "rw2", [64, 4, 1024], BF16)
            g2a = sb(st, "rg2a", [128, 1024], BF16)
            g2b = sb(st, "rg2b", [32, 1024], BF16)
            L = sb(st, "rL", [128, 416], BF16)
            LT = sb(st, "rLT", [128, 6, 128], BF16)
            asg = sb(st, "rasg", [128, 2, 1024], F32)
            o = [sb(st, "ro%d" % i, [128, 1024], F32) for i in range(4)]
            kk = sb(st, "rkk", [128, 1024], F32)
            ke = sb(st, "rke", [128, 2, 1024], F32)
            s16 = sb(st, "rs16", [128, 16], F32)
            pl = ps(st, "rpl", [128, 6, 128], BF16)
            pm = [ps(st, "rpm%d" % i, [128, 2, 512], F32) for i in range(2)]
            b_P = Buf(); b_Pz = Buf(); b_xs = Buf(); b_tt = Buf(); b_c = Buf(); b_L = Buf(); b_LT = Buf(); b_asg = Buf()
            b_o = [Buf() for _ in range(4)]; b_kk = Buf(); b_ke = Buf(); b_s16 = Buf(); b_pl = Buf(); b_pm = [Buf(), Buf()]
            sp.dma(mu[:], mu_in, writes=[b_c])
            sp.dma(w0[:], w0_in, pwrites=[b_c])
            sp.dma(a0[:], a0_in, pwrites=[b_c])
            sp.dma(kkc[:], kk_in, pwrites=[b_c])
            sp.dma(kac[:], ka_in, pwrites=[b_c])
            sp.dma(rkc[:], rk_in, pwrites=[b_c])
            pool.dma(w2[:], w2_in.rearrange("f k n -> k f n"), pwrites=[b_c])
            pool.dma(g2a[:], g2_in[0:128, :], pwrites=[b_c])
            pool.dma(g2b[:], g2_in[128:160, :], pwrites=[b_c])
            oi = [0]

            def outbuf():
                oi[0] += 1
                return oi[0] % 4

            def store(name, tt, i):
                sp.dma(RWS[name][tt * 128:(tt + 1) * 128, :], o[i][:], reads=[b_o[i]])

            for tt in range(NT):
                r0 = tt * 128
                dve.op(lambda e: e.memset(P[:, 1:3, :], 0.0), writes=[b_P, b_Pz])
                sp.dma(P[:, 0, :], proj_tm[r0:r0 + 128, DA_COLS:TM_COLS], pwrites=[b_P])
                if tt == 0:
                    sp.dma(P[1:128, 1, :], proj_tm[0:127, DA_COLS:TM_COLS], reads=[b_Pz], pwrites=[b_P])
                else:
                    sp.dma(P[:, 1, :], proj_tm[r0 - 1:r0 + 127, DA_COLS:TM_COLS], reads=[b_Pz], pwrites=[b_P])
                if tt == NT - 1:
                    sp.dma(P[0:127, 2, :], proj_tm[r0 + 1:r0 + 128, DA_COLS:TM_COLS], reads=[b_Pz], pwrites=[b_P])
                else:
                    sp.dma(P[:, 2, :], proj_tm[r0 + 1:r0 + 129, DA_COLS:TM_COLS], reads=[b_Pz], pwrites=[b_P])
                dve.op(lambda e: e.tensor_tensor(out=tt_[:], in0=P[:, 1, :], in1=P[:, 2, :], op=ALU.add), reads=[b_P], writes=[b_tt])
                dve.op(lambda e: e.scalar_tensor_tensor(out=tt_[:], in0=tt_[:], scalar=0.5, in1=P[:, 0, :], op0=ALU.mult, op1=ALU.subtract),
                       reads=[b_tt, b_P], writes=[b_tt])
                dve.op(lambda e: e.tensor_tensor(out=tt_[:], in0=tt_[:], in1=mu[:], op=ALU.mult), reads=[b_tt, b_c], writes=[b_tt])
                dve.op(lambda e: e.tensor_tensor(out=xs[:], in0=tt_[:], in1=P[:, 0, :], op=ALU.add), reads=[b_tt, b_P], writes=[b_xs])
                rr_ = xs[:, 0:1024]
                kk_ = xs[:, 1024:2048]
                vv_ = xs[:, 2048:3072]
                if CUT <= 1:
                    continue
                act.op(lambda e: e.activation(out=L[:, 0:128], in_=xs[:, 3072:3200], func=AF.Tanh), reads=[b_xs], writes=[b_L])
                act.op(lambda e: e.activation(out=L[:, 128:256], in_=xs[:, 3200:3328], func=AF.Copy), reads=[b_xs], pwrites=[b_L])
                act.op(lambda e: e.activation(out=L[:, 256:416], in_=xs[:, 3328:3488], func=AF.Sigmoid), reads=[b_xs], pwrites=[b_L])
                for i in range(4):
                    pe.op(lambda e: e.transpose(out=pl[0:64, i, :], in_=L[:, i * 64:(i + 1) * 64], identity=idb[:]),
                          reads=[b_L, b_idb], writes=[b_pl] if i == 0 else (), pwrites=() if i == 0 else [b_pl])
                pe.op(lambda e: e.transpose(out=pl[:, 4, :], in_=L[:, 256:384], identity=idb[:]), reads=[b_L, b_idb], pwrites=[b_pl])
                pe.op(lambda e: e.transpose(out=pl[0:32, 5, :], in_=L[:, 384:416], identity=idb[:]), reads=[b_L, b_idb], pwrites=[b_pl])
                dve.op(lambda e: e.tensor_copy(out=LT[0:64, 0:4, :], in_=pl[0:64, 0:4, :]), reads=[b_pl], writes=[b_LT])
                dve.op(lambda e: e.tensor_copy(out=LT[:, 4, :], in_=pl[:, 4, :]), reads=[b_pl], pwrites=[b_LT])
                dve.op(lambda e: e.tensor_copy(out=LT[0:32, 5, :], in_=pl[0:32, 5, :]), reads=[b_pl], pwrites=[b_LT])
                if CUT <= 2:
                    continue
                i = outbuf()
                dve.op(lambda e: e.tensor_copy(out=o[i][:], in_=rr_), reads=[b_xs], writes=[b_o[i]])
                store("R", tt, i)
                i = outbuf()
                dve.op(lambda e: e.tensor_copy(out=o[i][:], in_=vv_), reads=[b_xs], writes=[b_o[i]])
                store("V", tt, i)
                if CUT <= 3:
                    continue
                for d in range(2):
                    p = pm[d % 2]
                    for hf in range(2):
                        pe.op(lambda e: e.matmul(p[:, hf, :], lhsT=LT[0:64, d, :], rhs=w2[:, d, hf * 512:(hf + 1) * 512], start=True, stop=True),
                              reads=[b_LT, b_c], writes=[b_pm[d % 2]] if hf == 0 else (), pwrites=() if hf == 0 else [b_pm[d % 2]])
                    i = outbuf()
                    dve.op(lambda e: e.tensor_tensor(out=o[i][:], in0=p[:].rearrange("p a b -> p (a b)"), in1=w0[:, d, :], op=ALU.add),
                           reads=[b_pm[d % 2], b_c], writes=[b_o[i]])
                    act.op(lambda e: e.activation(out=o[i][:], in_=o[i][:], func=AF.Sigmoid), reads=[b_o[i]], writes=[b_o[i]])
                    dve.op(lambda e: e.tensor_scalar(out=o[i][:], in0=o[i][:], scalar1=-math.exp(-0.5), scalar2=None, op0=ALU.mult),
                           reads=[b_o[i]], writes=[b_o[i]])
                    store("LW%d" % d, tt, i)
                if CUT <= 4:
                    continue
                for d in range(2):
                    p = pm[d % 2]
                    for hf in range(2):
                        pe.op(lambda e: e.matmul(p[:, hf, :], lhsT=LT[0:64, 2 + d, :], rhs=w2[:, 2 + d, hf * 512:(hf + 1) * 512], start=True, stop=True),
                              reads=[b_LT, b_c], writes=[b_pm[d % 2]] if hf == 0 else (), pwrites=() if hf == 0 else [b_pm[d % 2]])
                    dve.op(lambda e: e.tensor_tensor(out=asg[:, d, :], in0=p[:].rearrange("p a b -> p (a b)"), in1=a0[:, d, :], op=ALU.add),
                           reads=[b_pm[d % 2], b_c], writes=[b_asg] if d == 0 else (), pwrites=() if d == 0 else [b_asg])
                act.op(lambda e: e.activation(out=asg[:], in_=asg[:], func=AF.Sigmoid), reads=[b_asg], writes=[b_asg])
                if CUT <= 5:
                    continue
                p = pm[0]
                for hf in range(2):
                    pe.op(lambda e: e.matmul(p[:, hf, :], lhsT=LT[:, 4, :], rhs=g2a[:, hf * 512:(hf + 1) * 512], start=True, stop=False),
                          reads=[b_LT, b_c], writes=[b_pm[0]] if hf == 0 else (), pwrites=() if hf == 0 else [b_pm[0]])
                    pe.op(lambda e: e.matmul(p[:, hf, :], lhsT=LT[0:32, 5, :], rhs=g2b[:, hf * 512:(hf + 1) * 512], start=False, stop=True),
                          reads=[b_LT, b_c], pwrites=[b_pm[0]])
                i = outbuf()
                act.op(lambda e: e.activation(out=o[i][:], in_=p[:].rearrange("p a b -> p (a b)"), func=AF.Copy), reads=[b_pm[0]], writes=[b_o[i]])
                store("G", tt, i)
                if CUT <= 6:
                    continue
                dve.op(lambda e: e.tensor_tensor(out=kk[:], in0=kk_, in1=kkc[:], op=ALU.mult), reads=[b_xs, b_c], writes=[b_kk])
                dve.op(lambda e: e.tensor_tensor(out=tt_[:, 0:1024], in0=kk[:], in1=kk[:], op=ALU.mult), reads=[b_kk], writes=[b_tt])
                dve.op(lambda e: e.tensor_reduce(out=s16[:], in_=tt_[:, 0:1024].rearrange("p (h d) -> p h d", d=64), axis=AX.X, op=ALU.add),
                       reads=[b_tt], writes=[b_s16])
                act.op(lambda e: e.activation(out=s16[:], in_=s16[:], func=AF.Sqrt), reads=[b_s16], writes=[b_s16])
                dve.op(lambda e: e.tensor_scalar(out=s16[:], in0=s16[:], scalar1=1e-12, scalar2=None, op0=ALU.max), reads=[b_s16], writes=[b_s16])
                dve.op(lambda e: e.reciprocal(out=s16[:], in_=s16[:]), reads=[b_s16], writes=[b_s16])
                i = outbuf()
                dve.op(lambda e: e.tensor_tensor(out=o[i][:].rearrange("p (h d) -> p h d", d=64), in0=kk[:].rearrange("p (h d) -> p h d", d=64),
                                                 in1=s16[:].unsqueeze(2).broadcast_to([128, 16, 64]), op=ALU.mult),
                       reads=[b_kk, b_s16], writes=[b_o[i]])
                ikk = i
                for d in range(2):
                    i = outbuf()
                    dve.op(lambda e: e.tensor_tensor(out=o[i][:], in0=o[ikk][:], in1=asg[:, d, :], op=ALU.mult), reads=[b_o[ikk], b_asg], writes=[b_o[i]])
                    store("BB%d" % d, tt, i)
                dve.op(lambda e: e.tensor_scalar(out=o[ikk][:], in0=o[ikk][:], scalar1=-1.0, scalar2=None, op0=ALU.mult), reads=[b_o[ikk]], writes=[b_o[ikk]])
                store("KK", tt, ikk)
                for d in range(2):
                    dve.op(lambda e: e.tensor_tensor(out=ke[:, d, :], in0=asg[:, d, :], in1=kac[:], op=ALU.mult),
                           reads=[b_asg, b_c], writes=[b_ke] if d == 0 else (), pwrites=() if d == 0 else [b_ke])
                    dve.op(lambda e: e.tensor_tensor(out=ke[:, d, :], in0=ke[:, d, :], in1=kac[:], op=ALU.subtract),
                           reads=[b_ke, b_c], writes=[b_ke])
                    dve.op(lambda e: e.tensor_tensor(out=ke[:, d, :], in0=ke[:, d, :], in1=kk_, op=ALU.mult),
                           reads=[b_ke, b_xs], writes=[b_ke])
                    dve.op(lambda e: e.tensor_tensor(out=ke[:, d, :], in0=ke[:, d, :], in1=kk_, op=ALU.add),
                           reads=[b_ke, b_xs], writes=[b_ke])
                    i = outbuf()
                    dve.op(lambda e: e.tensor_copy(out=o[i][:], in_=ke[:, d, :]), reads=[b_ke], writes=[b_o[i]])
                    store("KE%d" % d, tt, i)
                if CUT <= 7:
                    continue
                dve.op(lambda e: e.tensor_tensor(out=tt_[:, 0:1024], in0=ke[:, 0, :], in1=ke[:, 1, :], op=ALU.add), reads=[b_ke], writes=[b_tt])
                dve.op(lambda e: e.tensor_tensor(out=tt_[:, 0:1024], in0=tt_[:, 0:1024], in1=rr_, op=ALU.mult), reads=[b_tt, b_xs], writes=[b_tt])
                dve.op(lambda e: e.tensor_tensor(out=tt_[:, 0:1024], in0=tt_[:, 0:1024], in1=rkc[:], op=ALU.mult), reads=[b_tt, b_c], writes=[b_tt])
                dve.op(lambda e: e.tensor_reduce(out=s16[:], in_=tt_[:, 0:1024].rearrange("p (h d) -> p h d", d=64), axis=AX.X, op=ALU.add),
                       reads=[b_tt], writes=[b_s16])
                i = outbuf()
                dve.op(lambda e: e.tensor_tensor(out=o[i][:].rearrange("p (h d) -> p h d", d=64), in0=vv_.rearrange("p (h d) -> p h d", d=64),
                                                 in1=s16[:].unsqueeze(2).broadcast_to([128, 16, 64]), op=ALU.mult),
                       reads=[b_xs, b_s16], writes=[b_o[i]])
                store("BON", tt, i)
            K.barrier()

        for st in phase("3b"):
            mk = sb(st, "smk", [128, 4, 128], F32)
            ld = sb(st, "sld", [128, 2, 6, 1024], F32)
            e4 = sb(st, "se4", [128, 4, 1024], F32)
            bfs = sb(st, "sbfs", [128, 7, 1024], BF16)
            dG = sb(st, "sdG", [64, 1024], F32)
            XT = sb(st, "sXT", [64, 4, 4, 128], BF16)
            pr5 = sb(st, "spr5", [128, 5, 4, 128], BF16)
            Xp = sb(st, "sXp", [128, 2, 4, 192], BF16)
            Pp = sb(st, "sPp", [128, 2, 2, 4, 128], BF16)
            Rh = sb(st, "sRh", [64, 16, 128], BF16)
            Qm = sb(st, "sQm", [128, 16, 128], BF16)
            Gm = sb(st, "sGm", [64, 16, 64], BF16)
            Hm = sb(st, "sHm", [128, 16, 64], BF16)
            ST = sb(st, "sST", [64, 16, 64], BF16)
            yo = sb(st, "syo", [128, 2, 1024], F32)
            pT = [ps(st, "spT%d" % i, [64, 8, 128], BF16) for i in range(2)]
            pA = ps(st, "spA", [128, 2, 512], F32)
            pB = ps(st, "spB", [128, 2, 512], F32)
            pX = ps(st, "spX", [128, 2, 512], F32)
            b_mk = Buf(); b_ld = [Buf(), Buf()]; b_e4 = Buf(); b_bfs = Buf(); b_dG = Buf(); b_XT = Buf(); b_pr5 = Buf()
            b_Xp = [Buf(), Buf()]; b_Pp = [Buf(), Buf()]; b_Rh = Buf(); b_Qm = Buf(); b_Gm = Buf(); b_Hm = Buf(); b_ST = Buf()
            b_yo = [Buf(), Buf()]; b_pT = [Buf(), Buf()]; b_pA = [Buf(), Buf()]; b_pB = [Buf(), Buf()]; b_pX = [Buf(), Buf()]
            slot = [(pA, 0, b_pA[0]), (pA, 1, b_pA[1]), (pB, 0, b_pB[0]), (pB, 1, b_pB[1]), (pX, 0, b_pX[0]), (pX, 1, b_pX[1])]

            def sl(i, shape3):
                t_, j, b = slot[i]
                a, bb_ = shape3
                return t_[:, j, 0:a * bb_].rearrange("p (a b) -> p a b", b=bb_), b

            sp.dma(mk[:], masks_in, writes=[b_mk])
            SU, SL_, UI, LI = 0, 1, 2, 3
            li = 0
            yi = 0
            for d in range(2):
                cum_i, cum_s, mb_s, mb_i, ma_s = (UI, SL_, SU, UI, SL_) if d == 0 else (LI, SU, SL_, LI, SU)
                dve.op(lambda e: e.memset(ST[:], 0.0), writes=[b_ST])
                corder = range(NT) if d == 0 else range(NT - 1, -1, -1)
                names = ("R", "V", "KK", "LW%d" % d, "BB%d" % d, "KE%d" % d)
                for c in corder:
                    lb = li % 2
                    li += 1
                    for qi, nm in enumerate(names):
                        sp.dma(ld[:, lb, qi, :], RWS[nm][c * 128:(c + 1) * 128, :],
                               writes=[b_ld[lb]] if qi == 0 else (), pwrites=() if qi == 0 else [b_ld[lb]])
                    r_, v_, kk_, lw_, bb_, ke_ = (ld[:, lb, qi, :] for qi in range(6))
                    for hf in range(2):
                        pe.op(lambda e: e.matmul(pA[:, hf, :], lhsT=mk[:, cum_i, :], rhs=lw_[:, hf * 512:(hf + 1) * 512], start=True, stop=True),
                              reads=[b_mk, b_ld[lb]], writes=[b_pA[hf]])
                        pe.op(lambda e: e.matmul(pB[:, hf, :], lhsT=mk[:, cum_s, :], rhs=lw_[:, hf * 512:(hf + 1) * 512], start=True, stop=True),
                              reads=[b_mk, b_ld[lb]], writes=[b_pB[hf]])
                    gam = pA[:].rearrange("p a b -> p (a b)")
                    gsf = pB[:].rearrange("p a b -> p (a b)")
                    act.op(lambda e: e.activation(out=e4[:, 0, :], in_=gam, func=AF.Exp), reads=b_pA, writes=[b_e4])
                    act.op(lambda e: e.activation(out=e4[:, 2, :], in_=gam, func=AF.Exp, scale=-1.0), reads=b_pA, pwrites=[b_e4])
                    act.op(lambda e: e.activation(out=e4[:, 3, :], in_=gsf, func=AF.Exp), reads=b_pB, pwrites=[b_e4])
                    act.op(lambda e: e.activation(out=e4[:, 1, :], in_=lw_, func=AF.Exp, scale=-1.0), reads=[b_ld[lb]], pwrites=[b_e4])
                    dve.op(lambda e: e.tensor_tensor(out=e4[:, 1, :], in0=e4[:, 1, :], in1=e4[:, 0, :], op=ALU.mult), reads=[b_e4], pwrites=[b_e4])
                    dve.op(lambda e: e.tensor_tensor(out=dG[:], in0=e4[0:64, 0, :], in1=e4[0:64, 3, :], op=ALU.mult),
                           reads=[b_e4], writes=[b_dG])
                    dve.op(lambda e: e.tensor_tensor(out=dG[:].rearrange("p (h k) -> p h k", k=64), in0=dG[:].rearrange("p (h k) -> p h k", k=64),
                                                     in1=idf[0:64, 0:64].unsqueeze(1).broadcast_to([64, 16, 64]), op=ALU.mult),
                           reads=[b_dG, b_idf], writes=[b_dG])
                    dve.op(lambda e: e.tensor_tensor(out=bfs[:, 0, :], in0=kk_, in1=e4[:, 1, :], op=ALU.mult),
                           reads=[b_ld[lb], b_e4], writes=[b_bfs])
                    dve.op(lambda e: e.tensor_tensor(out=bfs[:, 1, :], in0=bb_, in1=e4[:, 2, :], op=ALU.mult), reads=[b_ld[lb], b_e4], pwrites=[b_bfs])
                    dve.op(lambda e: e.tensor_tensor(out=bfs[:, 2, :], in0=ke_, in1=e4[:, 2, :], op=ALU.mult), reads=[b_ld[lb], b_e4], pwrites=[b_bfs])
                    dve.op(lambda e: e.tensor_tensor(out=bfs[:, 3, :], in0=r_, in1=e4[:, 0, :], op=ALU.mult), reads=[b_ld[lb], b_e4], pwrites=[b_bfs])
                    dve.op(lambda e: e.tensor_tensor(out=bfs[:, 4, :], in0=bb_, in1=e4[:, 3, :], op=ALU.mult), reads=[b_ld[lb], b_e4], pwrites=[b_bfs])
                    dve.op(lambda e: e.tensor_tensor(out=bfs[:, 5, :], in0=ke_, in1=e4[:, 3, :], op=ALU.mult), reads=[b_ld[lb], b_e4], pwrites=[b_bfs])
                    act.op(lambda e: e.activation(out=bfs[:, 6, :], in_=v_, func=AF.Copy), reads=[b_ld[lb], b_bfs], pwrites=[b_bfs])
                    for g4 in range(4):
                        for hh in range(4):
                            h = g4 * 4 + hh
                            tb_ = hh // 2
                            for kd in range(4):
                                first = (hh % 2 == 0 and kd == 0)
                                pe.op(lambda e: e.transpose(out=pT[tb_][:, (hh % 2) * 4 + kd, :], in_=bfs[:, kd, h * 64:(h + 1) * 64], identity=idb[:]),
                                      reads=[b_bfs, b_idb], writes=[b_pT[tb_]] if first else (), pwrites=() if first else [b_pT[tb_]])
                        for tb_ in range(2):
                            evac_copy(XT[:, tb_ * 2:(tb_ + 1) * 2, :, :], pT[tb_][:].rearrange("p (a k) t -> p a k t", k=4), reads=[b_pT[tb_]],
                                      writes=[b_XT] if tb_ == 0 else (), pwrites=() if tb_ == 0 else [b_XT])
                        A_, B_, K_, R_ = 0, 1, 2, 3
                        prods = [(B_, A_, mb_s), (A_, B_, ma_s), (A_, K_, ma_s), (B_, R_, mb_i), (K_, R_, mb_i)]
                        for pi_, (l_, r2_, m_) in enumerate(prods):
                            ap3, bslot = sl(pi_, (4, 128))
                            for hh in range(4):
                                pe.op(lambda e: e.matmul(ap3[:, hh, :], lhsT=XT[:, hh, l_, :], rhs=XT[:, hh, r2_, :], start=True, stop=True),
                                      reads=[b_XT], writes=[bslot] if hh == 0 else (), pwrites=() if hh == 0 else [bslot])
                            dve.op(lambda e: e.tensor_tensor(out=pr5[:, pi_, :, :], in0=ap3, in1=mk[:, m_, :].unsqueeze(1).broadcast_to([128, 4, 128]), op=ALU.mult),
                                   reads=[bslot, b_mk], writes=[b_pr5] if pi_ == 0 else (), pwrites=() if pi_ == 0 else [b_pr5])
                        act.op(lambda e: e.activation(out=Xp[:, 0, :, 0:64], in_=bfs[:, 0, g4 * 256:(g4 + 1) * 256].rearrange("p (h k) -> p h k", k=64), func=AF.Copy),
                               reads=[b_bfs], writes=[b_Xp[0]])
                        act.op(lambda e: e.activation(out=Xp[:, 0, :, 64:192], in_=pr5[:, 2, :, :], func=AF.Copy), reads=[b_pr5], pwrites=[b_Xp[0]])
                        xi = 0
                        for it in range(7):
                            if it == 0:
                                Pc, PTc, bP = pr5[:, 0], pr5[:, 1], b_pr5
                            else:
                                Pc, PTc, bP = Pp[:, it % 2, 0], Pp[:, it % 2, 1], b_Pp[it % 2]
                            xa = pX[:].rearrange("p a (h x) -> p (a h) x", x=256)[:, :, 0:192]
                            for hh in range(4):
                                pe.op(lambda e: e.matmul(xa[:, hh, :], lhsT=Pc[:, hh, :], rhs=Xp[:, xi, hh, :], start=True, stop=True),
                                      reads=[bP, b_Xp[xi]], writes=[b_pX[0], b_pX[1]] if hh == 0 else (), pwrites=() if hh == 0 else [b_pX[0], b_pX[1]])
                            dve.op(lambda e: e.tensor_tensor(out=Xp[:, 1 - xi, :, :], in0=xa, in1=Xp[:, xi, :, :], op=ALU.add),
                                   reads=[b_pX[0], b_pX[1], b_Xp[xi]], writes=[b_Xp[1 - xi]])
                            xi = 1 - xi
                            if it < 6:
                                nP = (it + 1) % 2
                                a0_, bs0 = sl(0, (4, 128))
                                a1_, bs1 = sl(1, (4, 128))
                                for hh in range(4):
                                    pe.op(lambda e: e.matmul(a0_[:, hh, :], lhsT=PTc[:, hh, :], rhs=Pc[:, hh, :], start=True, stop=True),
                                          reads=[bP], writes=[bs0] if hh == 0 else (), pwrites=() if hh == 0 else [bs0])
                                for hh in range(4):
                                    pe.op(lambda e: e.matmul(a1_[:, hh, :], lhsT=Pc[:, hh, :], rhs=PTc[:, hh, :], start=True, stop=True),
                                          reads=[bP], writes=[bs1] if hh == 0 else (), pwrites=() if hh == 0 else [bs1])
                                act.op(lambda e: e.activation(out=Pp[:, nP, 0], in_=a0_, func=AF.Copy), reads=[bs0], writes=[b_Pp[nP]])
                                dve.op(lambda e: e.tensor_copy(out=Pp[:, nP, 1], in_=a1_), reads=[bs1], pwrites=[b_Pp[nP]])
                        Xf = Xp[:, xi]
                        bXf = b_Xp[xi]
                        hs = slice(g4 * 4, g4 * 4 + 4)
                        a2_, bs2 = sl(2, (4, 128))
                        for hh in range(4):
                            pe.op(lambda e: e.matmul(a2_[0:64, hh, :], lhsT=Xf[:, hh, 0:64], rhs=pr5[:, 3, hh, :], start=True, stop=True),
                                  reads=[bXf, b_pr5], writes=[bs2] if hh == 0 else (), pwrites=() if hh == 0 else [bs2])
                        dve.op(lambda e: e.tensor_tensor(out=Rh[:, hs, :], in0=a2_[0:64], in1=XT[:, :, 3, :], op=ALU.add),
                               reads=[bs2, b_XT], pwrites=[b_Rh])
                        a3_, bs3 = sl(3, (4, 128))
                        for hh in range(4):
                            pe.op(lambda e: e.matmul(a3_[:, hh, :], lhsT=Xf[:, hh, 64:192], rhs=pr5[:, 3, hh, :], start=True, stop=True),
                                  reads=[bXf, b_pr5], writes=[bs3] if hh == 0 else (), pwrites=() if hh == 0 else [bs3])
                        dve.op(lambda e: e.tensor_tensor(out=Qm[:, hs, :], in0=a3_, in1=pr5[:, 4, :, :], op=ALU.add),
                               reads=[bs3, b_pr5], pwrites=[b_Qm])
                        a0_, bs0 = sl(0, (4, 64))
                        a1_, bs1 = sl(1, (4, 64))
                        for hh in range(4):
                            h = g4 * 4 + hh
                            pe.op(lambda e: e.matmul(a0_[0:64, hh, :], lhsT=Xf[:, hh, 0:64], rhs=bfs[:, 4, h * 64:(h + 1) * 64], start=True, stop=True),
                                  reads=[bXf, b_bfs], writes=[bs0] if hh == 0 else (), pwrites=() if hh == 0 else [bs0])
                        for hh in range(4):
                            h = g4 * 4 + hh
                            pe.op(lambda e: e.matmul(a1_[:, hh, :], lhsT=Xf[:, hh, 64:192], rhs=bfs[:, 4, h * 64:(h + 1) * 64], start=True, stop=True),
                                  reads=[bXf, b_bfs], writes=[bs1] if hh == 0 else (), pwrites=() if hh == 0 else [bs1])
                        dve.op(lambda e: e.tensor_tensor(out=Gm[:, hs, :], in0=a0_[0:64], in1=dG[:, g4 * 256:(g4 + 1) * 256].rearrange("p (h k) -> p h k", k=64), op=ALU.add),
                               reads=[bs0, b_dG], pwrites=[b_Gm])
                        dve.op(lambda e: e.tensor_tensor(out=Hm[:, hs, :], in0=a1_, in1=bfs[:, 5, g4 * 256:(g4 + 1) * 256].rearrange("p (h k) -> p h k", k=64), op=ALU.add),
                               reads=[bs1, b_bfs], pwrites=[b_Hm])
                    yv = pA[:].rearrange("p a (h v) -> p (a h) v", v=64)
                    sv = pB[0:64].rearrange("p a (h v) -> p (a h) v", v=64)
                    for h in range(16):
                        vh = bfs[:, 6, h * 64:(h + 1) * 64]
                        pe.op(lambda e: e.matmul(yv[:, h, :], lhsT=Rh[:, h, :], rhs=ST[:, h, :], start=True, stop=False),
                              reads=[b_Rh, b_ST], writes=b_pA if h == 0 else (), pwrites=() if h == 0 else b_pA)
                        pe.op(lambda e: e.matmul(yv[:, h, :], lhsT=Qm[:, h, :], rhs=vh, start=False, stop=True),
                              reads=[b_Qm, b_bfs], pwrites=b_pA)
                        pe.op(lambda e: e.matmul(sv[:, h, :], lhsT=Gm[:, h, :], rhs=ST[:, h, :], start=True, stop=False),
                              reads=[b_Gm, b_ST], writes=b_pB if h == 0 else (), pwrites=() if h == 0 else b_pB)
                        pe.op(lambda e: e.matmul(sv[:, h, :], lhsT=Hm[:, h, :], rhs=vh, start=False, stop=True),
                              reads=[b_Hm, b_bfs], pwrites=b_pB)
                    yb2 = yi % 2
                    yi += 1
                    act.op(lambda e: e.activation(out=yo[:, yb2, :], in_=pA[:].rearrange("p a b -> p (a b)"), func=AF.Copy), reads=b_pA, writes=[b_yo[yb2]])
                    dve.op(lambda e: e.tensor_copy(out=ST[:], in_=sv), reads=b_pB, writes=[b_ST])
                    sp.dma(RWS["Y%d" % d][c * 128:(c + 1) * 128, :], yo[:, yb2, :], reads=[b_yo[yb2]])
            K.barrier()

        for st in phase("3c"):
            ld = sb(st, "cld", [128, 2, 4, 1024], F32)
            lw_ = sb(st, "clw", [128, 1024], F32)
            lb_ = sb(st, "clb", [128, 1024], F32)
            y = sb(st, "cy", [128, 1024], F32)
            sq = sb(st, "csq", [128, 1024], F32)
            m16 = sb(st, "cm16", [128, 16], F32)
            v16 = sb(st, "cv16", [128, 16], F32)
            yb16 = sb(st, "cyb", [128, 2, 1024], BF16)
            b_ld = [Buf(), Buf()]; b_c = Buf(); b_y = Buf(); b_sq = Buf(); b_m = Buf(); b_v = Buf(); b_yb = [Buf(), Buf()]
            sp.dma(lw_[:], lnw_in, writes=[b_c])
            sp.dma(lb_[:], lnb_in, pwrites=[b_c])
            h3 = lambda ap: ap.rearrange("p (h d) -> p h d", d=64)
            for tt in range(NT):
                lb = tt % 2
                for qi, nm in enumerate(("Y0", "Y1", "BON", "G")):
                    sp.dma(ld[:, lb, qi, :], RWS[nm][tt * 128:(tt + 1) * 128, :], writes=[b_ld[lb]] if qi == 0 else (), pwrites=() if qi == 0 else [b_ld[lb]])
                dve.op(lambda e: e.tensor_tensor(out=y[:], in0=ld[:, lb, 0, :], in1=ld[:, lb, 1, :], op=ALU.add), reads=[b_ld[lb]], writes=[b_y])
                dve.op(lambda e: e.tensor_reduce(out=m16[:], in_=h3(y[:]), axis=AX.X, op=ALU.add), reads=[b_y], writes=[b_m])
                dve.op(lambda e: e.tensor_scalar(out=m16[:], in0=m16[:], scalar1=1.0 / 64, scalar2=None, op0=ALU.mult), reads=[b_m], writes=[b_m])
                dve.op(lambda e: e.tensor_tensor(out=h3(y[:]), in0=h3(y[:]), in1=m16[:].unsqueeze(2).broadcast_to([128, 16, 64]), op=ALU.subtract),
                       reads=[b_y, b_m], writes=[b_y])
                dve.op(lambda e: e.tensor_tensor(out=sq[:], in0=y[:], in1=y[:], op=ALU.mult), reads=[b_y], writes=[b_sq])
                dve.op(lambda e: e.tensor_reduce(out=v16[:], in_=h3(sq[:]), axis=AX.X, op=ALU.add), reads=[b_sq], writes=[b_v])
                act.op(lambda e: e.activation(out=v16[:], in_=v16[:], func=AF.Sqrt, scale=1.0 / 64, bias=64e-5), reads=[b_v], writes=[b_v])
                dve.op(lambda e: e.reciprocal(out=v16[:], in_=v16[:]), reads=[b_v], writes=[b_v])
                dve.op(lambda e: e.tensor_tensor(out=h3(y[:]), in0=h3(y[:]), in1=v16[:].unsqueeze(2).broadcast_to([128, 16, 64]), op=ALU.mult),
                       reads=[b_y, b_v], writes=[b_y])
                dve.op(lambda e: e.tensor_tensor(out=y[:], in0=y[:], in1=lw_[:], op=ALU.mult), reads=[b_y, b_c], writes=[b_y])
                dve.op(lambda e: e.tensor_tensor(out=y[:], in0=y[:], in1=lb_[:], op=ALU.add), reads=[b_y, b_c], writes=[b_y])
                dve.op(lambda e: e.tensor_tensor(out=y[:], in0=y[:], in1=ld[:, lb, 2, :], op=ALU.add), reads=[b_y, b_ld[lb]], writes=[b_y])
                dve.op(lambda e: e.tensor_tensor(out=yb16[:, lb, :], in0=y[:], in1=ld[:, lb, 3, :], op=ALU.mult), reads=[b_y, b_ld[lb]], writes=[b_yb[lb]])
                sp.dma(y_b[tt * 128:(tt + 1) * 128, :], yb16[:, lb, :], reads=[b_yb[lb]])
            K.barrier()

    if SKIP_RWKV:
        for st in phase("zero"):
            z = sb(st, "zz", [128, 1024], BF16)
            b_z = Buf()
            dve.op(lambda e: e.memset(z[:], 0.0), writes=[b_z])
            for tt in range(NT):
                sp.dma(y_b[tt * 128:(tt + 1) * 128, :], z[:], reads=[b_z])
            K.barrier()
    else:
        rwkv_phase()

    for st in phase("4a"):
        Wa = sb(st, "Wa", [128, 8, D], BF16)
        Wb = sb(st, "Wb", [128, 8, D], BF16)
        yt = sb(st, "yt", [128, 2, 1024], BF16)
        yT = sb(st, "yT", [128, 2, 8, 512], BF16)
        gt = sb(st, "gt", [128, 2, 2, 512], BF16)
        ma = sb(st, "ma", [128, 512], F32)
        mbt = sb(st, "mbt", [128, 512], F32)
        mst = sb(st, "mst", [128, 2, 512], BF16)
        pt = [ps(st, "pt4a", [128, 8, 128], BF16), ps(st, "pt4b", [128, 8, 128], BF16)]
        pm = [ps(st, "pm4_%d" % i, [128, 512], F32) for i in range(4)]
        b_W = Buf(); b_yt = [Buf(), Buf()]; b_yT = [Buf(), Buf()]; b_gt = [Buf(), Buf()]; b_ma = Buf(); b_mb = Buf()
        b_mst = [Buf(), Buf()]; b_pt = [Buf(), Buf()]; b_pm = [Buf() for _ in range(4)]
        pool.dma(Wa[:], w_ba.rearrange("(c p) n -> p c n", p=128), writes=[b_W])
        pool.dma(Wb[:], w_bb.rearrange("(c p) n -> p c n", p=128), pwrites=[b_W])
        li = 0
        gi = 0
        for tb in range(T // 512):
            for br, ysrc in enumerate((y_a, y_b)):
                for t4 in range(4):
                    tt = tb * 4 + t4
                    lb = li % 2
                    li += 1
                    sp.dma(yt[:, lb, :], ysrc[tt * 128:(tt + 1) * 128, :], writes=[b_yt[lb]])
                    for c in range(8):
                        pe.op(lambda e: e.transpose(out=pt[lb][:, c, :], in_=yt[:, lb, c * 128:(c + 1) * 128], identity=idb[:]),
                              reads=[b_yt[lb], b_idb], writes=[b_pt[lb]] if c == 0 else (), pwrites=() if c == 0 else [b_pt[lb]])
                    evac_copy(yT[:, br, :, t4 * 128:(t4 + 1) * 128], pt[lb][:], reads=[b_pt[lb]],
                              writes=[b_yT[br]] if t4 == 0 else (), pwrites=() if t4 == 0 else [b_yT[br]])
            for fb in range(16):
                gb = gi % 2
                gi += 1
                sp.dma(gt[:, gb, 0, :], gateT[fb * 128:(fb + 1) * 128, tb * 512:(tb + 1) * 512], writes=[b_gt[gb]])
                sp.dma(gt[:, gb, 1, :], gateT[D + fb * 128:D + (fb + 1) * 128, tb * 512:(tb + 1) * 512], pwrites=[b_gt[gb]])
                pa = (2 * fb) % 4
                pb = (2 * fb + 1) % 4
                mm_group(pm[pa][:], b_pm[pa], [(Wa[:, c, fb * 128:(fb + 1) * 128], yT[:, 0, c, :]) for c in range(8)], reads=[b_W, b_yT[0]])
                mm_group(pm[pb][:], b_pm[pb], [(Wb[:, c, fb * 128:(fb + 1) * 128], yT[:, 1, c, :]) for c in range(8)], reads=[b_W, b_yT[1]])
                dve.op(lambda e: e.tensor_tensor(out=ma[:], in0=pm[pa][:], in1=gt[:, gb, 0, :], op=ALU.mult), reads=[b_pm[pa], b_gt[gb]], writes=[b_ma])
                dve.op(lambda e: e.tensor_tensor(out=mbt[:], in0=pm[pb][:], in1=gt[:, gb, 1, :], op=ALU.mult), reads=[b_pm[pb], b_gt[gb]], writes=[b_mb])
                dve.op(lambda e: e.tensor_tensor(out=mst[:, gb, :], in0=ma[:], in1=mbt[:], op=ALU.add), reads=[b_ma, b_mb], writes=[b_mst[gb]])
                sp.dma(mT_d[fb * 128:(fb + 1) * 128, tb * 512:(tb + 1) * 512], mst[:, gb, :], reads=[b_mst[gb]])
        K.barrier()

    aff = sb(gst, "aff", [128, NT, NE], F32)
    b_aff = Buf()
    for st in phase("4b"):
        Wo = sb(st, "Wo", [128, 16, D], BF16)
        Wr = sb(st, "Wr", [128, 16, NE], BF16)
        g2 = sb(st, "g2", [128, 16], F32)
        mT = sb(st, "mT", [128, 2, 16, 512], BF16)
        xt = sb(st, "xt4", [128, 2, D], F32)
        ht = sb(st, "ht", [128, 2, D], F32)
        junk = sb(st, "junk4", [128, D], BF16)
        xn = sb(st, "xn4", [128, D], BF16)
        ssq = sb(st, "ssq4", [128, 1], F32)
        hst = sb(st, "hst", [128, 2, 16, 512], BF16)
        lg = sb(st, "lg", [128, NE], F32)
        mx = sb(st, "mx", [128, 1], F32)
        sm = sb(st, "sm", [128, 1], F32)
        pt = [ps(st, "pt5a", [128, 8, 128], BF16), ps(st, "pt5b", [128, 8, 128], BF16)]
        pm = [ps(st, "pm5_%d" % i, [128, 512], F32) for i in range(4)]
        pr = ps(st, "pr5", [128, NE], F32)
        b_Wo = Buf(); b_Wr = Buf(); b_g2 = Buf(); b_mT = [Buf(), Buf()]; b_xt = [Buf(), Buf()]; b_ht = [Buf(), Buf()]
        b_junk = Buf(); b_xn = Buf(); b_ssq = Buf(); b_hst = [Buf(), Buf()]; b_lg = Buf(); b_mx = Buf(); b_sm = Buf()
        b_pt = [Buf(), Buf()]; b_pm = [Buf() for _ in range(4)]; b_pr = Buf()
        pool.dma(Wo[:], w_o.rearrange("(c p) n -> p c n", p=128), writes=[b_Wo])
        pool.dma(Wr[:], w_r.rearrange("(c p) n -> p c n", p=128), writes=[b_Wr])
        sp.dma(g2[:], g2T, writes=[b_g2])
        pi = 0
        for tb in range(T // 512):
            mb = tb % 2
            sp.dma(mT[:, mb, :, :], mT_d.rearrange("(c p) t -> p c t", p=128)[:, :, tb * 512:(tb + 1) * 512], writes=[b_mT[mb]])
            for t4 in range(4):
                tt = tb * 4 + t4
                bi = tt % 2
                sp.dma(xt[:, bi, :], x[tt * 128:(tt + 1) * 128, :], writes=[b_xt[bi]])
                for cc in range(4):
                    p = pi % 4
                    pi += 1
                    mm_group(pm[p][:], b_pm[p],
                             [(mT[:, mb, kc, t4 * 128:(t4 + 1) * 128], Wo[:, kc, cc * 512:(cc + 1) * 512]) for kc in range(16)],
                             reads=[b_mT[mb], b_Wo])
                    dve.op(lambda e: e.tensor_tensor(out=ht[:, bi, cc * 512:(cc + 1) * 512], in0=pm[p][:], in1=xt[:, bi, cc * 512:(cc + 1) * 512], op=ALU.add),
                           reads=[b_pm[p], b_xt[bi]], writes=[b_ht[bi]] if cc == 0 else (), pwrites=() if cc == 0 else [b_ht[bi]])
                sp.dma(out[tt * 128:(tt + 1) * 128, :], ht[:, bi, :], reads=[b_ht[bi]])
                rmsnorm_T(st, "p4", ht[:, bi, :], b_ht[bi], g2, b_g2, hst[:, mb, :, t4 * 128:(t4 + 1) * 128], b_hst[mb],
                          pt, b_pt, (junk, b_junk, ssq, b_ssq, xn, b_xn), store_ap=xn2_d[tt * 128:(tt + 1) * 128, :])
                mm_group(pr[:], b_pr, [(hst[:, mb, kc, t4 * 128:(t4 + 1) * 128], Wr[:, kc, :]) for kc in range(16)], reads=[b_hst[mb], b_Wr])
                dve.op(lambda e: e.tensor_reduce(out=mx[:], in_=pr[:], axis=AX.X, op=ALU.max), reads=[b_pr], writes=[b_mx])
                dve.op(lambda e: e.tensor_scalar(out=mx[:], in0=mx[:], scalar1=-1.0, scalar2=None, op0=ALU.mult), reads=[b_mx], writes=[b_mx])
                dve.op(lambda e: e.memset(sm[:], 0.0), writes=[b_sm])
                act.op(lambda e: e.activation(out=lg[:], in_=pr[:], func=AF.Exp, bias=mx[:, 0:1], accum_out=sm[:]),
                       reads=[b_pr, b_mx], writes=[b_lg, b_sm])
                dve.op(lambda e: e.reciprocal(out=sm[:], in_=sm[:]), reads=[b_sm], writes=[b_sm])
                dve.op(lambda e: e.tensor_scalar(out=aff[:, tt, :], in0=lg[:], scalar1=sm[:, 0:1], scalar2=None, op0=ALU.mult),
                       reads=[b_lg, b_sm], pwrites=[b_aff])
            sp.dma(hn2T_d.rearrange("(c p) t -> p c t", p=128)[:, :, tb * 512:(tb + 1) * 512], hst[:, mb, :, :], reads=[b_hst[mb]])
        if DEBUG:
            sp.dma(aff_d, aff[:], reads=[b_aff])
        K.barrier()

    coef = sb(gst, "coef", [128, NT, NE], F32)
    b_coef = Buf()
    for st in phase("5"):
        affT = sb(st, "affT", [NE, T], F32)
        work = sb(st, "work", [NE, T], F32)
        cT = sb(st, "cT", [NE, T], F32)
        m8 = sb(st, "m8", [NE, 8], F32)
        pa = [ps(st, "pa%d" % i, [NE, 4, 128], F32) for i in range(2)]
        pc = [ps(st, "pc%d" % i, [128, 4, NE], F32) for i in range(2)]
        b_affT = Buf(); b_work = Buf(); b_cT = Buf(); b_m8 = Buf(); b_pa = [Buf(), Buf()]; b_pc = [Buf(), Buf()]
        for g in range(NT // 4):
            pb = g % 2
            for j in range(4):
                tt = g * 4 + j
                pe.op(lambda e: e.transpose(out=pa[pb][:, j, :], in_=aff[:, tt, :], identity=idf[:]),
                      reads=[b_aff, b_idf], writes=[b_pa[pb]] if j == 0 else (), pwrites=() if j == 0 else [b_pa[pb]])
            dve.op(lambda e: e.tensor_copy(out=affT[:, g * 512:(g + 1) * 512], in_=pa[pb][:].rearrange("p a b -> p (a b)")),
                   reads=[b_pa[pb]], pwrites=[b_affT])
        dve.op(lambda e: e.tensor_copy(out=work[:], in_=affT[:]), reads=[b_affT], writes=[b_work])
        for r in range(CAP // 8):
            dve.op(lambda e: e.max(out=m8[:], in_=work[:]), reads=[b_work], writes=[b_m8])
            if r < CAP // 8 - 1:
                dve.op(lambda e: e.match_replace(out=work[:], in_to_replace=m8[:], in_values=work[:], imm_value=-1.0),
                       reads=[b_m8, b_work], writes=[b_work])
        dve.op(lambda e: e.scalar_tensor_tensor(out=cT[:], in0=affT[:], scalar=m8[:, 7:8], in1=affT[:], op0=ALU.is_ge, op1=ALU.mult),
               reads=[b_affT, b_m8], writes=[b_cT])
        for g in range(NT // 4):
            pb = g % 2
            for j in range(4):
                tt = g * 4 + j
                pe.op(lambda e: e.transpose(out=pc[pb][:, j, :], in_=cT[:, tt * 128:(tt + 1) * 128], identity=idf[0:NE, 0:NE]),
                      reads=[b_cT, b_idf], writes=[b_pc[pb]] if j == 0 else (), pwrites=() if j == 0 else [b_pc[pb]])
            dve.op(lambda e: e.tensor_copy(out=coef[:, g * 4:(g + 1) * 4, :], in_=pc[pb][:]), reads=[b_pc[pb]], pwrites=[b_coef])
        K.barrier()

    for st in phase("6"):
        Wg = sb(st, "Wg", [128, 16, FF], BF16)
        Wu = sb(st, "Wu", [128, 16, FF], BF16)
        Wd = sb(st, "Wd", [128, 8, D], BF16)
        hT = sb(st, "hT6", [128, 2, 16, 512], BF16)
        sg = sb(st, "sg6", [128, 2, 512], F32)
        hid = sb(st, "hid6", [128, 2, 8, 512], BF16)
        yo = sb(st, "yo6", [128, 2, D], F32)
        pg = [ps(st, "pg6_%d" % i, [128, 512], F32) for i in range(2)]
        pu = [ps(st, "pu6_%d" % i, [128, 512], F32) for i in range(2)]
        po = [ps(st, "po6_%d" % i, [128, 512], F32) for i in range(4)]
        b_Wg = Buf(); b_Wu = Buf(); b_Wd = Buf(); b_hT = [Buf(), Buf()]; b_sg = [Buf(), Buf()]; b_hid = [Buf(), Buf()]
        b_yo = [Buf(), Buf()]; b_pg = [Buf(), Buf()]; b_pu = [Buf(), Buf()]; b_po = [Buf() for _ in range(4)]
        b_out = [Buf() for _ in range(NT)]
        hi = 0
        fi = 0
        oi = 0
        yi = 0
        for ex in range(NE):
            pool.dma(Wg[:], w_g[ex].rearrange("(c p) n -> p c n", p=128), writes=[b_Wg])
            pool.dma(Wu[:], w_u[ex].rearrange("(c p) n -> p c n", p=128), writes=[b_Wu])
            pool.dma(Wd[:], w_d[ex].rearrange("(c p) n -> p c n", p=128), writes=[b_Wd])
            for tb in range(T // 512):
                hb = hi % 2
                hi += 1
                sp.dma(hT[:, hb, :, :], hn2T_d.rearrange("(c p) t -> p c t", p=128)[:, :, tb * 512:(tb + 1) * 512], writes=[b_hT[hb]])
                for fb in range(8):
                    f2 = fi % 2
                    fi += 1
                    mm_group(pg[f2][:], b_pg[f2], [(Wg[:, kc, fb * 128:(fb + 1) * 128], hT[:, hb, kc, :]) for kc in range(16)], reads=[b_Wg, b_hT[hb]])
                    mm_group(pu[f2][:], b_pu[f2], [(Wu[:, kc, fb * 128:(fb + 1) * 128], hT[:, hb, kc, :]) for kc in range(16)], reads=[b_Wu, b_hT[hb]])
                    act.op(lambda e: e.activation(out=sg[:, f2, :], in_=pg[f2][:], func=AF.Silu), reads=[b_pg[f2]], writes=[b_sg[f2]])
                    dve.op(lambda e: e.tensor_tensor(out=hid[:, hb, fb, :], in0=sg[:, f2, :], in1=pu[f2][:], op=ALU.mult),
                           reads=[b_sg[f2], b_pu[f2]], writes=[b_hid[hb]] if fb == 0 else (), pwrites=() if fb == 0 else [b_hid[hb]])
                for t4 in range(4):
                    tt = tb * 4 + t4
                    yb_ = yi % 2
                    yi += 1
                    for cc in range(4):
                        p = oi % 4
                        oi += 1
                        mm_group(po[p][:], b_po[p], [(hid[:, hb, fb, t4 * 128:(t4 + 1) * 128], Wd[:, fb, cc * 512:(cc + 1) * 512]) for fb in range(8)],
                                 reads=[b_hid[hb], b_Wd])
                        evq[0] += 1
                        if evq[0] % 2 == 0:
                            dve.op(lambda e: e.tensor_scalar(out=yo[:, yb_, cc * 512:(cc + 1) * 512], in0=po[p][:], scalar1=coef[:, tt, ex:ex + 1], scalar2=None, op0=ALU.mult),
                                   reads=[b_po[p], b_coef], writes=[b_yo[yb_]] if cc == 0 else (), pwrites=() if cc == 0 else [b_yo[yb_]])
                        else:
                            act.op(lambda e: e.activation(out=yo[:, yb_, cc * 512:(cc + 1) * 512], in_=po[p][:], func=AF.Copy, scale=coef[:, tt, ex:ex + 1]),
                                   reads=[b_po[p], b_coef], writes=[b_yo[yb_]] if cc == 0 else (), pwrites=() if cc == 0 else [b_yo[yb_]])
                    pool.dma(out[tt * 128:(tt + 1) * 128, :], yo[:, yb_, :], reads=[b_yo[yb_]], writes=[b_out[tt]], accum_op=ALU.add)
        K.barrier()

    for st in phase("5g"):
        affT = sb(st, "affTg", [NE, T], F32)
        work = sb(st, "workg", [NE, T], F32)
        vals = sb(st, "valsg", [NE, CAP], F32)
        idxu = sb(st, "idxug", [NE, CAP], U32)
        pa = [ps(st, "pag%d" % i, [NE, 4, 128], F32) for i in range(2)]
        b_affT = Buf(); b_work = Buf(); b_vals = Buf(); b_idx = Buf(); b_pa = [Buf(), Buf()]
        for g in range(NT // 4):
            pb = g % 2
            for j in range(4):
                tt = g * 4 + j
                pe.op(lambda e: e.transpose(out=pa[pb][:, j, :], in_=aff[:, tt, :], identity=idf[:]),
                      reads=[b_aff, b_idf], writes=[b_pa[pb]] if j == 0 else (), pwrites=() if j == 0 else [b_pa[pb]])
            dve.op(lambda e: e.tensor_copy(out=affT[:, g * 512:(g + 1) * 512], in_=pa[pb][:].rearrange("p a b -> p (a b)")),
                   reads=[b_pa[pb]], pwrites=[b_affT])
        dve.op(lambda e: e.tensor_copy(out=work[:], in_=affT[:]), reads=[b_affT], writes=[b_work])
        for r in range(CAP // 8):
            v8 = vals[:, r * 8:(r + 1) * 8]
            dve.op(lambda e: e.max(out=v8, in_=work[:]), reads=[b_work], pwrites=[b_vals])
            dve.op(lambda e: e.max_index(out=idxu[:, r * 8:(r + 1) * 8], in_max=v8, in_values=work[:]), reads=[b_work, b_vals], pwrites=[b_idx])
            dve.op(lambda e: e.match_replace(out=work[:], in_to_replace=v8, in_values=work[:], imm_value=-1.0),
                   reads=[b_vals, b_work, b_idx], writes=[b_work])
        sp.dma(idx_d, idxu[:].bitcast(I32), reads=[b_idx])
        sp.dma(val_d, vals[:], reads=[b_vals])
        K.barrier()

    for st in phase("6g"):
        NS = CAP // 128
        Wg = sb(st, "Wgg", [128, 16, FF], BF16)
        Wu = sb(st, "Wug", [128, 16, FF], BF16)
        Wd = sb(st, "Wdg", [128, 8, D], BF16)
        g2 = sb(st, "g2g", [128, 16], F32)
        idx_t = sb(st, "idx_t", [128, 2, NS], I32)
        gat = sb(st, "gat", [128, 2, NS], F32)
        xe = sb(st, "xeg", [128, 2, D], BF16)
        xeT = sb(st, "xeTg", [128, 16, CAP], BF16)
        sg = sb(st, "sgg", [128, 2, CAP], F32)
        hid = sb(st, "hidg", [128, 8, CAP], BF16)
        yo = sb(st, "yog", [128, 2, D], F32)
        pt = [ps(st, "ptg%d" % i, [128, 8, 128], BF16) for i in range(2)]
        pg = ps(st, "pgg", [128, 512], F32)
        pu = ps(st, "pug", [128, 512], F32)
        po = [ps(st, "pog%d" % i, [128, 512], F32) for i in range(4)]
        b_Wg = Buf(); b_Wu = Buf(); b_Wd = Buf(); b_g2 = Buf(); b_it = [Buf(), Buf()]; b_xe = [Buf(), Buf()]; b_xeT = Buf()
        b_sg = [Buf(), Buf()]; b_hid = Buf(); b_yo = [Buf(), Buf()]; b_pt = [Buf(), Buf()]; b_pg = Buf(); b_pu = Buf()
        b_po = [Buf() for _ in range(4)]; b_outall = Buf()
        sp.dma(g2[:], g2T, writes=[b_g2])
        xi = 0
        fi = 0
        oi = 0
        yi = 0
        for ex in range(NE):
            eb = ex % 2
            pool.dma(Wg[:], w_g[ex].rearrange("(c p) n -> p c n", p=128), writes=[b_Wg])
            pool.dma(Wu[:], w_u[ex].rearrange("(c p) n -> p c n", p=128), writes=[b_Wu])
            pool.dma(Wd[:], w_d[ex].rearrange("(c p) n -> p c n", p=128), writes=[b_Wd])
            for j in range(NS):
                sp.dma(idx_t[:, eb, j:j + 1], idx_d[ex:ex + 1, j * 128:(j + 1) * 128].rearrange("o p -> p o"),
                       writes=[b_it[eb]] if j == 0 else (), pwrites=() if j == 0 else [b_it[eb]])
                sp.dma(gat[:, eb, j:j + 1], val_d[ex:ex + 1, j * 128:(j + 1) * 128].rearrange("o p -> p o"), pwrites=[b_it[eb]])
            for j in range(NS):
                xb = xi % 2
                xi += 1
                pool.dma(None, None, reads=[b_it[eb]], writes=[b_xe[xb]],
                         fn=lambda e: e.indirect_dma_start(out=xe[:, xb, :], out_offset=None, in_=xn2_d[:, :],
                                                           in_offset=bass.IndirectOffsetOnAxis(ap=idx_t[:, eb, j:j + 1], axis=0)))
                for half in range(2):
                    for c in range(8):
                        cc = half * 8 + c
                        pe.op(lambda e: e.transpose(out=pt[half][:, c, :], in_=xe[:, xb, cc * 128:(cc + 1) * 128], identity=idb[:]),
                              reads=[b_xe[xb], b_idb], writes=[b_pt[half]] if c == 0 else (), pwrites=() if c == 0 else [b_pt[half]])
                    first = (j == 0 and half == 0)
                    dve.op(lambda e: e.tensor_tensor(out=xeT[:, half * 8:(half + 1) * 8, j * 128:(j + 1) * 128], in0=pt[half][:],
                                                     in1=g2[:, half * 8:(half + 1) * 8].unsqueeze(2).broadcast_to([128, 8, 128]), op=ALU.mult),
                           reads=[b_pt[half], b_g2], writes=[b_xeT] if first else (), pwrites=() if first else [b_xeT])
            for fb in range(8):
                f2 = fi % 2
                fi += 1
                mm_group(pg[:, 0:CAP], b_pg, [(Wg[:, kc, fb * 128:(fb + 1) * 128], xeT[:, kc, :]) for kc in range(16)], reads=[b_Wg, b_xeT])
                mm_group(pu[:, 0:CAP], b_pu, [(Wu[:, kc, fb * 128:(fb + 1) * 128], xeT[:, kc, :]) for kc in range(16)], reads=[b_Wu, b_xeT])
                act.op(lambda e: e.activation(out=sg[:, f2, :], in_=pg[:, 0:CAP], func=AF.Silu), reads=[b_pg], writes=[b_sg[f2]])
                dve.op(lambda e: e.tensor_tensor(out=hid[:, fb, :], in0=sg[:, f2, :], in1=pu[:, 0:CAP], op=ALU.mult),
                       reads=[b_sg[f2], b_pu], writes=[b_hid] if fb == 0 else (), pwrites=() if fb == 0 else [b_hid])
            for j in range(NS):
                yb_ = yi % 2
                yi += 1
                for cc in range(4):
                    p = oi % 4
                    oi += 1
                    mm_group(po[p][:], b_po[p], [(hid[:, fb, j * 128:(j + 1) * 128], Wd[:, fb, cc * 512:(cc + 1) * 512]) for fb in range(8)],
                             reads=[b_hid, b_Wd])
                    evq[0] += 1
                    if evq[0] % 2 == 0:
                        dve.op(lambda e: e.tensor_scalar(out=yo[:, yb_, cc * 512:(cc + 1) * 512], in0=po[p][:], scalar1=gat[:, eb, j:j + 1], scalar2=None, op0=ALU.mult),
                               reads=[b_po[p], b_it[eb]], writes=[b_yo[yb_]] if cc == 0 else (), pwrites=() if cc == 0 else [b_yo[yb_]])
                    else:
                        act.op(lambda e: e.activation(out=yo[:, yb_, cc * 512:(cc + 1) * 512], in_=po[p][:], func=AF.Copy, scale=gat[:, eb, j:j + 1]),
                               reads=[b_po[p], b_it[eb]], writes=[b_yo[yb_]] if cc == 0 else (), pwrites=() if cc == 0 else [b_yo[yb_]])
                pool.dma(None, None, reads=[b_yo[yb_], b_it[eb]], writes=[b_outall],
                         fn=lambda e: e.indirect_dma_start(out=out[:, :], out_offset=bass.IndirectOffsetOnAxis(ap=idx_t[:, eb, j:j + 1], axis=0),
                                                           in_=yo[:, yb_, :], in_offset=None, compute_op=ALU.add))
        K.barrier()
    gst.close()
    return nc


_NC_CACHE = {}


def kernel(**inputs):
    f32 = np.float32
    g = lambda k: np.asarray(inputs[k], dtype=f32)
    x = g("x")
    rep = lambda v, n=128: np.ascontiguousarray(np.broadcast_to(v[None], (n,) + v.shape)).astype(f32)
    colT = lambda v: np.ascontiguousarray(v.reshape(16, 128).T).astype(f32)
    inv = 500000.0 ** (-(np.arange(0, 16, 2, dtype=f32) / 16.0))
    ang = np.arange(T, dtype=f32)[:, None] * inv[None, :]
    cos = np.cos(ang).astype(f32).reshape(NT, 128, 8).transpose(1, 0, 2)
    sin = np.sin(ang).astype(f32).reshape(NT, 128, 8).transpose(1, 0, 2)
    common = {
        "w_in": g("w_in")[0],
        "g1T": colT(g("attn_norm_g")[0]),
        "g2T": colT(g("ffn_norm_g")[0]),
        "ident": np.eye(128, dtype=f32),
        "gqk": rep(np.stack([g("q_norm_g")[0], g("k_norm_g")[0]])),
        "rope_c": np.ascontiguousarray(cos),
        "rope_s": np.ascontiguousarray(sin),
        "lam_in": rep(np.stack([g("lambda_q1")[0], g("lambda_k1")[0], g("lambda_q2")[0], g("lambda_k2")[0]])),
        "subg": rep(g("subln_g")[0]),
        "w_ba": g("w_branch_a")[0],
        "w_bb": g("w_branch_b")[0],
        "w_o": g("w_out")[0],
        "w_r": g("w_router")[0],
        "w_g": g("w_gate_e")[0],
        "w_u": g("w_up_e")[0],
        "w_d": g("w_down_e")[0],
        "mu_rep": rep(g("shift_mu")[0]),
        "w0_rep": rep(np.stack([g("w0_f")[0], g("w0_b")[0]])),
        "a0_rep": rep(np.stack([g("a0_f")[0], g("a0_b")[0]])),
        "kk_rep": rep(g("k_k")[0]),
        "ka_rep": rep(g("k_a")[0]),
        "rk_rep": rep(g("r_k")[0].reshape(1024)),
        "lnw_rep": rep(g("ln_x_w")[0]),
        "lnb_rep": rep(g("ln_x_b")[0]),
        "w2_in": np.ascontiguousarray(np.stack([g("w2_f")[0], g("w2_b")[0], g("a2_f")[0], g("a2_b")[0]])),
        "g2_in": g("g2")[0],
        "masks_in": np.ascontiguousarray(np.stack([np.triu(np.ones((128, 128), f32), 1), np.tril(np.ones((128, 128), f32), -1),
                                                   np.triu(np.ones((128, 128), f32), 0), np.tril(np.ones((128, 128), f32), 0)], axis=1)),
    }
    if "nc" not in _NC_CACHE:
        _NC_CACHE["nc"] = build()
    nc = _NC_CACHE["nc"]
    in_maps = []
    for c in range(8):
        m = {k: v for k, v in common.items() if k in DECL}
        m["x"] = np.ascontiguousarray(x[c // 2][:T])
        in_maps.append(m)
    res = run_bass_kernel_spmd(nc, in_maps, core_ids=list(range(8)))
    outp = np.empty((4, T, D), dtype=f32)
    for c in range(8):
        b, half = c // 2, c % 2
        o = np.asarray(res.results[c]["out"])
        outp[b, half * 2048:(half + 1) * 2048] = o[half * 2048:(half + 1) * 2048]
    if DEBUG:
        kernel.dbg = res.results
    return outp
```

```python
import os
import math
from contextlib import ExitStack
import numpy as np
import concourse.bass as bass
import concourse.mybir as mybir
from concourse.bass_utils import run_bass_kernel_spmd

F32 = mybir.dt.float32
BF16 = mybir.dt.bfloat16
I32 = mybir.dt.int32
U32 = mybir.dt.uint32
AF = mybir.ActivationFunctionType
ALU = mybir.AluOpType
AX = mybir.AxisListType

D = 2048
T = int(os.environ.get("KT", "4096"))
NT = T // 128
DA_COLS = 3072
RW_COLS = 3488
GATE_COLS = 4096
IN_COLS = DA_COLS + RW_COLS + GATE_COLS
TM_COLS = DA_COLS + RW_COLS
NE = 16
FF = 1024
CAP = T // 8
LAM_INIT = 0.8 - 0.6 * math.exp(-0.3 * 0)

DEBUG = bool(int(os.environ.get("KDEBUG", "0")))
SKIP_RWKV = bool(int(os.environ.get("KSKIP_RWKV", "0")))
CUT = int(os.environ.get("KCUT", "99"))
PHASES = os.environ.get("KPHASES", "1,2a,2b,3a,3b,3c,4a,4b,5g,6g").split(",")


class Buf:
    __slots__ = ("w", "r", "pr", "name")

    def __init__(self, name=""):
        self.w = {}
        self.r = {}
        self.pr = {}
        self.name = name


class Eng:
    def __init__(self, K, name, eng, is_pe=False):
        self.K = K
        self.name = name
        self.eng = eng
        self.is_pe = is_pe
        self.sem = K.new_sem("s_" + name)
        self.count = 0
        self.seen = {}
        self.dma_pool = []
        self.dma_i = 0

    def _wait(self, toks):
        for sem, val in toks.items():
            if self.is_pe and sem is self.sem:
                continue
            if self.seen.get(sem, 0) >= val:
                continue
            self.eng.wait_ge(sem, val)
            self.seen[sem] = val

    @staticmethod
    def _deps(reads, writes, pwrites):
        toks = {}

        def add(d):
            for s, v in d.items():
                if toks.get(s, 0) < v:
                    toks[s] = v
        for b in reads:
            add(b.w)
        for b in writes:
            add(b.w)
            add(b.r)
        for b in pwrites:
            add(b.r)
            add(b.pr)
        return toks

    @staticmethod
    def _commit(tok, reads, writes, pwrites):
        s, v = tok
        for b in reads:
            b.r[s] = v
        for b in writes:
            b.w = {s: v}
            b.pr = b.r
            b.r = {}
        for b in pwrites:
            b.w[s] = v
            for s2, v2 in b.r.items():
                if b.pr.get(s2, 0) < v2:
                    b.pr[s2] = v2
            b.r = {}

    def op(self, fn, reads=(), writes=(), pwrites=()):
        self._wait(self._deps(reads, writes, pwrites))
        ins = fn(self.eng)
        self.count += 1
        ins.then_inc(self.sem, 1)
        self._commit((self.sem, self.count), reads, writes, pwrites)
        return ins

    def dma(self, out, in_, reads=(), writes=(), pwrites=(), fn=None, **kw):
        self._wait(self._deps(reads, writes, pwrites))
        if len(self.dma_pool) < self.K.n_dma_sems:
            self.dma_pool.append([self.K.new_sem("d_%s%d" % (self.name, len(self.dma_pool))), 0])
        ent = self.dma_pool[self.dma_i % self.K.n_dma_sems]
        self.dma_i += 1
        sem, cur = ent
        if cur > 0 and self.seen.get(sem, 0) < cur:
            self.eng.wait_ge(sem, cur)
            self.seen[sem] = cur
        ins = fn(self.eng) if fn is not None else self.eng.dma_start(out=out, in_=in_, **kw)
        ent[1] = cur + 16
        ins.then_inc(sem, 16)
        self._commit((sem, cur + 16), reads, writes, pwrites)
        return ins


class Kern:
    def __init__(self, nc, n_dma_sems=10):
        self.nc = nc
        self.n_dma_sems = n_dma_sems
        self._sems = []
        self.E = {}

    def new_sem(self, name):
        cm = self.nc.semaphore(name)
        s = cm.__enter__()
        self._sems.append(cm)
        return s

    def setup(self, engines):
        for name, eng in engines.items():
            self.E[name] = Eng(self, name, eng, is_pe=(name == "pe"))

    def barrier(self):
        toks = {}
        for e in self.E.values():
            if e.count > 0:
                toks[e.sem] = e.count
            for sem, cur in e.dma_pool:
                if cur > 0:
                    toks[sem] = cur
        for e in self.E.values():
            for sem, val in toks.items():
                if sem is e.sem:
                    continue
                if e.seen.get(sem, 0) >= val:
                    continue
                e.eng.wait_ge(sem, val)
                e.seen[sem] = val


DECL = set()


def phase(name):
    if name in PHASES or name == "zero":
        with ExitStack() as st:
            yield st


def build():
    nc = bass.Bass("TRN2", target_bir_lowering=False)

    def din(name, shape, dt=F32):
        DECL.add(name)
        return nc.dram_tensor(name, list(shape), dt, kind="ExternalInput").ap()

    def dscr(name, shape, dt, dbg=False):
        kind = "ExternalOutput" if (dbg and DEBUG) else "Internal"
        return nc.dram_tensor(name, list(shape), dt, kind=kind).ap()

    x = din("x", [T, D])
    w_in = din("w_in", [D, IN_COLS])
    g1T = din("g1T", [128, 16])
    g2T = din("g2T", [128, 16])
    ident_in = din("ident", [128, 128])
    gqk = din("gqk", [128, 2, 64])
    rope_c = din("rope_c", [128, NT, 8])
    rope_s = din("rope_s", [128, NT, 8])
    lam_in = din("lam_in", [128, 4, 64])
    subg = din("subg", [128, 128])
    w_ba = din("w_ba", [1024, D])
    w_bb = din("w_bb", [1024, D])
    w_o = din("w_o", [D, D])
    w_r = din("w_r", [D, NE])
    if "6" in PHASES or "6g" in PHASES:
        w_g = din("w_g", [NE, D, FF])
        w_u = din("w_u", [NE, D, FF])
        w_d = din("w_d", [NE, FF, D])

    out = nc.dram_tensor("out", [T, D], F32, kind="ExternalOutput").ap()

    proj_tm = dscr("proj_tm", [T, TM_COLS], BF16, dbg=True)
    gateT = dscr("gateT", [GATE_COLS, T], BF16)
    qkT_d = dscr("qkT_d", [2048, T], BF16)
    y_a = dscr("y_a", [T, 1024], BF16, dbg=True)
    y_b = dscr("y_b", [T, 1024], BF16, dbg=True)
    mT_d = dscr("mT_d", [D, T], BF16)
    hn2T_d = dscr("hn2T_d", [D, T], BF16)
    aff_d = dscr("aff_d", [128, NT, NE], F32, dbg=True)
    xn2_d = dscr("xn2_d", [T, D], BF16)
    idx_d = dscr("idx_d", [NE, CAP], I32)
    val_d = dscr("val_d", [NE, CAP], F32)

    K = Kern(nc)
    K.setup({"sp": nc.sync, "act": nc.scalar, "dve": nc.vector, "pe": nc.tensor, "pool": nc.gpsimd})
    sp, act, dve, pe, pool = (K.E[n] for n in ("sp", "act", "dve", "pe", "pool"))

    gst = ExitStack()

    def sb(st, name, shape, dt):
        return st.enter_context(nc.sbuf_tensor(name, list(shape), dt))

    def ps(st, name, shape, dt):
        return st.enter_context(nc.psum_tensor(name, list(shape), dt))

    idf = sb(gst, "idf", [128, 128], F32)
    idb = sb(gst, "idb", [128, 128], BF16)
    b_idf, b_idb = Buf(), Buf()
    sp.dma(idf[:], ident_in, writes=[b_idf])
    dve.op(lambda e: e.tensor_copy(out=idb[:], in_=idf[:]), reads=[b_idf], writes=[b_idb])

    evq = [0]

    def evac_copy(dst, src, reads, writes=(), pwrites=()):
        evq[0] += 1
        if evq[0] % 2 == 0:
            dve.op(lambda e: e.tensor_copy(out=dst, in_=src), reads=reads, writes=writes, pwrites=pwrites)
        else:
            act.op(lambda e: e.activation(out=dst, in_=src, func=AF.Copy), reads=reads, writes=writes, pwrites=pwrites)

    def mm_group(ps_ap, ps_buf, pairs, reads):
        n = len(pairs)
        for i, (l, r) in enumerate(pairs):
            if i == 0:
                pe.op(lambda e: e.matmul(ps_ap, lhsT=l, rhs=r, start=True, stop=(n == 1)), reads=reads, writes=[ps_buf])
            else:
                pe.op(lambda e: e.matmul(ps_ap, lhsT=l, rhs=r, start=False, stop=(i == n - 1)), reads=reads, pwrites=[ps_buf])

    def rmsnorm_T(st, tag, src_tile, b_src, gT_t, b_gT, dstT_ap, b_dst, pts, b_pts, scratch, store_ap=None):
        junk, b_junk, ssq, b_ssq, xn, b_xn = scratch
        dve.op(lambda e: e.memset(ssq[:], 0.0), writes=[b_ssq])
        act.op(lambda e: e.activation(out=junk[:], in_=src_tile, func=AF.Square, accum_out=ssq[:]),
               reads=[b_src], writes=[b_junk, b_ssq])
        act.op(lambda e: e.activation(out=ssq[:], in_=ssq[:], func=AF.Sqrt, scale=1.0 / D, bias=1e-6),
               reads=[b_ssq], writes=[b_ssq])
        dve.op(lambda e: e.reciprocal(out=ssq[:], in_=ssq[:]), reads=[b_ssq], writes=[b_ssq])
        dve.op(lambda e: e.tensor_scalar(out=xn[:], in0=src_tile, scalar1=ssq[:, 0:1], scalar2=None, op0=ALU.mult),
               reads=[b_src, b_ssq], writes=[b_xn])
        if store_ap is not None:
            sp.dma(store_ap, xn[:], reads=[b_xn])
        for half in range(2):
            for c in range(8):
                cc = half * 8 + c
                pe.op(lambda e: e.transpose(out=pts[half][:, c, :], in_=xn[:, cc * 128:(cc + 1) * 128], identity=idb[:]),
                      reads=[b_xn, b_idb], writes=[b_pts[half]] if c == 0 else (), pwrites=() if c == 0 else [b_pts[half]])
            dve.op(lambda e: e.tensor_tensor(out=dstT_ap[:, half * 8:(half + 1) * 8, :], in0=pts[half][:],
                                             in1=gT_t[:, half * 8:(half + 1) * 8].unsqueeze(2).broadcast_to([128, 8, 128]),
                                             op=ALU.mult),
                   reads=[b_pts[half], b_gT], pwrites=[b_dst])

    for st in phase("1"):
        HT = min(2048, T)
        hnT = sb(st, "hnT", [128, 16, HT], BF16)
        xt = sb(st, "xt", [128, 2, D], F32)
        junk = sb(st, "junk1", [128, D], BF16)
        xn = sb(st, "xn1", [128, D], BF16)
        ssq = sb(st, "ssq1", [128, 1], F32)
        g1 = sb(st, "g1", [128, 16], F32)
        wbf = sb(st, "wbf", [128, 2, 16, 512], BF16)
        stage = sb(st, "stage1", [128, 4, 512], BF16)
        pt = [ps(st, "pt1a", [128, 8, 128], BF16), ps(st, "pt1b", [128, 8, 128], BF16)]
        pm = [ps(st, "pm1_%d" % i, [128, 512], F32) for i in range(4)]
        b_hnT = Buf(); b_xt = [Buf(), Buf()]; b_junk = Buf(); b_xn = Buf(); b_ssq = Buf(); b_g1 = Buf()
        b_wbf = [Buf(), Buf()]; b_stage = [Buf() for _ in range(4)]; b_pt = [Buf(), Buf()]; b_pm = [Buf() for _ in range(4)]
        sp.dma(g1[:], g1T, writes=[b_g1])
        w_v = w_in.rearrange("(c p) n -> p c n", p=128)
        chunks = []
        c0 = 0
        while c0 < TM_COLS:
            cw = min(512, TM_COLS - c0)
            chunks.append(("tm", c0, cw))
            c0 += cw
        for gc in range(GATE_COLS // 512):
            chunks.append(("gate", TM_COLS + gc * 512, 512))
        wi = 0
        si = 0
        pi = 0
        NH16 = HT // 128
        for half in range(T // HT):
            for t16 in range(NH16):
                tt = half * NH16 + t16
                bi = tt % 2
                sp.dma(xt[:, bi, :], x[tt * 128:(tt + 1) * 128, :], writes=[b_xt[bi]])
                rmsnorm_T(st, "p1", xt[:, bi, :], b_xt[bi], g1, b_g1, hnT[:, :, t16 * 128:(t16 + 1) * 128], b_hnT,
                          pt, b_pt, (junk, b_junk, ssq, b_ssq, xn, b_xn))
            for kind, c0, cw in chunks:
                wb = wi % 2
                wi += 1
                pool.dma(wbf[:, wb, :, 0:cw], w_v[:, :, c0:c0 + cw], writes=[b_wbf[wb]])
                if kind == "tm":
                    for t16 in range(NH16):
                        tt = half * NH16 + t16
                        p = pi % 4
                        pi += 1
                        mm_group(pm[p][:, 0:cw], b_pm[p],
                                 [(hnT[:, kc, t16 * 128:(t16 + 1) * 128], wbf[:, wb, kc, 0:cw]) for kc in range(16)],
                                 reads=[b_hnT, b_wbf[wb]])
                        s = si % 4
                        si += 1
                        evac_copy(stage[:, s, 0:cw], pm[p][:, 0:cw], reads=[b_pm[p]], writes=[b_stage[s]])
                        sp.dma(proj_tm[tt * 128:(tt + 1) * 128, c0:c0 + cw], stage[:, s, 0:cw], reads=[b_stage[s]])
                else:
                    gcol = c0 - TM_COLS
                    for blk in range(4):
                        for tc4 in range(HT // 512):
                            p = pi % 4
                            pi += 1
                            mm_group(pm[p][:], b_pm[p],
                                     [(wbf[:, wb, kc, blk * 128:(blk + 1) * 128], hnT[:, kc, tc4 * 512:(tc4 + 1) * 512]) for kc in range(16)],
                                     reads=[b_hnT, b_wbf[wb]])
                            s = si % 4
                            si += 1
                            act.op(lambda e: e.activation(out=stage[:, s, :], in_=pm[p][:], func=AF.Sigmoid),
                                   reads=[b_pm[p]], writes=[b_stage[s]])
                            sp.dma(gateT[gcol + blk * 128:gcol + (blk + 1) * 128, half * HT + tc4 * 512:half * HT + (tc4 + 1) * 512],
                                   stage[:, s, :], reads=[b_stage[s]])
        K.barrier()

    for st in phase("2a"):
        qk = sb(st, "qk", [128, 2, 2048], BF16)
        sq = sb(st, "sq", [128, 32, 64], F32)
        ss = sb(st, "ss", [128, 32], F32)
        qn = sb(st, "qn", [128, 32, 64], F32)
        tmp = sb(st, "ropetmp", [128, 4, 32, 8], F32)
        qb = sb(st, "qb", [128, 32, 64], BF16)
        gq = sb(st, "gq", [128, 2, 64], F32)
        rc = sb(st, "rc", [128, NT, 8], F32)
        rs_ = sb(st, "rs", [128, NT, 8], F32)
        stg = sb(st, "stg2", [128, 2, 16, 512], BF16)
        pt = [ps(st, "pt2a", [128, 8, 128], BF16), ps(st, "pt2b", [128, 8, 128], BF16)]
        b_qk = [Buf(), Buf()]; b_sq = Buf(); b_ss = Buf(); b_qn = Buf(); b_tmp = Buf(); b_qb = Buf()
        b_c = Buf(); b_stg = [Buf(), Buf()]; b_pt = [Buf(), Buf()]
        sp.dma(gq[:], gqk, writes=[b_c])
        sp.dma(rc[:], rope_c, pwrites=[b_c])
        sp.dma(rs_[:], rope_s, pwrites=[b_c])
        for tt in range(NT):
            bi = tt % 2
            sp.dma(qk[:, bi, :], proj_tm[tt * 128:(tt + 1) * 128, 0:2048], writes=[b_qk[bi]])
            qv = qk[:, bi, :].rearrange("p (g d) -> p g d", d=64)
            dve.op(lambda e: e.tensor_tensor(out=sq[:], in0=qv, in1=qv, op=ALU.mult), reads=[b_qk[bi]], writes=[b_sq])
            dve.op(lambda e: e.tensor_reduce(out=ss[:], in_=sq[:], axis=AX.X, op=ALU.add), reads=[b_sq], writes=[b_ss])
            act.op(lambda e: e.activation(out=ss[:], in_=ss[:], func=AF.Sqrt, scale=1.0 / 64, bias=1e-6), reads=[b_ss], writes=[b_ss])
            dve.op(lambda e: e.reciprocal(out=ss[:], in_=ss[:]), reads=[b_ss], writes=[b_ss])
            dve.op(lambda e: e.tensor_tensor(out=qn[:], in0=qv, in1=ss[:].unsqueeze(2).broadcast_to([128, 32, 64]), op=ALU.mult),
                   reads=[b_qk[bi], b_ss], writes=[b_qn])
            for i in range(2):
                dve.op(lambda e: e.tensor_tensor(out=qn[:, i * 16:(i + 1) * 16, :], in0=qn[:, i * 16:(i + 1) * 16, :],
                                                 in1=gq[:, i:i + 1, :].broadcast_to([128, 16, 64]), op=ALU.mult),
                       reads=[b_qn, b_c], writes=[b_qn])
            cb = rc[:, tt:tt + 1, :].broadcast_to([128, 32, 8])
            sbb = rs_[:, tt:tt + 1, :].broadcast_to([128, 32, 8])
            x1 = qn[:, :, 0:8]
            x2 = qn[:, :, 8:16]
            dve.op(lambda e: e.tensor_tensor(out=tmp[:, 0], in0=x1, in1=cb, op=ALU.mult), reads=[b_qn, b_c], writes=[b_tmp])
            dve.op(lambda e: e.tensor_tensor(out=tmp[:, 1], in0=x2, in1=sbb, op=ALU.mult), reads=[b_qn, b_c], pwrites=[b_tmp])
            dve.op(lambda e: e.tensor_tensor(out=tmp[:, 2], in0=x2, in1=cb, op=ALU.mult), reads=[b_qn, b_c], pwrites=[b_tmp])
            dve.op(lambda e: e.tensor_tensor(out=tmp[:, 3], in0=x1, in1=sbb, op=ALU.mult), reads=[b_qn, b_c], pwrites=[b_tmp])
            dve.op(lambda e: e.tensor_copy(out=qb[:], in_=qn[:]), reads=[b_qn], writes=[b_qb])
            dve.op(lambda e: e.tensor_tensor(out=qb[:, :, 0:8], in0=tmp[:, 0], in1=tmp[:, 1], op=ALU.subtract), reads=[b_tmp], writes=[b_qb])
            dve.op(lambda e: e.tensor_tensor(out=qb[:, :, 8:16], in0=tmp[:, 2], in1=tmp[:, 3], op=ALU.add), reads=[b_tmp], writes=[b_qb])
            qbf = qb[:].rearrange("p g d -> p (g d)")
            sg = (tt // 4) % 2
            for half in range(2):
                for c in range(8):
                    cc = half * 8 + c
                    pe.op(lambda e: e.transpose(out=pt[half][:, c, :], in_=qbf[:, cc * 128:(cc + 1) * 128], identity=idb[:]),
                          reads=[b_qb, b_idb], writes=[b_pt[half]] if c == 0 else (), pwrites=() if c == 0 else [b_pt[half]])
                evac_copy(stg[:, sg, half * 8:(half + 1) * 8, (tt % 4) * 128:(tt % 4 + 1) * 128], pt[half][:],
                          reads=[b_pt[half]], writes=[b_stg[sg]] if (tt % 4 == 0 and half == 0) else (),
                          pwrites=() if (tt % 4 == 0 and half == 0) else [b_stg[sg]])
            if tt % 4 == 3:
                t0 = (tt // 4) * 512
                sp.dma(qkT_d.rearrange("(c p) t -> p c t", p=128)[:, :, t0:t0 + 512], stg[:, sg, :, :], reads=[b_stg[sg]])
        K.barrier()

    for st in phase("2b"):
        V1 = sb(st, "V1", [128, NT, 8, 144], BF16)
        qT = sb(st, "qT", [128, 2, T], BF16)
        kT1 = sb(st, "kT1", [128, 2, T], BF16)
        kT2 = sb(st, "kT2", [128, 2, T], BF16)
        PT = sb(st, "PT", [128, 3, 2, 512], BF16)
        lamt = sb(st, "lamt", [128, 4, 64], F32)
        lamp = sb(st, "lamp", [128, 2, 64], F32)
        lam2 = sb(st, "lam2", [128, 2], F32)
        lam = sb(st, "lam", [128, 1], F32)
        sgt = sb(st, "sgt", [128, 128], F32)
        rr = sb(st, "rr", [128, 2], F32)
        o1 = sb(st, "o1", [128, 128], F32)
        o2 = sb(st, "o2", [128, 128], F32)
        osq = sb(st, "osq", [128, 128], F32)
        oms = sb(st, "oms", [128, 1], F32)
        yst = sb(st, "yst", [128, 2, 128], BF16)
        pS = [ps(st, "pS%d" % i, [128, 2, 512], F32) for i in range(2)]
        pO = [ps(st, "pO%d" % i, [128, 2, 256], F32) for i in range(4)]
        b_V1 = Buf(); b_V1z = Buf(); b_q = [Buf(), Buf()]; b_k1 = [Buf(), Buf()]; b_k2 = [Buf(), Buf()]
        b_PT = [Buf() for _ in range(3)]; b_pS = [Buf(), Buf()]; b_pO = [[b_, b_] for b_ in (Buf(), Buf(), Buf(), Buf())]
        b_lam = Buf(); b_sg = Buf(); b_rr = Buf(); b_o1 = Buf(); b_o2 = Buf(); b_osq = Buf(); b_oms = Buf(); b_yst = [Buf(), Buf()]
        sp.dma(lamt[:], lam_in, writes=[b_lam])
        sp.dma(sgt[:], subg, writes=[b_sg])
        dve.op(lambda e: e.tensor_tensor(out=lamp[:, 0, :], in0=lamt[:, 0, :], in1=lamt[:, 1, :], op=ALU.mult), reads=[b_lam], writes=[b_lam])
        dve.op(lambda e: e.tensor_tensor(out=lamp[:, 1, :], in0=lamt[:, 2, :], in1=lamt[:, 3, :], op=ALU.mult), reads=[b_lam], writes=[b_lam])
        dve.op(lambda e: e.tensor_reduce(out=lam2[:], in_=lamp[:], axis=AX.X, op=ALU.add), reads=[b_lam], writes=[b_lam])
        act.op(lambda e: e.activation(out=lam2[:], in_=lam2[:], func=AF.Exp), reads=[b_lam], writes=[b_lam])
        dve.op(lambda e: e.tensor_tensor(out=lam[:], in0=lam2[:, 0:1], in1=lam2[:, 1:2], op=ALU.subtract), reads=[b_lam], writes=[b_lam])
        dve.op(lambda e: e.tensor_scalar(out=lam[:], in0=lam[:], scalar1=LAM_INIT, scalar2=None, op0=ALU.add), reads=[b_lam], writes=[b_lam])
        pool.op(lambda e: e.memset(kT1[:], 0.0), writes=[b_k1[0], b_k1[1]])
        pool.op(lambda e: e.memset(kT2[:], 0.0), writes=[b_k2[0], b_k2[1]])
        pool.op(lambda e: e.memset(V1[:], 1.0), writes=[b_V1, b_V1z])
        for tt in range(NT):
            sp.dma(V1[:, tt, :, 0:128], proj_tm[tt * 128:(tt + 1) * 128, 2048:3072].rearrange("p (h v) -> p h v", v=128), reads=[b_V1z], pwrites=[b_V1])
        sci = 0
        pti = 0
        for h in range(8):
            hb = h % 2
            sp.dma(qT[:, hb, :], qkT_d[h * 128:(h + 1) * 128, :], writes=[b_q[hb]])
            sp.dma(kT1[0:64, hb, :], qkT_d[1024 + h * 128:1024 + h * 128 + 64, :], writes=[b_k1[hb]])
            sp.dma(kT2[64:128, hb, :], qkT_d[1024 + h * 128 + 64:1024 + (h + 1) * 128, :], writes=[b_k2[hb]])
            kTs = [kT1, kT2]
            b_ks = [b_k1, b_k2]
            for qc in range(T // 512):
                for kt in range(NT):
                    pb = sci % 2
                    sci += 1
                    for s in range(2):
                        pe.op(lambda e: e.matmul(pS[pb][:, s, :], lhsT=kTs[s][:, hb, kt * 128:(kt + 1) * 128], rhs=qT[:, hb, qc * 512:(qc + 1) * 512],
                                                 start=True, stop=True),
                              reads=[b_ks[s][hb], b_q[hb]], writes=[b_pS[pb]] if s == 0 else (), pwrites=() if s == 0 else [b_pS[pb]])
                    pi_ = pti % 3
                    pti += 1
                    act.op(lambda e: e.activation(out=PT[:, pi_, :, :], in_=pS[pb][:], func=AF.Exp, scale=0.125),
                           reads=[b_pS[pb]], writes=[b_PT[pi_]])
                    for s in range(2):
                        for qs in range(4):
                            if kt == 0:
                                pe.op(lambda e: e.matmul(pO[qs][:, s, 0:129], lhsT=PT[:, pi_, s, qs * 128:(qs + 1) * 128], rhs=V1[:, kt, h, 0:129],
                                                         start=(s == 0), stop=False, skip_group_check=True),
                                      reads=[b_PT[pi_], b_V1], writes=[b_pO[qs][s]])
                            else:
                                pe.op(lambda e: e.matmul(pO[qs][:, s, 0:129], lhsT=PT[:, pi_, s, qs * 128:(qs + 1) * 128], rhs=V1[:, kt, h, 0:129],
                                                         start=False, stop=(kt == NT - 1), skip_group_check=True),
                                      reads=[b_PT[pi_], b_V1], pwrites=[b_pO[qs][s]])
                for qs in range(4):
                    qt = qc * 4 + qs
                    dve.op(lambda e: e.tensor_copy(out=rr[:, 0:1], in_=pO[qs][:, 0, 128:129]), reads=[b_pO[qs][0]], writes=[b_rr])
                    dve.op(lambda e: e.tensor_copy(out=rr[:, 1:2], in_=pO[qs][:, 1, 128:129]), reads=[b_pO[qs][1]], writes=[b_rr])
                    dve.op(lambda e: e.reciprocal(out=rr[:], in_=rr[:]), reads=[b_rr], writes=[b_rr])
                    dve.op(lambda e: e.tensor_tensor(out=rr[:, 1:2], in0=rr[:, 1:2], in1=lam[:], op=ALU.mult), reads=[b_rr, b_lam], writes=[b_rr])
                    dve.op(lambda e: e.tensor_scalar(out=o1[:], in0=pO[qs][:, 0, 0:128], scalar1=rr[:, 0:1], scalar2=None, op0=ALU.mult),
                           reads=[b_pO[qs][0], b_rr], writes=[b_o1])
                    dve.op(lambda e: e.tensor_scalar(out=o2[:], in0=pO[qs][:, 1, 0:128], scalar1=rr[:, 1:2], scalar2=None, op0=ALU.mult),
                           reads=[b_pO[qs][1], b_rr], writes=[b_o2])
                    dve.op(lambda e: e.tensor_tensor(out=o1[:], in0=o1[:], in1=o2[:], op=ALU.subtract), reads=[b_o1, b_o2], writes=[b_o1])
                    dve.op(lambda e: e.memset(oms[:], 0.0), writes=[b_oms])
                    act.op(lambda e: e.activation(out=osq[:], in_=o1[:], func=AF.Square, accum_out=oms[:]), reads=[b_o1], writes=[b_osq, b_oms])
                    act.op(lambda e: e.activation(out=oms[:], in_=oms[:], func=AF.Sqrt, scale=1.0 / 128, bias=1e-6), reads=[b_oms], writes=[b_oms])
                    dve.op(lambda e: e.reciprocal(out=oms[:], in_=oms[:]), reads=[b_oms], writes=[b_oms])
                    dve.op(lambda e: e.tensor_scalar(out=o1[:], in0=o1[:], scalar1=oms[:, 0:1], scalar2=(1.0 - LAM_INIT), op0=ALU.mult, op1=ALU.mult),
                           reads=[b_o1, b_oms], writes=[b_o1])
                    yb_ = qt % 2
                    dve.op(lambda e: e.tensor_tensor(out=yst[:, yb_, :], in0=o1[:], in1=sgt[:], op=ALU.mult), reads=[b_o1, b_sg], writes=[b_yst[yb_]])
                    sp.dma(y_a[qt * 128:(qt + 1) * 128, h * 128:(h + 1) * 128], yst[:, yb_, :], reads=[b_yst[yb_]])
        K.barrier()


    mu_in = din("mu_rep", [128, RW_COLS])
    w0_in = din("w0_rep", [128, 2, 1024])
    a0_in = din("a0_rep", [128, 2, 1024])
    kk_in = din("kk_rep", [128, 1024])
    ka_in = din("ka_rep", [128, 1024])
    rk_in = din("rk_rep", [128, 1024])
    lnw_in = din("lnw_rep", [128, 1024])
    lnb_in = din("lnb_rep", [128, 1024])
    w2_in = din("w2_in", [4, 64, 1024])
    g2_in = din("g2_in", [160, 1024])
    masks_in = din("masks_in", [128, 4, 128])
    RWS = {n: dscr("rw_" + n, [T, 1024], F32, dbg=True) for n in
           ("R", "V", "KK", "LW0", "LW1", "BB0", "BB1", "KE0", "KE1", "G", "BON", "Y0", "Y1")}

    def rwkv_phase():
        for st in phase("3a"):
            P = sb(st, "rP", [128, 3, RW_COLS], BF16)
            xs = sb(st, "rxs", [128, RW_COLS], F32)
            tt_ = sb(st, "rtt", [128, RW_COLS], F32)
            mu = sb(st, "rmu", [128, RW_COLS], F32)
            w0 = sb(st, "rw0", [128, 2, 1024], F32)
            a0 = sb(st, "ra0", [128, 2, 1024], F32)
            kkc = sb(st, "rkkc", [128, 1024], F32)
            kac = sb(st, "rkac", [128, 1024], F32)
            rkc = sb(st, "rrkc", [128, 1024], F32)
            w2 = sb(st, "rw2", [64, 4, 1024], BF16)
            g2a = sb(st, "rg2a", [128, 1024], BF16)
            g2b = sb(st, "rg2b", [32, 1024], BF16)
            L = sb(st, "rL", [128, 416], BF16)
            LT = sb(st, "rLT", [128, 6, 128], BF16)
            asg = sb(st, "rasg", [128, 2, 1024], F32)
            o = [sb(st, "ro%d" % i, [128, 1024], F32) for i in range(4)]
            kk = sb(st, "rkk", [128, 1024], F32)
            ke = sb(st, "rke", [128, 2, 1024], F32)
            s16 = sb(st, "rs16", [128, 16], F32)
            pl = ps(st, "rpl", [128, 6, 128], BF16)
            pm = [ps(st, "rpm%d" % i, [128, 2, 512], F32) for i in range(2)]
            b_P = Buf(); b_Pz = Buf(); b_xs = Buf(); b_tt = Buf(); b_c = Buf(); b_L = Buf(); b_LT = Buf(); b_asg = Buf()
            b_o = [Buf() for _ in range(4)]; b_kk = Buf(); b_ke = Buf(); b_s16 = Buf(); b_pl = Buf(); b_pm = [Buf(), Buf()]
            sp.dma(mu[:], mu_in, writes=[b_c])
            sp.dma(w0[:], w0_in, pwrites=[b_c])
            sp.dma(a0[:], a0_in, pwrites=[b_c])
            sp.dma(kkc[:], kk_in, pwrites=[b_c])
            sp.dma(kac[:], ka_in, pwrites=[b_c])
            sp.dma(rkc[:], rk_in, pwrites=[b_c])
            pool.dma(w2[:], w2_in.rearrange("f k n -> k f n"), pwrites=[b_c])
            pool.dma(g2a[:], g2_in[0:128, :], pwrites=[b_c])
            pool.dma(g2b[:], g2_in[128:160, :], pwrites=[b_c])
            oi = [0]

            def outbuf():
                oi[0] += 1
                return oi[0] % 4

            def store(name, tt, i):
                sp.dma(RWS[name][tt * 128:(tt + 1) * 128, :], o[i][:], reads=[b_o[i]])

            for tt in range(NT):
                r0 = tt * 128
                dve.op(lambda e: e.memset(P[:, 1:3, :], 0.0), writes=[b_P, b_Pz])
                sp.dma(P[:, 0, :], proj_tm[r0:r0 + 128, DA_COLS:TM_COLS], pwrites=[b_P])
                if tt == 0:
                    sp.dma(P[1:128, 1, :], proj_tm[0:127, DA_COLS:TM_COLS], reads=[b_Pz], pwrites=[b_P])
                else:
                    sp.dma(P[:, 1, :], proj_tm[r0 - 1:r0 + 127, DA_COLS:TM_COLS], reads=[b_Pz], pwrites=[b_P])
                if tt == NT - 1:
                    sp.dma(P[0:127, 2, :], proj_tm[r0 + 1:r0 + 128, DA_COLS:TM_COLS], reads=[b_Pz], pwrites=[b_P])
                else:
                    sp.dma(P[:, 2, :], proj_tm[r0 + 1:r0 + 129, DA_COLS:TM_COLS], reads=[b_Pz], pwrites=[b_P])
                dve.op(lambda e: e.tensor_tensor(out=tt_[:], in0=P[:, 1, :], in1=P[:, 2, :], op=ALU.add), reads=[b_P], writes=[b_tt])
                dve.op(lambda e: e.scalar_tensor_tensor(out=tt_[:], in0=tt_[:], scalar=0.5, in1=P[:, 0, :], op0=ALU.mult, op1=ALU.subtract),
                       reads=[b_tt, b_P], writes=[b_tt])
                dve.op(lambda e: e.tensor_tensor(out=tt_[:], in0=tt_[:], in1=mu[:], op=ALU.mult), reads=[b_tt, b_c], writes=[b_tt])
                dve.op(lambda e: e.tensor_tensor(out=xs[:], in0=tt_[:], in1=P[:, 0, :], op=ALU.add), reads=[b_tt, b_P], writes=[b_xs])
                rr_ = xs[:, 0:1024]
                kk_ = xs[:, 1024:2048]
                vv_ = xs[:, 2048:3072]
                if CUT <= 1:
                    continue
                act.op(lambda e: e.activation(out=L[:, 0:128], in_=xs[:, 3072:3200], func=AF.Tanh), reads=[b_xs], writes=[b_L])
                act.op(lambda e: e.activation(out=L[:, 128:256], in_=xs[:, 3200:3328], func=AF.Copy), reads=[b_xs], pwrites=[b_L])
                act.op(lambda e: e.activation(out=L[:, 256:416], in_=xs[:, 3328:3488], func=AF.Sigmoid), reads=[b_xs], pwrites=[b_L])
                for i in range(4):
                    pe.op(lambda e: e.transpose(out=pl[0:64, i, :], in_=L[:, i * 64:(i + 1) * 64], identity=idb[:]),
                          reads=[b_L, b_idb], writes=[b_pl] if i == 0 else (), pwrites=() if i == 0 else [b_pl])
                pe.op(lambda e: e.transpose(out=pl[:, 4, :], in_=L[:, 256:384], identity=idb[:]), reads=[b_L, b_idb], pwrites=[b_pl])
                pe.op(lambda e: e.transpose(out=pl[0:32, 5, :], in_=L[:, 384:416], identity=idb[:]), reads=[b_L, b_idb], pwrites=[b_pl])
                dve.op(lambda e: e.tensor_copy(out=LT[0:64, 0:4, :], in_=pl[0:64, 0:4, :]), reads=[b_pl], writes=[b_LT])
                dve.op(lambda e: e.tensor_copy(out=LT[:, 4, :], in_=pl[:, 4, :]), reads=[b_pl], pwrites=[b_LT])
                dve.op(lambda e: e.tensor_copy(out=LT[0:32, 5, :], in_=pl[0:32, 5, :]), reads=[b_pl], pwrites=[b_LT])
                if CUT <= 2:
                    continue
                i = outbuf()
                dve.op(lambda e: e.tensor_copy(out=o[i][:], in_=rr_), reads=[b_xs], writes=[b_o[i]])
                store("R", tt, i)
                i = outbuf()
                dve.op(lambda e: e.tensor_copy(out=o[i][:], in_=vv_), reads=[b_xs], writes=[b_o[i]])
                store("V", tt, i)
                if CUT <= 3:
                    continue
                for d in range(2):
                    p = pm[d % 2]
                    for hf in range(2):
                        pe.op(lambda e: e.matmul(p[:, hf, :], lhsT=LT[0:64, d, :], rhs=w2[:, d, hf * 512:(hf + 1) * 512], start=True, stop=True),
                              reads=[b_LT, b_c], writes=[b_pm[d % 2]] if hf == 0 else (), pwrites=() if hf == 0 else [b_pm[d % 2]])
                    i = outbuf()
                    dve.op(lambda e: e.tensor_tensor(out=o[i][:], in0=p[:].rearrange("p a b -> p (a b)"), in1=w0[:, d, :], op=ALU.add),
                           reads=[b_pm[d % 2], b_c], writes=[b_o[i]])
                    act.op(lambda e: e.activation(out=o[i][:], in_=o[i][:], func=AF.Sigmoid), reads=[b_o[i]], writes=[b_o[i]])
                    dve.op(lambda e: e.tensor_scalar(out=o[i][:], in0=o[i][:], scalar1=-math.exp(-0.5), scalar2=None, op0=ALU.mult),
                           reads=[b_o[i]], writes=[b_o[i]])
                    store("LW%d" % d, tt, i)
                if CUT <= 4:
                    continue
                for d in range(2):
                    p = pm[d % 2]
                    for hf in range(2):
                        pe.op(lambda e: e.matmul(p[:, hf, :], lhsT=LT[0:64, 2 + d, :], rhs=w2[:, 2 + d, hf * 512:(hf + 1) * 512], start=True, stop=True),
                              reads=[b_LT, b_c], writes=[b_pm[d % 2]] if hf == 0 else (), pwrites=() if hf == 0 else [b_pm[d % 2]])
                    dve.op(lambda e: e.tensor_tensor(out=asg[:, d, :], in0=p[:].rearrange("p a b -> p (a b)"), in1=a0[:, d, :], op=ALU.add),
                           reads=[b_pm[d % 2], b_c], writes=[b_asg] if d == 0 else (), pwrites=() if d == 0 else [b_asg])
                act.op(lambda e: e.activation(out=asg[:], in_=asg[:], func=AF.Sigmoid), reads=[b_asg], writes=[b_asg])
                if CUT <= 5:
                    continue
                p = pm[0]
                for hf in range(2):
                    pe.op(lambda e: e.matmul(p[:, hf, :], lhsT=LT[:, 4, :], rhs=g2a[:, hf * 512:(hf + 1) * 512], start=True, stop=False),
                          reads=[b_LT, b_c], writes=[b_pm[0]] if hf == 0 else (), pwrites=() if hf == 0 else [b_pm[0]])
                    pe.op(lambda e: e.matmul(p[:, hf, :], lhsT=LT[0:32, 5, :], rhs=g2b[:, hf * 512:(hf + 1) * 512], start=False, stop=True),
                          reads=[b_LT, b_c], pwrites=[b_pm[0]])
                i = outbuf()
                act.op(lambda e: e.activation(out=o[i][:], in_=p[:].rearrange("p a b -> p (a b)"), func=AF.Copy), reads=[b_pm[0]], writes=[b_o[i]])
                store("G", tt, i)
                if CUT <= 6:
                    continue
                dve.op(lambda e: e.tensor_tensor(out=kk[:], in0=kk_, in1=kkc[:], op=ALU.mult), reads=[b_xs, b_c], writes=[b_kk])
                dve.op(lambda e: e.tensor_tensor(out=tt_[:, 0:1024], in0=kk[:], in1=kk[:], op=ALU.mult), reads=[b_kk], writes=[b_tt])
                dve.op(lambda e: e.tensor_reduce(out=s16[:], in_=tt_[:, 0:1024].rearrange("p (h d) -> p h d", d=64), axis=AX.X, op=ALU.add),
                       reads=[b_tt], writes=[b_s16])
                act.op(lambda e: e.activation(out=s16[:], in_=s16[:], func=AF.Sqrt), reads=[b_s16], writes=[b_s16])
                dve.op(lambda e: e.tensor_scalar(out=s16[:], in0=s16[:], scalar1=1e-12, scalar2=None, op0=ALU.max), reads=[b_s16], writes=[b_s16])
                dve.op(lambda e: e.reciprocal(out=s16[:], in_=s16[:]), reads=[b_s16], writes=[b_s16])
                i = outbuf()
                dve.op(lambda e: e.tensor_tensor(out=o[i][:].rearrange("p (h d) -> p h d", d=64), in0=kk[:].rearrange("p (h d) -> p h d", d=64),
                                                 in1=s16[:].unsqueeze(2).broadcast_to([128, 16, 64]), op=ALU.mult),
                       reads=[b_kk, b_s16], writes=[b_o[i]])
                ikk = i
                for d in range(2):
                    i = outbuf()
                    dve.op(lambda e: e.tensor_tensor(out=o[i][:], in0=o[ikk][:], in1=asg[:, d, :], op=ALU.mult), reads=[b_o[ikk], b_asg], writes=[b_o[i]])
                    store("BB%d" % d, tt, i)
                dve.op(lambda e: e.tensor_scalar(out=o[ikk][:], in0=o[ikk][:], scalar1=-1.0, scalar2=None, op0=ALU.mult), reads=[b_o[ikk]], writes=[b_o[ikk]])
                store("KK", tt, ikk)
                for d in range(2):
                    dve.op(lambda e: e.tensor_tensor(out=ke[:, d, :], in0=asg[:, d, :], in1=kac[:], op=ALU.mult),
                           reads=[b_asg, b_c], writes=[b_ke] if d == 0 else (), pwrites=() if d == 0 else [b_ke])
                    dve.op(lambda e: e.tensor_tensor(out=ke[:, d, :], in0=ke[:, d, :], in1=kac[:], op=ALU.subtract),
                           reads=[b_ke, b_c], writes=[b_ke])
                    dve.op(lambda e: e.tensor_tensor(out=ke[:, d, :], in0=ke[:, d, :], in1=kk_, op=ALU.mult),
                           reads=[b_ke, b_xs], writes=[b_ke])
                    dve.op(lambda e: e.tensor_tensor(out=ke[:, d, :], in0=ke[:, d, :], in1=kk_, op=ALU.add),
                           reads=[b_ke, b_xs], writes=[b_ke])
                    i = outbuf()
                    dve.op(lambda e: e.tensor_copy(out=o[i][:], in_=ke[:, d, :]), reads=[b_ke], writes=[b_o[i]])
                    store("KE%d" % d, tt, i)
                if CUT <= 7:
                    continue
                dve.op(lambda e: e.tensor_tensor(out=tt_[:, 0:1024], in0=ke[:, 0, :], in1=ke[:, 1, :], op=ALU.add), reads=[b_ke], writes=[b_tt])
                dve.op(lambda e: e.tensor_tensor(out=tt_[:, 0:1024], in0=tt_[:, 0:1024], in1=rr_, op=ALU.mult), reads=[b_tt, b_xs], writes=[b_tt])
                dve.op(lambda e: e.tensor_tensor(out=tt_[:, 0:1024], in0=tt_[:, 0:1024], in1=rkc[:], op=ALU.mult), reads=[b_tt, b_c], writes=[b_tt])
                dve.op(lambda e: e.tensor_reduce(out=s16[:], in_=tt_[:, 0:1024].rearrange("p (h d) -> p h d", d=64), axis=AX.X, op=ALU.add),
                       reads=[b_tt], writes=[b_s16])
                i = outbuf()
                dve.op(lambda e: e.tensor_tensor(out=o[i][:].rearrange("p (h d) -> p h d", d=64), in0=vv_.rearrange("p (h d) -> p h d", d=64),
                                                 in1=s16[:].unsqueeze(2).broadcast_to([128, 16, 64]), op=ALU.mult),
                       reads=[b_xs, b_s16], writes=[b_o[i]])
                store("BON", tt, i)
            K.barrier()

        for st in phase("3b"):
            mk = sb(st, "smk", [128, 4, 128], F32)
            ld = sb(st, "sld", [128, 2, 6, 1024], F32)
            e4 = sb(st, "se4", [128, 4, 1024], F32)
            bfs = sb(st, "sbfs", [128, 7, 1024], BF16)
            dG = sb(st, "sdG", [64, 1024], F32)
            XTa = sb(st, "sXTa", [64, 4, 4, 4, 128], BF16)
            pr5a = sb(st, "spr5a", [128, 4, 5, 4, 128], BF16)
            Xp2 = sb(st, "sXp2", [128, 2, 2, 4, 192], BF16)
            Pp2 = sb(st, "sPp2", [128, 2, 2, 2, 4, 128], BF16)
            Rh = sb(st, "sRh", [64, 16, 128], BF16)
            Qm = sb(st, "sQm", [128, 16, 128], BF16)
            Gm = sb(st, "sGm", [64, 16, 64], BF16)
            Hm = sb(st, "sHm", [128, 16, 64], BF16)
            ST = sb(st, "sST", [64, 16, 64], BF16)
            yo = sb(st, "syo", [128, 2, 1024], F32)
            pT = [ps(st, "spT%d" % i, [128, 8, 128], BF16) for i in range(2)]
            pA = ps(st, "spA", [128, 2, 512], F32)
            pB = ps(st, "spB", [128, 2, 512], F32)
            pX = ps(st, "spX", [128, 2, 512], F32)
            b_mk = Buf(); b_ld = [Buf(), Buf()]; b_e4 = Buf(); b_bfs = Buf(); b_dG = Buf(); b_XT = [Buf() for _ in range(4)]; b_pr5 = [Buf() for _ in range(4)]
            b_Xp2 = [[Buf(), Buf()], [Buf(), Buf()]]; b_Pp2 = [[Buf(), Buf()], [Buf(), Buf()]]; b_Rh = Buf(); b_Qm = Buf(); b_Gm = Buf(); b_Hm = Buf(); b_ST = Buf()
            b_yo = [Buf(), Buf()]; b_pT = [Buf(), Buf()]; b_pA = [Buf(), Buf()]; b_pB = [Buf(), Buf()]; b_pX = [Buf(), Buf()]
            slot = [(pA, 0, b_pA[0]), (pA, 1, b_pA[1]), (pB, 0, b_pB[0]), (pB, 1, b_pB[1]), (pX, 0, b_pX[0]), (pX, 1, b_pX[1])]

            def sl(i, shape3):
                t_, j, b = slot[i]
                a, bb_ = shape3
                return t_[:, j, 0:a * bb_].rearrange("p (a b) -> p a b", b=bb_), b

            sp.dma(mk[:], masks_in, writes=[b_mk])
            SU, SL_, UI, LI = 0, 1, 2, 3
            li = 0
            yi = 0
            for d in range(2):
                cum_i, cum_s, mb_s, mb_i, ma_s = (UI, SL_, SU, UI, SL_) if d == 0 else (LI, SU, SL_, LI, SU)
                dve.op(lambda e: e.memset(ST[:], 0.0), writes=[b_ST])
                corder = range(NT) if d == 0 else range(NT - 1, -1, -1)
                names = ("R", "V", "KK", "LW%d" % d, "BB%d" % d, "KE%d" % d)
                for c in corder:
                    lb = li % 2
                    li += 1
                    for qi, nm in enumerate(names):
                        sp.dma(ld[:, lb, qi, :], RWS[nm][c * 128:(c + 1) * 128, :],
                               writes=[b_ld[lb]] if qi == 0 else (), pwrites=() if qi == 0 else [b_ld[lb]])
                    r_, v_, kk_, lw_, bb_, ke_ = (ld[:, lb, qi, :] for qi in range(6))
                    for hf in range(2):
                        pe.op(lambda e: e.matmul(pA[:, hf, :], lhsT=mk[:, cum_i, :], rhs=lw_[:, hf * 512:(hf + 1) * 512], start=True, stop=True),
                              reads=[b_mk, b_ld[lb]], writes=[b_pA[hf]])
                        pe.op(lambda e: e.matmul(pB[:, hf, :], lhsT=mk[:, cum_s, :], rhs=lw_[:, hf * 512:(hf + 1) * 512], start=True, stop=True),
                              reads=[b_mk, b_ld[lb]], writes=[b_pB[hf]])
                    gam = pA[:].rearrange("p a b -> p (a b)")
                    gsf = pB[:].rearrange("p a b -> p (a b)")
                    act.op(lambda e: e.activation(out=e4[:, 0, :], in_=gam, func=AF.Exp), reads=b_pA, writes=[b_e4])
                    act.op(lambda e: e.activation(out=e4[:, 2, :], in_=gam, func=AF.Exp, scale=-1.0), reads=b_pA, pwrites=[b_e4])
                    act.op(lambda e: e.activation(out=e4[:, 3, :], in_=gsf, func=AF.Exp), reads=b_pB, pwrites=[b_e4])
                    act.op(lambda e: e.activation(out=e4[:, 1, :], in_=lw_, func=AF.Exp, scale=-1.0), reads=[b_ld[lb]], pwrites=[b_e4])
                    dve.op(lambda e: e.tensor_tensor(out=e4[:, 1, :], in0=e4[:, 1, :], in1=e4[:, 0, :], op=ALU.mult), reads=[b_e4], pwrites=[b_e4])
                    dve.op(lambda e: e.tensor_tensor(out=dG[:], in0=e4[0:64, 0, :], in1=e4[0:64, 3, :], op=ALU.mult),
                           reads=[b_e4], writes=[b_dG])
                    dve.op(lambda e: e.tensor_tensor(out=dG[:].rearrange("p (h k) -> p h k", k=64), in0=dG[:].rearrange("p (h k) -> p h k", k=64),
                                                     in1=idf[0:64, 0:64].unsqueeze(1).broadcast_to([64, 16, 64]), op=ALU.mult),
                           reads=[b_dG, b_idf], writes=[b_dG])
                    dve.op(lambda e: e.tensor_tensor(out=bfs[:, 0, :], in0=kk_, in1=e4[:, 1, :], op=ALU.mult),
                           reads=[b_ld[lb], b_e4], writes=[b_bfs])
                    dve.op(lambda e: e.tensor_tensor(out=bfs[:, 1, :], in0=bb_, in1=e4[:, 2, :], op=ALU.mult), reads=[b_ld[lb], b_e4], pwrites=[b_bfs])
                    dve.op(lambda e: e.tensor_tensor(out=bfs[:, 2, :], in0=ke_, in1=e4[:, 2, :], op=ALU.mult), reads=[b_ld[lb], b_e4], pwrites=[b_bfs])
                    dve.op(lambda e: e.tensor_tensor(out=bfs[:, 3, :], in0=r_, in1=e4[:, 0, :], op=ALU.mult), reads=[b_ld[lb], b_e4], pwrites=[b_bfs])
                    dve.op(lambda e: e.tensor_tensor(out=bfs[:, 4, :], in0=bb_, in1=e4[:, 3, :], op=ALU.mult), reads=[b_ld[lb], b_e4], pwrites=[b_bfs])
                    dve.op(lambda e: e.tensor_tensor(out=bfs[:, 5, :], in0=ke_, in1=e4[:, 3, :], op=ALU.mult), reads=[b_ld[lb], b_e4], pwrites=[b_bfs])
                    act.op(lambda e: e.activation(out=bfs[:, 6, :], in_=v_, func=AF.Copy), reads=[b_ld[lb], b_bfs], pwrites=[b_bfs])
                    A_, B_, K_, R_ = 0, 1, 2, 3
                    prods = [(B_, A_, mb_s), (A_, B_, ma_s), (A_, K_, ma_s), (B_, R_, mb_i), (K_, R_, mb_i)]
                    for g4 in range(4):
                        for hh in range(4):
                            h = g4 * 4 + hh
                            tb_ = hh // 2
                            for kd in range(4):
                                first = (hh % 2 == 0 and kd == 0)
                                pe.op(lambda e: e.transpose(out=pT[tb_][0:64, (hh % 2) * 4 + kd, :], in_=bfs[:, kd, h * 64:(h + 1) * 64], identity=idb[:]),
                                      reads=[b_bfs, b_idb], writes=[b_pT[tb_]] if first else (), pwrites=() if first else [b_pT[tb_]])
                        for tb_ in range(2):
                            evac_copy(XTa[:, g4, tb_ * 2:(tb_ + 1) * 2, :, :], pT[tb_][0:64].rearrange("p (a k) t -> p a k t", k=4), reads=[b_pT[tb_]],
                                      writes=[b_XT[g4]] if tb_ == 0 else (), pwrites=() if tb_ == 0 else [b_XT[g4]])
                        for pi_, (l_, r2_, m_) in enumerate(prods):
                            ap3, bslot = sl(pi_, (4, 128))
                            for hh in range(4):
                                pe.op(lambda e: e.matmul(ap3[:, hh, :], lhsT=XTa[:, g4, hh, l_, :], rhs=XTa[:, g4, hh, r2_, :], start=True, stop=True),
                                      reads=[b_XT[g4]], writes=[bslot] if hh == 0 else (), pwrites=() if hh == 0 else [bslot])
                            dve.op(lambda e: e.tensor_tensor(out=pr5a[:, g4, pi_, :, :], in0=ap3, in1=mk[:, m_, :].unsqueeze(1).broadcast_to([128, 4, 128]), op=ALU.mult),
                                   reads=[bslot, b_mk], writes=[b_pr5[g4]] if pi_ == 0 else (), pwrites=() if pi_ == 0 else [b_pr5[g4]])

                    def neumann(g4, side):
                        pr5 = pr5a[:, g4]
                        bpr = b_pr5[g4]
                        Xp_ = Xp2[:, side]
                        bXp_ = b_Xp2[side]
                        Pp_ = Pp2[:, side]
                        bPp_ = b_Pp2[side]
                        if side == 0:
                            xa = pX[:].rearrange("p a (h x) -> p (a h) x", x=256)[:, :, 0:192]
                            bxa = [b_pX[0], b_pX[1]]
                            s0, bs0 = sl(0, (4, 128))
                            s1, bs1 = sl(1, (4, 128))
                        else:
                            xa = pB[:].rearrange("p a (h x) -> p (a h) x", x=256)[:, :, 0:192]
                            bxa = [b_pB[0], b_pB[1]]
                            s0 = pT[0][:].rearrange("p a b -> p (a b)").bitcast(F32).rearrange("p (a b) -> p a b", b=128)
                            s1 = pT[1][:].rearrange("p a b -> p (a b)").bitcast(F32).rearrange("p (a b) -> p a b", b=128)
                            bs0, bs1 = b_pT[0], b_pT[1]
                        act.op(lambda e: e.activation(out=Xp_[:, 0, :, 0:64], in_=bfs[:, 0, g4 * 256:(g4 + 1) * 256].rearrange("p (h k) -> p h k", k=64), func=AF.Copy),
                               reads=[b_bfs], writes=[bXp_[0]])
                        act.op(lambda e: e.activation(out=Xp_[:, 0, :, 64:192], in_=pr5[:, 2, :, :], func=AF.Copy), reads=[bpr], pwrites=[bXp_[0]])
                        xi = 0
                        for it in range(7):
                            if it == 0:
                                Pc, PTc, bP = pr5[:, 0], pr5[:, 1], bpr
                            else:
                                Pc, PTc, bP = Pp_[:, it % 2, 0], Pp_[:, it % 2, 1], bPp_[it % 2]
                            for hh in range(4):
                                pe.op(lambda e: e.matmul(xa[:, hh, :], lhsT=Pc[:, hh, :], rhs=Xp_[:, xi, hh, :], start=True, stop=True),
                                      reads=[bP, bXp_[xi]], writes=bxa if hh == 0 else (), pwrites=() if hh == 0 else bxa)
                            dve.op(lambda e: e.tensor_tensor(out=Xp_[:, 1 - xi, :, :], in0=xa, in1=Xp_[:, xi, :, :], op=ALU.add),
                                   reads=bxa + [bXp_[xi]], writes=[bXp_[1 - xi]])
                            xi = 1 - xi
                            if it < 6:
                                nP = (it + 1) % 2
                                for hh in range(4):
                                    pe.op(lambda e: e.matmul(s0[:, hh, :], lhsT=PTc[:, hh, :], rhs=Pc[:, hh, :], start=True, stop=True),
                                          reads=[bP], writes=[bs0] if hh == 0 else (), pwrites=() if hh == 0 else [bs0])
                                for hh in range(4):
                                    pe.op(lambda e: e.matmul(s1[:, hh, :], lhsT=Pc[:, hh, :], rhs=PTc[:, hh, :], start=True, stop=True),
                                          reads=[bP], writes=[bs1] if hh == 0 else (), pwrites=() if hh == 0 else [bs1])
                                act.op(lambda e: e.activation(out=Pp_[:, nP, 0], in_=s0, func=AF.Copy), reads=[bs0], writes=[bPp_[nP]])
                                dve.op(lambda e: e.tensor_copy(out=Pp_[:, nP, 1], in_=s1), reads=[bs1], pwrites=[bPp_[nP]])
                            yield xi
                    finals = {}
                    for (ga, gb) in ((0, 1), (2, 3)):
                        gA = neumann(ga, 0)
                        gB = neumann(gb, 1)
                        for it in range(7):
                            finals[ga] = (0, next(gA))
                            finals[gb] = (1, next(gB))
                        for g4 in (ga, gb):
                            side, xi = finals[g4]
                            Xf = Xp2[:, side, xi]
                            bXf = b_Xp2[side][xi]
                            pr5 = pr5a[:, g4]
                            bpr = b_pr5[g4]
                            hs = slice(g4 * 4, g4 * 4 + 4)
                            a2_, bs2 = sl(0, (4, 128))
                            for hh in range(4):
                                pe.op(lambda e: e.matmul(a2_[0:64, hh, :], lhsT=Xf[:, hh, 0:64], rhs=pr5[:, 3, hh, :], start=True, stop=True),
                                      reads=[bXf, bpr], writes=[bs2] if hh == 0 else (), pwrites=() if hh == 0 else [bs2])
                            dve.op(lambda e: e.tensor_tensor(out=Rh[:, hs, :], in0=a2_[0:64], in1=XTa[:, g4, :, 3, :], op=ALU.add),
                                   reads=[bs2, b_XT[g4]], pwrites=[b_Rh])
                            a3_, bs3 = sl(1, (4, 128))
                            for hh in range(4):
                                pe.op(lambda e: e.matmul(a3_[:, hh, :], lhsT=Xf[:, hh, 64:192], rhs=pr5[:, 3, hh, :], start=True, stop=True),
                                      reads=[bXf, bpr], writes=[bs3] if hh == 0 else (), pwrites=() if hh == 0 else [bs3])
                            dve.op(lambda e: e.tensor_tensor(out=Qm[:, hs, :], in0=a3_, in1=pr5[:, 4, :, :], op=ALU.add),
                                   reads=[bs3, bpr], pwrites=[b_Qm])
                            a0_, bs0 = sl(4, (4, 64))
                            a1_, bs1 = sl(5, (4, 64))
                            for hh in range(4):
                                h = g4 * 4 + hh
                                pe.op(lambda e: e.matmul(a0_[0:64, hh, :], lhsT=Xf[:, hh, 0:64], rhs=bfs[:, 4, h * 64:(h + 1) * 64], start=True, stop=True),
                                      reads=[bXf, b_bfs], writes=[bs0] if hh == 0 else (), pwrites=() if hh == 0 else [bs0])
                            for hh in range(4):
                                h = g4 * 4 + hh
                                pe.op(lambda e: e.matmul(a1_[:, hh, :], lhsT=Xf[:, hh, 64:192], rhs=bfs[:, 4, h * 64:(h + 1) * 64], start=True, stop=True),
                                      reads=[bXf, b_bfs], writes=[bs1] if hh == 0 else (), pwrites=() if hh == 0 else [bs1])
                            dve.op(lambda e: e.tensor_tensor(out=Gm[:, hs, :], in0=a0_[0:64], in1=dG[:, g4 * 256:(g4 + 1) * 256].rearrange("p (h k) -> p h k", k=64), op=ALU.add),
                                   reads=[bs0, b_dG], pwrites=[b_Gm])
                            dve.op(lambda e: e.tensor_tensor(out=Hm[:, hs, :], in0=a1_, in1=bfs[:, 5, g4 * 256:(g4 + 1) * 256].rearrange("p (h k) -> p h k", k=64), op=ALU.add),
                                   reads=[bs1, b_bfs], pwrites=[b_Hm])
                    yv = pA[:].rearrange("p a (h v) -> p (a h) v", v=64)
                    sv = pB[0:64].rearrange("p a (h v) -> p (a h) v", v=64)
                    for h in range(16):
                        vh = bfs[:, 6, h * 64:(h + 1) * 64]
                        pe.op(lambda e: e.matmul(yv[:, h, :], lhsT=Rh[:, h, :], rhs=ST[:, h, :], start=True, stop=False),
                              reads=[b_Rh, b_ST], writes=b_pA if h == 0 else (), pwrites=() if h == 0 else b_pA)
                        pe.op(lambda e: e.matmul(yv[:, h, :], lhsT=Qm[:, h, :], rhs=vh, start=False, stop=True),
                              reads=[b_Qm, b_bfs], pwrites=b_pA)
                        pe.op(lambda e: e.matmul(sv[:, h, :], lhsT=Gm[:, h, :], rhs=ST[:, h, :], start=True, stop=False),
                              reads=[b_Gm, b_ST], writes=b_pB if h == 0 else (), pwrites=() if h == 0 else b_pB)
                        pe.op(lambda e: e.matmul(sv[:, h, :], lhsT=Hm[:, h, :], rhs=vh, start=False, stop=True),
                              reads=[b_Hm, b_bfs], pwrites=b_pB)
                    yb2 = yi % 2
                    yi += 1
                    act.op(lambda e: e.activation(out=yo[:, yb2, :], in_=pA[:].rearrange("p a b -> p (a b)"), func=AF.Copy), reads=b_pA, writes=[b_yo[yb2]])
                    dve.op(lambda e: e.tensor_copy(out=ST[:], in_=sv), reads=b_pB, writes=[b_ST])
                    sp.dma(RWS["Y%d" % d][c * 128:(c + 1) * 128, :], yo[:, yb2, :], reads=[b_yo[yb2]])
            K.barrier()

        for st in phase("3c"):
            ld = sb(st, "cld", [128, 2, 4, 1024], F32)
            lw_ = sb(st, "clw", [128, 1024], F32)
            lb_ = sb(st, "clb", [128, 1024], F32)
            y = sb(st, "cy", [128, 1024], F32)
            sq = sb(st, "csq", [128, 1024], F32)
            m16 = sb(st, "cm16", [128, 16], F32)
            v16 = sb(st, "cv16", [128, 16], F32)
            yb16 = sb(st, "cyb", [128, 2, 1024], BF16)
            b_ld = [Buf(), Buf()]; b_c = Buf(); b_y = Buf(); b_sq = Buf(); b_m = Buf(); b_v = Buf(); b_yb = [Buf(), Buf()]
            sp.dma(lw_[:], lnw_in, writes=[b_c])
            sp.dma(lb_[:], lnb_in, pwrites=[b_c])
            h3 = lambda ap: ap.rearrange("p (h d) -> p h d", d=64)
            for tt in range(NT):
                lb = tt % 2
                for qi, nm in enumerate(("Y0", "Y1", "BON", "G")):
                    sp.dma(ld[:, lb, qi, :], RWS[nm][tt * 128:(tt + 1) * 128, :], writes=[b_ld[lb]] if qi == 0 else (), pwrites=() if qi == 0 else [b_ld[lb]])
                dve.op(lambda e: e.tensor_tensor(out=y[:], in0=ld[:, lb, 0, :], in1=ld[:, lb, 1, :], op=ALU.add), reads=[b_ld[lb]], writes=[b_y])
                dve.op(lambda e: e.tensor_reduce(out=m16[:], in_=h3(y[:]), axis=AX.X, op=ALU.add), reads=[b_y], writes=[b_m])
                dve.op(lambda e: e.tensor_scalar(out=m16[:], in0=m16[:], scalar1=1.0 / 64, scalar2=None, op0=ALU.mult), reads=[b_m], writes=[b_m])
                dve.op(lambda e: e.tensor_tensor(out=h3(y[:]), in0=h3(y[:]), in1=m16[:].unsqueeze(2).broadcast_to([128, 16, 64]), op=ALU.subtract),
                       reads=[b_y, b_m], writes=[b_y])
                dve.op(lambda e: e.tensor_tensor(out=sq[:], in0=y[:], in1=y[:], op=ALU.mult), reads=[b_y], writes=[b_sq])
                dve.op(lambda e: e.tensor_reduce(out=v16[:], in_=h3(sq[:]), axis=AX.X, op=ALU.add), reads=[b_sq], writes=[b_v])
                act.op(lambda e: e.activation(out=v16[:], in_=v16[:], func=AF.Sqrt, scale=1.0 / 64, bias=64e-5), reads=[b_v], writes=[b_v])
                dve.op(lambda e: e.reciprocal(out=v16[:], in_=v16[:]), reads=[b_v], writes=[b_v])
                dve.op(lambda e: e.tensor_tensor(out=h3(y[:]), in0=h3(y[:]), in1=v16[:].unsqueeze(2).broadcast_to([128, 16, 64]), op=ALU.mult),
                       reads=[b_y, b_v], writes=[b_y])
                dve.op(lambda e: e.tensor_tensor(out=y[:], in0=y[:], in1=lw_[:], op=ALU.mult), reads=[b_y, b_c], writes=[b_y])
                dve.op(lambda e: e.tensor_tensor(out=y[:], in0=y[:], in1=lb_[:], op=ALU.add), reads=[b_y, b_c], writes=[b_y])
                dve.op(lambda e: e.tensor_tensor(out=y[:], in0=y[:], in1=ld[:, lb, 2, :], op=ALU.add), reads=[b_y, b_ld[lb]], writes=[b_y])
                dve.op(lambda e: e.tensor_tensor(out=yb16[:, lb, :], in0=y[:], in1=ld[:, lb, 3, :], op=ALU.mult), reads=[b_y, b_ld[lb]], writes=[b_yb[lb]])
                sp.dma(y_b[tt * 128:(tt + 1) * 128, :], yb16[:, lb, :], reads=[b_yb[lb]])
            K.barrier()

    if SKIP_RWKV:
        for st in phase("zero"):
            z = sb(st, "zz", [128, 1024], BF16)
            b_z = Buf()
            dve.op(lambda e: e.memset(z[:], 0.0), writes=[b_z])
            for tt in range(NT):
                sp.dma(y_b[tt * 128:(tt + 1) * 128, :], z[:], reads=[b_z])
            K.barrier()
    else:
        rwkv_phase()

    for st in phase("4a"):
        Wa = sb(st, "Wa", [128, 8, D], BF16)
        Wb = sb(st, "Wb", [128, 8, D], BF16)
        yt = sb(st, "yt", [128, 2, 1024], BF16)
        yT = sb(st, "yT", [128, 2, 8, 512], BF16)
        gt = sb(st, "gt", [128, 2, 2, 512], BF16)
        ma = sb(st, "ma", [128, 512], F32)
        mbt = sb(st, "mbt", [128, 512], F32)
        mst = sb(st, "mst", [128, 2, 512], BF16)
        pt = [ps(st, "pt4a", [128, 8, 128], BF16), ps(st, "pt4b", [128, 8, 128], BF16)]
        pm = [ps(st, "pm4_%d" % i, [128, 512], F32) for i in range(4)]
        b_W = Buf(); b_yt = [Buf(), Buf()]; b_yT = [Buf(), Buf()]; b_gt = [Buf(), Buf()]; b_ma = Buf(); b_mb = Buf()
        b_mst = [Buf(), Buf()]; b_pt = [Buf(), Buf()]; b_pm = [Buf() for _ in range(4)]
        pool.dma(Wa[:], w_ba.rearrange("(c p) n -> p c n", p=128), writes=[b_W])
        pool.dma(Wb[:], w_bb.rearrange("(c p) n -> p c n", p=128), pwrites=[b_W])
        li = 0
        gi = 0
        for tb in range(T // 512):
            for br, ysrc in enumerate((y_a, y_b)):
                for t4 in range(4):
                    tt = tb * 4 + t4
                    lb = li % 2
                    li += 1
                    sp.dma(yt[:, lb, :], ysrc[tt * 128:(tt + 1) * 128, :], writes=[b_yt[lb]])
                    for c in range(8):
                        pe.op(lambda e: e.transpose(out=pt[lb][:, c, :], in_=yt[:, lb, c * 128:(c + 1) * 128], identity=idb[:]),
                              reads=[b_yt[lb], b_idb], writes=[b_pt[lb]] if c == 0 else (), pwrites=() if c == 0 else [b_pt[lb]])
                    evac_copy(yT[:, br, :, t4 * 128:(t4 + 1) * 128], pt[lb][:], reads=[b_pt[lb]],
                              writes=[b_yT[br]] if t4 == 0 else (), pwrites=() if t4 == 0 else [b_yT[br]])
            for fb in range(16):
                gb = gi % 2
                gi += 1
                sp.dma(gt[:, gb, 0, :], gateT[fb * 128:(fb + 1) * 128, tb * 512:(tb + 1) * 512], writes=[b_gt[gb]])
                sp.dma(gt[:, gb, 1, :], gateT[D + fb * 128:D + (fb + 1) * 128, tb * 512:(tb + 1) * 512], pwrites=[b_gt[gb]])
                pa = (2 * fb) % 4
                pb = (2 * fb + 1) % 4
                mm_group(pm[pa][:], b_pm[pa], [(Wa[:, c, fb * 128:(fb + 1) * 128], yT[:, 0, c, :]) for c in range(8)], reads=[b_W, b_yT[0]])
                mm_group(pm[pb][:], b_pm[pb], [(Wb[:, c, fb * 128:(fb + 1) * 128], yT[:, 1, c, :]) for c in range(8)], reads=[b_W, b_yT[1]])
                dve.op(lambda e: e.tensor_tensor(out=ma[:], in0=pm[pa][:], in1=gt[:, gb, 0, :], op=ALU.mult), reads=[b_pm[pa], b_gt[gb]], writes=[b_ma])
                dve.op(lambda e: e.tensor_tensor(out=mbt[:], in0=pm[pb][:], in1=gt[:, gb, 1, :], op=ALU.mult), reads=[b_pm[pb], b_gt[gb]], writes=[b_mb])
                dve.op(lambda e: e.tensor_tensor(out=mst[:, gb, :], in0=ma[:], in1=mbt[:], op=ALU.add), reads=[b_ma, b_mb], writes=[b_mst[gb]])
                sp.dma(mT_d[fb * 128:(fb + 1) * 128, tb * 512:(tb + 1) * 512], mst[:, gb, :], reads=[b_mst[gb]])
        K.barrier()

    aff = sb(gst, "aff", [128, NT, NE], F32)
    b_aff = Buf()
    for st in phase("4b"):
        Wo = sb(st, "Wo", [128, 16, D], BF16)
        Wr = sb(st, "Wr", [128, 16, NE], BF16)
        g2 = sb(st, "g2", [128, 16], F32)
        mT = sb(st, "mT", [128, 2, 16, 512], BF16)
        xt = sb(st, "xt4", [128, 2, D], F32)
        ht = sb(st, "ht", [128, 2, D], F32)
        junk = sb(st, "junk4", [128, D], BF16)
        xn = sb(st, "xn4", [128, D], BF16)
        ssq = sb(st, "ssq4", [128, 1], F32)
        hst = sb(st, "hst", [128, 2, 16, 512], BF16)
        lg = sb(st, "lg", [128, NE], F32)
        mx = sb(st, "mx", [128, 1], F32)
        sm = sb(st, "sm", [128, 1], F32)
        pt = [ps(st, "pt5a", [128, 8, 128], BF16), ps(st, "pt5b", [128, 8, 128], BF16)]
        pm = [ps(st, "pm5_%d" % i, [128, 512], F32) for i in range(4)]
        pr = ps(st, "pr5", [128, NE], F32)
        b_Wo = Buf(); b_Wr = Buf(); b_g2 = Buf(); b_mT = [Buf(), Buf()]; b_xt = [Buf(), Buf()]; b_ht = [Buf(), Buf()]
        b_junk = Buf(); b_xn = Buf(); b_ssq = Buf(); b_hst = [Buf(), Buf()]; b_lg = Buf(); b_mx = Buf(); b_sm = Buf()
        b_pt = [Buf(), Buf()]; b_pm = [Buf() for _ in range(4)]; b_pr = Buf()
        pool.dma(Wo[:], w_o.rearrange("(c p) n -> p c n", p=128), writes=[b_Wo])
        pool.dma(Wr[:], w_r.rearrange("(c p) n -> p c n", p=128), writes=[b_Wr])
        sp.dma(g2[:], g2T, writes=[b_g2])
        pi = 0
        for tb in range(T // 512):
            mb = tb % 2
            sp.dma(mT[:, mb, :, :], mT_d.rearrange("(c p) t -> p c t", p=128)[:, :, tb * 512:(tb + 1) * 512], writes=[b_mT[mb]])
            for t4 in range(4):
                tt = tb * 4 + t4
                bi = tt % 2
                sp.dma(xt[:, bi, :], x[tt * 128:(tt + 1) * 128, :], writes=[b_xt[bi]])
                for cc in range(4):
                    p = pi % 4
                    pi += 1
                    mm_group(pm[p][:], b_pm[p],
                             [(mT[:, mb, kc, t4 * 128:(t4 + 1) * 128], Wo[:, kc, cc * 512:(cc + 1) * 512]) for kc in range(16)],
                             reads=[b_mT[mb], b_Wo])
                    dve.op(lambda e: e.tensor_tensor(out=ht[:, bi, cc * 512:(cc + 1) * 512], in0=pm[p][:], in1=xt[:, bi, cc * 512:(cc + 1) * 512], op=ALU.add),
                           reads=[b_pm[p], b_xt[bi]], writes=[b_ht[bi]] if cc == 0 else (), pwrites=() if cc == 0 else [b_ht[bi]])
                sp.dma(out[tt * 128:(tt + 1) * 128, :], ht[:, bi, :], reads=[b_ht[bi]])
                rmsnorm_T(st, "p4", ht[:, bi, :], b_ht[bi], g2, b_g2, hst[:, mb, :, t4 * 128:(t4 + 1) * 128], b_hst[mb],
                          pt, b_pt, (junk, b_junk, ssq, b_ssq, xn, b_xn), store_ap=xn2_d[tt * 128:(tt + 1) * 128, :])
                mm_group(pr[:], b_pr, [(hst[:, mb, kc, t4 * 128:(t4 + 1) * 128], Wr[:, kc, :]) for kc in range(16)], reads=[b_hst[mb], b_Wr])
                dve.op(lambda e: e.tensor_reduce(out=mx[:], in_=pr[:], axis=AX.X, op=ALU.max), reads=[b_pr], writes=[b_mx])
                dve.op(lambda e: e.tensor_scalar(out=mx[:], in0=mx[:], scalar1=-1.0, scalar2=None, op0=ALU.mult), reads=[b_mx], writes=[b_mx])
                dve.op(lambda e: e.memset(sm[:], 0.0), writes=[b_sm])
                act.op(lambda e: e.activation(out=lg[:], in_=pr[:], func=AF.Exp, bias=mx[:, 0:1], accum_out=sm[:]),
                       reads=[b_pr, b_mx], writes=[b_lg, b_sm])
                dve.op(lambda e: e.reciprocal(out=sm[:], in_=sm[:]), reads=[b_sm], writes=[b_sm])
                dve.op(lambda e: e.tensor_scalar(out=aff[:, tt, :], in0=lg[:], scalar1=sm[:, 0:1], scalar2=None, op0=ALU.mult),
                       reads=[b_lg, b_sm], pwrites=[b_aff])
            sp.dma(hn2T_d.rearrange("(c p) t -> p c t", p=128)[:, :, tb * 512:(tb + 1) * 512], hst[:, mb, :, :], reads=[b_hst[mb]])
        if DEBUG:
            sp.dma(aff_d, aff[:], reads=[b_aff])
        K.barrier()

    coef = sb(gst, "coef", [128, NT, NE], F32)
    b_coef = Buf()
    for st in phase("5"):
        affT = sb(st, "affT", [NE, T], F32)
        work = sb(st, "work", [NE, T], F32)
        cT = sb(st, "cT", [NE, T], F32)
        m8 = sb(st, "m8", [NE, 8], F32)
        pa = [ps(st, "pa%d" % i, [NE, 4, 128], F32) for i in range(2)]
        pc = [ps(st, "pc%d" % i, [128, 4, NE], F32) for i in range(2)]
        b_affT = Buf(); b_work = Buf(); b_cT = Buf(); b_m8 = Buf(); b_pa = [Buf(), Buf()]; b_pc = [Buf(), Buf()]
        for g in range(NT // 4):
            pb = g % 2
            for j in range(4):
                tt = g * 4 + j
                pe.op(lambda e: e.transpose(out=pa[pb][:, j, :], in_=aff[:, tt, :], identity=idf[:]),
                      reads=[b_aff, b_idf], writes=[b_pa[pb]] if j == 0 else (), pwrites=() if j == 0 else [b_pa[pb]])
            dve.op(lambda e: e.tensor_copy(out=affT[:, g * 512:(g + 1) * 512], in_=pa[pb][:].rearrange("p a b -> p (a b)")),
                   reads=[b_pa[pb]], pwrites=[b_affT])
        dve.op(lambda e: e.tensor_copy(out=work[:], in_=affT[:]), reads=[b_affT], writes=[b_work])
        for r in range(CAP // 8):
            dve.op(lambda e: e.max(out=m8[:], in_=work[:]), reads=[b_work], writes=[b_m8])
            if r < CAP // 8 - 1:
                dve.op(lambda e: e.match_replace(out=work[:], in_to_replace=m8[:], in_values=work[:], imm_value=-1.0),
                       reads=[b_m8, b_work], writes=[b_work])
        dve.op(lambda e: e.scalar_tensor_tensor(out=cT[:], in0=affT[:], scalar=m8[:, 7:8], in1=affT[:], op0=ALU.is_ge, op1=ALU.mult),
               reads=[b_affT, b_m8], writes=[b_cT])
        for g in range(NT // 4):
            pb = g % 2
            for j in range(4):
                tt = g * 4 + j
                pe.op(lambda e: e.transpose(out=pc[pb][:, j, :], in_=cT[:, tt * 128:(tt + 1) * 128], identity=idf[0:NE, 0:NE]),
                      reads=[b_cT, b_idf], writes=[b_pc[pb]] if j == 0 else (), pwrites=() if j == 0 else [b_pc[pb]])
            dve.op(lambda e: e.tensor_copy(out=coef[:, g * 4:(g + 1) * 4, :], in_=pc[pb][:]), reads=[b_pc[pb]], pwrites=[b_coef])
        K.barrier()

    for st in phase("6"):
        Wg = sb(st, "Wg", [128, 16, FF], BF16)
        Wu = sb(st, "Wu", [128, 16, FF], BF16)
        Wd = sb(st, "Wd", [128, 8, D], BF16)
        hT = sb(st, "hT6", [128, 2, 16, 512], BF16)
        sg = sb(st, "sg6", [128, 2, 512], F32)
        hid = sb(st, "hid6", [128, 2, 8, 512], BF16)
        yo = sb(st, "yo6", [128, 2, D], F32)
        pg = [ps(st, "pg6_%d" % i, [128, 512], F32) for i in range(2)]
        pu = [ps(st, "pu6_%d" % i, [128, 512], F32) for i in range(2)]
        po = [ps(st, "po6_%d" % i, [128, 512], F32) for i in range(4)]
        b_Wg = Buf(); b_Wu = Buf(); b_Wd = Buf(); b_hT = [Buf(), Buf()]; b_sg = [Buf(), Buf()]; b_hid = [Buf(), Buf()]
        b_yo = [Buf(), Buf()]; b_pg = [Buf(), Buf()]; b_pu = [Buf(), Buf()]; b_po = [Buf() for _ in range(4)]
        b_out = [Buf() for _ in range(NT)]
        hi = 0
        fi = 0
        oi = 0
        yi = 0
        for ex in range(NE):
            pool.dma(Wg[:], w_g[ex].rearrange("(c p) n -> p c n", p=128), writes=[b_Wg])
            pool.dma(Wu[:], w_u[ex].rearrange("(c p) n -> p c n", p=128), writes=[b_Wu])
            pool.dma(Wd[:], w_d[ex].rearrange("(c p) n -> p c n", p=128), writes=[b_Wd])
            for tb in range(T // 512):
                hb = hi % 2
                hi += 1
                sp.dma(hT[:, hb, :, :], hn2T_d.rearrange("(c p) t -> p c t", p=128)[:, :, tb * 512:(tb + 1) * 512], writes=[b_hT[hb]])
                for fb in range(8):
                    f2 = fi % 2
                    fi += 1
                    mm_group(pg[f2][:], b_pg[f2], [(Wg[:, kc, fb * 128:(fb + 1) * 128], hT[:, hb, kc, :]) for kc in range(16)], reads=[b_Wg, b_hT[hb]])
                    mm_group(pu[f2][:], b_pu[f2], [(Wu[:, kc, fb * 128:(fb + 1) * 128], hT[:, hb, kc, :]) for kc in range(16)], reads=[b_Wu, b_hT[hb]])
                    act.op(lambda e: e.activation(out=sg[:, f2, :], in_=pg[f2][:], func=AF.Silu), reads=[b_pg[f2]], writes=[b_sg[f2]])
                    dve.op(lambda e: e.tensor_tensor(out=hid[:, hb, fb, :], in0=sg[:, f2, :], in1=pu[f2][:], op=ALU.mult),
                           reads=[b_sg[f2], b_pu[f2]], writes=[b_hid[hb]] if fb == 0 else (), pwrites=() if fb == 0 else [b_hid[hb]])
                for t4 in range(4):
                    tt = tb * 4 + t4
                    yb_ = yi % 2
                    yi += 1
                    for cc in range(4):
                        p = oi % 4
                        oi += 1
                        mm_group(po[p][:], b_po[p], [(hid[:, hb, fb, t4 * 128:(t4 + 1) * 128], Wd[:, fb, cc * 512:(cc + 1) * 512]) for fb in range(8)],
                                 reads=[b_hid[hb], b_Wd])
                        evq[0] += 1
                        if evq[0] % 2 == 0:
                            dve.op(lambda e: e.tensor_scalar(out=yo[:, yb_, cc * 512:(cc + 1) * 512], in0=po[p][:], scalar1=coef[:, tt, ex:ex + 1], scalar2=None, op0=ALU.mult),
                                   reads=[b_po[p], b_coef], writes=[b_yo[yb_]] if cc == 0 else (), pwrites=() if cc == 0 else [b_yo[yb_]])
                        else:
                            act.op(lambda e: e.activation(out=yo[:, yb_, cc * 512:(cc + 1) * 512], in_=po[p][:], func=AF.Copy, scale=coef[:, tt, ex:ex + 1]),
                                   reads=[b_po[p], b_coef], writes=[b_yo[yb_]] if cc == 0 else (), pwrites=() if cc == 0 else [b_yo[yb_]])
                    pool.dma(out[tt * 128:(tt + 1) * 128, :], yo[:, yb_, :], reads=[b_yo[yb_]], writes=[b_out[tt]], accum_op=ALU.add)
        K.barrier()

    for st in phase("5g"):
        affT = sb(st, "affTg", [NE, T], F32)
        work = sb(st, "workg", [NE, T], F32)
        vals = sb(st, "valsg", [NE, CAP], F32)
        idxu = sb(st, "idxug", [NE, CAP], U32)
        pa = [ps(st, "pag%d" % i, [NE, 4, 128], F32) for i in range(2)]
        b_affT = Buf(); b_work = Buf(); b_vals = Buf(); b_idx = Buf(); b_pa = [Buf(), Buf()]
        for g in range(NT // 4):
            pb = g % 2
            for j in range(4):
                tt = g * 4 + j
                pe.op(lambda e: e.transpose(out=pa[pb][:, j, :], in_=aff[:, tt, :], identity=idf[:]),
                      reads=[b_aff, b_idf], writes=[b_pa[pb]] if j == 0 else (), pwrites=() if j == 0 else [b_pa[pb]])
            dve.op(lambda e: e.tensor_copy(out=affT[:, g * 512:(g + 1) * 512], in_=pa[pb][:].rearrange("p a b -> p (a b)")),
                   reads=[b_pa[pb]], pwrites=[b_affT])
        dve.op(lambda e: e.tensor_copy(out=work[:], in_=affT[:]), reads=[b_affT], writes=[b_work])
        for r in range(CAP // 8):
            v8 = vals[:, r * 8:(r + 1) * 8]
            dve.op(lambda e: e.max(out=v8, in_=work[:]), reads=[b_work], pwrites=[b_vals])
            dve.op(lambda e: e.max_index(out=idxu[:, r * 8:(r + 1) * 8], in_max=v8, in_values=work[:]), reads=[b_work, b_vals], pwrites=[b_idx])
            dve.op(lambda e: e.match_replace(out=work[:], in_to_replace=v8, in_values=work[:], imm_value=-1.0),
                   reads=[b_vals, b_work, b_idx], writes=[b_work])
        sp.dma(idx_d, idxu[:].bitcast(I32), reads=[b_idx])
        sp.dma(val_d, vals[:], reads=[b_vals])
        K.barrier()

    for st in phase("6g"):
        NS = CAP // 128
        Wg = sb(st, "Wgg", [128, 16, FF], BF16)
        Wu = sb(st, "Wug", [128, 16, FF], BF16)
        Wd = sb(st, "Wdg", [128, 8, D], BF16)
        g2 = sb(st, "g2g", [128, 16], F32)
        idx_t = sb(st, "idx_t", [128, 2, NS], I32)
        gat = sb(st, "gat", [128, 2, NS], F32)
        xe = sb(st, "xeg", [128, 2, D], BF16)
        xeT = sb(st, "xeTg", [128, 16, CAP], BF16)
        sg = sb(st, "sgg", [128, 2, CAP], F32)
        hid = sb(st, "hidg", [128, 8, CAP], BF16)
        yo = sb(st, "yog", [128, 2, D], F32)
        pt = [ps(st, "ptg%d" % i, [128, 8, 128], BF16) for i in range(2)]
        pg = ps(st, "pgg", [128, 512], F32)
        pu = ps(st, "pug", [128, 512], F32)
        po = [ps(st, "pog%d" % i, [128, 512], F32) for i in range(4)]
        b_Wg = Buf(); b_Wu = Buf(); b_Wd = Buf(); b_g2 = Buf(); b_it = [Buf(), Buf()]; b_xe = [Buf(), Buf()]; b_xeT = Buf()
        b_sg = [Buf(), Buf()]; b_hid = Buf(); b_yo = [Buf(), Buf()]; b_pt = [Buf(), Buf()]; b_pg = Buf(); b_pu = Buf()
        b_po = [Buf() for _ in range(4)]; b_outall = Buf()
        sp.dma(g2[:], g2T, writes=[b_g2])
        xi = 0
        fi = 0
        oi = 0
        yi = 0
        for ex in range(NE):
            eb = ex % 2
            pool.dma(Wg[:], w_g[ex].rearrange("(c p) n -> p c n", p=128), writes=[b_Wg])
            pool.dma(Wu[:], w_u[ex].rearrange("(c p) n -> p c n", p=128), writes=[b_Wu])
            pool.dma(Wd[:], w_d[ex].rearrange("(c p) n -> p c n", p=128), writes=[b_Wd])
            for j in range(NS):
                sp.dma(idx_t[:, eb, j:j + 1], idx_d[ex:ex + 1, j * 128:(j + 1) * 128].rearrange("o p -> p o"),
                       writes=[b_it[eb]] if j == 0 else (), pwrites=() if j == 0 else [b_it[eb]])
                sp.dma(gat[:, eb, j:j + 1], val_d[ex:ex + 1, j * 128:(j + 1) * 128].rearrange("o p -> p o"), pwrites=[b_it[eb]])
            for j in range(NS):
                xb = xi % 2
                xi += 1
                pool.dma(None, None, reads=[b_it[eb]], writes=[b_xe[xb]],
                         fn=lambda e: e.indirect_dma_start(out=xe[:, xb, :], out_offset=None, in_=xn2_d[:, :],
                                                           in_offset=bass.IndirectOffsetOnAxis(ap=idx_t[:, eb, j:j + 1], axis=0)))
                for half in range(2):
                    for c in range(8):
                        cc = half * 8 + c
                        pe.op(lambda e: e.transpose(out=pt[half][:, c, :], in_=xe[:, xb, cc * 128:(cc + 1) * 128], identity=idb[:]),
                              reads=[b_xe[xb], b_idb], writes=[b_pt[half]] if c == 0 else (), pwrites=() if c == 0 else [b_pt[half]])
                    first = (j == 0 and half == 0)
                    dve.op(lambda e: e.tensor_tensor(out=xeT[:, half * 8:(half + 1) * 8, j * 128:(j + 1) * 128], in0=pt[half][:],
                                                     in1=g2[:, half * 8:(half + 1) * 8].unsqueeze(2).broadcast_to([128, 8, 128]), op=ALU.mult),
                           reads=[b_pt[half], b_g2], writes=[b_xeT] if first else (), pwrites=() if first else [b_xeT])
            for fb in range(8):
                f2 = fi % 2
                fi += 1
                mm_group(pg[:, 0:CAP], b_pg, [(Wg[:, kc, fb * 128:(fb + 1) * 128], xeT[:, kc, :]) for kc in range(16)], reads=[b_Wg, b_xeT])
                mm_group(pu[:, 0:CAP], b_pu, [(Wu[:, kc, fb * 128:(fb + 1) * 128], xeT[:, kc, :]) for kc in range(16)], reads=[b_Wu, b_xeT])
                act.op(lambda e: e.activation(out=sg[:, f2, :], in_=pg[:, 0:CAP], func=AF.Silu), reads=[b_pg], writes=[b_sg[f2]])
                dve.op(lambda e: e.tensor_tensor(out=hid[:, fb, :], in0=sg[:, f2, :], in1=pu[:, 0:CAP], op=ALU.mult),
                       reads=[b_sg[f2], b_pu], writes=[b_hid] if fb == 0 else (), pwrites=() if fb == 0 else [b_hid])
            for j in range(NS):
                yb_ = yi % 2
                yi += 1
                for cc in range(4):
                    p = oi % 4
                    oi += 1
                    mm_group(po[p][:], b_po[p], [(hid[:, fb, j * 128:(j + 1) * 128], Wd[:, fb, cc * 512:(cc + 1) * 512]) for fb in range(8)],
                             reads=[b_hid, b_Wd])
                    evq[0] += 1
                    if evq[0] % 2 == 0:
                        dve.op(lambda e: e.tensor_scalar(out=yo[:, yb_, cc * 512:(cc + 1) * 512], in0=po[p][:], scalar1=gat[:, eb, j:j + 1], scalar2=None, op0=ALU.mult),
                               reads=[b_po[p], b_it[eb]], writes=[b_yo[yb_]] if cc == 0 else (), pwrites=() if cc == 0 else [b_yo[yb_]])
                    else:
                        act.op(lambda e: e.activation(out=yo[:, yb_, cc * 512:(cc + 1) * 512], in_=po[p][:], func=AF.Copy, scale=gat[:, eb, j:j + 1]),
                               reads=[b_po[p], b_it[eb]], writes=[b_yo[yb_]] if cc == 0 else (), pwrites=() if cc == 0 else [b_yo[yb_]])
                pool.dma(None, None, reads=[b_yo[yb_], b_it[eb]], writes=[b_outall],
                         fn=lambda e: e.indirect_dma_start(out=out[:, :], out_offset=bass.IndirectOffsetOnAxis(ap=idx_t[:, eb, j:j + 1], axis=0),
                                                           in_=yo[:, yb_, :], in_offset=None, compute_op=ALU.add))
        K.barrier()
    gst.close()
    return nc


_NC_CACHE = {}


def kernel(**inputs):
    f32 = np.float32
    g = lambda k: np.asarray(inputs[k], dtype=f32)
    x = g("x")
    rep = lambda v, n=128: np.ascontiguousarray(np.broadcast_to(v[None], (n,) + v.shape)).astype(f32)
    colT = lambda v: np.ascontiguousarray(v.reshape(16, 128).T).astype(f32)
    inv = 500000.0 ** (-(np.arange(0, 16, 2, dtype=f32) / 16.0))
    ang = np.arange(T, dtype=f32)[:, None] * inv[None, :]
    cos = np.cos(ang).astype(f32).reshape(NT, 128, 8).transpose(1, 0, 2)
    sin = np.sin(ang).astype(f32).reshape(NT, 128, 8).transpose(1, 0, 2)
    common = {
        "w_in": g("w_in")[0],
        "g1T": colT(g("attn_norm_g")[0]),
        "g2T": colT(g("ffn_norm_g")[0]),
        "ident": np.eye(128, dtype=f32),
        "gqk": rep(np.stack([g("q_norm_g")[0], g("k_norm_g")[0]])),
        "rope_c": np.ascontiguousarray(cos),
        "rope_s": np.ascontiguousarray(sin),
        "lam_in": rep(np.stack([g("lambda_q1")[0], g("lambda_k1")[0], g("lambda_q2")[0], g("lambda_k2")[0]])),
        "subg": rep(g("subln_g")[0]),
        "w_ba": g("w_branch_a")[0],
        "w_bb": g("w_branch_b")[0],
        "w_o": g("w_out")[0],
        "w_r": g("w_router")[0],
        "w_g": g("w_gate_e")[0],
        "w_u": g("w_up_e")[0],
        "w_d": g("w_down_e")[0],
        "mu_rep": rep(g("shift_mu")[0]),
        "w0_rep": rep(np.stack([g("w0_f")[0], g("w0_b")[0]])),
        "a0_rep": rep(np.stack([g("a0_f")[0], g("a0_b")[0]])),
        "kk_rep": rep(g("k_k")[0]),
        "ka_rep": rep(g("k_a")[0]),
        "rk_rep": rep(g("r_k")[0].reshape(1024)),
        "lnw_rep": rep(g("ln_x_w")[0]),
        "lnb_rep": rep(g("ln_x_b")[0]),
        "w2_in": np.ascontiguousarray(np.stack([g("w2_f")[0], g("w2_b")[0], g("a2_f")[0], g("a2_b")[0]])),
        "g2_in": g("g2")[0],
        "masks_in": np.ascontiguousarray(np.stack([np.triu(np.ones((128, 128), f32), 1), np.tril(np.ones((128, 128), f32), -1),
                                                   np.triu(np.ones((128, 128), f32), 0), np.tril(np.ones((128, 128), f32), 0)], axis=1)),
    }
    if "nc" not in _NC_CACHE:
        _NC_CACHE["nc"] = build()
    nc = _NC_CACHE["nc"]
    in_maps = []
    for c in range(8):
        m = {k: v for k, v in common.items() if k in DECL}
        m["x"] = np.ascontiguousarray(x[c // 2][:T])
        in_maps.append(m)
    res = run_bass_kernel_spmd(nc, in_maps, core_ids=list(range(8)))
    outp = np.empty((4, T, D), dtype=f32)
    for c in range(8):
        b, half = c // 2, c % 2
        o = np.asarray(res.results[c]["out"])
        outp[b, half * 2048:(half + 1) * 2048] = o[half * 2048:(half + 1) * 2048]
    if DEBUG:
        kernel.dbg = res.results
    return outp
```

```python
import os
import math
from contextlib import ExitStack
import numpy as np
import concourse.bass as bass
import concourse.mybir as mybir
from concourse.bass_utils import run_bass_kernel_spmd

F32 = mybir.dt.float32
BF16 = mybir.dt.bfloat16
I32 = mybir.dt.int32
U32 = mybir.dt.uint32
AF = mybir.ActivationFunctionType
ALU = mybir.AluOpType
AX = mybir.AxisListType

D = 2048
T = int(os.environ.get("KT", "4096"))
NT = T // 128
DA_COLS = 3072
RW_COLS = 3488
GATE_COLS = 4096
IN_COLS = DA_COLS + RW_COLS + GATE_COLS
TM_COLS = DA_COLS + RW_COLS
NE = 16
FF = 1024
CAP = T // 8
LAM_INIT = 0.8 - 0.6 * math.exp(-0.3 * 0)

DEBUG = bool(int(os.environ.get("KDEBUG", "0")))
SKIP_RWKV = bool(int(os.environ.get("KSKIP_RWKV", "0")))
CUT = int(os.environ.get("KCUT", "99"))
PHASES = os.environ.get("KPHASES", "1,2a,2b,3a,3b,3c,4a,4b,5g,6g").split(",")


class Buf:
    __slots__ = ("w", "r", "pr", "name")

    def __init__(self, name=""):
        self.w = {}
        self.r = {}
        self.pr = {}
        self.name = name


class Eng:
    def __init__(self, K, name, eng, is_pe=False):
        self.K = K
        self.name = name
        self.eng = eng
        self.is_pe = is_pe
        self.sem = K.new_sem("s_" + name)
        self.count = 0
        self.seen = {}
        self.dma_pool = []
        self.dma_i = 0

    def _wait(self, toks):
        for sem, val in toks.items():
            if self.is_pe and sem is self.sem:
                continue
            if self.seen.get(sem, 0) >= val:
                continue
            self.eng.wait_ge(sem, val)
            self.seen[sem] = val

    @staticmethod
    def _deps(reads, writes, pwrites):
        toks = {}

        def add(d):
            for s, v in d.items():
                if toks.get(s, 0) < v:
                    toks[s] = v
        for b in reads:
            add(b.w)
        for b in writes:
            add(b.w)
            add(b.r)
        for b in pwrites:
            add(b.r)
            add(b.pr)
        return toks

    @staticmethod
    def _commit(tok, reads, writes, pwrites):
        s, v = tok
        for b in reads:
            b.r[s] = v
        for b in writes:
            b.w = {s: v}
            b.pr = b.r
            b.r = {}
        for b in pwrites:
            b.w[s] = v
            for s2, v2 in b.r.items():
                if b.pr.get(s2, 0) < v2:
                    b.pr[s2] = v2
            b.r = {}

    def op(self, fn, reads=(), writes=(), pwrites=()):
        self._wait(self._deps(reads, writes, pwrites))
        ins = fn(self.eng)
        self.count += 1
        ins.then_inc(self.sem, 1)
        self._commit((self.sem, self.count), reads, writes, pwrites)
        return ins

    def dma(self, out, in_, reads=(), writes=(), pwrites=(), fn=None, **kw):
        self._wait(self._deps(reads, writes, pwrites))
        if len(self.dma_pool) < self.K.n_dma_sems:
            self.dma_pool.append([self.K.new_sem("d_%s%d" % (self.name, len(self.dma_pool))), 0])
        ent = self.dma_pool[self.dma_i % self.K.n_dma_sems]
        self.dma_i += 1
        sem, cur = ent
        if cur > 0 and self.seen.get(sem, 0) < cur:
            self.eng.wait_ge(sem, cur)
            self.seen[sem] = cur
        ins = fn(self.eng) if fn is not None else self.eng.dma_start(out=out, in_=in_, **kw)
        ent[1] = cur + 16
        ins.then_inc(sem, 16)
        self._commit((sem, cur + 16), reads, writes, pwrites)
        return ins


class Kern:
    def __init__(self, nc, n_dma_sems=10):
        self.nc = nc
        self.n_dma_sems = n_dma_sems
        self._sems = []
        self.E = {}

    def new_sem(self, name):
        cm = self.nc.semaphore(name)
        s = cm.__enter__()
        self._sems.append(cm)
        return s

    def setup(self, engines):
        for name, eng in engines.items():
            self.E[name] = Eng(self, name, eng, is_pe=(name == "pe"))

    def barrier(self):
        toks = {}
        for e in self.E.values():
            if e.count > 0:
                toks[e.sem] = e.count
            for sem, cur in e.dma_pool:
                if cur > 0:
                    toks[sem] = cur
        for e in self.E.values():
            for sem, val in toks.items():
                if sem is e.sem:
                    continue
                if e.seen.get(sem, 0) >= val:
                    continue
                e.eng.wait_ge(sem, val)
                e.seen[sem] = val


DECL = set()


def phase(name):
    if name in PHASES or name == "zero":
        with ExitStack() as st:
            yield st


def build():
    nc = bass.Bass("TRN2", target_bir_lowering=False)

    def din(name, shape, dt=F32):
        DECL.add(name)
        return nc.dram_tensor(name, list(shape), dt, kind="ExternalInput").ap()

    def dscr(name, shape, dt, dbg=False):
        kind = "ExternalOutput" if (dbg and DEBUG) else "Internal"
        return nc.dram_tensor(name, list(shape), dt, kind=kind).ap()

    x = din("x", [T, D])
    w_in = din("w_in", [D, IN_COLS])
    g1T = din("g1T", [128, 16])
    g2T = din("g2T", [128, 16])
    ident_in = din("ident", [128, 128])
    gqk = din("gqk", [128, 2, 64])
    rope_c = din("rope_c", [128, NT, 8])
    rope_s = din("rope_s", [128, NT, 8])
    lam_in = din("lam_in", [128, 4, 64])
    subg = din("subg", [128, 128])
    w_ba = din("w_ba", [1024, D])
    w_bb = din("w_bb", [1024, D])
    w_o = din("w_o", [D, D])
    w_r = din("w_r", [D, NE])
    if "6" in PHASES or "6g" in PHASES:
        w_g = din("w_g", [NE, D, FF])
        w_u = din("w_u", [NE, D, FF])
        w_d = din("w_d", [NE, FF, D])

    out = nc.dram_tensor("out", [T, D], F32, kind="ExternalOutput").ap()

    proj_tm = dscr("proj_tm", [T, TM_COLS], BF16, dbg=True)
    gateT = dscr("gateT", [GATE_COLS, T], BF16)
    qkT_d = dscr("qkT_d", [2048, T], BF16)
    y_a = dscr("y_a", [T, 1024], BF16, dbg=True)
    y_b = dscr("y_b", [T, 1024], BF16, dbg=True)
    mT_d = dscr("mT_d", [D, T], BF16)
    hn2T_d = dscr("hn2T_d", [D, T], BF16)
    aff_d = dscr("aff_d", [128, NT, NE], F32, dbg=True)
    xn2_d = dscr("xn2_d", [T, D], BF16)
    idx_d = dscr("idx_d", [NE, CAP], I32)
    val_d = dscr("val_d", [NE, CAP], F32)

    K = Kern(nc)
    K.setup({"sp": nc.sync, "act": nc.scalar, "dve": nc.vector, "pe": nc.tensor, "pool": nc.gpsimd})
    sp, act, dve, pe, pool = (K.E[n] for n in ("sp", "act", "dve", "pe", "pool"))

    gst = ExitStack()

    def sb(st, name, shape, dt):
        return st.enter_context(nc.sbuf_tensor(name, list(shape), dt))

    def ps(st, name, shape, dt):
        return st.enter_context(nc.psum_tensor(name, list(shape), dt))

    idf = sb(gst, "idf", [128, 128], F32)
    idb = sb(gst, "idb", [128, 128], BF16)
    b_idf, b_idb = Buf(), Buf()
    sp.dma(idf[:], ident_in, writes=[b_idf])
    dve.op(lambda e: e.tensor_copy(out=idb[:], in_=idf[:]), reads=[b_idf], writes=[b_idb])

    evq = [0]

    def evac_copy(dst, src, reads, writes=(), pwrites=()):
        evq[0] += 1
        if evq[0] % 2 == 0:
            dve.op(lambda e: e.tensor_copy(out=dst, in_=src), reads=reads, writes=writes, pwrites=pwrites)
        else:
            act.op(lambda e: e.activation(out=dst, in_=src, func=AF.Copy), reads=reads, writes=writes, pwrites=pwrites)

    def mm_group(ps_ap, ps_buf, pairs, reads):
        n = len(pairs)
        for i, (l, r) in enumerate(pairs):
            if i == 0:
                pe.op(lambda e: e.matmul(ps_ap, lhsT=l, rhs=r, start=True, stop=(n == 1)), reads=reads, writes=[ps_buf])
            else:
                pe.op(lambda e: e.matmul(ps_ap, lhsT=l, rhs=r, start=False, stop=(i == n - 1)), reads=reads, pwrites=[ps_buf])

    def rmsnorm_T(st, tag, src_tile, b_src, gT_t, b_gT, dstT_ap, b_dst, pts, b_pts, scratch, store_ap=None):
        junk, b_junk, ssq, b_ssq, xn, b_xn = scratch
        dve.op(lambda e: e.memset(ssq[:], 0.0), writes=[b_ssq])
        act.op(lambda e: e.activation(out=junk[:], in_=src_tile, func=AF.Square, accum_out=ssq[:]),
               reads=[b_src], writes=[b_junk, b_ssq])
        act.op(lambda e: e.activation(out=ssq[:], in_=ssq[:], func=AF.Sqrt, scale=1.0 / D, bias=1e-6),
               reads=[b_ssq], writes=[b_ssq])
        dve.op(lambda e: e.reciprocal(out=ssq[:], in_=ssq[:]), reads=[b_ssq], writes=[b_ssq])
        dve.op(lambda e: e.tensor_scalar(out=xn[:], in0=src_tile, scalar1=ssq[:, 0:1], scalar2=None, op0=ALU.mult),
               reads=[b_src, b_ssq], writes=[b_xn])
        if store_ap is not None:
            sp.dma(store_ap, xn[:], reads=[b_xn])
        for half in range(2):
            for c in range(8):
                cc = half * 8 + c
                pe.op(lambda e: e.transpose(out=pts[half][:, c, :], in_=xn[:, cc * 128:(cc + 1) * 128], identity=idb[:]),
                      reads=[b_xn, b_idb], writes=[b_pts[half]] if c == 0 else (), pwrites=() if c == 0 else [b_pts[half]])
            dve.op(lambda e: e.tensor_tensor(out=dstT_ap[:, half * 8:(half + 1) * 8, :], in0=pts[half][:],
                                             in1=gT_t[:, half * 8:(half + 1) * 8].unsqueeze(2).broadcast_to([128, 8, 128]),
                                             op=ALU.mult),
                   reads=[b_pts[half], b_gT], pwrites=[b_dst])

    for st in phase("1"):
        HT = min(2048, T)
        hnT = sb(st, "hnT", [128, 16, HT], BF16)
        xt = sb(st, "xt", [128, 2, D], F32)
        junk = sb(st, "junk1", [128, D], BF16)
        xn = sb(st, "xn1", [128, D], BF16)
        ssq = sb(st, "ssq1", [128, 1], F32)
        g1 = sb(st, "g1", [128, 16], F32)
        wbf = sb(st, "wbf", [128, 2, 16, 512], BF16)
        stage = sb(st, "stage1", [128, 4, 512], BF16)
        pt = [ps(st, "pt1a", [128, 8, 128], BF16), ps(st, "pt1b", [128, 8, 128], BF16)]
        pm = [ps(st, "pm1_%d" % i, [128, 512], F32) for i in range(4)]
        b_hnT = Buf(); b_xt = [Buf(), Buf()]; b_junk = Buf(); b_xn = Buf(); b_ssq = Buf(); b_g1 = Buf()
        b_wbf = [Buf(), Buf()]; b_stage = [Buf() for _ in range(4)]; b_pt = [Buf(), Buf()]; b_pm = [Buf() for _ in range(4)]
        sp.dma(g1[:], g1T, writes=[b_g1])
        w_v = w_in.rearrange("(c p) n -> p c n", p=128)
        chunks = []
        c0 = 0
        while c0 < TM_COLS:
            cw = min(512, TM_COLS - c0)
            chunks.append(("tm", c0, cw))
            c0 += cw
        for gc in range(GATE_COLS // 512):
            chunks.append(("gate", TM_COLS + gc * 512, 512))
        wi = 0
        si = 0
        pi = 0
        NH16 = HT // 128
        for half in range(T // HT):
            for t16 in range(NH16):
                tt = half * NH16 + t16
                bi = tt % 2
                sp.dma(xt[:, bi, :], x[tt * 128:(tt + 1) * 128, :], writes=[b_xt[bi]])
                rmsnorm_T(st, "p1", xt[:, bi, :], b_xt[bi], g1, b_g1, hnT[:, :, t16 * 128:(t16 + 1) * 128], b_hnT,
                          pt, b_pt, (junk, b_junk, ssq, b_ssq, xn, b_xn))
            for kind, c0, cw in chunks:
                wb = wi % 2
                wi += 1
                pool.dma(wbf[:, wb, :, 0:cw], w_v[:, :, c0:c0 + cw], writes=[b_wbf[wb]])
                if kind == "tm":
                    for t16 in range(NH16):
                        tt = half * NH16 + t16
                        p = pi % 4
                        pi += 1
                        mm_group(pm[p][:, 0:cw], b_pm[p],
                                 [(hnT[:, kc, t16 * 128:(t16 + 1) * 128], wbf[:, wb, kc, 0:cw]) for kc in range(16)],
                                 reads=[b_hnT, b_wbf[wb]])
                        s = si % 4
                        si += 1
                        evac_copy(stage[:, s, 0:cw], pm[p][:, 0:cw], reads=[b_pm[p]], writes=[b_stage[s]])
                        sp.dma(proj_tm[tt * 128:(tt + 1) * 128, c0:c0 + cw], stage[:, s, 0:cw], reads=[b_stage[s]])
                else:
                    gcol = c0 - TM_COLS
                    for blk in range(4):
                        for tc4 in range(HT // 512):
                            p = pi % 4
                            pi += 1
                            mm_group(pm[p][:], b_pm[p],
                                     [(wbf[:, wb, kc, blk * 128:(blk + 1) * 128], hnT[:, kc, tc4 * 512:(tc4 + 1) * 512]) for kc in range(16)],
                                     reads=[b_hnT, b_wbf[wb]])
                            s = si % 4
                            si += 1
                            act.op(lambda e: e.activation(out=stage[:, s, :], in_=pm[p][:], func=AF.Sigmoid),
                                   reads=[b_pm[p]], writes=[b_stage[s]])
                            sp.dma(gateT[gcol + blk * 128:gcol + (blk + 1) * 128, half * HT + tc4 * 512:half * HT + (tc4 + 1) * 512],
                                   stage[:, s, :], reads=[b_stage[s]])
        K.barrier()

    for st in phase("2a"):
        qk = sb(st, "qk", [128, 2, 2048], BF16)
        sq = sb(st, "sq", [128, 32, 64], F32)
        ss = sb(st, "ss", [128, 32], F32)
        qn = sb(st, "qn", [128, 32, 64], F32)
        tmp = sb(st, "ropetmp", [128, 4, 32, 8], F32)
        qb = sb(st, "qb", [128, 32, 64], BF16)
        gq = sb(st, "gq", [128, 2, 64], F32)
        rc = sb(st, "rc", [128, NT, 8], F32)
        rs_ = sb(st, "rs", [128, NT, 8], F32)
        stg = sb(st, "stg2", [128, 2, 16, 512], BF16)
        pt = [ps(st, "pt2a", [128, 8, 128], BF16), ps(st, "pt2b", [128, 8, 128], BF16)]
        b_qk = [Buf(), Buf()]; b_sq = Buf(); b_ss = Buf(); b_qn = Buf(); b_tmp = Buf(); b_qb = Buf()
        b_c = Buf(); b_stg = [Buf(), Buf()]; b_pt = [Buf(), Buf()]
        sp.dma(gq[:], gqk, writes=[b_c])
        sp.dma(rc[:], rope_c, pwrites=[b_c])
        sp.dma(rs_[:], rope_s, pwrites=[b_c])
        for tt in range(NT):
            bi = tt % 2
            sp.dma(qk[:, bi, :], proj_tm[tt * 128:(tt + 1) * 128, 0:2048], writes=[b_qk[bi]])
            qv = qk[:, bi, :].rearrange("p (g d) -> p g d", d=64)
            dve.op(lambda e: e.tensor_tensor(out=sq[:], in0=qv, in1=qv, op=ALU.mult), reads=[b_qk[bi]], writes=[b_sq])
            dve.op(lambda e: e.tensor_reduce(out=ss[:], in_=sq[:], axis=AX.X, op=ALU.add), reads=[b_sq], writes=[b_ss])
            act.op(lambda e: e.activation(out=ss[:], in_=ss[:], func=AF.Sqrt, scale=1.0 / 64, bias=1e-6), reads=[b_ss], writes=[b_ss])
            dve.op(lambda e: e.reciprocal(out=ss[:], in_=ss[:]), reads=[b_ss], writes=[b_ss])
            dve.op(lambda e: e.tensor_tensor(out=qn[:], in0=qv, in1=ss[:].unsqueeze(2).broadcast_to([128, 32, 64]), op=ALU.mult),
                   reads=[b_qk[bi], b_ss], writes=[b_qn])
            for i in range(2):
                dve.op(lambda e: e.tensor_tensor(out=qn[:, i * 16:(i + 1) * 16, :], in0=qn[:, i * 16:(i + 1) * 16, :],
                                                 in1=gq[:, i:i + 1, :].broadcast_to([128, 16, 64]), op=ALU.mult),
                       reads=[b_qn, b_c], writes=[b_qn])
            cb = rc[:, tt:tt + 1, :].broadcast_to([128, 32, 8])
            sbb = rs_[:, tt:tt + 1, :].broadcast_to([128, 32, 8])
            x1 = qn[:, :, 0:8]
            x2 = qn[:, :, 8:16]
            dve.op(lambda e: e.tensor_tensor(out=tmp[:, 0], in0=x1, in1=cb, op=ALU.mult), reads=[b_qn, b_c], writes=[b_tmp])
            dve.op(lambda e: e.tensor_tensor(out=tmp[:, 1], in0=x2, in1=sbb, op=ALU.mult), reads=[b_qn, b_c], pwrites=[b_tmp])
            dve.op(lambda e: e.tensor_tensor(out=tmp[:, 2], in0=x2, in1=cb, op=ALU.mult), reads=[b_qn, b_c], pwrites=[b_tmp])
            dve.op(lambda e: e.tensor_tensor(out=tmp[:, 3], in0=x1, in1=sbb, op=ALU.mult), reads=[b_qn, b_c], pwrites=[b_tmp])
            dve.op(lambda e: e.tensor_copy(out=qb[:], in_=qn[:]), reads=[b_qn], writes=[b_qb])
            dve.op(lambda e: e.tensor_tensor(out=qb[:, :, 0:8], in0=tmp[:, 0], in1=tmp[:, 1], op=ALU.subtract), reads=[b_tmp], writes=[b_qb])
            dve.op(lambda e: e.tensor_tensor(out=qb[:, :, 8:16], in0=tmp[:, 2], in1=tmp[:, 3], op=ALU.add), reads=[b_tmp], writes=[b_qb])
            qbf = qb[:].rearrange("p g d -> p (g d)")
            sg = (tt // 4) % 2
            for half in range(2):
                for c in range(8):
                    cc = half * 8 + c
                    pe.op(lambda e: e.transpose(out=pt[half][:, c, :], in_=qbf[:, cc * 128:(cc + 1) * 128], identity=idb[:]),
                          reads=[b_qb, b_idb], writes=[b_pt[half]] if c == 0 else (), pwrites=() if c == 0 else [b_pt[half]])
                evac_copy(stg[:, sg, half * 8:(half + 1) * 8, (tt % 4) * 128:(tt % 4 + 1) * 128], pt[half][:],
                          reads=[b_pt[half]], writes=[b_stg[sg]] if (tt % 4 == 0 and half == 0) else (),
                          pwrites=() if (tt % 4 == 0 and half == 0) else [b_stg[sg]])
            if tt % 4 == 3:
                t0 = (tt // 4) * 512
                sp.dma(qkT_d.rearrange("(c p) t -> p c t", p=128)[:, :, t0:t0 + 512], stg[:, sg, :, :], reads=[b_stg[sg]])
        K.barrier()

    for st in phase("2b"):
        V1 = sb(st, "V1", [128, NT, 8, 144], BF16)
        qT = sb(st, "qT", [128, 2, T], BF16)
        kT1 = sb(st, "kT1", [128, 2, T], BF16)
        kT2 = sb(st, "kT2", [128, 2, T], BF16)
        PT = sb(st, "PT", [128, 3, 2, 512], BF16)
        lamt = sb(st, "lamt", [128, 4, 64], F32)
        lamp = sb(st, "lamp", [128, 2, 64], F32)
        lam2 = sb(st, "lam2", [128, 2], F32)
        lam = sb(st, "lam", [128, 1], F32)
        sgt = sb(st, "sgt", [128, 128], F32)
        rr = sb(st, "rr", [128, 2], F32)
        o1 = sb(st, "o1", [128, 128], F32)
        o2 = sb(st, "o2", [128, 128], F32)
        osq = sb(st, "osq", [128, 128], F32)
        oms = sb(st, "oms", [128, 1], F32)
        yst = sb(st, "yst", [128, 2, 128], BF16)
        pS = [ps(st, "pS%d" % i, [128, 2, 512], F32) for i in range(2)]
        pO = [ps(st, "pO%d" % i, [128, 2, 256], F32) for i in range(4)]
        b_V1 = Buf(); b_V1z = Buf(); b_q = [Buf(), Buf()]; b_k1 = [Buf(), Buf()]; b_k2 = [Buf(), Buf()]
        b_PT = [Buf() for _ in range(3)]; b_pS = [Buf(), Buf()]; b_pO = [[b_, b_] for b_ in (Buf(), Buf(), Buf(), Buf())]
        b_lam = Buf(); b_sg = Buf(); b_rr = Buf(); b_o1 = Buf(); b_o2 = Buf(); b_osq = Buf(); b_oms = Buf(); b_yst = [Buf(), Buf()]
        sp.dma(lamt[:], lam_in, writes=[b_lam])
        sp.dma(sgt[:], subg, writes=[b_sg])
        dve.op(lambda e: e.tensor_tensor(out=lamp[:, 0, :], in0=lamt[:, 0, :], in1=lamt[:, 1, :], op=ALU.mult), reads=[b_lam], writes=[b_lam])
        dve.op(lambda e: e.tensor_tensor(out=lamp[:, 1, :], in0=lamt[:, 2, :], in1=lamt[:, 3, :], op=ALU.mult), reads=[b_lam], writes=[b_lam])
        dve.op(lambda e: e.tensor_reduce(out=lam2[:], in_=lamp[:], axis=AX.X, op=ALU.add), reads=[b_lam], writes=[b_lam])
        act.op(lambda e: e.activation(out=lam2[:], in_=lam2[:], func=AF.Exp), reads=[b_lam], writes=[b_lam])
        dve.op(lambda e: e.tensor_tensor(out=lam[:], in0=lam2[:, 0:1], in1=lam2[:, 1:2], op=ALU.subtract), reads=[b_lam], writes=[b_lam])
        dve.op(lambda e: e.tensor_scalar(out=lam[:], in0=lam[:], scalar1=LAM_INIT, scalar2=None, op0=ALU.add), reads=[b_lam], writes=[b_lam])
        pool.op(lambda e: e.memset(kT1[:], 0.0), writes=[b_k1[0], b_k1[1]])
        pool.op(lambda e: e.memset(kT2[:], 0.0), writes=[b_k2[0], b_k2[1]])
        pool.op(lambda e: e.memset(V1[:], 1.0), writes=[b_V1, b_V1z])
        for tt in range(NT):
            sp.dma(V1[:, tt, :, 0:128], proj_tm[tt * 128:(tt + 1) * 128, 2048:3072].rearrange("p (h v) -> p h v", v=128), reads=[b_V1z], pwrites=[b_V1])
        sci = 0
        pti = 0
        for h in range(8):
            hb = h % 2
            sp.dma(qT[:, hb, :], qkT_d[h * 128:(h + 1) * 128, :], writes=[b_q[hb]])
            sp.dma(kT1[0:64, hb, :], qkT_d[1024 + h * 128:1024 + h * 128 + 64, :], writes=[b_k1[hb]])
            sp.dma(kT2[64:128, hb, :], qkT_d[1024 + h * 128 + 64:1024 + (h + 1) * 128, :], writes=[b_k2[hb]])
            kTs = [kT1, kT2]
            b_ks = [b_k1, b_k2]
            for qc in range(T // 512):
                for kt in range(NT):
                    pb = sci % 2
                    sci += 1
                    for s in range(2):
                        pe.op(lambda e: e.matmul(pS[pb][:, s, :], lhsT=kTs[s][:, hb, kt * 128:(kt + 1) * 128], rhs=qT[:, hb, qc * 512:(qc + 1) * 512],
                                                 start=True, stop=True),
                              reads=[b_ks[s][hb], b_q[hb]], writes=[b_pS[pb]] if s == 0 else (), pwrites=() if s == 0 else [b_pS[pb]])
                    pi_ = pti % 3
                    pti += 1
                    act.op(lambda e: e.activation(out=PT[:, pi_, :, :], in_=pS[pb][:], func=AF.Exp, scale=0.125),
                           reads=[b_pS[pb]], writes=[b_PT[pi_]])
                    for s in range(2):
                        for qs in range(4):
                            if kt == 0:
                                pe.op(lambda e: e.matmul(pO[qs][:, s, 0:129], lhsT=PT[:, pi_, s, qs * 128:(qs + 1) * 128], rhs=V1[:, kt, h, 0:129],
                                                         start=(s == 0), stop=False, skip_group_check=True),
                                      reads=[b_PT[pi_], b_V1], writes=[b_pO[qs][s]])
                            else:
                                pe.op(lambda e: e.matmul(pO[qs][:, s, 0:129], lhsT=PT[:, pi_, s, qs * 128:(qs + 1) * 128], rhs=V1[:, kt, h, 0:129],
                                                         start=False, stop=(kt == NT - 1), skip_group_check=True),
                                      reads=[b_PT[pi_], b_V1], pwrites=[b_pO[qs][s]])
                for qs in range(4):
                    qt = qc * 4 + qs
                    dve.op(lambda e: e.tensor_copy(out=rr[:, 0:1], in_=pO[qs][:, 0, 128:129]), reads=[b_pO[qs][0]], writes=[b_rr])
                    dve.op(lambda e: e.tensor_copy(out=rr[:, 1:2], in_=pO[qs][:, 1, 128:129]), reads=[b_pO[qs][1]], writes=[b_rr])
                    dve.op(lambda e: e.reciprocal(out=rr[:], in_=rr[:]), reads=[b_rr], writes=[b_rr])
                    dve.op(lambda e: e.tensor_tensor(out=rr[:, 1:2], in0=rr[:, 1:2], in1=lam[:], op=ALU.mult), reads=[b_rr, b_lam], writes=[b_rr])
                    dve.op(lambda e: e.tensor_scalar(out=o1[:], in0=pO[qs][:, 0, 0:128], scalar1=rr[:, 0:1], scalar2=None, op0=ALU.mult),
                           reads=[b_pO[qs][0], b_rr], writes=[b_o1])
                    dve.op(lambda e: e.tensor_scalar(out=o2[:], in0=pO[qs][:, 1, 0:128], scalar1=rr[:, 1:2], scalar2=None, op0=ALU.mult),
                           reads=[b_pO[qs][1], b_rr], writes=[b_o2])
                    dve.op(lambda e: e.tensor_tensor(out=o1[:], in0=o1[:], in1=o2[:], op=ALU.subtract), reads=[b_o1, b_o2], writes=[b_o1])
                    dve.op(lambda e: e.memset(oms[:], 0.0), writes=[b_oms])
                    act.op(lambda e: e.activation(out=osq[:], in_=o1[:], func=AF.Square, accum_out=oms[:]), reads=[b_o1], writes=[b_osq, b_oms])
                    act.op(lambda e: e.activation(out=oms[:], in_=oms[:], func=AF.Sqrt, scale=1.0 / 128, bias=1e-6), reads=[b_oms], writes=[b_oms])
                    dve.op(lambda e: e.reciprocal(out=oms[:], in_=oms[:]), reads=[b_oms], writes=[b_oms])
                    dve.op(lambda e: e.tensor_scalar(out=o1[:], in0=o1[:], scalar1=oms[:, 0:1], scalar2=(1.0 - LAM_INIT), op0=ALU.mult, op1=ALU.mult),
                           reads=[b_o1, b_oms], writes=[b_o1])
                    yb_ = qt % 2
                    dve.op(lambda e: e.tensor_tensor(out=yst[:, yb_, :], in0=o1[:], in1=sgt[:], op=ALU.mult), reads=[b_o1, b_sg], writes=[b_yst[yb_]])
                    sp.dma(y_a[qt * 128:(qt + 1) * 128, h * 128:(h + 1) * 128], yst[:, yb_, :], reads=[b_yst[yb_]])
        K.barrier()


    mu_in = din("mu_rep", [128, RW_COLS])
    w0_in = din("w0_rep", [128, 2, 1024])
    a0_in = din("a0_rep", [128, 2, 1024])
    kk_in = din("kk_rep", [128, 1024])
    ka_in = din("ka_rep", [128, 1024])
    rk_in = din("rk_rep", [128, 1024])
    lnw_in = din("lnw_rep", [128, 1024])
    lnb_in = din("lnb_rep", [128, 1024])
    w2_in = din("w2_in", [4, 64, 1024])
    g2_in = din("g2_in", [160, 1024])
    masks_in = din("masks_in", [128, 4, 128])
    RWS = {n: dscr("rw_" + n, [T, 1024], F32, dbg=True) for n in
           ("R", "V", "KK", "LW0", "LW1", "BB0", "BB1", "KE0", "KE1", "G", "BON", "Y0", "Y1")}

    def rwkv_phase():
        for st in phase("3a"):
            P = sb(st, "rP", [128, 3, RW_COLS], BF16)
            xs = sb(st, "rxs", [128, RW_COLS], F32)
            tt_ = sb(st, "rtt", [128, RW_COLS], F32)
            mu = sb(st, "rmu", [128, RW_COLS], F32)
            w0 = sb(st, "rw0", [128, 2, 1024], F32)
            a0 = sb(st, "ra0", [128, 2, 1024], F32)
            kkc = sb(st, "rkkc", [128, 1024], F32)
            kac = sb(st, "rkac", [128, 1024], F32)
            rkc = sb(st, "rrkc", [128, 1024], F32)
            w2 = sb(st, "rw2", [64, 4, 1024], BF16)
            g2a = sb(st, "rg2a", [128, 1024], BF16)
            g2b = sb(st, "rg2b", [32, 1024], BF16)
            L = sb(st, "rL", [128, 416], BF16)
            LT = sb(st, "rLT", [128, 6, 128], BF16)
            asg = sb(st, "rasg", [128, 2, 1024], F32)
            o = [sb(st, "ro%d" % i, [128, 1024], F32) for i in range(4)]
            kk = sb(st, "rkk", [128, 1024], F32)
            ke = sb(st, "rke", [128, 2, 1024], F32)
            s16 = sb(st, "rs16", [128, 16], F32)
            pl = ps(st, "rpl", [128, 6, 128], BF16)
            pm = [ps(st, "rpm%d" % i, [128, 2, 512], F32) for i in range(2)]
            b_P = Buf(); b_Pz = Buf(); b_xs = Buf(); b_tt = Buf(); b_c = Buf(); b_L = Buf(); b_LT = Buf(); b_asg = Buf()
            b_o = [Buf() for _ in range(4)]; b_kk = Buf(); b_ke = Buf(); b_s16 = Buf(); b_pl = Buf(); b_pm = [Buf(), Buf()]
            sp.dma(mu[:], mu_in, writes=[b_c])
            sp.dma(w0[:], w0_in, pwrites=[b_c])
            sp.dma(a0[:], a0_in, pwrites=[b_c])
            sp.dma(kkc[:], kk_in, pwrites=[b_c])
            sp.dma(kac[:], ka_in, pwrites=[b_c])
            sp.dma(rkc[:], rk_in, pwrites=[b_c])
            pool.dma(w2[:], w2_in.rearrange("f k n -> k f n"), pwrites=[b_c])
            pool.dma(g2a[:], g2_in[0:128, :], pwrites=[b_c])
            pool.dma(g2b[:], g2_in[128:160, :], pwrites=[b_c])
            oi = [0]

            def outbuf():
                oi[0] += 1
                return oi[0] % 4

            def store(name, tt, i):
                sp.dma(RWS[name][tt * 128:(tt + 1) * 128, :], o[i][:], reads=[b_o[i]])

            for tt in range(NT):
                r0 = tt * 128
                dve.op(lambda e: e.memset(P[:, 1:3, :], 0.0), writes=[b_P, b_Pz])
                sp.dma(P[:, 0, :], proj_tm[r0:r0 + 128, DA_COLS:TM_COLS], pwrites=[b_P])
                if tt == 0:
                    sp.dma(P[1:128, 1, :], proj_tm[0:127, DA_COLS:TM_COLS], reads=[b_Pz], pwrites=[b_P])
                else:
                    sp.dma(P[:, 1, :], proj_tm[r0 - 1:r0 + 127, DA_COLS:TM_COLS], reads=[b_Pz], pwrites=[b_P])
                if tt == NT - 1:
                    sp.dma(P[0:127, 2, :], proj_tm[r0 + 1:r0 + 128, DA_COLS:TM_COLS], reads=[b_Pz], pwrites=[b_P])
                else:
                    sp.dma(P[:, 2, :], proj_tm[r0 + 1:r0 + 129, DA_COLS:TM_COLS], reads=[b_Pz], pwrites=[b_P])
                dve.op(lambda e: e.tensor_tensor(out=tt_[:], in0=P[:, 1, :], in1=P[:, 2, :], op=ALU.add), reads=[b_P], writes=[b_tt])
                dve.op(lambda e: e.scalar_tensor_tensor(out=tt_[:], in0=tt_[:], scalar=0.5, in1=P[:, 0, :], op0=ALU.mult, op1=ALU.subtract),
                       reads=[b_tt, b_P], writes=[b_tt])
                dve.op(lambda e: e.tensor_tensor(out=tt_[:], in0=tt_[:], in1=mu[:], op=ALU.mult), reads=[b_tt, b_c], writes=[b_tt])
                dve.op(lambda e: e.tensor_tensor(out=xs[:], in0=tt_[:], in1=P[:, 0, :], op=ALU.add), reads=[b_tt, b_P], writes=[b_xs])
                rr_ = xs[:, 0:1024]
                kk_ = xs[:, 1024:2048]
                vv_ = xs[:, 2048:3072]
                if CUT <= 1:
                    continue
                act.op(lambda e: e.activation(out=L[:, 0:128], in_=xs[:, 3072:3200], func=AF.Tanh), reads=[b_xs], writes=[b_L])
                act.op(lambda e: e.activation(out=L[:, 128:256], in_=xs[:, 3200:3328], func=AF.Copy), reads=[b_xs], pwrites=[b_L])
                act.op(lambda e: e.activation(out=L[:, 256:416], in_=xs[:, 3328:3488], func=AF.Sigmoid), reads=[b_xs], pwrites=[b_L])
                for i in range(4):
                    pe.op(lambda e: e.transpose(out=pl[0:64, i, :], in_=L[:, i * 64:(i + 1) * 64], identity=idb[:]),
                          reads=[b_L, b_idb], writes=[b_pl] if i == 0 else (), pwrites=() if i == 0 else [b_pl])
                pe.op(lambda e: e.transpose(out=pl[:, 4, :], in_=L[:, 256:384], identity=idb[:]), reads=[b_L, b_idb], pwrites=[b_pl])
                pe.op(lambda e: e.transpose(out=pl[0:32, 5, :], in_=L[:, 384:416], identity=idb[:]), reads=[b_L, b_idb], pwrites=[b_pl])
                dve.op(lambda e: e.tensor_copy(out=LT[0:64, 0:4, :], in_=pl[0:64, 0:4, :]), reads=[b_pl], writes=[b_LT])
                dve.op(lambda e: e.tensor_copy(out=LT[:, 4, :], in_=pl[:, 4, :]), reads=[b_pl], pwrites=[b_LT])
                dve.op(lambda e: e.tensor_copy(out=LT[0:32, 5, :], in_=pl[0:32, 5, :]), reads=[b_pl], pwrites=[b_LT])
                if CUT <= 2:
                    continue
                i = outbuf()
                dve.op(lambda e: e.tensor_copy(out=o[i][:], in_=rr_), reads=[b_xs], writes=[b_o[i]])
                store("R", tt, i)
                i = outbuf()
                dve.op(lambda e: e.tensor_copy(out=o[i][:], in_=vv_), reads=[b_xs], writes=[b_o[i]])
                store("V", tt, i)
                if CUT <= 3:
                    continue
                for d in range(2):
                    p = pm[d % 2]
                    for hf in range(2):
                        pe.op(lambda e: e.matmul(p[:, hf, :], lhsT=LT[0:64, d, :], rhs=w2[:, d, hf * 512:(hf + 1) * 512], start=True, stop=True),
                              reads=[b_LT, b_c], writes=[b_pm[d % 2]] if hf == 0 else (), pwrites=() if hf == 0 else [b_pm[d % 2]])
                    i = outbuf()
                    dve.op(lambda e: e.tensor_tensor(out=o[i][:], in0=p[:].rearrange("p a b -> p (a b)"), in1=w0[:, d, :], op=ALU.add),
                           reads=[b_pm[d % 2], b_c], writes=[b_o[i]])
                    act.op(lambda e: e.activation(out=o[i][:], in_=o[i][:], func=AF.Sigmoid), reads=[b_o[i]], writes=[b_o[i]])
                    dve.op(lambda e: e.tensor_scalar(out=o[i][:], in0=o[i][:], scalar1=-math.exp(-0.5), scalar2=None, op0=ALU.mult),
                           reads=[b_o[i]], writes=[b_o[i]])
                    store("LW%d" % d, tt, i)
                if CUT <= 4:
                    continue
                for d in range(2):
                    p = pm[d % 2]
                    for hf in range(2):
                        pe.op(lambda e: e.matmul(p[:, hf, :], lhsT=LT[0:64, 2 + d, :], rhs=w2[:, 2 + d, hf * 512:(hf + 1) * 512], start=True, stop=True),
                              reads=[b_LT, b_c], writes=[b_pm[d % 2]] if hf == 0 else (), pwrites=() if hf == 0 else [b_pm[d % 2]])
                    dve.op(lambda e: e.tensor_tensor(out=asg[:, d, :], in0=p[:].rearrange("p a b -> p (a b)"), in1=a0[:, d, :], op=ALU.add),
                           reads=[b_pm[d % 2], b_c], writes=[b_asg] if d == 0 else (), pwrites=() if d == 0 else [b_asg])
                act.op(lambda e: e.activation(out=asg[:], in_=asg[:], func=AF.Sigmoid), reads=[b_asg], writes=[b_asg])
                if CUT <= 5:
                    continue
                p = pm[0]
                for hf in range(2):
                    pe.op(lambda e: e.matmul(p[:, hf, :], lhsT=LT[:, 4, :], rhs=g2a[:, hf * 512:(hf + 1) * 512], start=True, stop=False),
                          reads=[b_LT, b_c], writes=[b_pm[0]] if hf == 0 else (), pwrites=() if hf == 0 else [b_pm[0]])
                    pe.op(lambda e: e.matmul(p[:, hf, :], lhsT=LT[0:32, 5, :], rhs=g2b[:, hf * 512:(hf + 1) * 512], start=False, stop=True),
                          reads=[b_LT, b_c], pwrites=[b_pm[0]])
                i = outbuf()
                act.op(lambda e: e.activation(out=o[i][:], in_=p[:].rearrange("p a b -> p (a b)"), func=AF.Copy), reads=[b_pm[0]], writes=[b_o[i]])
                store("G", tt, i)
                if CUT <= 6:
                    continue
                dve.op(lambda e: e.tensor_tensor(out=kk[:], in0=kk_, in1=kkc[:], op=ALU.mult), reads=[b_xs, b_c], writes=[b_kk])
                dve.op(lambda e: e.tensor_tensor(out=tt_[:, 0:1024], in0=kk[:], in1=kk[:], op=ALU.mult), reads=[b_kk], writes=[b_tt])
                dve.op(lambda e: e.tensor_reduce(out=s16[:], in_=tt_[:, 0:1024].rearrange("p (h d) -> p h d", d=64), axis=AX.X, op=ALU.add),
                       reads=[b_tt], writes=[b_s16])
                act.op(lambda e: e.activation(out=s16[:], in_=s16[:], func=AF.Sqrt), reads=[b_s16], writes=[b_s16])
                dve.op(lambda e: e.tensor_scalar(out=s16[:], in0=s16[:], scalar1=1e-12, scalar2=None, op0=ALU.max), reads=[b_s16], writes=[b_s16])
                dve.op(lambda e: e.reciprocal(out=s16[:], in_=s16[:]), reads=[b_s16], writes=[b_s16])
                i = outbuf()
                dve.op(lambda e: e.tensor_tensor(out=o[i][:].rearrange("p (h d) -> p h d", d=64), in0=kk[:].rearrange("p (h d) -> p h d", d=64),
                                                 in1=s16[:].unsqueeze(2).broadcast_to([128, 16, 64]), op=ALU.mult),
                       reads=[b_kk, b_s16], writes=[b_o[i]])
                ikk = i
                for d in range(2):
                    i = outbuf()
                    dve.op(lambda e: e.tensor_tensor(out=o[i][:], in0=o[ikk][:], in1=asg[:, d, :], op=ALU.mult), reads=[b_o[ikk], b_asg], writes=[b_o[i]])
                    store("BB%d" % d, tt, i)
                dve.op(lambda e: e.tensor_scalar(out=o[ikk][:], in0=o[ikk][:], scalar1=-1.0, scalar2=None, op0=ALU.mult), reads=[b_o[ikk]], writes=[b_o[ikk]])
                store("KK", tt, ikk)
                for d in range(2):
                    dve.op(lambda e: e.tensor_tensor(out=ke[:, d, :], in0=asg[:, d, :], in1=kac[:], op=ALU.mult),
                           reads=[b_asg, b_c], writes=[b_ke] if d == 0 else (), pwrites=() if d == 0 else [b_ke])
                    dve.op(lambda e: e.tensor_tensor(out=ke[:, d, :], in0=ke[:, d, :], in1=kac[:], op=ALU.subtract),
                           reads=[b_ke, b_c], writes=[b_ke])
                    dve.op(lambda e: e.tensor_tensor(out=ke[:, d, :], in0=ke[:, d, :], in1=kk_, op=ALU.mult),
                           reads=[b_ke, b_xs], writes=[b_ke])
                    dve.op(lambda e: e.tensor_tensor(out=ke[:, d, :], in0=ke[:, d, :], in1=kk_, op=ALU.add),
                           reads=[b_ke, b_xs], writes=[b_ke])
                    i = outbuf()
                    dve.op(lambda e: e.tensor_copy(out=o[i][:], in_=ke[:, d, :]), reads=[b_ke], writes=[b_o[i]])
                    store("KE%d" % d, tt, i)
                if CUT <= 7:
                    continue
                dve.op(lambda e: e.tensor_tensor(out=tt_[:, 0:1024], in0=ke[:, 0, :], in1=ke[:, 1, :], op=ALU.add), reads=[b_ke], writes=[b_tt])
                dve.op(lambda e: e.tensor_tensor(out=tt_[:, 0:1024], in0=tt_[:, 0:1024], in1=rr_, op=ALU.mult), reads=[b_tt, b_xs], writes=[b_tt])
                dve.op(lambda e: e.tensor_tensor(out=tt_[:, 0:1024], in0=tt_[:, 0:1024], in1=rkc[:], op=ALU.mult), reads=[b_tt, b_c], writes=[b_tt])
                dve.op(lambda e: e.tensor_reduce(out=s16[:], in_=tt_[:, 0:1024].rearrange("p (h d) -> p h d", d=64), axis=AX.X, op=ALU.add),
                       reads=[b_tt], writes=[b_s16])
                i = outbuf()
                dve.op(lambda e: e.tensor_tensor(out=o[i][:].rearrange("p (h d) -> p h d", d=64), in0=vv_.rearrange("p (h d) -> p h d", d=64),
                                                 in1=s16[:].unsqueeze(2).broadcast_to([128, 16, 64]), op=ALU.mult),
                       reads=[b_xs, b_s16], writes=[b_o[i]])
                store("BON", tt, i)
            K.barrier()

        for st in phase("3b"):
            mk = sb(st, "smk", [128, 4, 128], F32)
            ld = sb(st, "sld", [128, 2, 6, 1024], F32)
            e4 = sb(st, "se4", [128, 4, 1024], F32)
            bfs = sb(st, "sbfs", [128, 7, 1024], BF16)
            dG = sb(st, "sdG", [64, 1024], F32)
            XTa = sb(st, "sXTa", [64, 4, 4, 4, 128], BF16)
            pr5a = sb(st, "spr5a", [128, 4, 5, 4, 128], BF16)
            Xp2 = sb(st, "sXp2", [128, 2, 2, 4, 192], BF16)
            Pp2 = sb(st, "sPp2", [128, 2, 2, 2, 4, 128], BF16)
            Rh = sb(st, "sRh", [64, 16, 128], BF16)
            Qm = sb(st, "sQm", [128, 16, 128], BF16)
            Gm = sb(st, "sGm", [64, 16, 64], BF16)
            Hm = sb(st, "sHm", [128, 16, 64], BF16)
            ST = sb(st, "sST", [64, 16, 64], BF16)
            yo = sb(st, "syo", [128, 2, 1024], F32)
            pT = [ps(st, "spT%d" % i, [128, 8, 128], BF16) for i in range(2)]
            pA = ps(st, "spA", [128, 2, 512], F32)
            pB = ps(st, "spB", [128, 2, 512], F32)
            pX = ps(st, "spX", [128, 2, 512], F32)
            b_mk = Buf(); b_ld = [Buf(), Buf()]; b_e4 = Buf(); b_bfs = Buf(); b_dG = Buf(); b_XT = [Buf() for _ in range(4)]; b_pr5 = [Buf() for _ in range(4)]
            b_Xp2 = [[Buf(), Buf()], [Buf(), Buf()]]; b_Pp2 = [[Buf(), Buf()], [Buf(), Buf()]]; b_Rh = Buf(); b_Qm = Buf(); b_Gm = Buf(); b_Hm = Buf(); b_ST = Buf()
            b_yo = [Buf(), Buf()]; b_pT = [Buf(), Buf()]; b_pA = [Buf(), Buf()]; b_pB = [Buf(), Buf()]; b_pX = [Buf(), Buf()]
            slot = [(pA, 0, b_pA[0]), (pA, 1, b_pA[1]), (pB, 0, b_pB[0]), (pB, 1, b_pB[1]), (pX, 0, b_pX[0]), (pX, 1, b_pX[1])]

            def sl(i, shape3):
                t_, j, b = slot[i]
                a, bb_ = shape3
                return t_[:, j, 0:a * bb_].rearrange("p (a b) -> p a b", b=bb_), b

            sp.dma(mk[:], masks_in, writes=[b_mk])
            SU, SL_, UI, LI = 0, 1, 2, 3
            li = 0
            yi = 0
            for d in range(2):
                cum_i, cum_s, mb_s, mb_i, ma_s = (UI, SL_, SU, UI, SL_) if d == 0 else (LI, SU, SL_, LI, SU)
                dve.op(lambda e: e.memset(ST[:], 0.0), writes=[b_ST])
                corder = range(NT) if d == 0 else range(NT - 1, -1, -1)
                names = ("R", "V", "KK", "LW%d" % d, "BB%d" % d, "KE%d" % d)
                for c in corder:
                    lb = li % 2
                    li += 1
                    for qi, nm in enumerate(names):
                        sp.dma(ld[:, lb, qi, :], RWS[nm][c * 128:(c + 1) * 128, :],
                               writes=[b_ld[lb]] if qi == 0 else (), pwrites=() if qi == 0 else [b_ld[lb]])
                    r_, v_, kk_, lw_, bb_, ke_ = (ld[:, lb, qi, :] for qi in range(6))
                    for hf in range(2):
                        pe.op(lambda e: e.matmul(pA[:, hf, :], lhsT=mk[:, cum_i, :], rhs=lw_[:, hf * 512:(hf + 1) * 512], start=True, stop=True),
                              reads=[b_mk, b_ld[lb]], writes=[b_pA[hf]])
                        pe.op(lambda e: e.matmul(pB[:, hf, :], lhsT=mk[:, cum_s, :], rhs=lw_[:, hf * 512:(hf + 1) * 512], start=True, stop=True),
                              reads=[b_mk, b_ld[lb]], writes=[b_pB[hf]])
                    gam = pA[:].rearrange("p a b -> p (a b)")
                    gsf = pB[:].rearrange("p a b -> p (a b)")
                    act.op(lambda e: e.activation(out=e4[:, 0, :], in_=gam, func=AF.Exp), reads=b_pA, writes=[b_e4])
                    act.op(lambda e: e.activation(out=e4[:, 2, :], in_=gam, func=AF.Exp, scale=-1.0), reads=b_pA, pwrites=[b_e4])
                    act.op(lambda e: e.activation(out=e4[:, 3, :], in_=gsf, func=AF.Exp), reads=b_pB, pwrites=[b_e4])
                    act.op(lambda e: e.activation(out=e4[:, 1, :], in_=lw_, func=AF.Exp, scale=-1.0), reads=[b_ld[lb]], pwrites=[b_e4])
                    dve.op(lambda e: e.tensor_tensor(out=e4[:, 1, :], in0=e4[:, 1, :], in1=e4[:, 0, :], op=ALU.mult), reads=[b_e4], pwrites=[b_e4])
                    dve.op(lambda e: e.tensor_tensor(out=dG[:], in0=e4[0:64, 0, :], in1=e4[0:64, 3, :], op=ALU.mult),
                           reads=[b_e4], writes=[b_dG])
                    dve.op(lambda e: e.tensor_tensor(out=dG[:].rearrange("p (h k) -> p h k", k=64), in0=dG[:].rearrange("p (h k) -> p h k", k=64),
                                                     in1=idf[0:64, 0:64].unsqueeze(1).broadcast_to([64, 16, 64]), op=ALU.mult),
                           reads=[b_dG, b_idf], writes=[b_dG])
                    dve.op(lambda e: e.tensor_tensor(out=bfs[:, 0, :], in0=kk_, in1=e4[:, 1, :], op=ALU.mult),
                           reads=[b_ld[lb], b_e4], writes=[b_bfs])
                    dve.op(lambda e: e.tensor_tensor(out=bfs[:, 1, :], in0=bb_, in1=e4[:, 2, :], op=ALU.mult), reads=[b_ld[lb], b_e4], pwrites=[b_bfs])
                    dve.op(lambda e: e.tensor_tensor(out=bfs[:, 2, :], in0=ke_, in1=e4[:, 2, :], op=ALU.mult), reads=[b_ld[lb], b_e4], pwrites=[b_bfs])
                    dve.op(lambda e: e.tensor_tensor(out=bfs[:, 3, :], in0=r_, in1=e4[:, 0, :], op=ALU.mult), reads=[b_ld[lb], b_e4], pwrites=[b_bfs])
                    dve.op(lambda e: e.tensor_tensor(out=bfs[:, 4, :], in0=bb_, in1=e4[:, 3, :], op=ALU.mult), reads=[b_ld[lb], b_e4], pwrites=[b_bfs])
                    dve.op(lambda e: e.tensor_tensor(out=bfs[:, 5, :], in0=ke_, in1=e4[:, 3, :], op=ALU.mult), reads=[b_ld[lb], b_e4], pwrites=[b_bfs])
                    act.op(lambda e: e.activation(out=bfs[:, 6, :], in_=v_, func=AF.Copy), reads=[b_ld[lb], b_bfs], pwrites=[b_bfs])
                    A_, B_, K_, R_ = 0, 1, 2, 3
                    prods = [(B_, A_, mb_s), (A_, B_, ma_s), (A_, K_, ma_s), (B_, R_, mb_i), (K_, R_, mb_i)]
                    for g4 in range(4):
                        for hh in range(4):
                            h = g4 * 4 + hh
                            tb_ = hh // 2
                            for kd in range(4):
                                first = (hh % 2 == 0 and kd == 0)
                                pe.op(lambda e: e.transpose(out=pT[tb_][0:64, (hh % 2) * 4 + kd, :], in_=bfs[:, kd, h * 64:(h + 1) * 64], identity=idb[:]),
                                      reads=[b_bfs, b_idb], writes=[b_pT[tb_]] if first else (), pwrites=() if first else [b_pT[tb_]])
                        for tb_ in range(2):
                            evac_copy(XTa[:, g4, tb_ * 2:(tb_ + 1) * 2, :, :], pT[tb_][0:64].rearrange("p (a k) t -> p a k t", k=4), reads=[b_pT[tb_]],
                                      writes=[b_XT[g4]] if tb_ == 0 else (), pwrites=() if tb_ == 0 else [b_XT[g4]])
                        for pi_, (l_, r2_, m_) in enumerate(prods):
                            ap3, bslot = sl(pi_, (4, 128))
                            for hh in range(4):
                                pe.op(lambda e: e.matmul(ap3[:, hh, :], lhsT=XTa[:, g4, hh, l_, :], rhs=XTa[:, g4, hh, r2_, :], start=True, stop=True),
                                      reads=[b_XT[g4]], writes=[bslot] if hh == 0 else (), pwrites=() if hh == 0 else [bslot])
                            dve.op(lambda e: e.tensor_tensor(out=pr5a[:, g4, pi_, :, :], in0=ap3, in1=mk[:, m_, :].unsqueeze(1).broadcast_to([128, 4, 128]), op=ALU.mult),
                                   reads=[bslot, b_mk], writes=[b_pr5[g4]] if pi_ == 0 else (), pwrites=() if pi_ == 0 else [b_pr5[g4]])

                    def neumann(g4, side):
                        pr5 = pr5a[:, g4]
                        bpr = b_pr5[g4]
                        Xp_ = Xp2[:, side]
                        bXp_ = b_Xp2[side]
                        Pp_ = Pp2[:, side]
                        bPp_ = b_Pp2[side]
                        if side == 0:
                            xa = pX[:].rearrange("p a (h x) -> p (a h) x", x=256)[:, :, 0:192]
                            bxa = [b_pX[0], b_pX[1]]
                            s0, bs0 = sl(0, (4, 128))
                            s1, bs1 = sl(1, (4, 128))
                        else:
                            xa = pB[:].rearrange("p a (h x) -> p (a h) x", x=256)[:, :, 0:192]
                            bxa = [b_pB[0], b_pB[1]]
                            s0 = pT[0][:].rearrange("p a b -> p (a b)").bitcast(F32).rearrange("p (a b) -> p a b", b=128)
                            s1 = pT[1][:].rearrange("p a b -> p (a b)").bitcast(F32).rearrange("p (a b) -> p a b", b=128)
                            bs0, bs1 = b_pT[0], b_pT[1]
                        act.op(lambda e: e.activation(out=Xp_[:, 0, :, 0:64], in_=bfs[:, 0, g4 * 256:(g4 + 1) * 256].rearrange("p (h k) -> p h k", k=64), func=AF.Copy),
                               reads=[b_bfs], writes=[bXp_[0]])
                        act.op(lambda e: e.activation(out=Xp_[:, 0, :, 64:192], in_=pr5[:, 2, :, :], func=AF.Copy), reads=[bpr], pwrites=[bXp_[0]])
                        xi = 0
                        for it in range(7):
                            if it == 0:
                                Pc, PTc, bP = pr5[:, 0], pr5[:, 1], bpr
                            else:
                                Pc, PTc, bP = Pp_[:, it % 2, 0], Pp_[:, it % 2, 1], bPp_[it % 2]
                            for hh in range(4):
                                pe.op(lambda e: e.matmul(xa[:, hh, :], lhsT=Pc[:, hh, :], rhs=Xp_[:, xi, hh, :], start=True, stop=True),
                                      reads=[bP, bXp_[xi]], writes=bxa if hh == 0 else (), pwrites=() if hh == 0 else bxa)
                            dve.op(lambda e: e.tensor_tensor(out=Xp_[:, 1 - xi, :, :], in0=xa, in1=Xp_[:, xi, :, :], op=ALU.add),
                                   reads=bxa + [bXp_[xi]], writes=[bXp_[1 - xi]])
                            xi = 1 - xi
                            if it < 6:
                                nP = (it + 1) % 2
                                for hh in range(4):
                                    pe.op(lambda e: e.matmul(s0[:, hh, :], lhsT=PTc[:, hh, :], rhs=Pc[:, hh, :], start=True, stop=True),
                                          reads=[bP], writes=[bs0] if hh == 0 else (), pwrites=() if hh == 0 else [bs0])
                                for hh in range(4):
                                    pe.op(lambda e: e.matmul(s1[:, hh, :], lhsT=Pc[:, hh, :], rhs=PTc[:, hh, :], start=True, stop=True),
                                          reads=[bP], writes=[bs1] if hh == 0 else (), pwrites=() if hh == 0 else [bs1])
                                act.op(lambda e: e.activation(out=Pp_[:, nP, 0], in_=s0, func=AF.Copy), reads=[bs0], writes=[bPp_[nP]])
                                dve.op(lambda e: e.tensor_copy(out=Pp_[:, nP, 1], in_=s1), reads=[bs1], pwrites=[bPp_[nP]])
                            yield xi
                    finals = {}
                    for (ga, gb) in ((0, 1), (2, 3)):
                        gA = neumann(ga, 0)
                        gB = neumann(gb, 1)
                        for it in range(7):
                            finals[ga] = (0, next(gA))
                            finals[gb] = (1, next(gB))
                        for g4 in (ga, gb):
                            side, xi = finals[g4]
                            Xf = Xp2[:, side, xi]
                            bXf = b_Xp2[side][xi]
                            pr5 = pr5a[:, g4]
                            bpr = b_pr5[g4]
                            hs = slice(g4 * 4, g4 * 4 + 4)
                            a2_, bs2 = sl(0, (4, 128))
                            for hh in range(4):
                                pe.op(lambda e: e.matmul(a2_[0:64, hh, :], lhsT=Xf[:, hh, 0:64], rhs=pr5[:, 3, hh, :], start=True, stop=True),
                                      reads=[bXf, bpr], writes=[bs2] if hh == 0 else (), pwrites=() if hh == 0 else [bs2])
                            dve.op(lambda e: e.tensor_tensor(out=Rh[:, hs, :], in0=a2_[0:64], in1=XTa[:, g4, :, 3, :], op=ALU.add),
                                   reads=[bs2, b_XT[g4]], pwrites=[b_Rh])
                            a3_, bs3 = sl(1, (4, 128))
                            for hh in range(4):
                                pe.op(lambda e: e.matmul(a3_[:, hh, :], lhsT=Xf[:, hh, 64:192], rhs=pr5[:, 3, hh, :], start=True, stop=True),
                                      reads=[bXf, bpr], writes=[bs3] if hh == 0 else (), pwrites=() if hh == 0 else [bs3])
                            dve.op(lambda e: e.tensor_tensor(out=Qm[:, hs, :], in0=a3_, in1=pr5[:, 4, :, :], op=ALU.add),
                                   reads=[bs3, bpr], pwrites=[b_Qm])
                            a0_, bs0 = sl(4, (4, 64))
                            a1_, bs1 = sl(5, (4, 64))
                            for hh in range(4):
                                h = g4 * 4 + hh
                                pe.op(lambda e: e.matmul(a0_[0:64, hh, :], lhsT=Xf[:, hh, 0:64], rhs=bfs[:, 4, h * 64:(h + 1) * 64], start=True, stop=True),
                                      reads=[bXf, b_bfs], writes=[bs0] if hh == 0 else (), pwrites=() if hh == 0 else [bs0])
                            for hh in range(4):
                                h = g4 * 4 + hh
                                pe.op(lambda e: e.matmul(a1_[:, hh, :], lhsT=Xf[:, hh, 64:192], rhs=bfs[:, 4, h * 64:(h + 1) * 64], start=True, stop=True),
                                      reads=[bXf, b_bfs], writes=[bs1] if hh == 0 else (), pwrites=() if hh == 0 else [bs1])
                            dve.op(lambda e: e.tensor_tensor(out=Gm[:, hs, :], in0=a0_[0:64], in1=dG[:, g4 * 256:(g4 + 1) * 256].rearrange("p (h k) -> p h k", k=64), op=ALU.add),
                                   reads=[bs0, b_dG], pwrites=[b_Gm])
                            dve.op(lambda e: e.tensor_tensor(out=Hm[:, hs, :], in0=a1_, in1=bfs[:, 5, g4 * 256:(g4 + 1) * 256].rearrange("p (h k) -> p h k", k=64), op=ALU.add),
                                   reads=[bs1, b_bfs], pwrites=[b_Hm])
                    yv = pA[:].rearrange("p a (h v) -> p (a h) v", v=64)
                    sv = pB[0:64].rearrange("p a (h v) -> p (a h) v", v=64)
                    for h in range(16):
                        vh = bfs[:, 6, h * 64:(h + 1) * 64]
                        pe.op(lambda e: e.matmul(yv[:, h, :], lhsT=Rh[:, h, :], rhs=ST[:, h, :], start=True, stop=False),
                              reads=[b_Rh, b_ST], writes=b_pA if h == 0 else (), pwrites=() if h == 0 else b_pA)
                        pe.op(lambda e: e.matmul(yv[:, h, :], lhsT=Qm[:, h, :], rhs=vh, start=False, stop=True),
                              reads=[b_Qm, b_bfs], pwrites=b_pA)
                        pe.op(lambda e: e.matmul(sv[:, h, :], lhsT=Gm[:, h, :], rhs=ST[:, h, :], start=True, stop=False),
                              reads=[b_Gm, b_ST], writes=b_pB if h == 0 else (), pwrites=() if h == 0 else b_pB)
                        pe.op(lambda e: e.matmul(sv[:, h, :], lhsT=Hm[:, h, :], rhs=vh, start=False, stop=True),
                              reads=[b_Hm, b_bfs], pwrites=b_pB)
                    yb2 = yi % 2
                    yi += 1
                    act.op(lambda e: e.activation(out=yo[:, yb2, :], in_=pA[:].rearrange("p a b -> p (a b)"), func=AF.Copy), reads=b_pA, writes=[b_yo[yb2]])
                    dve.op(lambda e: e.tensor_copy(out=ST[:], in_=sv), reads=b_pB, writes=[b_ST])
                    sp.dma(RWS["Y%d" % d][c * 128:(c + 1) * 128, :], yo[:, yb2, :], reads=[b_yo[yb2]])
            K.barrier()

        for st in phase("3c"):
            ld = sb(st, "cld", [128, 2, 4, 1024], F32)
            lw_ = sb(st, "clw", [128, 1024], F32)
            lb_ = sb(st, "clb", [128, 1024], F32)
            y = sb(st, "cy", [128, 1024], F32)
            sq = sb(st, "csq", [128, 1024], F32)
            m16 = sb(st, "cm16", [128, 16], F32)
            v16 = sb(st, "cv16", [128, 16], F32)
            yb16 = sb(st, "cyb", [128, 2, 1024], BF16)
            b_ld = [Buf(), Buf()]; b_c = Buf(); b_y = Buf(); b_sq = Buf(); b_m = Buf(); b_v = Buf(); b_yb = [Buf(), Buf()]
            sp.dma(lw_[:], lnw_in, writes=[b_c])
            sp.dma(lb_[:], lnb_in, pwrites=[b_c])
            h3 = lambda ap: ap.rearrange("p (h d) -> p h d", d=64)
            for tt in range(NT):
                lb = tt % 2
                for qi, nm in enumerate(("Y0", "Y1", "BON", "G")):
                    sp.dma(ld[:, lb, qi, :], RWS[nm][tt * 128:(tt + 1) * 128, :], writes=[b_ld[lb]] if qi == 0 else (), pwrites=() if qi == 0 else [b_ld[lb]])
                dve.op(lambda e: e.tensor_tensor(out=y[:], in0=ld[:, lb, 0, :], in1=ld[:, lb, 1, :], op=ALU.add), reads=[b_ld[lb]], writes=[b_y])
                dve.op(lambda e: e.tensor_reduce(out=m16[:], in_=h3(y[:]), axis=AX.X, op=ALU.add), reads=[b_y], writes=[b_m])
                dve.op(lambda e: e.tensor_scalar(out=m16[:], in0=m16[:], scalar1=1.0 / 64, scalar2=None, op0=ALU.mult), reads=[b_m], writes=[b_m])
                dve.op(lambda e: e.tensor_tensor(out=h3(y[:]), in0=h3(y[:]), in1=m16[:].unsqueeze(2).broadcast_to([128, 16, 64]), op=ALU.subtract),
                       reads=[b_y, b_m], writes=[b_y])
                dve.op(lambda e: e.tensor_tensor(out=sq[:], in0=y[:], in1=y[:], op=ALU.mult), reads=[b_y], writes=[b_sq])
                dve.op(lambda e: e.tensor_reduce(out=v16[:], in_=h3(sq[:]), axis=AX.X, op=ALU.add), reads=[b_sq], writes=[b_v])
                act.op(lambda e: e.activation(out=v16[:], in_=v16[:], func=AF.Sqrt, scale=1.0 / 64, bias=64e-5), reads=[b_v], writes=[b_v])
                dve.op(lambda e: e.reciprocal(out=v16[:], in_=v16[:]), reads=[b_v], writes=[b_v])
                dve.op(lambda e: e.tensor_tensor(out=h3(y[:]), in0=h3(y[:]), in1=v16[:].unsqueeze(2).broadcast_to([128, 16, 64]), op=ALU.mult),
                       reads=[b_y, b_v], writes=[b_y])
                dve.op(lambda e: e.tensor_tensor(out=y[:], in0=y[:], in1=lw_[:], op=ALU.mult), reads=[b_y, b_c], writes=[b_y])
                dve.op(lambda e: e.tensor_tensor(out=y[:], in0=y[:], in1=lb_[:], op=ALU.add), reads=[b_y, b_c], writes=[b_y])
                dve.op(lambda e: e.tensor_tensor(out=y[:], in0=y[:], in1=ld[:, lb, 2, :], op=ALU.add), reads=[b_y, b_ld[lb]], writes=[b_y])
                dve.op(lambda e: e.tensor_tensor(out=yb16[:, lb, :], in0=y[:], in1=ld[:, lb, 3, :], op=ALU.mult), reads=[b_y, b_ld[lb]], writes=[b_yb[lb]])
                sp.dma(y_b[tt * 128:(tt + 1) * 128, :], yb16[:, lb, :], reads=[b_yb[lb]])
            K.barrier()

    if SKIP_RWKV:
        for st in phase("zero"):
            z = sb(st, "zz", [128, 1024], BF16)
            b_z = Buf()
            dve.op(lambda e: e.memset(z[:], 0.0), writes=[b_z])
            for tt in range(NT):
                sp.dma(y_b[tt * 128:(tt + 1) * 128, :], z[:], reads=[b_z])
            K.barrier()
    else:
        rwkv_phase()

    for st in phase("4a"):
        Wa = sb(st, "Wa", [128, 8, D], BF16)
        Wb = sb(st, "Wb", [128, 8, D], BF16)
        yt = sb(st, "yt", [128, 2, 1024], BF16)
        yT = sb(st, "yT", [128, 2, 8, 512], BF16)
        gt = sb(st, "gt", [128, 2, 2, 512], BF16)
        ma = sb(st, "ma", [128, 512], F32)
        mbt = sb(st, "mbt", [128, 512], F32)
        mst = sb(st, "mst", [128, 2, 512], BF16)
        pt = [ps(st, "pt4a", [128, 8, 128], BF16), ps(st, "pt4b", [128, 8, 128], BF16)]
        pm = [ps(st, "pm4_%d" % i, [128, 512], F32) for i in range(4)]
        b_W = Buf(); b_yt = [Buf(), Buf()]; b_yT = [Buf(), Buf()]; b_gt = [Buf(), Buf()]; b_ma = Buf(); b_mb = Buf()
        b_mst = [Buf(), Buf()]; b_pt = [Buf(), Buf()]; b_pm = [Buf() for _ in range(4)]
        pool.dma(Wa[:], w_ba.rearrange("(c p) n -> p c n", p=128), writes=[b_W])
        pool.dma(Wb[:], w_bb.rearrange("(c p) n -> p c n", p=128), pwrites=[b_W])
        li = 0
        gi = 0
        for tb in range(T // 512):
            for br, ysrc in enumerate((y_a, y_b)):
                for t4 in range(4):
                    tt = tb * 4 + t4
                    lb = li % 2
                    li += 1
                    sp.dma(yt[:, lb, :], ysrc[tt * 128:(tt + 1) * 128, :], writes=[b_yt[lb]])
                    for c in range(8):
                        pe.op(lambda e: e.transpose(out=pt[lb][:, c, :], in_=yt[:, lb, c * 128:(c + 1) * 128], identity=idb[:]),
                              reads=[b_yt[lb], b_idb], writes=[b_pt[lb]] if c == 0 else (), pwrites=() if c == 0 else [b_pt[lb]])
                    evac_copy(yT[:, br, :, t4 * 128:(t4 + 1) * 128], pt[lb][:], reads=[b_pt[lb]],
                              writes=[b_yT[br]] if t4 == 0 else (), pwrites=() if t4 == 0 else [b_yT[br]])
            for fb in range(16):
                gb = gi % 2
                gi += 1
                sp.dma(gt[:, gb, 0, :], gateT[fb * 128:(fb + 1) * 128, tb * 512:(tb + 1) * 512], writes=[b_gt[gb]])
                sp.dma(gt[:, gb, 1, :], gateT[D + fb * 128:D + (fb + 1) * 128, tb * 512:(tb + 1) * 512], pwrites=[b_gt[gb]])
                pa = (2 * fb) % 4
                pb = (2 * fb + 1) % 4
                mm_group(pm[pa][:], b_pm[pa], [(Wa[:, c, fb * 128:(fb + 1) * 128], yT[:, 0, c, :]) for c in range(8)], reads=[b_W, b_yT[0]])
                mm_group(pm[pb][:], b_pm[pb], [(Wb[:, c, fb * 128:(fb + 1) * 128], yT[:, 1, c, :]) for c in range(8)], reads=[b_W, b_yT[1]])
                dve.op(lambda e: e.tensor_tensor(out=ma[:], in0=pm[pa][:], in1=gt[:, gb, 0, :], op=ALU.mult), reads=[b_pm[pa], b_gt[gb]], writes=[b_ma])
                dve.op(lambda e: e.tensor_tensor(out=mbt[:], in0=pm[pb][:], in1=gt[:, gb, 1, :], op=ALU.mult), reads=[b_pm[pb], b_gt[gb]], writes=[b_mb])
                dve.op(lambda e: e.tensor_tensor(out=mst[:, gb, :], in0=ma[:], in1=mbt[:], op=ALU.add), reads=[b_ma, b_mb], writes=[b_mst[gb]])
                sp.dma(mT_d[fb * 128:(fb + 1) * 128, tb * 512:(tb + 1) * 512], mst[:, gb, :], reads=[b_mst[gb]])
        K.barrier()

    aff = sb(gst, "aff", [128, NT, NE], F32)
    b_aff = Buf()
    for st in phase("4b"):
        Wo = sb(st, "Wo", [128, 16, D], BF16)
        Wr = sb(st, "Wr", [128, 16, NE], BF16)
        g2 = sb(st, "g2", [128, 16], F32)
        mT = sb(st, "mT", [128, 2, 16, 512], BF16)
        xt = sb(st, "xt4", [128, 2, D], F32)
        ht = sb(st, "ht", [128, 2, D], F32)
        junk = sb(st, "junk4", [128, D], BF16)
        xn = sb(st, "xn4", [128, D], BF16)
        ssq = sb(st, "ssq4", [128, 1], F32)
        hst = sb(st, "hst", [128, 2, 16, 512], BF16)
        lg = sb(st, "lg", [128, NE], F32)
        mx = sb(st, "mx", [128, 1], F32)
        sm = sb(st, "sm", [128, 1], F32)
        pt = [ps(st, "pt5a", [128, 8, 128], BF16), ps(st, "pt5b", [128, 8, 128], BF16)]
        pm = [ps(st, "pm5_%d" % i, [128, 512], F32) for i in range(4)]
        pr = ps(st, "pr5", [128, NE], F32)
        b_Wo = Buf(); b_Wr = Buf(); b_g2 = Buf(); b_mT = [Buf(), Buf()]; b_xt = [Buf(), Buf()]; b_ht = [Buf(), Buf()]
        b_junk = Buf(); b_xn = Buf(); b_ssq = Buf(); b_hst = [Buf(), Buf()]; b_lg = Buf(); b_mx = Buf(); b_sm = Buf()
        b_pt = [Buf(), Buf()]; b_pm = [Buf() for _ in range(4)]; b_pr = Buf()
        pool.dma(Wo[:], w_o.rearrange("(c p) n -> p c n", p=128), writes=[b_Wo])
        pool.dma(Wr[:], w_r.rearrange("(c p) n -> p c n", p=128), writes=[b_Wr])
        sp.dma(g2[:], g2T, writes=[b_g2])
        pi = 0
        for tb in range(T // 512):
            mb = tb % 2
            sp.dma(mT[:, mb, :, :], mT_d.rearrange("(c p) t -> p c t", p=128)[:, :, tb * 512:(tb + 1) * 512], writes=[b_mT[mb]])
            for t4 in range(4):
                tt = tb * 4 + t4
                bi = tt % 2
                sp.dma(xt[:, bi, :], x[tt * 128:(tt + 1) * 128, :], writes=[b_xt[bi]])
                for cc in range(4):
                    p = pi % 4
                    pi += 1
                    mm_group(pm[p][:], b_pm[p],
                             [(mT[:, mb, kc, t4 * 128:(t4 + 1) * 128], Wo[:, kc, cc * 512:(cc + 1) * 512]) for kc in range(16)],
                             reads=[b_mT[mb], b_Wo])
                    dve.op(lambda e: e.tensor_tensor(out=ht[:, bi, cc * 512:(cc + 1) * 512], in0=pm[p][:], in1=xt[:, bi, cc * 512:(cc + 1) * 512], op=ALU.add),
                           reads=[b_pm[p], b_xt[bi]], writes=[b_ht[bi]] if cc == 0 else (), pwrites=() if cc == 0 else [b_ht[bi]])
                sp.dma(out[tt * 128:(tt + 1) * 128, :], ht[:, bi, :], reads=[b_ht[bi]])
                rmsnorm_T(st, "p4", ht[:, bi, :], b_ht[bi], g2, b_g2, hst[:, mb, :, t4 * 128:(t4 + 1) * 128], b_hst[mb],
                          pt, b_pt, (junk, b_junk, ssq, b_ssq, xn, b_xn), store_ap=xn2_d[tt * 128:(tt + 1) * 128, :])
                mm_group(pr[:], b_pr, [(hst[:, mb, kc, t4 * 128:(t4 + 1) * 128], Wr[:, kc, :]) for kc in range(16)], reads=[b_hst[mb], b_Wr])
                dve.op(lambda e: e.tensor_reduce(out=mx[:], in_=pr[:], axis=AX.X, op=ALU.max), reads=[b_pr], writes=[b_mx])
                dve.op(lambda e: e.tensor_scalar(out=mx[:], in0=mx[:], scalar1=-1.0, scalar2=None, op0=ALU.mult), reads=[b_mx], writes=[b_mx])
                dve.op(lambda e: e.memset(sm[:], 0.0), writes=[b_sm])
                act.op(lambda e: e.activation(out=lg[:], in_=pr[:], func=AF.Exp, bias=mx[:, 0:1], accum_out=sm[:]),
                       reads=[b_pr, b_mx], writes=[b_lg, b_sm])
                dve.op(lambda e: e.reciprocal(out=sm[:], in_=sm[:]), reads=[b_sm], writes=[b_sm])
                dve.op(lambda e: e.tensor_scalar(out=aff[:, tt, :], in0=lg[:], scalar1=sm[:, 0:1], scalar2=None, op0=ALU.mult),
                       reads=[b_lg, b_sm], pwrites=[b_aff])
            sp.dma(hn2T_d.rearrange("(c p) t -> p c t", p=128)[:, :, tb * 512:(tb + 1) * 512], hst[:, mb, :, :], reads=[b_hst[mb]])
        if DEBUG:
            sp.dma(aff_d, aff[:], reads=[b_aff])
        K.barrier()

    coef = sb(gst, "coef", [128, NT, NE], F32)
    b_coef = Buf()
    for st in phase("5"):
        affT = sb(st, "affT", [NE, T], F32)
        work = sb(st, "work", [NE, T], F32)
        cT = sb(st, "cT", [NE, T], F32)
        m8 = sb(st, "m8", [NE, 8], F32)
        pa = [ps(st, "pa%d" % i, [NE, 4, 128], F32) for i in range(2)]
        pc = [ps(st, "pc%d" % i, [128, 4, NE], F32) for i in range(2)]
        b_affT = Buf(); b_work = Buf(); b_cT = Buf(); b_m8 = Buf(); b_pa = [Buf(), Buf()]; b_pc = [Buf(), Buf()]
        for g in range(NT // 4):
            pb = g % 2
            for j in range(4):
                tt = g * 4 + j
                pe.op(lambda e: e.transpose(out=pa[pb][:, j, :], in_=aff[:, tt, :], identity=idf[:]),
                      reads=[b_aff, b_idf], writes=[b_pa[pb]] if j == 0 else (), pwrites=() if j == 0 else [b_pa[pb]])
            dve.op(lambda e: e.tensor_copy(out=affT[:, g * 512:(g + 1) * 512], in_=pa[pb][:].rearrange("p a b -> p (a b)")),
                   reads=[b_pa[pb]], pwrites=[b_affT])
        dve.op(lambda e: e.tensor_copy(out=work[:], in_=affT[:]), reads=[b_affT], writes=[b_work])
        for r in range(CAP // 8):
            dve.op(lambda e: e.max(out=m8[:], in_=work[:]), reads=[b_work], writes=[b_m8])
            if r < CAP // 8 - 1:
                dve.op(lambda e: e.match_replace(out=work[:], in_to_replace=m8[:], in_values=work[:], imm_value=-1.0),
                       reads=[b_m8, b_work], writes=[b_work])
        dve.op(lambda e: e.scalar_tensor_tensor(out=cT[:], in0=affT[:], scalar=m8[:, 7:8], in1=affT[:], op0=ALU.is_ge, op1=ALU.mult),
               reads=[b_affT, b_m8], writes=[b_cT])
        for g in range(NT // 4):
            pb = g % 2
            for j in range(4):
                tt = g * 4 + j
                pe.op(lambda e: e.transpose(out=pc[pb][:, j, :], in_=cT[:, tt * 128:(tt + 1) * 128], identity=idf[0:NE, 0:NE]),
                      reads=[b_cT, b_idf], writes=[b_pc[pb]] if j == 0 else (), pwrites=() if j == 0 else [b_pc[pb]])
            dve.op(lambda e: e.tensor_copy(out=coef[:, g * 4:(g + 1) * 4, :], in_=pc[pb][:]), reads=[b_pc[pb]], pwrites=[b_coef])
        K.barrier()

    for st in phase("6"):
        Wg = sb(st, "Wg", [128, 16, FF], BF16)
        Wu = sb(st, "Wu", [128, 16, FF], BF16)
        Wd = sb(st, "Wd", [128, 8, D], BF16)
        hT = sb(st, "hT6", [128, 2, 16, 512], BF16)
        sg = sb(st, "sg6", [128, 2, 512], F32)
        hid = sb(st, "hid6", [128, 2, 8, 512], BF16)
        yo = sb(st, "yo6", [128, 2, D], F32)
        pg = [ps(st, "pg6_%d" % i, [128, 512], F32) for i in range(2)]
        pu = [ps(st, "pu6_%d" % i, [128, 512], F32) for i in range(2)]
        po = [ps(st, "po6_%d" % i, [128, 512], F32) for i in range(4)]
        b_Wg = Buf(); b_Wu = Buf(); b_Wd = Buf(); b_hT = [Buf(), Buf()]; b_sg = [Buf(), Buf()]; b_hid = [Buf(), Buf()]
        b_yo = [Buf(), Buf()]; b_pg = [Buf(), Buf()]; b_pu = [Buf(), Buf()]; b_po = [Buf() for _ in range(4)]
        b_out = [Buf() for _ in range(NT)]
        hi = 0
        fi = 0
        oi = 0
        yi = 0
        for ex in range(NE):
            pool.dma(Wg[:], w_g[ex].rearrange("(c p) n -> p c n", p=128), writes=[b_Wg])
            pool.dma(Wu[:], w_u[ex].rearrange("(c p) n -> p c n", p=128), writes=[b_Wu])
            pool.dma(Wd[:], w_d[ex].rearrange("(c p) n -> p c n", p=128), writes=[b_Wd])
            for tb in range(T // 512):
                hb = hi % 2
                hi += 1
                sp.dma(hT[:, hb, :, :], hn2T_d.rearrange("(c p) t -> p c t", p=128)[:, :, tb * 512:(tb + 1) * 512], writes=[b_hT[hb]])
                for fb in range(8):
                    f2 = fi % 2
                    fi += 1
                    mm_group(pg[f2][:], b_pg[f2], [(Wg[:, kc, fb * 128:(fb + 1) * 128], hT[:, hb, kc, :]) for kc in range(16)], reads=[b_Wg, b_hT[hb]])
                    mm_group(pu[f2][:], b_pu[f2], [(Wu[:, kc, fb * 128:(fb + 1) * 128], hT[:, hb, kc, :]) for kc in range(16)], reads=[b_Wu, b_hT[hb]])
                    act.op(lambda e: e.activation(out=sg[:, f2, :], in_=pg[f2][:], func=AF.Silu), reads=[b_pg[f2]], writes=[b_sg[f2]])
                    dve.op(lambda e: e.tensor_tensor(out=hid[:, hb, fb, :], in0=sg[:, f2, :], in1=pu[f2][:], op=ALU.mult),
                           reads=[b_sg[f2], b_pu[f2]], writes=[b_hid[hb]] if fb == 0 else (), pwrites=() if fb == 0 else [b_hid[hb]])
                for t4 in range(4):
                    tt = tb * 4 + t4
                    yb_ = yi % 2
                    yi += 1
                    for cc in range(4):
                        p = oi % 4
                        oi += 1
                        mm_group(po[p][:], b_po[p], [(hid[:, hb, fb, t4 * 128:(t4 + 1) * 128], Wd[:, fb, cc * 512:(cc + 1) * 512]) for fb in range(8)],
                                 reads=[b_hid[hb], b_Wd])
                        evq[0] += 1
                        if evq[0] % 2 == 0:
                            dve.op(lambda e: e.tensor_scalar(out=yo[:, yb_, cc * 512:(cc + 1) * 512], in0=po[p][:], scalar1=coef[:, tt, ex:ex + 1], scalar2=None, op0=ALU.mult),
                                   reads=[b_po[p], b_coef], writes=[b_yo[yb_]] if cc == 0 else (), pwrites=() if cc == 0 else [b_yo[yb_]])
                        else:
                            act.op(lambda e: e.activation(out=yo[:, yb_, cc * 512:(cc + 1) * 512], in_=po[p][:], func=AF.Copy, scale=coef[:, tt, ex:ex + 1]),
                                   reads=[b_po[p], b_coef], writes=[b_yo[yb_]] if cc == 0 else (), pwrites=() if cc == 0 else [b_yo[yb_]])
                    pool.dma(out[tt * 128:(tt + 1) * 128, :], yo[:, yb_, :], reads=[b_yo[yb_]], writes=[b_out[tt]], accum_op=ALU.add)
        K.barrier()

    for st in phase("5g"):
        affT = sb(st, "affTg", [NE, T], F32)
        work = sb(st, "workg", [NE, T], F32)
        vals = sb(st, "valsg", [NE, CAP], F32)
        idxu = sb(st, "idxug", [NE, CAP], U32)
        pa = [ps(st, "pag%d" % i, [NE, 4, 128], F32) for i in range(2)]
        b_affT = Buf(); b_work = Buf(); b_vals = Buf(); b_idx = Buf(); b_pa = [Buf(), Buf()]
        for g in range(NT // 4):
            pb = g % 2
            for j in range(4):
                tt = g * 4 + j
                pe.op(lambda e: e.transpose(out=pa[pb][:, j, :], in_=aff[:, tt, :], identity=idf[:]),
                      reads=[b_aff, b_idf], writes=[b_pa[pb]] if j == 0 else (), pwrites=() if j == 0 else [b_pa[pb]])
            dve.op(lambda e: e.tensor_copy(out=affT[:, g * 512:(g + 1) * 512], in_=pa[pb][:].rearrange("p a b -> p (a b)")),
                   reads=[b_pa[pb]], pwrites=[b_affT])
        dve.op(lambda e: e.tensor_copy(out=work[:], in_=affT[:]), reads=[b_affT], writes=[b_work])
        for r in range(CAP // 8):
            v8 = vals[:, r * 8:(r + 1) * 8]
            dve.op(lambda e: e.max(out=v8, in_=work[:]), reads=[b_work], pwrites=[b_vals])
            dve.op(lambda e: e.max_index(out=idxu[:, r * 8:(r + 1) * 8], in_max=v8, in_values=work[:]), reads=[b_work, b_vals], pwrites=[b_idx])
            dve.op(lambda e: e.match_replace(out=work[:], in_to_replace=v8, in_values=work[:], imm_value=-1.0),
                   reads=[b_vals, b_work, b_idx], writes=[b_work])
        sp.dma(idx_d, idxu[:].bitcast(I32), reads=[b_idx])
        sp.dma(val_d, vals[:], reads=[b_vals])
        K.barrier()

    for st in phase("6g"):
        NS = CAP // 128
        Wg = sb(st, "Wgg", [128, 16, FF], BF16)
        Wu = sb(st, "Wug", [128, 16, FF], BF16)
        Wd = sb(st, "Wdg", [128, 8, D], BF16)
        g2 = sb(st, "g2g", [128, 16], F32)
        idx_t = sb(st, "idx_t", [128, 2, NS], I32)
        gat = sb(st, "gat", [128, 2, NS], F32)
        xe = sb(st, "xeg", [128, 2, D], BF16)
        xeT = sb(st, "xeTg", [128, 16, CAP], BF16)
        sg = sb(st, "sgg", [128, 2, CAP], F32)
        hid = sb(st, "hidg", [128, 8, CAP], BF16)
        yo = sb(st, "yog", [128, 2, D], F32)
        pt = [ps(st, "ptg%d" % i, [128, 8, 128], BF16) for i in range(2)]
        pg = ps(st, "pgg", [128, 512], F32)
        pu = ps(st, "pug", [128, 512], F32)
        po = [ps(st, "pog%d" % i, [128, 512], F32) for i in range(4)]
        b_Wg = Buf(); b_Wu = Buf(); b_Wd = Buf(); b_g2 = Buf(); b_it = [Buf(), Buf()]; b_xe = [Buf(), Buf()]; b_xeT = Buf()
        b_sg = [Buf(), Buf()]; b_hid = Buf(); b_yo = [Buf(), Buf()]; b_pt = [Buf(), Buf()]; b_pg = Buf(); b_pu = Buf()
        b_po = [Buf() for _ in range(4)]; b_outall = Buf()
        sp.dma(g2[:], g2T, writes=[b_g2])
        xi = 0
        fi = 0
        oi = 0
        yi = 0
        b_Wgp = [Buf() for _ in range(4)]
        b_Wup = [Buf() for _ in range(4)]

        def load_gu(e_):
            for q in range(4):
                pool.dma(Wg[:, :, q * 256:(q + 1) * 256], w_g[e_].rearrange("(c p) n -> p c n", p=128)[:, :, q * 256:(q + 1) * 256], writes=[b_Wgp[q]])
                pool.dma(Wu[:, :, q * 256:(q + 1) * 256], w_u[e_].rearrange("(c p) n -> p c n", p=128)[:, :, q * 256:(q + 1) * 256], writes=[b_Wup[q]])

        def load_d(e_):
            pool.dma(Wd[:], w_d[e_].rearrange("(c p) n -> p c n", p=128), writes=[b_Wd])

        load_gu(0)
        load_d(0)
        for ex in range(NE):
            eb = ex % 2
            for j in range(NS):
                sp.dma(idx_t[:, eb, j:j + 1], idx_d[ex:ex + 1, j * 128:(j + 1) * 128].rearrange("o p -> p o"),
                       writes=[b_it[eb]] if j == 0 else (), pwrites=() if j == 0 else [b_it[eb]])
                sp.dma(gat[:, eb, j:j + 1], val_d[ex:ex + 1, j * 128:(j + 1) * 128].rearrange("o p -> p o"), pwrites=[b_it[eb]])
            for j in range(NS):
                xb = xi % 2
                xi += 1
                pool.dma(None, None, reads=[b_it[eb]], writes=[b_xe[xb]],
                         fn=lambda e: e.indirect_dma_start(out=xe[:, xb, :], out_offset=None, in_=xn2_d[:, :],
                                                           in_offset=bass.IndirectOffsetOnAxis(ap=idx_t[:, eb, j:j + 1], axis=0)))
                for half in range(2):
                    for c in range(8):
                        cc = half * 8 + c
                        pe.op(lambda e: e.transpose(out=pt[half][:, c, :], in_=xe[:, xb, cc * 128:(cc + 1) * 128], identity=idb[:]),
                              reads=[b_xe[xb], b_idb], writes=[b_pt[half]] if c == 0 else (), pwrites=() if c == 0 else [b_pt[half]])
                    first = (j == 0 and half == 0)
                    dve.op(lambda e: e.tensor_tensor(out=xeT[:, half * 8:(half + 1) * 8, j * 128:(j + 1) * 128], in0=pt[half][:],
                                                     in1=g2[:, half * 8:(half + 1) * 8].unsqueeze(2).broadcast_to([128, 8, 128]), op=ALU.mult),
                           reads=[b_pt[half], b_g2], writes=[b_xeT] if first else (), pwrites=() if first else [b_xeT])
            for fb in range(8):
                f2 = fi % 2
                fi += 1
                mm_group(pg[:, 0:CAP], b_pg, [(Wg[:, kc, fb * 128:(fb + 1) * 128], xeT[:, kc, :]) for kc in range(16)], reads=[b_Wgp[fb // 2], b_xeT])
                mm_group(pu[:, 0:CAP], b_pu, [(Wu[:, kc, fb * 128:(fb + 1) * 128], xeT[:, kc, :]) for kc in range(16)], reads=[b_Wup[fb // 2], b_xeT])
                act.op(lambda e: e.activation(out=sg[:, f2, :], in_=pg[:, 0:CAP], func=AF.Silu), reads=[b_pg], writes=[b_sg[f2]])
                dve.op(lambda e: e.tensor_tensor(out=hid[:, fb, :], in0=sg[:, f2, :], in1=pu[:, 0:CAP], op=ALU.mult),
                       reads=[b_sg[f2], b_pu], writes=[b_hid] if fb == 0 else (), pwrites=() if fb == 0 else [b_hid])
            if ex + 1 < NE:
                load_gu(ex + 1)
            for j in range(NS):
                yb_ = yi % 2
                yi += 1
                for cc in range(4):
                    p = oi % 4
                    oi += 1
                    mm_group(po[p][:], b_po[p], [(hid[:, fb, j * 128:(j + 1) * 128], Wd[:, fb, cc * 512:(cc + 1) * 512]) for fb in range(8)],
                             reads=[b_hid, b_Wd])
                    evq[0] += 1
                    if evq[0] % 2 == 0:
                        dve.op(lambda e: e.tensor_scalar(out=yo[:, yb_, cc * 512:(cc + 1) * 512], in0=po[p][:], scalar1=gat[:, eb, j:j + 1], scalar2=None, op0=ALU.mult),
                               reads=[b_po[p], b_it[eb]], writes=[b_yo[yb_]] if cc == 0 else (), pwrites=() if cc == 0 else [b_yo[yb_]])
                    else:
                        act.op(lambda e: e.activation(out=yo[:, yb_, cc * 512:(cc + 1) * 512], in_=po[p][:], func=AF.Copy, scale=gat[:, eb, j:j + 1]),
                               reads=[b_po[p], b_it[eb]], writes=[b_yo[yb_]] if cc == 0 else (), pwrites=() if cc == 0 else [b_yo[yb_]])
                pool.dma(None, None, reads=[b_yo[yb_], b_it[eb]], writes=[b_outall],
                         fn=lambda e, j=j, yb_=yb_: e.indirect_dma_start(out=out[:, :], out_offset=bass.IndirectOffsetOnAxis(ap=idx_t[:, eb, j:j + 1], axis=0),
                                                                         in_=yo[:, yb_, :], in_offset=None, compute_op=ALU.add))
            if ex + 1 < NE:
                load_d(ex + 1)
        K.barrier()
    gst.close()
    return nc


_NC_CACHE = {}


def kernel(**inputs):
    f32 = np.float32
    g = lambda k: np.asarray(inputs[k], dtype=f32)
    x = g("x")
    rep = lambda v, n=128: np.ascontiguousarray(np.broadcast_to(v[None], (n,) + v.shape)).astype(f32)
    colT = lambda v: np.ascontiguousarray(v.reshape(16, 128).T).astype(f32)
    inv = 500000.0 ** (-(np.arange(0, 16, 2, dtype=f32) / 16.0))
    ang = np.arange(T, dtype=f32)[:, None] * inv[None, :]
    cos = np.cos(ang).astype(f32).reshape(NT, 128, 8).transpose(1, 0, 2)
    sin = np.sin(ang).astype(f32).reshape(NT, 128, 8).transpose(1, 0, 2)
    common = {
        "w_in": g("w_in")[0],
        "g1T": colT(g("attn_norm_g")[0]),
        "g2T": colT(g("ffn_norm_g")[0]),
        "ident": np.eye(128, dtype=f32),
        "gqk": rep(np.stack([g("q_norm_g")[0], g("k_norm_g")[0]])),
        "rope_c": np.ascontiguousarray(cos),
        "rope_s": np.ascontiguousarray(sin),
        "lam_in": rep(np.stack([g("lambda_q1")[0], g("lambda_k1")[0], g("lambda_q2")[0], g("lambda_k2")[0]])),
        "subg": rep(g("subln_g")[0]),
        "w_ba": g("w_branch_a")[0],
        "w_bb": g("w_branch_b")[0],
        "w_o": g("w_out")[0],
        "w_r": g("w_router")[0],
        "w_g": g("w_gate_e")[0],
        "w_u": g("w_up_e")[0],
        "w_d": g("w_down_e")[0],
        "mu_rep": rep(g("shift_mu")[0]),
        "w0_rep": rep(np.stack([g("w0_f")[0], g("w0_b")[0]])),
        "a0_rep": rep(np.stack([g("a0_f")[0], g("a0_b")[0]])),
        "kk_rep": rep(g("k_k")[0]),
        "ka_rep": rep(g("k_a")[0]),
        "rk_rep": rep(g("r_k")[0].reshape(1024)),
        "lnw_rep": rep(g("ln_x_w")[0]),
        "lnb_rep": rep(g("ln_x_b")[0]),
        "w2_in": np.ascontiguousarray(np.stack([g("w2_f")[0], g("w2_b")[0], g("a2_f")[0], g("a2_b")[0]])),
        "g2_in": g("g2")[0],
        "masks_in": np.ascontiguousarray(np.stack([np.triu(np.ones((128, 128), f32), 1), np.tril(np.ones((128, 128), f32), -1),
                                                   np.triu(np.ones((128, 128), f32), 0), np.tril(np.ones((128, 128), f32), 0)], axis=1)),
    }
    if "nc" not in _NC_CACHE:
        _NC_CACHE["nc"] = build()
    nc = _NC_CACHE["nc"]
    in_maps = []
    for c in range(8):
        m = {k: v for k, v in common.items() if k in DECL}
        m["x"] = np.ascontiguousarray(x[c // 2][:T])
        in_maps.append(m)
    res = run_bass_kernel_spmd(nc, in_maps, core_ids=list(range(8)))
    outp = np.empty((4, T, D), dtype=f32)
    for c in range(8):
        b, half = c // 2, c % 2
        o = np.asarray(res.results[c]["out"])
        outp[b, half * 2048:(half + 1) * 2048] = o[half * 2048:(half + 1) * 2048]
    if DEBUG:
        kernel.dbg = res.results
    return outp
```

```python
import os
import math
from contextlib import ExitStack
import numpy as np
import concourse.bass as bass
import concourse.mybir as mybir
from concourse.bass_utils import run_bass_kernel_spmd

F32 = mybir.dt.float32
BF16 = mybir.dt.bfloat16
I32 = mybir.dt.int32
U32 = mybir.dt.uint32
AF = mybir.ActivationFunctionType
ALU = mybir.AluOpType
AX = mybir.AxisListType

D = 2048
T = int(os.environ.get("KT", "4096"))
NT = T // 128
DA_COLS = 3072
RW_COLS = 3488
GATE_COLS = 4096
IN_COLS = DA_COLS + RW_COLS + GATE_COLS
TM_COLS = DA_COLS + RW_COLS
NE = 16
FF = 1024
CAP = T // 8
LAM_INIT = 0.8 - 0.6 * math.exp(-0.3 * 0)

DEBUG = bool(int(os.environ.get("KDEBUG", "0")))
SKIP_RWKV = bool(int(os.environ.get("KSKIP_RWKV", "0")))
CUT = int(os.environ.get("KCUT", "99"))
PHASES = os.environ.get("KPHASES", "1,2a,2b,3a,3b,3c,4a,4b,5g,6g").split(",")


class Buf:
    __slots__ = ("w", "r", "pr", "name")

    def __init__(self, name=""):
        self.w = {}
        self.r = {}
        self.pr = {}
        self.name = name


class Eng:
    def __init__(self, K, name, eng, is_pe=False):
        self.K = K
        self.name = name
        self.eng = eng
        self.is_pe = is_pe
        self.sem = K.new_sem("s_" + name)
        self.count = 0
        self.seen = {}
        self.dma_pool = []
        self.dma_i = 0

    def _wait(self, toks):
        for sem, val in toks.items():
            if self.is_pe and sem is self.sem:
                continue
            if self.seen.get(sem, 0) >= val:
                continue
            self.eng.wait_ge(sem, val)
            self.seen[sem] = val

    @staticmethod
    def _deps(reads, writes, pwrites):
        toks = {}

        def add(d):
            for s, v in d.items():
                if toks.get(s, 0) < v:
                    toks[s] = v
        for b in reads:
            add(b.w)
        for b in writes:
            add(b.w)
            add(b.r)
        for b in pwrites:
            add(b.r)
            add(b.pr)
        return toks

    @staticmethod
    def _commit(tok, reads, writes, pwrites):
        s, v = tok
        for b in reads:
            b.r[s] = v
        for b in writes:
            b.w = {s: v}
            b.pr = b.r
            b.r = {}
        for b in pwrites:
            b.w[s] = v
            for s2, v2 in b.r.items():
                if b.pr.get(s2, 0) < v2:
                    b.pr[s2] = v2
            b.r = {}

    def op(self, fn, reads=(), writes=(), pwrites=()):
        self._wait(self._deps(reads, writes, pwrites))
        ins = fn(self.eng)
        self.count += 1
        ins.then_inc(self.sem, 1)
        self._commit((self.sem, self.count), reads, writes, pwrites)
        return ins

    def dma(self, out, in_, reads=(), writes=(), pwrites=(), fn=None, **kw):
        self._wait(self._deps(reads, writes, pwrites))
        if len(self.dma_pool) < self.K.n_dma_sems:
            self.dma_pool.append([self.K.new_sem("d_%s%d" % (self.name, len(self.dma_pool))), 0])
        ent = self.dma_pool[self.dma_i % self.K.n_dma_sems]
        self.dma_i += 1
        sem, cur = ent
        if cur > 0 and self.seen.get(sem, 0) < cur:
            self.eng.wait_ge(sem, cur)
            self.seen[sem] = cur
        ins = fn(self.eng) if fn is not None else self.eng.dma_start(out=out, in_=in_, **kw)
        ent[1] = cur + 16
        ins.then_inc(sem, 16)
        self._commit((sem, cur + 16), reads, writes, pwrites)
        return ins


class Kern:
    def __init__(self, nc, n_dma_sems=10):
        self.nc = nc
        self.n_dma_sems = n_dma_sems
        self._sems = []
        self.E = {}

    def new_sem(self, name):
        cm = self.nc.semaphore(name)
        s = cm.__enter__()
        self._sems.append(cm)
        return s

    def setup(self, engines):
        for name, eng in engines.items():
            self.E[name] = Eng(self, name, eng, is_pe=(name == "pe"))

    def barrier(self):
        toks = {}
        for e in self.E.values():
            if e.count > 0:
                toks[e.sem] = e.count
            for sem, cur in e.dma_pool:
                if cur > 0:
                    toks[sem] = cur
        for e in self.E.values():
            for sem, val in toks.items():
                if sem is e.sem:
                    continue
                if e.seen.get(sem, 0) >= val:
                    continue
                e.eng.wait_ge(sem, val)
                e.seen[sem] = val


DECL = set()


def phase(name):
    if name in PHASES or name == "zero":
        with ExitStack() as st:
            yield st


def build():
    nc = bass.Bass("TRN2", target_bir_lowering=False)

    def din(name, shape, dt=F32):
        DECL.add(name)
        return nc.dram_tensor(name, list(shape), dt, kind="ExternalInput").ap()

    def dscr(name, shape, dt, dbg=False):
        kind = "ExternalOutput" if (dbg and DEBUG) else "Internal"
        return nc.dram_tensor(name, list(shape), dt, kind=kind).ap()

    x = din("x", [T, D])
    w_in = din("w_in", [D, IN_COLS])
    g1T = din("g1T", [128, 16])
    g2T = din("g2T", [128, 16])
    ident_in = din("ident", [128, 128])
    gqk = din("gqk", [128, 2, 64])
    rope_c = din("rope_c", [128, NT, 8])
    rope_s = din("rope_s", [128, NT, 8])
    lam_in = din("lam_in", [128, 4, 64])
    subg = din("subg", [128, 128])
    w_ba = din("w_ba", [1024, D])
    w_bb = din("w_bb", [1024, D])
    w_o = din("w_o", [D, D])
    w_r = din("w_r", [D, NE])
    if "6" in PHASES or "6g" in PHASES:
        w_g = din("w_g", [NE, D, FF])
        w_u = din("w_u", [NE, D, FF])
        w_d = din("w_d", [NE, FF, D])

    out = nc.dram_tensor("out", [T, D], F32, kind="ExternalOutput").ap()

    proj_tm = dscr("proj_tm", [T, TM_COLS], BF16, dbg=True)
    gateT = dscr("gateT", [GATE_COLS, T], BF16)
    qkT_d = dscr("qkT_d", [2048, T], BF16)
    y_a = dscr("y_a", [T, 1024], BF16, dbg=True)
    y_b = dscr("y_b", [T, 1024], BF16, dbg=True)
    mT_d = dscr("mT_d", [D, T], BF16)
    hn2T_d = dscr("hn2T_d", [D, T], BF16)
    aff_d = dscr("aff_d", [128, NT, NE], F32, dbg=True)
    xn2_d = dscr("xn2_d", [T, D], BF16)
    idx_d = dscr("idx_d", [NE, CAP], I32)
    val_d = dscr("val_d", [NE, CAP], F32)

    K = Kern(nc)
    K.setup({"sp": nc.sync, "act": nc.scalar, "dve": nc.vector, "pe": nc.tensor, "pool": nc.gpsimd})
    sp, act, dve, pe, pool = (K.E[n] for n in ("sp", "act", "dve", "pe", "pool"))

    gst = ExitStack()

    def sb(st, name, shape, dt):
        return st.enter_context(nc.sbuf_tensor(name, list(shape), dt))

    def ps(st, name, shape, dt):
        return st.enter_context(nc.psum_tensor(name, list(shape), dt))

    idf = sb(gst, "idf", [128, 128], F32)
    idb = sb(gst, "idb", [128, 128], BF16)
    b_idf, b_idb = Buf(), Buf()
    sp.dma(idf[:], ident_in, writes=[b_idf])
    dve.op(lambda e: e.tensor_copy(out=idb[:], in_=idf[:]), reads=[b_idf], writes=[b_idb])

    evq = [0]

    def evac_copy(dst, src, reads, writes=(), pwrites=()):
        evq[0] += 1
        if evq[0] % 2 == 0:
            dve.op(lambda e: e.tensor_copy(out=dst, in_=src), reads=reads, writes=writes, pwrites=pwrites)
        else:
            act.op(lambda e: e.activation(out=dst, in_=src, func=AF.Copy), reads=reads, writes=writes, pwrites=pwrites)

    def mm_group(ps_ap, ps_buf, pairs, reads):
        n = len(pairs)
        for i, (l, r) in enumerate(pairs):
            if i == 0:
                pe.op(lambda e: e.matmul(ps_ap, lhsT=l, rhs=r, start=True, stop=(n == 1)), reads=reads, writes=[ps_buf])
            else:
                pe.op(lambda e: e.matmul(ps_ap, lhsT=l, rhs=r, start=False, stop=(i == n - 1)), reads=reads, pwrites=[ps_buf])

    def rmsnorm_T(st, tag, src_tile, b_src, gT_t, b_gT, dstT_ap, b_dst, pts, b_pts, scratch, store_ap=None):
        junk, b_junk, ssq, b_ssq, xn, b_xn = scratch
        dve.op(lambda e: e.memset(ssq[:], 0.0), writes=[b_ssq])
        act.op(lambda e: e.activation(out=junk[:], in_=src_tile, func=AF.Square, accum_out=ssq[:]),
               reads=[b_src], writes=[b_junk, b_ssq])
        act.op(lambda e: e.activation(out=ssq[:], in_=ssq[:], func=AF.Sqrt, scale=1.0 / D, bias=1e-6),
               reads=[b_ssq], writes=[b_ssq])
        dve.op(lambda e: e.reciprocal(out=ssq[:], in_=ssq[:]), reads=[b_ssq], writes=[b_ssq])
        dve.op(lambda e: e.tensor_scalar(out=xn[:], in0=src_tile, scalar1=ssq[:, 0:1], scalar2=None, op0=ALU.mult),
               reads=[b_src, b_ssq], writes=[b_xn])
        if store_ap is not None:
            sp.dma(store_ap, xn[:], reads=[b_xn])
        for half in range(2):
            for c in range(8):
                cc = half * 8 + c
                pe.op(lambda e: e.transpose(out=pts[half][:, c, :], in_=xn[:, cc * 128:(cc + 1) * 128], identity=idb[:]),
                      reads=[b_xn, b_idb], writes=[b_pts[half]] if c == 0 else (), pwrites=() if c == 0 else [b_pts[half]])
            dve.op(lambda e: e.tensor_tensor(out=dstT_ap[:, half * 8:(half + 1) * 8, :], in0=pts[half][:],
                                             in1=gT_t[:, half * 8:(half + 1) * 8].unsqueeze(2).broadcast_to([128, 8, 128]),
                                             op=ALU.mult),
                   reads=[b_pts[half], b_gT], pwrites=[b_dst])

    for st in phase("1"):
        HT = min(2048, T)
        hnT = sb(st, "hnT", [128, 16, HT], BF16)
        xt = sb(st, "xt", [128, 2, D], F32)
        junk = sb(st, "junk1", [128, D], BF16)
        xn = sb(st, "xn1", [128, D], BF16)
        ssq = sb(st, "ssq1", [128, 1], F32)
        g1 = sb(st, "g1", [128, 16], F32)
        wbf = sb(st, "wbf", [128, 2, 16, 512], BF16)
        stage = sb(st, "stage1", [128, 4, 512], BF16)
        pt = [ps(st, "pt1a", [128, 8, 128], BF16), ps(st, "pt1b", [128, 8, 128], BF16)]
        pm = [ps(st, "pm1_%d" % i, [128, 512], F32) for i in range(4)]
        b_hnT = Buf(); b_xt = [Buf(), Buf()]; b_junk = Buf(); b_xn = Buf(); b_ssq = Buf(); b_g1 = Buf()
        b_wbf = [Buf(), Buf()]; b_stage = [Buf() for _ in range(4)]; b_pt = [Buf(), Buf()]; b_pm = [Buf() for _ in range(4)]
        sp.dma(g1[:], g1T, writes=[b_g1])
        w_v = w_in.rearrange("(c p) n -> p c n", p=128)
        chunks = []
        c0 = 0
        while c0 < TM_COLS:
            cw = min(512, TM_COLS - c0)
            chunks.append(("tm", c0, cw))
            c0 += cw
        for gc in range(GATE_COLS // 512):
            chunks.append(("gate", TM_COLS + gc * 512, 512))
        wi = 0
        si = 0
        pi = 0
        NH16 = HT // 128
        for half in range(T // HT):
            for t16 in range(NH16):
                tt = half * NH16 + t16
                bi = tt % 2
                sp.dma(xt[:, bi, :], x[tt * 128:(tt + 1) * 128, :], writes=[b_xt[bi]])
                rmsnorm_T(st, "p1", xt[:, bi, :], b_xt[bi], g1, b_g1, hnT[:, :, t16 * 128:(t16 + 1) * 128], b_hnT,
                          pt, b_pt, (junk, b_junk, ssq, b_ssq, xn, b_xn))
            for kind, c0, cw in chunks:
                wb = wi % 2
                wi += 1
                pool.dma(wbf[:, wb, :, 0:cw], w_v[:, :, c0:c0 + cw], writes=[b_wbf[wb]])
                if kind == "tm":
                    for t16 in range(NH16):
                        tt = half * NH16 + t16
                        p = pi % 4
                        pi += 1
                        mm_group(pm[p][:, 0:cw], b_pm[p],
                                 [(hnT[:, kc, t16 * 128:(t16 + 1) * 128], wbf[:, wb, kc, 0:cw]) for kc in range(16)],
                                 reads=[b_hnT, b_wbf[wb]])
                        s = si % 4
                        si += 1
                        evac_copy(stage[:, s, 0:cw], pm[p][:, 0:cw], reads=[b_pm[p]], writes=[b_stage[s]])
                        sp.dma(proj_tm[tt * 128:(tt + 1) * 128, c0:c0 + cw], stage[:, s, 0:cw], reads=[b_stage[s]])
                else:
                    gcol = c0 - TM_COLS
                    for blk in range(4):
                        for tc4 in range(HT // 512):
                            p = pi % 4
                            pi += 1
                            mm_group(pm[p][:], b_pm[p],
                                     [(wbf[:, wb, kc, blk * 128:(blk + 1) * 128], hnT[:, kc, tc4 * 512:(tc4 + 1) * 512]) for kc in range(16)],
                                     reads=[b_hnT, b_wbf[wb]])
                            s = si % 4
                            si += 1
                            act.op(lambda e: e.activation(out=stage[:, s, :], in_=pm[p][:], func=AF.Sigmoid),
                                   reads=[b_pm[p]], writes=[b_stage[s]])
                            sp.dma(gateT[gcol + blk * 128:gcol + (blk + 1) * 128, half * HT + tc4 * 512:half * HT + (tc4 + 1) * 512],
                                   stage[:, s, :], reads=[b_stage[s]])
        K.barrier()

    for st in phase("2a"):
        qk = sb(st, "qk", [128, 2, 2048], BF16)
        sq = sb(st, "sq", [128, 32, 64], F32)
        ss = sb(st, "ss", [128, 32], F32)
        qn = sb(st, "qn", [128, 32, 64], F32)
        tmp = sb(st, "ropetmp", [128, 4, 32, 8], F32)
        qb = sb(st, "qb", [128, 32, 64], BF16)
        gq = sb(st, "gq", [128, 2, 64], F32)
        rc = sb(st, "rc", [128, NT, 8], F32)
        rs_ = sb(st, "rs", [128, NT, 8], F32)
        stg = sb(st, "stg2", [128, 2, 16, 512], BF16)
        pt = [ps(st, "pt2a", [128, 8, 128], BF16), ps(st, "pt2b", [128, 8, 128], BF16)]
        b_qk = [Buf(), Buf()]; b_sq = Buf(); b_ss = Buf(); b_qn = Buf(); b_tmp = Buf(); b_qb = Buf()
        b_c = Buf(); b_stg = [Buf(), Buf()]; b_pt = [Buf(), Buf()]
        sp.dma(gq[:], gqk, writes=[b_c])
        sp.dma(rc[:], rope_c, pwrites=[b_c])
        sp.dma(rs_[:], rope_s, pwrites=[b_c])
        for tt in range(NT):
            bi = tt % 2
            sp.dma(qk[:, bi, :], proj_tm[tt * 128:(tt + 1) * 128, 0:2048], writes=[b_qk[bi]])
            qv = qk[:, bi, :].rearrange("p (g d) -> p g d", d=64)
            dve.op(lambda e: e.tensor_tensor(out=sq[:], in0=qv, in1=qv, op=ALU.mult), reads=[b_qk[bi]], writes=[b_sq])
            dve.op(lambda e: e.tensor_reduce(out=ss[:], in_=sq[:], axis=AX.X, op=ALU.add), reads=[b_sq], writes=[b_ss])
            act.op(lambda e: e.activation(out=ss[:], in_=ss[:], func=AF.Sqrt, scale=1.0 / 64, bias=1e-6), reads=[b_ss], writes=[b_ss])
            dve.op(lambda e: e.reciprocal(out=ss[:], in_=ss[:]), reads=[b_ss], writes=[b_ss])
            dve.op(lambda e: e.tensor_tensor(out=qn[:], in0=qv, in1=ss[:].unsqueeze(2).broadcast_to([128, 32, 64]), op=ALU.mult),
                   reads=[b_qk[bi], b_ss], writes=[b_qn])
            for i in range(2):
                dve.op(lambda e: e.tensor_tensor(out=qn[:, i * 16:(i + 1) * 16, :], in0=qn[:, i * 16:(i + 1) * 16, :],
                                                 in1=gq[:, i:i + 1, :].broadcast_to([128, 16, 64]), op=ALU.mult),
                       reads=[b_qn, b_c], writes=[b_qn])
            cb = rc[:, tt:tt + 1, :].broadcast_to([128, 32, 8])
            sbb = rs_[:, tt:tt + 1, :].broadcast_to([128, 32, 8])
            x1 = qn[:, :, 0:8]
            x2 = qn[:, :, 8:16]
            dve.op(lambda e: e.tensor_tensor(out=tmp[:, 0], in0=x1, in1=cb, op=ALU.mult), reads=[b_qn, b_c], writes=[b_tmp])
            dve.op(lambda e: e.tensor_tensor(out=tmp[:, 1], in0=x2, in1=sbb, op=ALU.mult), reads=[b_qn, b_c], pwrites=[b_tmp])
            dve.op(lambda e: e.tensor_tensor(out=tmp[:, 2], in0=x2, in1=cb, op=ALU.mult), reads=[b_qn, b_c], pwrites=[b_tmp])
            dve.op(lambda e: e.tensor_tensor(out=tmp[:, 3], in0=x1, in1=sbb, op=ALU.mult), reads=[b_qn, b_c], pwrites=[b_tmp])
            dve.op(lambda e: e.tensor_copy(out=qb[:], in_=qn[:]), reads=[b_qn], writes=[b_qb])
            dve.op(lambda e: e.tensor_tensor(out=qb[:, :, 0:8], in0=tmp[:, 0], in1=tmp[:, 1], op=ALU.subtract), reads=[b_tmp], writes=[b_qb])
            dve.op(lambda e: e.tensor_tensor(out=qb[:, :, 8:16], in0=tmp[:, 2], in1=tmp[:, 3], op=ALU.add), reads=[b_tmp], writes=[b_qb])
            qbf = qb[:].rearrange("p g d -> p (g d)")
            sg = (tt // 4) % 2
            for half in range(2):
                for c in range(8):
                    cc = half * 8 + c
                    pe.op(lambda e: e.transpose(out=pt[half][:, c, :], in_=qbf[:, cc * 128:(cc + 1) * 128], identity=idb[:]),
                          reads=[b_qb, b_idb], writes=[b_pt[half]] if c == 0 else (), pwrites=() if c == 0 else [b_pt[half]])
                evac_copy(stg[:, sg, half * 8:(half + 1) * 8, (tt % 4) * 128:(tt % 4 + 1) * 128], pt[half][:],
                          reads=[b_pt[half]], writes=[b_stg[sg]] if (tt % 4 == 0 and half == 0) else (),
                          pwrites=() if (tt % 4 == 0 and half == 0) else [b_stg[sg]])
            if tt % 4 == 3:
                t0 = (tt // 4) * 512
                sp.dma(qkT_d.rearrange("(c p) t -> p c t", p=128)[:, :, t0:t0 + 512], stg[:, sg, :, :], reads=[b_stg[sg]])
        K.barrier()

    for st in phase("2b"):
        V1 = sb(st, "V1", [128, NT, 8, 144], BF16)
        qT = sb(st, "qT", [128, 2, T], BF16)
        kT1 = sb(st, "kT1", [128, 2, T], BF16)
        kT2 = sb(st, "kT2", [128, 2, T], BF16)
        PT = sb(st, "PT", [128, 3, 2, 512], BF16)
        lamt = sb(st, "lamt", [128, 4, 64], F32)
        lamp = sb(st, "lamp", [128, 2, 64], F32)
        lam2 = sb(st, "lam2", [128, 2], F32)
        lam = sb(st, "lam", [128, 1], F32)
        sgt = sb(st, "sgt", [128, 128], F32)
        rr = sb(st, "rr", [128, 2], F32)
        osb = sb(st, "osb", [128, 4, 2, 129], F32)
        b_osb = [Buf() for _ in range(4)]
        o1 = sb(st, "o1", [128, 128], F32)
        o2 = sb(st, "o2", [128, 128], F32)
        osq = sb(st, "osq", [128, 128], F32)
        oms = sb(st, "oms", [128, 1], F32)
        yst = sb(st, "yst", [128, 2, 128], BF16)
        pS = [ps(st, "pS%d" % i, [128, 2, 512], F32) for i in range(2)]
        pO = [ps(st, "pO%d" % i, [128, 2, 256], F32) for i in range(4)]
        b_V1 = Buf(); b_V1z = Buf(); b_q = [Buf(), Buf()]; b_k1 = [Buf(), Buf()]; b_k2 = [Buf(), Buf()]
        b_PT = [Buf() for _ in range(3)]; b_pS = [Buf(), Buf()]; b_pO = [[b_, b_] for b_ in (Buf(), Buf(), Buf(), Buf())]
        b_lam = Buf(); b_sg = Buf(); b_rr = Buf(); b_o1 = Buf(); b_o2 = Buf(); b_osq = Buf(); b_oms = Buf(); b_yst = [Buf(), Buf()]
        sp.dma(lamt[:], lam_in, writes=[b_lam])
        sp.dma(sgt[:], subg, writes=[b_sg])
        dve.op(lambda e: e.tensor_tensor(out=lamp[:, 0, :], in0=lamt[:, 0, :], in1=lamt[:, 1, :], op=ALU.mult), reads=[b_lam], writes=[b_lam])
        dve.op(lambda e: e.tensor_tensor(out=lamp[:, 1, :], in0=lamt[:, 2, :], in1=lamt[:, 3, :], op=ALU.mult), reads=[b_lam], writes=[b_lam])
        dve.op(lambda e: e.tensor_reduce(out=lam2[:], in_=lamp[:], axis=AX.X, op=ALU.add), reads=[b_lam], writes=[b_lam])
        act.op(lambda e: e.activation(out=lam2[:], in_=lam2[:], func=AF.Exp), reads=[b_lam], writes=[b_lam])
        dve.op(lambda e: e.tensor_tensor(out=lam[:], in0=lam2[:, 0:1], in1=lam2[:, 1:2], op=ALU.subtract), reads=[b_lam], writes=[b_lam])
        dve.op(lambda e: e.tensor_scalar(out=lam[:], in0=lam[:], scalar1=LAM_INIT, scalar2=None, op0=ALU.add), reads=[b_lam], writes=[b_lam])
        pool.op(lambda e: e.memset(kT1[:], 0.0), writes=[b_k1[0], b_k1[1]])
        pool.op(lambda e: e.memset(kT2[:], 0.0), writes=[b_k2[0], b_k2[1]])
        pool.op(lambda e: e.memset(V1[:], 1.0), writes=[b_V1, b_V1z])
        for tt in range(NT):
            sp.dma(V1[:, tt, :, 0:128], proj_tm[tt * 128:(tt + 1) * 128, 2048:3072].rearrange("p (h v) -> p h v", v=128), reads=[b_V1z], pwrites=[b_V1])
        sci = 0
        pti = 0
        for h in range(8):
            hb = h % 2
            sp.dma(qT[:, hb, :], qkT_d[h * 128:(h + 1) * 128, :], writes=[b_q[hb]])
            sp.dma(kT1[0:64, hb, :], qkT_d[1024 + h * 128:1024 + h * 128 + 64, :], writes=[b_k1[hb]])
            sp.dma(kT2[64:128, hb, :], qkT_d[1024 + h * 128 + 64:1024 + (h + 1) * 128, :], writes=[b_k2[hb]])
            kTs = [kT1, kT2]
            b_ks = [b_k1, b_k2]
            for qc in range(T // 512):
                for kt in range(NT):
                    pb = sci % 2
                    sci += 1
                    for s in range(2):
                        pe.op(lambda e: e.matmul(pS[pb][:, s, :], lhsT=kTs[s][:, hb, kt * 128:(kt + 1) * 128], rhs=qT[:, hb, qc * 512:(qc + 1) * 512],
                                                 start=True, stop=True),
                              reads=[b_ks[s][hb], b_q[hb]], writes=[b_pS[pb]] if s == 0 else (), pwrites=() if s == 0 else [b_pS[pb]])
                    pi_ = pti % 3
                    pti += 1
                    act.op(lambda e: e.activation(out=PT[:, pi_, :, :], in_=pS[pb][:], func=AF.Exp, scale=0.125),
                           reads=[b_pS[pb]], writes=[b_PT[pi_]])
                    for s in range(2):
                        for qs in range(4):
                            if kt == 0:
                                pe.op(lambda e: e.matmul(pO[qs][:, s, 0:129], lhsT=PT[:, pi_, s, qs * 128:(qs + 1) * 128], rhs=V1[:, kt, h, 0:129],
                                                         start=(s == 0), stop=False, skip_group_check=True),
                                      reads=[b_PT[pi_], b_V1], writes=[b_pO[qs][s]])
                            else:
                                pe.op(lambda e: e.matmul(pO[qs][:, s, 0:129], lhsT=PT[:, pi_, s, qs * 128:(qs + 1) * 128], rhs=V1[:, kt, h, 0:129],
                                                         start=False, stop=(kt == NT - 1), skip_group_check=True),
                                      reads=[b_PT[pi_], b_V1], pwrites=[b_pO[qs][s]])
                for qs in range(4):
                    evac_copy(osb[:, qs, :, :], pO[qs][:, :, 0:129], reads=[b_pO[qs][0]], writes=[b_osb[qs]])
                for qs in range(4):
                    qt = qc * 4 + qs
                    dve.op(lambda e: e.tensor_copy(out=rr[:], in_=osb[:, qs, :, 128]), reads=[b_osb[qs]], writes=[b_rr])
                    dve.op(lambda e: e.reciprocal(out=rr[:], in_=rr[:]), reads=[b_rr], writes=[b_rr])
                    dve.op(lambda e: e.tensor_tensor(out=rr[:, 1:2], in0=rr[:, 1:2], in1=lam[:], op=ALU.mult), reads=[b_rr, b_lam], writes=[b_rr])
                    dve.op(lambda e: e.tensor_scalar(out=o1[:], in0=osb[:, qs, 0, 0:128], scalar1=rr[:, 0:1], scalar2=None, op0=ALU.mult),
                           reads=[b_osb[qs], b_rr], writes=[b_o1])
                    dve.op(lambda e: e.tensor_scalar(out=o2[:], in0=osb[:, qs, 1, 0:128], scalar1=rr[:, 1:2], scalar2=None, op0=ALU.mult),
                           reads=[b_osb[qs], b_rr], writes=[b_o2])
                    dve.op(lambda e: e.tensor_tensor(out=o1[:], in0=o1[:], in1=o2[:], op=ALU.subtract), reads=[b_o1, b_o2], writes=[b_o1])
                    dve.op(lambda e: e.memset(oms[:], 0.0), writes=[b_oms])
                    act.op(lambda e: e.activation(out=osq[:], in_=o1[:], func=AF.Square, accum_out=oms[:]), reads=[b_o1], writes=[b_osq, b_oms])
                    act.op(lambda e: e.activation(out=oms[:], in_=oms[:], func=AF.Sqrt, scale=1.0 / 128, bias=1e-6), reads=[b_oms], writes=[b_oms])
                    dve.op(lambda e: e.reciprocal(out=oms[:], in_=oms[:]), reads=[b_oms], writes=[b_oms])
                    dve.op(lambda e: e.tensor_scalar(out=o1[:], in0=o1[:], scalar1=oms[:, 0:1], scalar2=(1.0 - LAM_INIT), op0=ALU.mult, op1=ALU.mult),
                           reads=[b_o1, b_oms], writes=[b_o1])
                    yb_ = qt % 2
                    dve.op(lambda e: e.tensor_tensor(out=yst[:, yb_, :], in0=o1[:], in1=sgt[:], op=ALU.mult), reads=[b_o1, b_sg], writes=[b_yst[yb_]])
                    sp.dma(y_a[qt * 128:(qt + 1) * 128, h * 128:(h + 1) * 128], yst[:, yb_, :], reads=[b_yst[yb_]])
        K.barrier()


    mu_in = din("mu_rep", [128, RW_COLS])
    w0_in = din("w0_rep", [128, 2, 1024])
    a0_in = din("a0_rep", [128, 2, 1024])
    kk_in = din("kk_rep", [128, 1024])
    ka_in = din("ka_rep", [128, 1024])
    rk_in = din("rk_rep", [128, 1024])
    lnw_in = din("lnw_rep", [128, 1024])
    lnb_in = din("lnb_rep", [128, 1024])
    w2_in = din("w2_in", [4, 64, 1024])
    g2_in = din("g2_in", [160, 1024])
    masks_in = din("masks_in", [128, 4, 128])
    RWS = {n: dscr("rw_" + n, [T, 1024], F32, dbg=True) for n in
           ("R", "V", "KK", "LW0", "LW1", "BB0", "BB1", "KE0", "KE1", "G", "BON", "Y0", "Y1")}

    def rwkv_phase():
        for st in phase("3a"):
            P = sb(st, "rP", [128, 3, RW_COLS], BF16)
            xs = sb(st, "rxs", [128, RW_COLS], F32)
            tt_ = sb(st, "rtt", [128, RW_COLS], F32)
            mu = sb(st, "rmu", [128, RW_COLS], F32)
            w0 = sb(st, "rw0", [128, 2, 1024], F32)
            a0 = sb(st, "ra0", [128, 2, 1024], F32)
            kkc = sb(st, "rkkc", [128, 1024], F32)
            kac = sb(st, "rkac", [128, 1024], F32)
            rkc = sb(st, "rrkc", [128, 1024], F32)
            w2 = sb(st, "rw2", [64, 4, 1024], BF16)
            g2a = sb(st, "rg2a", [128, 1024], BF16)
            g2b = sb(st, "rg2b", [32, 1024], BF16)
            L = sb(st, "rL", [128, 416], BF16)
            LT = sb(st, "rLT", [128, 6, 128], BF16)
            asg = sb(st, "rasg", [128, 2, 1024], F32)
            o = [sb(st, "ro%d" % i, [128, 1024], F32) for i in range(4)]
            kk = sb(st, "rkk", [128, 1024], F32)
            ke = sb(st, "rke", [128, 2, 1024], F32)
            s16 = sb(st, "rs16", [128, 16], F32)
            pl = ps(st, "rpl", [128, 6, 128], BF16)
            pm = [ps(st, "rpm%d" % i, [128, 2, 512], F32) for i in range(2)]
            b_P = Buf(); b_Pz = Buf(); b_xs = Buf(); b_tt = Buf(); b_c = Buf(); b_L = Buf(); b_LT = Buf(); b_asg = Buf()
            b_o = [Buf() for _ in range(4)]; b_kk = Buf(); b_ke = Buf(); b_s16 = Buf(); b_pl = Buf(); b_pm = [Buf(), Buf()]
            sp.dma(mu[:], mu_in, writes=[b_c])
            sp.dma(w0[:], w0_in, pwrites=[b_c])
            sp.dma(a0[:], a0_in, pwrites=[b_c])
            sp.dma(kkc[:], kk_in, pwrites=[b_c])
            sp.dma(kac[:], ka_in, pwrites=[b_c])
            sp.dma(rkc[:], rk_in, pwrites=[b_c])
            pool.dma(w2[:], w2_in.rearrange("f k n -> k f n"), pwrites=[b_c])
            pool.dma(g2a[:], g2_in[0:128, :], pwrites=[b_c])
            pool.dma(g2b[:], g2_in[128:160, :], pwrites=[b_c])
            oi = [0]

            def outbuf():
                oi[0] += 1
                return oi[0] % 4

            def store(name, tt, i):
                sp.dma(RWS[name][tt * 128:(tt + 1) * 128, :], o[i][:], reads=[b_o[i]])

            for tt in range(NT):
                r0 = tt * 128
                dve.op(lambda e: e.memset(P[:, 1:3, :], 0.0), writes=[b_P, b_Pz])
                sp.dma(P[:, 0, :], proj_tm[r0:r0 + 128, DA_COLS:TM_COLS], pwrites=[b_P])
                if tt == 0:
                    sp.dma(P[1:128, 1, :], proj_tm[0:127, DA_COLS:TM_COLS], reads=[b_Pz], pwrites=[b_P])
                else:
                    sp.dma(P[:, 1, :], proj_tm[r0 - 1:r0 + 127, DA_COLS:TM_COLS], reads=[b_Pz], pwrites=[b_P])
                if tt == NT - 1:
                    sp.dma(P[0:127, 2, :], proj_tm[r0 + 1:r0 + 128, DA_COLS:TM_COLS], reads=[b_Pz], pwrites=[b_P])
                else:
                    sp.dma(P[:, 2, :], proj_tm[r0 + 1:r0 + 129, DA_COLS:TM_COLS], reads=[b_Pz], pwrites=[b_P])
                dve.op(lambda e: e.tensor_tensor(out=tt_[:], in0=P[:, 1, :], in1=P[:, 2, :], op=ALU.add), reads=[b_P], writes=[b_tt])
                dve.op(lambda e: e.scalar_tensor_tensor(out=tt_[:], in0=tt_[:], scalar=0.5, in1=P[:, 0, :], op0=ALU.mult, op1=ALU.subtract),
                       reads=[b_tt, b_P], writes=[b_tt])
                dve.op(lambda e: e.tensor_tensor(out=tt_[:], in0=tt_[:], in1=mu[:], op=ALU.mult), reads=[b_tt, b_c], writes=[b_tt])
                dve.op(lambda e: e.tensor_tensor(out=xs[:], in0=tt_[:], in1=P[:, 0, :], op=ALU.add), reads=[b_tt, b_P], writes=[b_xs])
                rr_ = xs[:, 0:1024]
                kk_ = xs[:, 1024:2048]
                vv_ = xs[:, 2048:3072]
                if CUT <= 1:
                    continue
                act.op(lambda e: e.activation(out=L[:, 0:128], in_=xs[:, 3072:3200], func=AF.Tanh), reads=[b_xs], writes=[b_L])
                act.op(lambda e: e.activation(out=L[:, 128:256], in_=xs[:, 3200:3328], func=AF.Copy), reads=[b_xs], pwrites=[b_L])
                act.op(lambda e: e.activation(out=L[:, 256:416], in_=xs[:, 3328:3488], func=AF.Sigmoid), reads=[b_xs], pwrites=[b_L])
                for i in range(4):
                    pe.op(lambda e: e.transpose(out=pl[0:64, i, :], in_=L[:, i * 64:(i + 1) * 64], identity=idb[:]),
                          reads=[b_L, b_idb], writes=[b_pl] if i == 0 else (), pwrites=() if i == 0 else [b_pl])
                pe.op(lambda e: e.transpose(out=pl[:, 4, :], in_=L[:, 256:384], identity=idb[:]), reads=[b_L, b_idb], pwrites=[b_pl])
                pe.op(lambda e: e.transpose(out=pl[0:32, 5, :], in_=L[:, 384:416], identity=idb[:]), reads=[b_L, b_idb], pwrites=[b_pl])
                dve.op(lambda e: e.tensor_copy(out=LT[0:64, 0:4, :], in_=pl[0:64, 0:4, :]), reads=[b_pl], writes=[b_LT])
                dve.op(lambda e: e.tensor_copy(out=LT[:, 4, :], in_=pl[:, 4, :]), reads=[b_pl], pwrites=[b_LT])
                dve.op(lambda e: e.tensor_copy(out=LT[0:32, 5, :], in_=pl[0:32, 5, :]), reads=[b_pl], pwrites=[b_LT])
                if CUT <= 2:
                    continue
                i = outbuf()
                dve.op(lambda e: e.tensor_copy(out=o[i][:], in_=rr_), reads=[b_xs], writes=[b_o[i]])
                store("R", tt, i)
                i = outbuf()
                dve.op(lambda e: e.tensor_copy(out=o[i][:], in_=vv_), reads=[b_xs], writes=[b_o[i]])
                store("V", tt, i)
                if CUT <= 3:
                    continue
                for d in range(2):
                    p = pm[d % 2]
                    for hf in range(2):
                        pe.op(lambda e: e.matmul(p[:, hf, :], lhsT=LT[0:64, d, :], rhs=w2[:, d, hf * 512:(hf + 1) * 512], start=True, stop=True),
                              reads=[b_LT, b_c], writes=[b_pm[d % 2]] if hf == 0 else (), pwrites=() if hf == 0 else [b_pm[d % 2]])
                    i = outbuf()
                    dve.op(lambda e: e.tensor_tensor(out=o[i][:], in0=p[:].rearrange("p a b -> p (a b)"), in1=w0[:, d, :], op=ALU.add),
                           reads=[b_pm[d % 2], b_c], writes=[b_o[i]])
                    act.op(lambda e: e.activation(out=o[i][:], in_=o[i][:], func=AF.Sigmoid), reads=[b_o[i]], writes=[b_o[i]])
                    dve.op(lambda e: e.tensor_scalar(out=o[i][:], in0=o[i][:], scalar1=-math.exp(-0.5), scalar2=None, op0=ALU.mult),
                           reads=[b_o[i]], writes=[b_o[i]])
                    store("LW%d" % d, tt, i)
                if CUT <= 4:
                    continue
                for d in range(2):
                    p = pm[d % 2]
                    for hf in range(2):
                        pe.op(lambda e: e.matmul(p[:, hf, :], lhsT=LT[0:64, 2 + d, :], rhs=w2[:, 2 + d, hf * 512:(hf + 1) * 512], start=True, stop=True),
                              reads=[b_LT, b_c], writes=[b_pm[d % 2]] if hf == 0 else (), pwrites=() if hf == 0 else [b_pm[d % 2]])
                    dve.op(lambda e: e.tensor_tensor(out=asg[:, d, :], in0=p[:].rearrange("p a b -> p (a b)"), in1=a0[:, d, :], op=ALU.add),
                           reads=[b_pm[d % 2], b_c], writes=[b_asg] if d == 0 else (), pwrites=() if d == 0 else [b_asg])
                act.op(lambda e: e.activation(out=asg[:], in_=asg[:], func=AF.Sigmoid), reads=[b_asg], writes=[b_asg])
                if CUT <= 5:
                    continue
                p = pm[0]
                for hf in range(2):
                    pe.op(lambda e: e.matmul(p[:, hf, :], lhsT=LT[:, 4, :], rhs=g2a[:, hf * 512:(hf + 1) * 512], start=True, stop=False),
                          reads=[b_LT, b_c], writes=[b_pm[0]] if hf == 0 else (), pwrites=() if hf == 0 else [b_pm[0]])
                    pe.op(lambda e: e.matmul(p[:, hf, :], lhsT=LT[0:32, 5, :], rhs=g2b[:, hf * 512:(hf + 1) * 512], start=False, stop=True),
                          reads=[b_LT, b_c], pwrites=[b_pm[0]])
                i = outbuf()
                act.op(lambda e: e.activation(out=o[i][:], in_=p[:].rearrange("p a b -> p (a b)"), func=AF.Copy), reads=[b_pm[0]], writes=[b_o[i]])
                store("G", tt, i)
                if CUT <= 6:
                    continue
                dve.op(lambda e: e.tensor_tensor(out=kk[:], in0=kk_, in1=kkc[:], op=ALU.mult), reads=[b_xs, b_c], writes=[b_kk])
                dve.op(lambda e: e.tensor_tensor(out=tt_[:, 0:1024], in0=kk[:], in1=kk[:], op=ALU.mult), reads=[b_kk], writes=[b_tt])
                dve.op(lambda e: e.tensor_reduce(out=s16[:], in_=tt_[:, 0:1024].rearrange("p (h d) -> p h d", d=64), axis=AX.X, op=ALU.add),
                       reads=[b_tt], writes=[b_s16])
                act.op(lambda e: e.activation(out=s16[:], in_=s16[:], func=AF.Sqrt), reads=[b_s16], writes=[b_s16])
                dve.op(lambda e: e.tensor_scalar(out=s16[:], in0=s16[:], scalar1=1e-12, scalar2=None, op0=ALU.max), reads=[b_s16], writes=[b_s16])
                dve.op(lambda e: e.reciprocal(out=s16[:], in_=s16[:]), reads=[b_s16], writes=[b_s16])
                i = outbuf()
                dve.op(lambda e: e.tensor_tensor(out=o[i][:].rearrange("p (h d) -> p h d", d=64), in0=kk[:].rearrange("p (h d) -> p h d", d=64),
                                                 in1=s16[:].unsqueeze(2).broadcast_to([128, 16, 64]), op=ALU.mult),
                       reads=[b_kk, b_s16], writes=[b_o[i]])
                ikk = i
                for d in range(2):
                    i = outbuf()
                    dve.op(lambda e: e.tensor_tensor(out=o[i][:], in0=o[ikk][:], in1=asg[:, d, :], op=ALU.mult), reads=[b_o[ikk], b_asg], writes=[b_o[i]])
                    store("BB%d" % d, tt, i)
                dve.op(lambda e: e.tensor_scalar(out=o[ikk][:], in0=o[ikk][:], scalar1=-1.0, scalar2=None, op0=ALU.mult), reads=[b_o[ikk]], writes=[b_o[ikk]])
                store("KK", tt, ikk)
                for d in range(2):
                    dve.op(lambda e: e.tensor_tensor(out=ke[:, d, :], in0=asg[:, d, :], in1=kac[:], op=ALU.mult),
                           reads=[b_asg, b_c], writes=[b_ke] if d == 0 else (), pwrites=() if d == 0 else [b_ke])
                    dve.op(lambda e: e.tensor_tensor(out=ke[:, d, :], in0=ke[:, d, :], in1=kac[:], op=ALU.subtract),
                           reads=[b_ke, b_c], writes=[b_ke])
                    dve.op(lambda e: e.tensor_tensor(out=ke[:, d, :], in0=ke[:, d, :], in1=kk_, op=ALU.mult),
                           reads=[b_ke, b_xs], writes=[b_ke])
                    dve.op(lambda e: e.tensor_tensor(out=ke[:, d, :], in0=ke[:, d, :], in1=kk_, op=ALU.add),
                           reads=[b_ke, b_xs], writes=[b_ke])
                    i = outbuf()
                    dve.op(lambda e: e.tensor_copy(out=o[i][:], in_=ke[:, d, :]), reads=[b_ke], writes=[b_o[i]])
                    store("KE%d" % d, tt, i)
                if CUT <= 7:
                    continue
                dve.op(lambda e: e.tensor_tensor(out=tt_[:, 0:1024], in0=ke[:, 0, :], in1=ke[:, 1, :], op=ALU.add), reads=[b_ke], writes=[b_tt])
                dve.op(lambda e: e.tensor_tensor(out=tt_[:, 0:1024], in0=tt_[:, 0:1024], in1=rr_, op=ALU.mult), reads=[b_tt, b_xs], writes=[b_tt])
                dve.op(lambda e: e.tensor_tensor(out=tt_[:, 0:1024], in0=tt_[:, 0:1024], in1=rkc[:], op=ALU.mult), reads=[b_tt, b_c], writes=[b_tt])
                dve.op(lambda e: e.tensor_reduce(out=s16[:], in_=tt_[:, 0:1024].rearrange("p (h d) -> p h d", d=64), axis=AX.X, op=ALU.add),
                       reads=[b_tt], writes=[b_s16])
                i = outbuf()
                dve.op(lambda e: e.tensor_tensor(out=o[i][:].rearrange("p (h d) -> p h d", d=64), in0=vv_.rearrange("p (h d) -> p h d", d=64),
                                                 in1=s16[:].unsqueeze(2).broadcast_to([128, 16, 64]), op=ALU.mult),
                       reads=[b_xs, b_s16], writes=[b_o[i]])
                store("BON", tt, i)
            K.barrier()

        for st in phase("3b"):
            mk = sb(st, "smk", [128, 4, 128], F32)
            ld = sb(st, "sld", [128, 2, 6, 1024], F32)
            e4 = sb(st, "se4", [128, 4, 1024], F32)
            bfs = sb(st, "sbfs", [128, 7, 1024], BF16)
            dG = sb(st, "sdG", [64, 1024], F32)
            XTa = sb(st, "sXTa", [64, 4, 4, 4, 128], BF16)
            pr5a = sb(st, "spr5a", [128, 4, 5, 4, 128], BF16)
            Xp2 = sb(st, "sXp2", [128, 2, 2, 4, 192], BF16)
            Pp2 = sb(st, "sPp2", [128, 2, 2, 2, 4, 128], BF16)
            Rh = sb(st, "sRh", [64, 16, 128], BF16)
            Qm = sb(st, "sQm", [128, 16, 128], BF16)
            Gm = sb(st, "sGm", [64, 16, 64], BF16)
            Hm = sb(st, "sHm", [128, 16, 64], BF16)
            ST = sb(st, "sST", [64, 16, 64], BF16)
            yo = sb(st, "syo", [128, 2, 1024], F32)
            pT = [ps(st, "spT%d" % i, [128, 8, 128], BF16) for i in range(2)]
            pA = ps(st, "spA", [128, 2, 512], F32)
            pB = ps(st, "spB", [128, 2, 512], F32)
            pX = ps(st, "spX", [128, 2, 512], F32)
            b_mk = Buf(); b_ld = [Buf(), Buf()]; b_e4 = Buf(); b_bfs = Buf(); b_dG = Buf(); b_XT = [Buf() for _ in range(4)]; b_pr5 = [Buf() for _ in range(4)]
            b_Xp2 = [[Buf(), Buf()], [Buf(), Buf()]]; b_Pp2 = [[Buf(), Buf()], [Buf(), Buf()]]; b_Rh = Buf(); b_Qm = Buf(); b_Gm = Buf(); b_Hm = Buf(); b_ST = Buf()
            b_yo = [Buf(), Buf()]; b_pT = [Buf(), Buf()]; b_pA = [Buf(), Buf()]; b_pB = [Buf(), Buf()]; b_pX = [Buf(), Buf()]
            slot = [(pA, 0, b_pA[0]), (pA, 1, b_pA[1]), (pB, 0, b_pB[0]), (pB, 1, b_pB[1]), (pX, 0, b_pX[0]), (pX, 1, b_pX[1])]

            def sl(i, shape3):
                t_, j, b = slot[i]
                a, bb_ = shape3
                return t_[:, j, 0:a * bb_].rearrange("p (a b) -> p a b", b=bb_), b

            sp.dma(mk[:], masks_in, writes=[b_mk])
            SU, SL_, UI, LI = 0, 1, 2, 3
            li = 0
            yi = 0
            for d in range(2):
                cum_i, cum_s, mb_s, mb_i, ma_s = (UI, SL_, SU, UI, SL_) if d == 0 else (LI, SU, SL_, LI, SU)
                dve.op(lambda e: e.memset(ST[:], 0.0), writes=[b_ST])
                corder = range(NT) if d == 0 else range(NT - 1, -1, -1)
                names = ("R", "V", "KK", "LW%d" % d, "BB%d" % d, "KE%d" % d)
                for c in corder:
                    lb = li % 2
                    li += 1
                    for qi, nm in enumerate(names):
                        sp.dma(ld[:, lb, qi, :], RWS[nm][c * 128:(c + 1) * 128, :],
                               writes=[b_ld[lb]] if qi == 0 else (), pwrites=() if qi == 0 else [b_ld[lb]])
                    r_, v_, kk_, lw_, bb_, ke_ = (ld[:, lb, qi, :] for qi in range(6))
                    for hf in range(2):
                        pe.op(lambda e: e.matmul(pA[:, hf, :], lhsT=mk[:, cum_i, :], rhs=lw_[:, hf * 512:(hf + 1) * 512], start=True, stop=True),
                              reads=[b_mk, b_ld[lb]], writes=[b_pA[hf]])
                        pe.op(lambda e: e.matmul(pB[:, hf, :], lhsT=mk[:, cum_s, :], rhs=lw_[:, hf * 512:(hf + 1) * 512], start=True, stop=True),
                              reads=[b_mk, b_ld[lb]], writes=[b_pB[hf]])
                    gam = pA[:].rearrange("p a b -> p (a b)")
                    gsf = pB[:].rearrange("p a b -> p (a b)")
                    act.op(lambda e: e.activation(out=e4[:, 0, :], in_=gam, func=AF.Exp), reads=b_pA, writes=[b_e4])
                    act.op(lambda e: e.activation(out=e4[:, 2, :], in_=gam, func=AF.Exp, scale=-1.0), reads=b_pA, pwrites=[b_e4])
                    act.op(lambda e: e.activation(out=e4[:, 3, :], in_=gsf, func=AF.Exp), reads=b_pB, pwrites=[b_e4])
                    act.op(lambda e: e.activation(out=e4[:, 1, :], in_=lw_, func=AF.Exp, scale=-1.0), reads=[b_ld[lb]], pwrites=[b_e4])
                    dve.op(lambda e: e.tensor_tensor(out=e4[:, 1, :], in0=e4[:, 1, :], in1=e4[:, 0, :], op=ALU.mult), reads=[b_e4], pwrites=[b_e4])
                    dve.op(lambda e: e.tensor_tensor(out=dG[:], in0=e4[0:64, 0, :], in1=e4[0:64, 3, :], op=ALU.mult),
                           reads=[b_e4], writes=[b_dG])
                    dve.op(lambda e: e.tensor_tensor(out=dG[:].rearrange("p (h k) -> p h k", k=64), in0=dG[:].rearrange("p (h k) -> p h k", k=64),
                                                     in1=idf[0:64, 0:64].unsqueeze(1).broadcast_to([64, 16, 64]), op=ALU.mult),
                           reads=[b_dG, b_idf], writes=[b_dG])
                    dve.op(lambda e: e.tensor_tensor(out=bfs[:, 0, :], in0=kk_, in1=e4[:, 1, :], op=ALU.mult),
                           reads=[b_ld[lb], b_e4], writes=[b_bfs])
                    dve.op(lambda e: e.tensor_tensor(out=bfs[:, 1, :], in0=bb_, in1=e4[:, 2, :], op=ALU.mult), reads=[b_ld[lb], b_e4], pwrites=[b_bfs])
                    dve.op(lambda e: e.tensor_tensor(out=bfs[:, 2, :], in0=ke_, in1=e4[:, 2, :], op=ALU.mult), reads=[b_ld[lb], b_e4], pwrites=[b_bfs])
                    dve.op(lambda e: e.tensor_tensor(out=bfs[:, 3, :], in0=r_, in1=e4[:, 0, :], op=ALU.mult), reads=[b_ld[lb], b_e4], pwrites=[b_bfs])
                    dve.op(lambda e: e.tensor_tensor(out=bfs[:, 4, :], in0=bb_, in1=e4[:, 3, :], op=ALU.mult), reads=[b_ld[lb], b_e4], pwrites=[b_bfs])
                    dve.op(lambda e: e.tensor_tensor(out=bfs[:, 5, :], in0=ke_, in1=e4[:, 3, :], op=ALU.mult), reads=[b_ld[lb], b_e4], pwrites=[b_bfs])
                    act.op(lambda e: e.activation(out=bfs[:, 6, :], in_=v_, func=AF.Copy), reads=[b_ld[lb], b_bfs], pwrites=[b_bfs])
                    A_, B_, K_, R_ = 0, 1, 2, 3
                    prods = [(B_, A_, mb_s), (A_, B_, ma_s), (A_, K_, ma_s), (B_, R_, mb_i), (K_, R_, mb_i)]
                    for g4 in range(4):
                        for hh in range(4):
                            h = g4 * 4 + hh
                            tb_ = hh // 2
                            for kd in range(4):
                                first = (hh % 2 == 0 and kd == 0)
                                pe.op(lambda e: e.transpose(out=pT[tb_][0:64, (hh % 2) * 4 + kd, :], in_=bfs[:, kd, h * 64:(h + 1) * 64], identity=idb[:]),
                                      reads=[b_bfs, b_idb], writes=[b_pT[tb_]] if first else (), pwrites=() if first else [b_pT[tb_]])
                        for tb_ in range(2):
                            evac_copy(XTa[:, g4, tb_ * 2:(tb_ + 1) * 2, :, :], pT[tb_][0:64].rearrange("p (a k) t -> p a k t", k=4), reads=[b_pT[tb_]],
                                      writes=[b_XT[g4]] if tb_ == 0 else (), pwrites=() if tb_ == 0 else [b_XT[g4]])
                        for pi_, (l_, r2_, m_) in enumerate(prods):
                            ap3, bslot = sl(pi_, (4, 128))
                            for hh in range(4):
                                pe.op(lambda e: e.matmul(ap3[:, hh, :], lhsT=XTa[:, g4, hh, l_, :], rhs=XTa[:, g4, hh, r2_, :], start=True, stop=True),
                                      reads=[b_XT[g4]], writes=[bslot] if hh == 0 else (), pwrites=() if hh == 0 else [bslot])
                            dve.op(lambda e: e.tensor_tensor(out=pr5a[:, g4, pi_, :, :], in0=ap3, in1=mk[:, m_, :].unsqueeze(1).broadcast_to([128, 4, 128]), op=ALU.mult),
                                   reads=[bslot, b_mk], writes=[b_pr5[g4]] if pi_ == 0 else (), pwrites=() if pi_ == 0 else [b_pr5[g4]])

                    def neumann(g4, side):
                        pr5 = pr5a[:, g4]
                        bpr = b_pr5[g4]
                        Xp_ = Xp2[:, side]
                        bXp_ = b_Xp2[side]
                        Pp_ = Pp2[:, side]
                        bPp_ = b_Pp2[side]
                        if side == 0:
                            xa = pX[:].rearrange("p a (h x) -> p (a h) x", x=256)[:, :, 0:192]
                            bxa = [b_pX[0], b_pX[1]]
                            s0, bs0 = sl(0, (4, 128))
                            s1, bs1 = sl(1, (4, 128))
                        else:
                            xa = pB[:].rearrange("p a (h x) -> p (a h) x", x=256)[:, :, 0:192]
                            bxa = [b_pB[0], b_pB[1]]
                            s0 = pT[0][:].rearrange("p a b -> p (a b)").bitcast(F32).rearrange("p (a b) -> p a b", b=128)
                            s1 = pT[1][:].rearrange("p a b -> p (a b)").bitcast(F32).rearrange("p (a b) -> p a b", b=128)
                            bs0, bs1 = b_pT[0], b_pT[1]
                        act.op(lambda e: e.activation(out=Xp_[:, 0, :, 0:64], in_=bfs[:, 0, g4 * 256:(g4 + 1) * 256].rearrange("p (h k) -> p h k", k=64), func=AF.Copy),
                               reads=[b_bfs], writes=[bXp_[0]])
                        act.op(lambda e: e.activation(out=Xp_[:, 0, :, 64:192], in_=pr5[:, 2, :, :], func=AF.Copy), reads=[bpr], pwrites=[bXp_[0]])
                        xi = 0
                        for it in range(7):
                            if it == 0:
                                Pc, PTc, bP = pr5[:, 0], pr5[:, 1], bpr
                            else:
                                Pc, PTc, bP = Pp_[:, it % 2, 0], Pp_[:, it % 2, 1], bPp_[it % 2]
                            for hh in range(4):
                                pe.op(lambda e: e.matmul(xa[:, hh, :], lhsT=Pc[:, hh, :], rhs=Xp_[:, xi, hh, :], start=True, stop=True),
                                      reads=[bP, bXp_[xi]], writes=bxa if hh == 0 else (), pwrites=() if hh == 0 else bxa)
                            dve.op(lambda e: e.tensor_tensor(out=Xp_[:, 1 - xi, :, :], in0=xa, in1=Xp_[:, xi, :, :], op=ALU.add),
                                   reads=bxa + [bXp_[xi]], writes=[bXp_[1 - xi]])
                            xi = 1 - xi
                            if it < 6:
                                nP = (it + 1) % 2
                                for hh in range(4):
                                    pe.op(lambda e: e.matmul(s0[:, hh, :], lhsT=PTc[:, hh, :], rhs=Pc[:, hh, :], start=True, stop=True),
                                          reads=[bP], writes=[bs0] if hh == 0 else (), pwrites=() if hh == 0 else [bs0])
                                for hh in range(4):
                                    pe.op(lambda e: e.matmul(s1[:, hh, :], lhsT=Pc[:, hh, :], rhs=PTc[:, hh, :], start=True, stop=True),
                                          reads=[bP], writes=[bs1] if hh == 0 else (), pwrites=() if hh == 0 else [bs1])
                                act.op(lambda e: e.activation(out=Pp_[:, nP, 0], in_=s0, func=AF.Copy), reads=[bs0], writes=[bPp_[nP]])
                                dve.op(lambda e: e.tensor_copy(out=Pp_[:, nP, 1], in_=s1), reads=[bs1], pwrites=[bPp_[nP]])
                            yield xi
                    finals = {}
                    for (ga, gb) in ((0, 1), (2, 3)):
                        gA = neumann(ga, 0)
                        gB = neumann(gb, 1)
                        for it in range(7):
                            finals[ga] = (0, next(gA))
                            finals[gb] = (1, next(gB))
                        for g4 in (ga, gb):
                            side, xi = finals[g4]
                            Xf = Xp2[:, side, xi]
                            bXf = b_Xp2[side][xi]
                            pr5 = pr5a[:, g4]
                            bpr = b_pr5[g4]
                            hs = slice(g4 * 4, g4 * 4 + 4)
                            a2_, bs2 = sl(0, (4, 128))
                            for hh in range(4):
                                pe.op(lambda e: e.matmul(a2_[0:64, hh, :], lhsT=Xf[:, hh, 0:64], rhs=pr5[:, 3, hh, :], start=True, stop=True),
                                      reads=[bXf, bpr], writes=[bs2] if hh == 0 else (), pwrites=() if hh == 0 else [bs2])
                            dve.op(lambda e: e.tensor_tensor(out=Rh[:, hs, :], in0=a2_[0:64], in1=XTa[:, g4, :, 3, :], op=ALU.add),
                                   reads=[bs2, b_XT[g4]], pwrites=[b_Rh])
                            a3_, bs3 = sl(1, (4, 128))
                            for hh in range(4):
                                pe.op(lambda e: e.matmul(a3_[:, hh, :], lhsT=Xf[:, hh, 64:192], rhs=pr5[:, 3, hh, :], start=True, stop=True),
                                      reads=[bXf, bpr], writes=[bs3] if hh == 0 else (), pwrites=() if hh == 0 else [bs3])
                            dve.op(lambda e: e.tensor_tensor(out=Qm[:, hs, :], in0=a3_, in1=pr5[:, 4, :, :], op=ALU.add),
                                   reads=[bs3, bpr], pwrites=[b_Qm])
                            a0_, bs0 = sl(4, (4, 64))
                            a1_, bs1 = sl(5, (4, 64))
                            for hh in range(4):
                                h = g4 * 4 + hh
                                pe.op(lambda e: e.matmul(a0_[0:64, hh, :], lhsT=Xf[:, hh, 0:64], rhs=bfs[:, 4, h * 64:(h + 1) * 64], start=True, stop=True),
                                      reads=[bXf, b_bfs], writes=[bs0] if hh == 0 else (), pwrites=() if hh == 0 else [bs0])
                            for hh in range(4):
                                h = g4 * 4 + hh
                                pe.op(lambda e: e.matmul(a1_[:, hh, :], lhsT=Xf[:, hh, 64:192], rhs=bfs[:, 4, h * 64:(h + 1) * 64], start=True, stop=True),
                                      reads=[bXf, b_bfs], writes=[bs1] if hh == 0 else (), pwrites=() if hh == 0 else [bs1])
                            dve.op(lambda e: e.tensor_tensor(out=Gm[:, hs, :], in0=a0_[0:64], in1=dG[:, g4 * 256:(g4 + 1) * 256].rearrange("p (h k) -> p h k", k=64), op=ALU.add),
                                   reads=[bs0, b_dG], pwrites=[b_Gm])
                            dve.op(lambda e: e.tensor_tensor(out=Hm[:, hs, :], in0=a1_, in1=bfs[:, 5, g4 * 256:(g4 + 1) * 256].rearrange("p (h k) -> p h k", k=64), op=ALU.add),
                                   reads=[bs1, b_bfs], pwrites=[b_Hm])
                    yv = pA[:].rearrange("p a (h v) -> p (a h) v", v=64)
                    sv = pB[0:64].rearrange("p a (h v) -> p (a h) v", v=64)
                    for h in range(16):
                        vh = bfs[:, 6, h * 64:(h + 1) * 64]
                        pe.op(lambda e: e.matmul(yv[:, h, :], lhsT=Rh[:, h, :], rhs=ST[:, h, :], start=True, stop=False),
                              reads=[b_Rh, b_ST], writes=b_pA if h == 0 else (), pwrites=() if h == 0 else b_pA)
                        pe.op(lambda e: e.matmul(yv[:, h, :], lhsT=Qm[:, h, :], rhs=vh, start=False, stop=True),
                              reads=[b_Qm, b_bfs], pwrites=b_pA)
                        pe.op(lambda e: e.matmul(sv[:, h, :], lhsT=Gm[:, h, :], rhs=ST[:, h, :], start=True, stop=False),
                              reads=[b_Gm, b_ST], writes=b_pB if h == 0 else (), pwrites=() if h == 0 else b_pB)
                        pe.op(lambda e: e.matmul(sv[:, h, :], lhsT=Hm[:, h, :], rhs=vh, start=False, stop=True),
                              reads=[b_Hm, b_bfs], pwrites=b_pB)
                    yb2 = yi % 2
                    yi += 1
                    act.op(lambda e: e.activation(out=yo[:, yb2, :], in_=pA[:].rearrange("p a b -> p (a b)"), func=AF.Copy), reads=b_pA, writes=[b_yo[yb2]])
                    dve.op(lambda e: e.tensor_copy(out=ST[:], in_=sv), reads=b_pB, writes=[b_ST])
                    sp.dma(RWS["Y%d" % d][c * 128:(c + 1) * 128, :], yo[:, yb2, :], reads=[b_yo[yb2]])
            K.barrier()

        for st in phase("3c"):
            ld = sb(st, "cld", [128, 2, 4, 1024], F32)
            lw_ = sb(st, "clw", [128, 1024], F32)
            lb_ = sb(st, "clb", [128, 1024], F32)
            y = sb(st, "cy", [128, 1024], F32)
            sq = sb(st, "csq", [128, 1024], F32)
            m16 = sb(st, "cm16", [128, 16], F32)
            v16 = sb(st, "cv16", [128, 16], F32)
            yb16 = sb(st, "cyb", [128, 2, 1024], BF16)
            b_ld = [Buf(), Buf()]; b_c = Buf(); b_y = Buf(); b_sq = Buf(); b_m = Buf(); b_v = Buf(); b_yb = [Buf(), Buf()]
            sp.dma(lw_[:], lnw_in, writes=[b_c])
            sp.dma(lb_[:], lnb_in, pwrites=[b_c])
            h3 = lambda ap: ap.rearrange("p (h d) -> p h d", d=64)
            for tt in range(NT):
                lb = tt % 2
                for qi, nm in enumerate(("Y0", "Y1", "BON", "G")):
                    sp.dma(ld[:, lb, qi, :], RWS[nm][tt * 128:(tt + 1) * 128, :], writes=[b_ld[lb]] if qi == 0 else (), pwrites=() if qi == 0 else [b_ld[lb]])
                dve.op(lambda e: e.tensor_tensor(out=y[:], in0=ld[:, lb, 0, :], in1=ld[:, lb, 1, :], op=ALU.add), reads=[b_ld[lb]], writes=[b_y])
                dve.op(lambda e: e.tensor_reduce(out=m16[:], in_=h3(y[:]), axis=AX.X, op=ALU.add), reads=[b_y], writes=[b_m])
                dve.op(lambda e: e.tensor_scalar(out=m16[:], in0=m16[:], scalar1=1.0 / 64, scalar2=None, op0=ALU.mult), reads=[b_m], writes=[b_m])
                dve.op(lambda e: e.tensor_tensor(out=h3(y[:]), in0=h3(y[:]), in1=m16[:].unsqueeze(2).broadcast_to([128, 16, 64]), op=ALU.subtract),
                       reads=[b_y, b_m], writes=[b_y])
                dve.op(lambda e: e.tensor_tensor(out=sq[:], in0=y[:], in1=y[:], op=ALU.mult), reads=[b_y], writes=[b_sq])
                dve.op(lambda e: e.tensor_reduce(out=v16[:], in_=h3(sq[:]), axis=AX.X, op=ALU.add), reads=[b_sq], writes=[b_v])
                act.op(lambda e: e.activation(out=v16[:], in_=v16[:], func=AF.Sqrt, scale=1.0 / 64, bias=64e-5), reads=[b_v], writes=[b_v])
                dve.op(lambda e: e.reciprocal(out=v16[:], in_=v16[:]), reads=[b_v], writes=[b_v])
                dve.op(lambda e: e.tensor_tensor(out=h3(y[:]), in0=h3(y[:]), in1=v16[:].unsqueeze(2).broadcast_to([128, 16, 64]), op=ALU.mult),
                       reads=[b_y, b_v], writes=[b_y])
                dve.op(lambda e: e.tensor_tensor(out=y[:], in0=y[:], in1=lw_[:], op=ALU.mult), reads=[b_y, b_c], writes=[b_y])
                dve.op(lambda e: e.tensor_tensor(out=y[:], in0=y[:], in1=lb_[:], op=ALU.add), reads=[b_y, b_c], writes=[b_y])
                dve.op(lambda e: e.tensor_tensor(out=y[:], in0=y[:], in1=ld[:, lb, 2, :], op=ALU.add), reads=[b_y, b_ld[lb]], writes=[b_y])
                dve.op(lambda e: e.tensor_tensor(out=yb16[:, lb, :], in0=y[:], in1=ld[:, lb, 3, :], op=ALU.mult), reads=[b_y, b_ld[lb]], writes=[b_yb[lb]])
                sp.dma(y_b[tt * 128:(tt + 1) * 128, :], yb16[:, lb, :], reads=[b_yb[lb]])
            K.barrier()

    if SKIP_RWKV:
        for st in phase("zero"):
            z = sb(st, "zz", [128, 1024], BF16)
            b_z = Buf()
            dve.op(lambda e: e.memset(z[:], 0.0), writes=[b_z])
            for tt in range(NT):
                sp.dma(y_b[tt * 128:(tt + 1) * 128, :], z[:], reads=[b_z])
            K.barrier()
    else:
        rwkv_phase()

    for st in phase("4a"):
        Wa = sb(st, "Wa", [128, 8, D], BF16)
        Wb = sb(st, "Wb", [128, 8, D], BF16)
        yt = sb(st, "yt", [128, 2, 1024], BF16)
        yT = sb(st, "yT", [128, 2, 8, 512], BF16)
        gt = sb(st, "gt", [128, 2, 2, 512], BF16)
        ma = sb(st, "ma", [128, 512], F32)
        mbt = sb(st, "mbt", [128, 512], F32)
        mst = sb(st, "mst", [128, 2, 512], BF16)
        pt = [ps(st, "pt4a", [128, 8, 128], BF16), ps(st, "pt4b", [128, 8, 128], BF16)]
        pm = [ps(st, "pm4_%d" % i, [128, 512], F32) for i in range(4)]
        b_W = Buf(); b_yt = [Buf(), Buf()]; b_yT = [Buf(), Buf()]; b_gt = [Buf(), Buf()]; b_ma = Buf(); b_mb = Buf()
        b_mst = [Buf(), Buf()]; b_pt = [Buf(), Buf()]; b_pm = [Buf() for _ in range(4)]
        pool.dma(Wa[:], w_ba.rearrange("(c p) n -> p c n", p=128), writes=[b_W])
        pool.dma(Wb[:], w_bb.rearrange("(c p) n -> p c n", p=128), pwrites=[b_W])
        li = 0
        gi = 0
        for tb in range(T // 512):
            for br, ysrc in enumerate((y_a, y_b)):
                for t4 in range(4):
                    tt = tb * 4 + t4
                    lb = li % 2
                    li += 1
                    sp.dma(yt[:, lb, :], ysrc[tt * 128:(tt + 1) * 128, :], writes=[b_yt[lb]])
                    for c in range(8):
                        pe.op(lambda e: e.transpose(out=pt[lb][:, c, :], in_=yt[:, lb, c * 128:(c + 1) * 128], identity=idb[:]),
                              reads=[b_yt[lb], b_idb], writes=[b_pt[lb]] if c == 0 else (), pwrites=() if c == 0 else [b_pt[lb]])
                    evac_copy(yT[:, br, :, t4 * 128:(t4 + 1) * 128], pt[lb][:], reads=[b_pt[lb]],
                              writes=[b_yT[br]] if t4 == 0 else (), pwrites=() if t4 == 0 else [b_yT[br]])
            for fb in range(16):
                gb = gi % 2
                gi += 1
                sp.dma(gt[:, gb, 0, :], gateT[fb * 128:(fb + 1) * 128, tb * 512:(tb + 1) * 512], writes=[b_gt[gb]])
                sp.dma(gt[:, gb, 1, :], gateT[D + fb * 128:D + (fb + 1) * 128, tb * 512:(tb + 1) * 512], pwrites=[b_gt[gb]])
                pa = (2 * fb) % 4
                pb = (2 * fb + 1) % 4
                mm_group(pm[pa][:], b_pm[pa], [(Wa[:, c, fb * 128:(fb + 1) * 128], yT[:, 0, c, :]) for c in range(8)], reads=[b_W, b_yT[0]])
                mm_group(pm[pb][:], b_pm[pb], [(Wb[:, c, fb * 128:(fb + 1) * 128], yT[:, 1, c, :]) for c in range(8)], reads=[b_W, b_yT[1]])
                dve.op(lambda e: e.tensor_tensor(out=ma[:], in0=pm[pa][:], in1=gt[:, gb, 0, :], op=ALU.mult), reads=[b_pm[pa], b_gt[gb]], writes=[b_ma])
                dve.op(lambda e: e.tensor_tensor(out=mbt[:], in0=pm[pb][:], in1=gt[:, gb, 1, :], op=ALU.mult), reads=[b_pm[pb], b_gt[gb]], writes=[b_mb])
                dve.op(lambda e: e.tensor_tensor(out=mst[:, gb, :], in0=ma[:], in1=mbt[:], op=ALU.add), reads=[b_ma, b_mb], writes=[b_mst[gb]])
                sp.dma(mT_d[fb * 128:(fb + 1) * 128, tb * 512:(tb + 1) * 512], mst[:, gb, :], reads=[b_mst[gb]])
        K.barrier()

    aff = sb(gst, "aff", [128, NT, NE], F32)
    b_aff = Buf()
    for st in phase("4b"):
        Wo = sb(st, "Wo", [128, 16, D], BF16)
        Wr = sb(st, "Wr", [128, 16, NE], BF16)
        g2 = sb(st, "g2", [128, 16], F32)
        mT = sb(st, "mT", [128, 2, 16, 512], BF16)
        xt = sb(st, "xt4", [128, 2, D], F32)
        ht = sb(st, "ht", [128, 2, D], F32)
        junk = sb(st, "junk4", [128, D], BF16)
        xn = sb(st, "xn4", [128, D], BF16)
        ssq = sb(st, "ssq4", [128, 1], F32)
        hst = sb(st, "hst", [128, 2, 16, 512], BF16)
        lg = sb(st, "lg", [128, NE], F32)
        mx = sb(st, "mx", [128, 1], F32)
        sm = sb(st, "sm", [128, 1], F32)
        pt = [ps(st, "pt5a", [128, 8, 128], BF16), ps(st, "pt5b", [128, 8, 128], BF16)]
        pm = [ps(st, "pm5_%d" % i, [128, 512], F32) for i in range(4)]
        pr = ps(st, "pr5", [128, NE], F32)
        b_Wo = Buf(); b_Wr = Buf(); b_g2 = Buf(); b_mT = [Buf(), Buf()]; b_xt = [Buf(), Buf()]; b_ht = [Buf(), Buf()]
        b_junk = Buf(); b_xn = Buf(); b_ssq = Buf(); b_hst = [Buf(), Buf()]; b_lg = Buf(); b_mx = Buf(); b_sm = Buf()
        b_pt = [Buf(), Buf()]; b_pm = [Buf() for _ in range(4)]; b_pr = Buf()
        pool.dma(Wo[:], w_o.rearrange("(c p) n -> p c n", p=128), writes=[b_Wo])
        pool.dma(Wr[:], w_r.rearrange("(c p) n -> p c n", p=128), writes=[b_Wr])
        sp.dma(g2[:], g2T, writes=[b_g2])
        pi = 0
        for tb in range(T // 512):
            mb = tb % 2
            sp.dma(mT[:, mb, :, :], mT_d.rearrange("(c p) t -> p c t", p=128)[:, :, tb * 512:(tb + 1) * 512], writes=[b_mT[mb]])
            for t4 in range(4):
                tt = tb * 4 + t4
                bi = tt % 2
                sp.dma(xt[:, bi, :], x[tt * 128:(tt + 1) * 128, :], writes=[b_xt[bi]])
                for cc in range(4):
                    p = pi % 4
                    pi += 1
                    mm_group(pm[p][:], b_pm[p],
                             [(mT[:, mb, kc, t4 * 128:(t4 + 1) * 128], Wo[:, kc, cc * 512:(cc + 1) * 512]) for kc in range(16)],
                             reads=[b_mT[mb], b_Wo])
                    dve.op(lambda e: e.tensor_tensor(out=ht[:, bi, cc * 512:(cc + 1) * 512], in0=pm[p][:], in1=xt[:, bi, cc * 512:(cc + 1) * 512], op=ALU.add),
                           reads=[b_pm[p], b_xt[bi]], writes=[b_ht[bi]] if cc == 0 else (), pwrites=() if cc == 0 else [b_ht[bi]])
                sp.dma(out[tt * 128:(tt + 1) * 128, :], ht[:, bi, :], reads=[b_ht[bi]])
                rmsnorm_T(st, "p4", ht[:, bi, :], b_ht[bi], g2, b_g2, hst[:, mb, :, t4 * 128:(t4 + 1) * 128], b_hst[mb],
                          pt, b_pt, (junk, b_junk, ssq, b_ssq, xn, b_xn), store_ap=xn2_d[tt * 128:(tt + 1) * 128, :])
                mm_group(pr[:], b_pr, [(hst[:, mb, kc, t4 * 128:(t4 + 1) * 128], Wr[:, kc, :]) for kc in range(16)], reads=[b_hst[mb], b_Wr])
                dve.op(lambda e: e.tensor_reduce(out=mx[:], in_=pr[:], axis=AX.X, op=ALU.max), reads=[b_pr], writes=[b_mx])
                dve.op(lambda e: e.tensor_scalar(out=mx[:], in0=mx[:], scalar1=-1.0, scalar2=None, op0=ALU.mult), reads=[b_mx], writes=[b_mx])
                dve.op(lambda e: e.memset(sm[:], 0.0), writes=[b_sm])
                act.op(lambda e: e.activation(out=lg[:], in_=pr[:], func=AF.Exp, bias=mx[:, 0:1], accum_out=sm[:]),
                       reads=[b_pr, b_mx], writes=[b_lg, b_sm])
                dve.op(lambda e: e.reciprocal(out=sm[:], in_=sm[:]), reads=[b_sm], writes=[b_sm])
                dve.op(lambda e: e.tensor_scalar(out=aff[:, tt, :], in0=lg[:], scalar1=sm[:, 0:1], scalar2=None, op0=ALU.mult),
                       reads=[b_lg, b_sm], pwrites=[b_aff])
            sp.dma(hn2T_d.rearrange("(c p) t -> p c t", p=128)[:, :, tb * 512:(tb + 1) * 512], hst[:, mb, :, :], reads=[b_hst[mb]])
        if DEBUG:
            sp.dma(aff_d, aff[:], reads=[b_aff])
        K.barrier()

    coef = sb(gst, "coef", [128, NT, NE], F32)
    b_coef = Buf()
    for st in phase("5"):
        affT = sb(st, "affT", [NE, T], F32)
        work = sb(st, "work", [NE, T], F32)
        cT = sb(st, "cT", [NE, T], F32)
        m8 = sb(st, "m8", [NE, 8], F32)
        pa = [ps(st, "pa%d" % i, [NE, 4, 128], F32) for i in range(2)]
        pc = [ps(st, "pc%d" % i, [128, 4, NE], F32) for i in range(2)]
        b_affT = Buf(); b_work = Buf(); b_cT = Buf(); b_m8 = Buf(); b_pa = [Buf(), Buf()]; b_pc = [Buf(), Buf()]
        for g in range(NT // 4):
            pb = g % 2
            for j in range(4):
                tt = g * 4 + j
                pe.op(lambda e: e.transpose(out=pa[pb][:, j, :], in_=aff[:, tt, :], identity=idf[:]),
                      reads=[b_aff, b_idf], writes=[b_pa[pb]] if j == 0 else (), pwrites=() if j == 0 else [b_pa[pb]])
            dve.op(lambda e: e.tensor_copy(out=affT[:, g * 512:(g + 1) * 512], in_=pa[pb][:].rearrange("p a b -> p (a b)")),
                   reads=[b_pa[pb]], pwrites=[b_affT])
        dve.op(lambda e: e.tensor_copy(out=work[:], in_=affT[:]), reads=[b_affT], writes=[b_work])
        for r in range(CAP // 8):
            dve.op(lambda e: e.max(out=m8[:], in_=work[:]), reads=[b_work], writes=[b_m8])
            if r < CAP // 8 - 1:
                dve.op(lambda e: e.match_replace(out=work[:], in_to_replace=m8[:], in_values=work[:], imm_value=-1.0),
                       reads=[b_m8, b_work], writes=[b_work])
        dve.op(lambda e: e.scalar_tensor_tensor(out=cT[:], in0=affT[:], scalar=m8[:, 7:8], in1=affT[:], op0=ALU.is_ge, op1=ALU.mult),
               reads=[b_affT, b_m8], writes=[b_cT])
        for g in range(NT // 4):
            pb = g % 2
            for j in range(4):
                tt = g * 4 + j
                pe.op(lambda e: e.transpose(out=pc[pb][:, j, :], in_=cT[:, tt * 128:(tt + 1) * 128], identity=idf[0:NE, 0:NE]),
                      reads=[b_cT, b_idf], writes=[b_pc[pb]] if j == 0 else (), pwrites=() if j == 0 else [b_pc[pb]])
            dve.op(lambda e: e.tensor_copy(out=coef[:, g * 4:(g + 1) * 4, :], in_=pc[pb][:]), reads=[b_pc[pb]], pwrites=[b_coef])
        K.barrier()

    for st in phase("6"):
        Wg = sb(st, "Wg", [128, 16, FF], BF16)
        Wu = sb(st, "Wu", [128, 16, FF], BF16)
        Wd = sb(st, "Wd", [128, 8, D], BF16)
        hT = sb(st, "hT6", [128, 2, 16, 512], BF16)
        sg = sb(st, "sg6", [128, 2, 512], F32)
        hid = sb(st, "hid6", [128, 2, 8, 512], BF16)
        yo = sb(st, "yo6", [128, 2, D], F32)
        pg = [ps(st, "pg6_%d" % i, [128, 512], F32) for i in range(2)]
        pu = [ps(st, "pu6_%d" % i, [128, 512], F32) for i in range(2)]
        po = [ps(st, "po6_%d" % i, [128, 512], F32) for i in range(4)]
        b_Wg = Buf(); b_Wu = Buf(); b_Wd = Buf(); b_hT = [Buf(), Buf()]; b_sg = [Buf(), Buf()]; b_hid = [Buf(), Buf()]
        b_yo = [Buf(), Buf()]; b_pg = [Buf(), Buf()]; b_pu = [Buf(), Buf()]; b_po = [Buf() for _ in range(4)]
        b_out = [Buf() for _ in range(NT)]
        hi = 0
        fi = 0
        oi = 0
        yi = 0
        for ex in range(NE):
            pool.dma(Wg[:], w_g[ex].rearrange("(c p) n -> p c n", p=128), writes=[b_Wg])
            pool.dma(Wu[:], w_u[ex].rearrange("(c p) n -> p c n", p=128), writes=[b_Wu])
            pool.dma(Wd[:], w_d[ex].rearrange("(c p) n -> p c n", p=128), writes=[b_Wd])
            for tb in range(T // 512):
                hb = hi % 2
                hi += 1
                sp.dma(hT[:, hb, :, :], hn2T_d.rearrange("(c p) t -> p c t", p=128)[:, :, tb * 512:(tb + 1) * 512], writes=[b_hT[hb]])
                for fb in range(8):
                    f2 = fi % 2
                    fi += 1
                    mm_group(pg[f2][:], b_pg[f2], [(Wg[:, kc, fb * 128:(fb + 1) * 128], hT[:, hb, kc, :]) for kc in range(16)], reads=[b_Wg, b_hT[hb]])
                    mm_group(pu[f2][:], b_pu[f2], [(Wu[:, kc, fb * 128:(fb + 1) * 128], hT[:, hb, kc, :]) for kc in range(16)], reads=[b_Wu, b_hT[hb]])
                    act.op(lambda e: e.activation(out=sg[:, f2, :], in_=pg[f2][:], func=AF.Silu), reads=[b_pg[f2]], writes=[b_sg[f2]])
                    dve.op(lambda e: e.tensor_tensor(out=hid[:, hb, fb, :], in0=sg[:, f2, :], in1=pu[f2][:], op=ALU.mult),
                           reads=[b_sg[f2], b_pu[f2]], writes=[b_hid[hb]] if fb == 0 else (), pwrites=() if fb == 0 else [b_hid[hb]])
                for t4 in range(4):
                    tt = tb * 4 + t4
                    yb_ = yi % 2
                    yi += 1
                    for cc in range(4):
                        p = oi % 4
                        oi += 1
                        mm_group(po[p][:], b_po[p], [(hid[:, hb, fb, t4 * 128:(t4 + 1) * 128], Wd[:, fb, cc * 512:(cc + 1) * 512]) for fb in range(8)],
                                 reads=[b_hid[hb], b_Wd])
                        evq[0] += 1
                        if evq[0] % 2 == 0:
                            dve.op(lambda e: e.tensor_scalar(out=yo[:, yb_, cc * 512:(cc + 1) * 512], in0=po[p][:], scalar1=coef[:, tt, ex:ex + 1], scalar2=None, op0=ALU.mult),
                                   reads=[b_po[p], b_coef], writes=[b_yo[yb_]] if cc == 0 else (), pwrites=() if cc == 0 else [b_yo[yb_]])
                        else:
                            act.op(lambda e: e.activation(out=yo[:, yb_, cc * 512:(cc + 1) * 512], in_=po[p][:], func=AF.Copy, scale=coef[:, tt, ex:ex + 1]),
                                   reads=[b_po[p], b_coef], writes=[b_yo[yb_]] if cc == 0 else (), pwrites=() if cc == 0 else [b_yo[yb_]])
                    pool.dma(out[tt * 128:(tt + 1) * 128, :], yo[:, yb_, :], reads=[b_yo[yb_]], writes=[b_out[tt]], accum_op=ALU.add)
        K.barrier()

    for st in phase("5g"):
        affT = sb(st, "affTg", [NE, T], F32)
        work = sb(st, "workg", [NE, T], F32)
        vals = sb(st, "valsg", [NE, CAP], F32)
        idxu = sb(st, "idxug", [NE, CAP], U32)
        pa = [ps(st, "pag%d" % i, [NE, 4, 128], F32) for i in range(2)]
        b_affT = Buf(); b_work = Buf(); b_vals = Buf(); b_idx = Buf(); b_pa = [Buf(), Buf()]
        for g in range(NT // 4):
            pb = g % 2
            for j in range(4):
                tt = g * 4 + j
                pe.op(lambda e: e.transpose(out=pa[pb][:, j, :], in_=aff[:, tt, :], identity=idf[:]),
                      reads=[b_aff, b_idf], writes=[b_pa[pb]] if j == 0 else (), pwrites=() if j == 0 else [b_pa[pb]])
            dve.op(lambda e: e.tensor_copy(out=affT[:, g * 512:(g + 1) * 512], in_=pa[pb][:].rearrange("p a b -> p (a b)")),
                   reads=[b_pa[pb]], pwrites=[b_affT])
        dve.op(lambda e: e.tensor_copy(out=work[:], in_=affT[:]), reads=[b_affT], writes=[b_work])
        for r in range(CAP // 8):
            v8 = vals[:, r * 8:(r + 1) * 8]
            dve.op(lambda e: e.max(out=v8, in_=work[:]), reads=[b_work], pwrites=[b_vals])
            dve.op(lambda e: e.max_index(out=idxu[:, r * 8:(r + 1) * 8], in_max=v8, in_values=work[:]), reads=[b_work, b_vals], pwrites=[b_idx])
            dve.op(lambda e: e.match_replace(out=work[:], in_to_replace=v8, in_values=work[:], imm_value=-1.0),
                   reads=[b_vals, b_work, b_idx], writes=[b_work])
        sp.dma(idx_d, idxu[:].bitcast(I32), reads=[b_idx])
        sp.dma(val_d, vals[:], reads=[b_vals])
        K.barrier()

    for st in phase("6g"):
        NS = CAP // 128
        Wg = sb(st, "Wgg", [128, 16, FF], BF16)
        Wu = sb(st, "Wug", [128, 16, FF], BF16)
        Wd = sb(st, "Wdg", [128, 8, D], BF16)
        g2 = sb(st, "g2g", [128, 16], F32)
        idx_t = sb(st, "idx_t", [128, 2, NS], I32)
        gat = sb(st, "gat", [128, 2, NS], F32)
        xe = sb(st, "xeg", [128, 2, D], BF16)
        xeT = sb(st, "xeTg", [128, 16, CAP], BF16)
        sg = sb(st, "sgg", [128, 2, CAP], F32)
        hid = sb(st, "hidg", [128, 8, CAP], BF16)
        yo = sb(st, "yog", [128, 2, D], F32)
        pt = [ps(st, "ptg%d" % i, [128, 8, 128], BF16) for i in range(2)]
        pg = ps(st, "pgg", [128, 512], F32)
        pu = ps(st, "pug", [128, 512], F32)
        po = [ps(st, "pog%d" % i, [128, 512], F32) for i in range(4)]
        b_Wg = Buf(); b_Wu = Buf(); b_Wd = Buf(); b_g2 = Buf(); b_it = [Buf(), Buf()]; b_xe = [Buf(), Buf()]; b_xeT = Buf()
        b_sg = [Buf(), Buf()]; b_hid = Buf(); b_yo = [Buf(), Buf()]; b_pt = [Buf(), Buf()]; b_pg = Buf(); b_pu = Buf()
        b_po = [Buf() for _ in range(4)]; b_outall = Buf()
        sp.dma(g2[:], g2T, writes=[b_g2])
        xi = 0
        fi = 0
        oi = 0
        yi = 0
        b_Wgp = [Buf() for _ in range(4)]
        b_Wup = [Buf() for _ in range(4)]

        def load_gu(e_):
            for q in range(4):
                pool.dma(Wg[:, :, q * 256:(q + 1) * 256], w_g[e_].rearrange("(c p) n -> p c n", p=128)[:, :, q * 256:(q + 1) * 256], writes=[b_Wgp[q]])
                pool.dma(Wu[:, :, q * 256:(q + 1) * 256], w_u[e_].rearrange("(c p) n -> p c n", p=128)[:, :, q * 256:(q + 1) * 256], writes=[b_Wup[q]])

        def load_d(e_):
            pool.dma(Wd[:], w_d[e_].rearrange("(c p) n -> p c n", p=128), writes=[b_Wd])

        load_gu(0)
        load_d(0)
        for ex in range(NE):
            eb = ex % 2
            for j in range(NS):
                sp.dma(idx_t[:, eb, j:j + 1], idx_d[ex:ex + 1, j * 128:(j + 1) * 128].rearrange("o p -> p o"),
                       writes=[b_it[eb]] if j == 0 else (), pwrites=() if j == 0 else [b_it[eb]])
                sp.dma(gat[:, eb, j:j + 1], val_d[ex:ex + 1, j * 128:(j + 1) * 128].rearrange("o p -> p o"), pwrites=[b_it[eb]])
            for j in range(NS):
                xb = xi % 2
                xi += 1
                pool.dma(None, None, reads=[b_it[eb]], writes=[b_xe[xb]],
                         fn=lambda e: e.indirect_dma_start(out=xe[:, xb, :], out_offset=None, in_=xn2_d[:, :],
                                                           in_offset=bass.IndirectOffsetOnAxis(ap=idx_t[:, eb, j:j + 1], axis=0)))
                for half in range(2):
                    for c in range(8):
                        cc = half * 8 + c
                        pe.op(lambda e: e.transpose(out=pt[half][:, c, :], in_=xe[:, xb, cc * 128:(cc + 1) * 128], identity=idb[:]),
                              reads=[b_xe[xb], b_idb], writes=[b_pt[half]] if c == 0 else (), pwrites=() if c == 0 else [b_pt[half]])
                    first = (j == 0 and half == 0)
                    dve.op(lambda e: e.tensor_tensor(out=xeT[:, half * 8:(half + 1) * 8, j * 128:(j + 1) * 128], in0=pt[half][:],
                                                     in1=g2[:, half * 8:(half + 1) * 8].unsqueeze(2).broadcast_to([128, 8, 128]), op=ALU.mult),
                           reads=[b_pt[half], b_g2], writes=[b_xeT] if first else (), pwrites=() if first else [b_xeT])
            for fb in range(8):
                f2 = fi % 2
                fi += 1
                mm_group(pg[:, 0:CAP], b_pg, [(Wg[:, kc, fb * 128:(fb + 1) * 128], xeT[:, kc, :]) for kc in range(16)], reads=[b_Wgp[fb // 2], b_xeT])
                mm_group(pu[:, 0:CAP], b_pu, [(Wu[:, kc, fb * 128:(fb + 1) * 128], xeT[:, kc, :]) for kc in range(16)], reads=[b_Wup[fb // 2], b_xeT])
                act.op(lambda e: e.activation(out=sg[:, f2, :], in_=pg[:, 0:CAP], func=AF.Silu), reads=[b_pg], writes=[b_sg[f2]])
                dve.op(lambda e: e.tensor_tensor(out=hid[:, fb, :], in0=sg[:, f2, :], in1=pu[:, 0:CAP], op=ALU.mult),
                       reads=[b_sg[f2], b_pu], writes=[b_hid] if fb == 0 else (), pwrites=() if fb == 0 else [b_hid])
            if ex + 1 < NE:
                load_gu(ex + 1)
            for j in range(NS):
                yb_ = yi % 2
                yi += 1
                for cc in range(4):
                    p = oi % 4
                    oi += 1
                    mm_group(po[p][:], b_po[p], [(hid[:, fb, j * 128:(j + 1) * 128], Wd[:, fb, cc * 512:(cc + 1) * 512]) for fb in range(8)],
                             reads=[b_hid, b_Wd])
                    evq[0] += 1
                    if evq[0] % 2 == 0:
                        dve.op(lambda e: e.tensor_scalar(out=yo[:, yb_, cc * 512:(cc + 1) * 512], in0=po[p][:], scalar1=gat[:, eb, j:j + 1], scalar2=None, op0=ALU.mult),
                               reads=[b_po[p], b_it[eb]], writes=[b_yo[yb_]] if cc == 0 else (), pwrites=() if cc == 0 else [b_yo[yb_]])
                    else:
                        act.op(lambda e: e.activation(out=yo[:, yb_, cc * 512:(cc + 1) * 512], in_=po[p][:], func=AF.Copy, scale=gat[:, eb, j:j + 1]),
                               reads=[b_po[p], b_it[eb]], writes=[b_yo[yb_]] if cc == 0 else (), pwrites=() if cc == 0 else [b_yo[yb_]])
                pool.dma(None, None, reads=[b_yo[yb_], b_it[eb]], writes=[b_outall],
                         fn=lambda e, j=j, yb_=yb_: e.indirect_dma_start(out=out[:, :], out_offset=bass.IndirectOffsetOnAxis(ap=idx_t[:, eb, j:j + 1], axis=0),
                                                                         in_=yo[:, yb_, :], in_offset=None, compute_op=ALU.add))
            if ex + 1 < NE:
                load_d(ex + 1)
        K.barrier()
    gst.close()
    return nc


_NC_CACHE = {}


def kernel(**inputs):
    f32 = np.float32
    g = lambda k: np.asarray(inputs[k], dtype=f32)
    x = g("x")
    rep = lambda v, n=128: np.ascontiguousarray(np.broadcast_to(v[None], (n,) + v.shape)).astype(f32)
    colT = lambda v: np.ascontiguousarray(v.reshape(16, 128).T).astype(f32)
    inv = 500000.0 ** (-(np.arange(0, 16, 2, dtype=f32) / 16.0))
    ang = np.arange(T, dtype=f32)[:, None] * inv[None, :]
    cos = np.cos(ang).astype(f32).reshape(NT, 128, 8).transpose(1, 0, 2)
    sin = np.sin(ang).astype(f32).reshape(NT, 128, 8).transpose(1, 0, 2)
    common = {
        "w_in": g("w_in")[0],
        "g1T": colT(g("attn_norm_g")[0]),
        "g2T": colT(g("ffn_norm_g")[0]),
        "ident": np.eye(128, dtype=f32),
        "gqk": rep(np.stack([g("q_norm_g")[0], g("k_norm_g")[0]])),
        "rope_c": np.ascontiguousarray(cos),
        "rope_s": np.ascontiguousarray(sin),
        "lam_in": rep(np.stack([g("lambda_q1")[0], g("lambda_k1")[0], g("lambda_q2")[0], g("lambda_k2")[0]])),
        "subg": rep(g("subln_g")[0]),
        "w_ba": g("w_branch_a")[0],
        "w_bb": g("w_branch_b")[0],
        "w_o": g("w_out")[0],
        "w_r": g("w_router")[0],
        "w_g": g("w_gate_e")[0],
        "w_u": g("w_up_e")[0],
        "w_d": g("w_down_e")[0],
        "mu_rep": rep(g("shift_mu")[0]),
        "w0_rep": rep(np.stack([g("w0_f")[0], g("w0_b")[0]])),
        "a0_rep": rep(np.stack([g("a0_f")[0], g("a0_b")[0]])),
        "kk_rep": rep(g("k_k")[0]),
        "ka_rep": rep(g("k_a")[0]),
        "rk_rep": rep(g("r_k")[0].reshape(1024)),
        "lnw_rep": rep(g("ln_x_w")[0]),
        "lnb_rep": rep(g("ln_x_b")[0]),
        "w2_in": np.ascontiguousarray(np.stack([g("w2_f")[0], g("w2_b")[0], g("a2_f")[0], g("a2_b")[0]])),
        "g2_in": g("g2")[0],
        "masks_in": np.ascontiguousarray(np.stack([np.triu(np.ones((128, 128), f32), 1), np.tril(np.ones((128, 128), f32), -1),
                                                   np.triu(np.ones((128, 128), f32), 0), np.tril(np.ones((128, 128), f32), 0)], axis=1)),
    }
    if "nc" not in _NC_CACHE:
        _NC_CACHE["nc"] = build()
    nc = _NC_CACHE["nc"]
    in_maps = []
    for c in range(8):
        m = {k: v for k, v in common.items() if k in DECL}
        m["x"] = np.ascontiguousarray(x[c // 2][:T])
        in_maps.append(m)
    res = run_bass_kernel_spmd(nc, in_maps, core_ids=list(range(8)))
    outp = np.empty((4, T, D), dtype=f32)
    for c in range(8):
        b, half = c // 2, c % 2
        o = np.asarray(res.results[c]["out"])
        outp[b, half * 2048:(half + 1) * 2048] = o[half * 2048:(half + 1) * 2048]
    if DEBUG:
        kernel.dbg = res.results
    return outp
```
